# Optimizing a Trainium2 kernel written in Bass

```python
import jax, jax.numpy as jnp
from jax import lax
import numpy as np

D_MODEL = 1024
BATCH = 16
SEQ = 2048
DEPTH = 2

HEAD_DIM = 64
GLA_HEADS = 4
GLA_DK = 64
GLA_DV = 128
GLA_GATE_RANK = 16
GLA_GATE_NORM = 16.0
GLA_CHUNK = 64
MOBA_HEADS = 4
MOBA_BLOCK = 256
MOBA_TOPK = 3
MOBA_Q_CHUNK = 16
DSA_HEADS = 4
DSA_TOPK = 256
IDX_HEADS = 8
IDX_DIM = 64
DSA_Q_CHUNK = 64
ROPE_THETA = 500000.0
ROPE_FRACTION = 4
MIX_WIDTH = GLA_HEADS * GLA_DV + MOBA_HEADS * HEAD_DIM + DSA_HEADS * HEAD_DIM
N_EXPERTS = 16
N_GROUPS = 4
EXPERTS_PER_GROUP = N_EXPERTS // N_GROUPS
TOP_K = 2
D_FF = 512
DN_ALPHA = (2 * DEPTH) ** 0.25
DN_BETA = (8 * DEPTH) ** -0.25
LN_EPS = 1e-5
RMS_EPS = 1e-6

IN_WIDTHS = (
    GLA_HEADS * GLA_DK, GLA_HEADS * GLA_DK, GLA_HEADS * GLA_DV, GLA_GATE_RANK, GLA_HEADS * GLA_DV,
    MOBA_HEADS * HEAD_DIM, MOBA_HEADS * HEAD_DIM, MOBA_HEADS * HEAD_DIM,
    DSA_HEADS * HEAD_DIM, DSA_HEADS * HEAD_DIM, DSA_HEADS * HEAD_DIM,
    IDX_HEADS * IDX_DIM, IDX_DIM, IDX_HEADS,
)
IN_WIDTH = sum(IN_WIDTHS)

kernel_name = 'hybrid_gla_moba_dsa_moe'


def layer_norm(x, g, b):
    xf = x.astype(jnp.float32)
    mu = jnp.mean(xf, -1, keepdims=True)
    var = jnp.mean(jnp.square(xf - mu), -1, keepdims=True)
    y = (xf - mu) * lax.rsqrt(var + LN_EPS) * g.astype(jnp.float32) + b.astype(jnp.float32)
    return y.astype(x.dtype)


def rms_norm(x, g):
    xf = x.astype(jnp.float32)
    y = xf * lax.rsqrt(jnp.mean(jnp.square(xf), -1, keepdims=True) + RMS_EPS) * g.astype(jnp.float32)
    return y.astype(x.dtype)


def split_columns(y, widths):
    parts, start = [], 0
    for w in widths:
        parts.append(y[..., start:start + w])
        start += w
    return parts


def partial_rope(x, positions):
    rot = x.shape[-1] // ROPE_FRACTION
    half = rot // 2
    inv_freq = ROPE_THETA ** (-jnp.arange(half, dtype=jnp.float32) / half)
    ang = positions.astype(jnp.float32)[:, :, None, None] * inv_freq
    cos, sin = jnp.cos(ang), jnp.sin(ang)
    xf = x.astype(jnp.float32)
    x1, x2, rest = xf[..., :half], xf[..., half:rot], xf[..., rot:]
    out = jnp.concatenate([x1 * cos - x2 * sin, x2 * cos + x1 * sin, rest], -1)
    return out.astype(x.dtype)


def gla_mixer(q, k, v, gate_rank, g_out, gate_up, gate_bias, norm_gain):
    B, L, _ = q.shape
    f32 = jnp.float32
    C = GLA_CHUNK
    N = L // C
    log_a = jax.nn.log_sigmoid((gate_rank @ gate_up + gate_bias).astype(f32)) / GLA_GATE_NORM

    def heads(t, d):
        return t.astype(f32).reshape(B, N, C, GLA_HEADS, d).transpose(0, 3, 1, 2, 4)

    qh = heads(q, GLA_DK) * GLA_DK ** -0.5
    kh = heads(k, GLA_DK)
    vh = heads(v, GLA_DV)
    b = jnp.cumsum(heads(log_a, GLA_DK), axis=3)
    b_last = b[:, :, :, -1:, :]
    q_dec = qh * jnp.exp(b)
    k_inv = kh * jnp.exp(-b)
    causal = jnp.tril(jnp.ones((C, C), bool))
    a = jnp.where(causal, jnp.einsum('bhnid,bhnjd->bhnij', q_dec, k_inv), 0.0)
    o_intra = jnp.einsum('bhnij,bhnjv->bhniv', a, vh)
    kv = jnp.einsum('bhncd,bhncv->bhndv', kh * jnp.exp(b_last - b), vh)
    decay = jnp.exp(b_last[:, :, :, 0, :])

    def step(S, inp):
        kv_n, dec_n = inp
        return dec_n[..., None] * S + kv_n, S

    S0 = jnp.zeros((B, GLA_HEADS, GLA_DK, GLA_DV), f32)
    _, S_in = lax.scan(step, S0, (jnp.moveaxis(kv, 2, 0), jnp.moveaxis(decay, 2, 0)))
    S_in = jnp.moveaxis(S_in, 0, 2)
    o = o_intra + jnp.einsum('bhncd,bhndv->bhncv', q_dec, S_in)
    o = o.transpose(0, 2, 3, 1, 4).reshape(B, L, GLA_HEADS, GLA_DV)
    o = rms_norm(o, norm_gain).reshape(B, L, GLA_HEADS * GLA_DV)
    return (o * jax.nn.silu(g_out.astype(f32))).astype(q.dtype)


def moba_mixer(q, k, v):
    B, L, H, d = q.shape
    f32 = jnp.float32
    NB = -(-L // MOBA_BLOCK)
    Lp = NB * MOBA_BLOCK
    n_sel = min(MOBA_TOPK, NB - 1)
    qh = (q * d ** -0.5).transpose(0, 2, 1, 3)
    pad = ((0, 0), (0, 0), (0, Lp - L), (0, 0))
    kb = jnp.pad(k.transpose(0, 2, 1, 3), pad).reshape(B, H, NB, MOBA_BLOCK, d)
    vb = jnp.pad(v.transpose(0, 2, 1, 3), pad).reshape(B, H, NB, MOBA_BLOCK, d)
    k_mean = jnp.mean(kb.astype(f32), axis=3).astype(q.dtype)
    n_chunks = L // MOBA_Q_CHUNK
    q_chunks = jnp.moveaxis(qh.reshape(B, H, n_chunks, MOBA_Q_CHUNK, d), 2, 0)
    gather = jax.vmap(jax.vmap(lambda blocks, idx: blocks[idx]))

    def one_chunk(args):
        qc, c = args
        pos_q = c * MOBA_Q_CHUNK + jnp.arange(MOBA_Q_CHUNK)
        blk = (c * MOBA_Q_CHUNK) // MOBA_BLOCK
        k_own = lax.dynamic_index_in_dim(kb, blk, axis=2, keepdims=False)
        v_own = lax.dynamic_index_in_dim(vb, blk, axis=2, keepdims=False)
        pos_k = blk * MOBA_BLOCK + jnp.arange(MOBA_BLOCK)
        s_own = jnp.einsum('bhqd,bhkd->bhqk', qc, k_own).astype(f32)
        s_own = jnp.where(pos_k[None, :] <= pos_q[:, None], s_own, -jnp.inf)
        if n_sel == 0:
            p = jax.nn.softmax(s_own, -1).astype(v.dtype)
            return jnp.einsum('bhqk,bhkd->bhqd', p, v_own)
        gate = jnp.einsum('bhqd,bhnd->bhqn', qc, k_mean).astype(f32)
        gate = jnp.where(jnp.arange(NB) < blk, gate, -jnp.inf)
        _, idx = lax.top_k(gate, n_sel)
        valid = jnp.arange(n_sel) < blk
        k_sel = gather(kb, idx)
        v_sel = gather(vb, idx)
        s_sel = jnp.einsum('bhqd,bhqnkd->bhqnk', qc, k_sel).astype(f32)
        s_sel = jnp.where(valid[:, None], s_sel, -jnp.inf).reshape(B, H, MOBA_Q_CHUNK, n_sel * MOBA_BLOCK)
        p = jax.nn.softmax(jnp.concatenate([s_sel, s_own], -1), -1).astype(v.dtype)
        p_sel = p[..., :n_sel * MOBA_BLOCK].reshape(B, H, MOBA_Q_CHUNK, n_sel, MOBA_BLOCK)
        p_own = p[..., n_sel * MOBA_BLOCK:]
        return (jnp.einsum('bhqnk,bhqnkd->bhqd', p_sel, v_sel)
                + jnp.einsum('bhqk,bhkd->bhqd', p_own, v_own))

    out = lax.map(one_chunk, (q_chunks, jnp.arange(n_chunks)))
    out = jnp.moveaxis(out, 0, 2).reshape(B, H, L, d).transpose(0, 2, 1, 3)
    return out.reshape(B, L, H * d)


def dsa_mixer(q, k, v, q_idx, k_idx, w_idx):
    B, L, H, d = q.shape
    f32 = jnp.float32
    top = min(DSA_TOPK, L // 4)
    n_chunks = L // DSA_Q_CHUNK

    def chunks(t):
        return jnp.moveaxis(t.reshape((B, n_chunks, DSA_Q_CHUNK) + t.shape[2:]), 1, 0)

    qs = chunks(q * d ** -0.5)
    qis = chunks(q_idx)
    ws = chunks(w_idx)
    kh = k.transpose(0, 2, 1, 3)
    vh = v.transpose(0, 2, 1, 3)
    k_idx_f = k_idx.astype(f32)
    gather = jax.vmap(lambda t, idx: t[:, idx])
    pos_k = jnp.arange(L)

    def one_chunk(args):
        qc, qic, wc, c = args
        pos_q = c * DSA_Q_CHUNK + jnp.arange(DSA_Q_CHUNK)
        logits = jnp.einsum('bqhe,bse->bqhs', qic.astype(f32), k_idx_f) * IDX_DIM ** -0.5
        iscore = jnp.einsum('bqhs,bqh->bqs', jax.nn.relu(logits), wc.astype(f32) * IDX_HEADS ** -0.5)
        iscore = jnp.where(pos_k[None, :] <= pos_q[:, None], iscore, -jnp.inf)
        _, idx = lax.top_k(iscore, top)
        valid = idx <= pos_q[None, :, None]
        k_sel = gather(kh, idx)
        v_sel = gather(vh, idx)
        s = jnp.einsum('bqhd,bhqkd->bhqk', qc, k_sel).astype(f32)
        s = jnp.where(valid[:, None], s, -jnp.inf)
        p = jax.nn.softmax(s, -1).astype(v.dtype)
        return jnp.einsum('bhqk,bhqkd->bqhd', p, v_sel)

    out = lax.map(one_chunk, (qs, qis, ws, jnp.arange(n_chunks)))
    return jnp.moveaxis(out, 0, 1).reshape(B, L, H * d)


def token_mixers(h, positions, w_in, gla_gate_up, gla_gate_bias, gla_norm_gain, w_out):
    B, L, _ = h.shape
    y = h @ w_in
    (g_q, g_k, g_v, g_rank, g_gate, m_q, m_k, m_v,
     s_q, s_k, s_v, i_q, i_k, i_w) = split_columns(y, IN_WIDTHS)
    out_a = gla_mixer(g_q, g_k, g_v, g_rank, g_gate, gla_gate_up, gla_gate_bias, gla_norm_gain)

    def hd(t, n, d):
        return t.reshape(B, L, n, d)

    out_b = moba_mixer(partial_rope(hd(m_q, MOBA_HEADS, HEAD_DIM), positions),
                       partial_rope(hd(m_k, MOBA_HEADS, HEAD_DIM), positions),
                       hd(m_v, MOBA_HEADS, HEAD_DIM))
    out_c = dsa_mixer(partial_rope(hd(s_q, DSA_HEADS, HEAD_DIM), positions),
                      partial_rope(hd(s_k, DSA_HEADS, HEAD_DIM), positions),
                      hd(s_v, DSA_HEADS, HEAD_DIM),
                      partial_rope(hd(i_q, IDX_HEADS, IDX_DIM), positions),
                      partial_rope(hd(i_k, 1, IDX_DIM), positions)[:, :, 0],
                      i_w)
    mixed = jnp.concatenate([out_a, out_b, out_c], -1)
    return mixed @ w_out


def moe_ffn(h, router_w, router_bias, w_gate, w_up, w_down):
    B, L, D = h.shape
    f32 = jnp.float32
    t = h.reshape(B * L, D)
    scores = jax.nn.sigmoid((t @ router_w).astype(f32))
    sel = scores + router_bias.astype(f32)
    grouped = sel.reshape(-1, N_GROUPS, EXPERTS_PER_GROUP)
    group_score = jnp.sum(lax.top_k(grouped, TOP_K)[0], -1)
    best = jnp.argmax(group_score, -1)
    in_group = (jnp.arange(N_EXPERTS) // EXPERTS_PER_GROUP)[None, :] == best[:, None]
    _, top_idx = lax.top_k(jnp.where(in_group, sel, -jnp.inf), TOP_K)
    top_w = jnp.take_along_axis(scores, top_idx, -1)
    top_w = top_w / jnp.sum(top_w, -1, keepdims=True)
    combine = jnp.einsum('tk,tke->te', top_w, jax.nn.one_hot(top_idx, N_EXPERTS, dtype=f32)).astype(h.dtype)
    out = jnp.zeros_like(t)
    for e in range(N_EXPERTS):
        act = jax.nn.silu(t @ w_gate[e]) * (t @ w_up[e])
        out = out + combine[:, e:e + 1] * (act @ w_down[e])
    return out.reshape(B, L, D)


def setup_inputs(seed: int = 0) -> dict:
    key = jax.random.key(seed)
    ks = jax.random.split(key, 16)
    f32 = jnp.float32

    def normal(k, shape, scale):
        return jax.random.normal(k, shape, f32) * scale

    x = normal(ks[0], (BATCH, SEQ, D_MODEL), 1.0)
    offsets = jax.random.randint(ks[1], (BATCH, 1), 0, 4096, dtype=jnp.int32)
    positions = offsets + jnp.arange(SEQ, dtype=jnp.int32)[None, :]
    w_in = normal(ks[2], (DEPTH, D_MODEL, IN_WIDTH), D_MODEL ** -0.5)
    gla_gate_up = normal(ks[3], (DEPTH, GLA_GATE_RANK, GLA_HEADS * GLA_DK), GLA_GATE_RANK ** -0.5)
    gla_gate_bias = normal(ks[4], (DEPTH, GLA_HEADS * GLA_DK), 0.1)
    gla_norm_gain = 1.0 + normal(ks[5], (DEPTH, GLA_DV), 0.01)
    w_out = normal(ks[6], (DEPTH, MIX_WIDTH, D_MODEL), MIX_WIDTH ** -0.5 * DN_BETA)
    ln_mix_g = 1.0 + normal(ks[7], (DEPTH, D_MODEL), 0.01)
    ln_mix_b = normal(ks[8], (DEPTH, D_MODEL), 0.01)
    router_w = normal(ks[9], (D_MODEL, N_EXPERTS), D_MODEL ** -0.5)
    router_bias = normal(ks[10], (N_EXPERTS,), 0.01)
    w_expert_gate = normal(ks[11], (DEPTH, N_EXPERTS, D_MODEL, D_FF), D_MODEL ** -0.5)
    w_expert_up = normal(ks[12], (DEPTH, N_EXPERTS, D_MODEL, D_FF), D_MODEL ** -0.5)
    w_expert_down = normal(ks[13], (DEPTH, N_EXPERTS, D_FF, D_MODEL), D_FF ** -0.5 * DN_BETA)
    ln_ffn_g = 1.0 + normal(ks[14], (DEPTH, D_MODEL), 0.01)
    ln_ffn_b = normal(ks[15], (DEPTH, D_MODEL), 0.01)
    return {'x': x, 'positions': positions, 'w_in': w_in, 'gla_gate_up': gla_gate_up,
            'gla_gate_bias': gla_gate_bias, 'gla_norm_gain': gla_norm_gain, 'w_out': w_out,
            'ln_mix_g': ln_mix_g, 'ln_mix_b': ln_mix_b, 'router_w': router_w,
            'router_bias': router_bias, 'w_expert_gate': w_expert_gate,
            'w_expert_up': w_expert_up, 'w_expert_down': w_expert_down,
            'ln_ffn_g': ln_ffn_g, 'ln_ffn_b': ln_ffn_b}


def reference(x, positions, w_in, gla_gate_up, gla_gate_bias, gla_norm_gain, w_out,
              ln_mix_g, ln_mix_b, router_w, router_bias, w_expert_gate, w_expert_up,
              w_expert_down, ln_ffn_g, ln_ffn_b):
    h = x
    for layer in range(DEPTH):
        mix = token_mixers(h, positions, w_in[layer], gla_gate_up[layer], gla_gate_bias[layer],
                           gla_norm_gain[layer], w_out[layer])
        h = layer_norm(DN_ALPHA * h + mix, ln_mix_g[layer], ln_mix_b[layer])
        ffn = moe_ffn(h, router_w, router_bias, w_expert_gate[layer], w_expert_up[layer],
                      w_expert_down[layer])
        h = layer_norm(DN_ALPHA * h + ffn, ln_ffn_g[layer], ln_ffn_b[layer])
    return h
```

```python
import numpy as np
import concourse.bass as bass
import concourse.mybir as mybir
from concourse.bass_utils import run_bass_kernel_spmd

F32 = mybir.dt.float32
BF16 = mybir.dt.bfloat16
I32 = mybir.dt.int32
ALU = mybir.AluOpType
AF = mybir.ActivationFunctionType
AX = mybir.AxisListType

L = 2048
D = 1024
NT = 16
NEG = -30000.0
BIG = 1.0e30
ALPHA = 4.0 ** 0.25
N_BISECT = 18
SBUF_BASE = 16512
SBUF_BYTES = 229000

IN_WIDTHS = (256, 256, 512, 16, 512, 256, 256, 256, 256, 256, 256, 512, 64, 8)
IN_OFF = np.concatenate([[0], np.cumsum(IN_WIDTHS)]).astype(int)
(O_GQ, O_GK, O_GV, O_GR, O_GG, O_MQ, O_MK, O_MV, O_SQ, O_SK, O_SV, O_IQ, O_IK, O_IW) = [int(v) for v in IN_OFF[:-1]]


def _rot_cols(base, nheads):
    cols = []
    for h in range(nheads):
        for d in range(64):
            if d < 8:
                pd = d + 8
            elif d < 16:
                pd = d - 8
            else:
                pd = d
            cols.append(base + h * 64 + pd)
    return cols


def _build_colmap():
    cols = []
    off = {}

    def add(name, c):
        off[name] = len(cols)
        cols.extend(c)

    for p in range(2):
        add(f"g{p}", list(range(O_GQ + p * 128, O_GQ + p * 128 + 128)) + list(range(O_GK + p * 128, O_GK + p * 128 + 128))
            + list(range(O_GV + p * 256, O_GV + p * 256 + 256)) + list(range(O_GG + p * 256, O_GG + p * 256 + 256))
            + list(range(O_GR, O_GR + 16)))
    add("mq", list(range(O_MQ, O_MQ + 256)))
    add("mqr", _rot_cols(O_MQ, 4))
    add("mk", list(range(O_MK, O_MK + 256)))
    add("mkr", _rot_cols(O_MK, 4))
    add("mv", list(range(O_MV, O_MV + 256)))
    add("sq", list(range(O_SQ, O_SQ + 256)))
    add("sqr", _rot_cols(O_SQ, 4))
    add("sk", list(range(O_SK, O_SK + 256)))
    add("skr", _rot_cols(O_SK, 4))
    add("sv", list(range(O_SV, O_SV + 256)))
    add("iq", list(range(O_IQ, O_IQ + 512)))
    add("iqr", _rot_cols(O_IQ, 8))
    add("ik", list(range(O_IK, O_IK + 64)) * 2)
    add("ikr", _rot_cols(O_IK, 1) * 2)
    add("iw", list(range(O_IW, O_IW + 8)))
    return np.array(cols, dtype=np.int64), off


COLMAP, COFF = _build_colmap()
WX = len(COLMAP)


def _consts():
    c = np.zeros((128, 1024), np.float32)
    c[:, 0:128] = np.eye(128, dtype=np.float32)
    j = np.arange(128)[:, None]
    i = np.arange(128)[None, :]
    c[:, 128:256] = (i >= j).astype(np.float32)
    c[:, 256:384] = np.where(i <= j, 0.0, NEG)
    c[:, 384:512] = np.where(i <= j, 0.0, -BIG)
    p = np.arange(128)
    d = p % 64
    invf = np.where(d < 16, 500000.0 ** (-(d % 8) / 8.0), 0.0)
    c[:, 512] = invf / (2 * np.pi)
    c[:, 513] = np.where(d < 8, -invf, invf) / (2 * np.pi)
    c8 = np.zeros((8, 2048 + 512 + 8), np.float32)
    for k in range(8):
        for n in range(8):
            c8[k, 2560 + n] = 0.0 if n < k else (BIG if n == k else -BIG)
    for n in range(8):
        c8[n, n * 256:(n + 1) * 256] = 1.0
    for k in range(8):
        pr, hh = k // 2, k % 2
        c8[k, 2048 + pr * 128 + hh * 64: 2048 + pr * 128 + hh * 64 + 64] = 1.0
    return c, c8


class Buf:
    __slots__ = ("name", "writer", "readers", "dsem", "dcnt")

    def __init__(self, name):
        self.name = name
        self.writer = None
        self.readers = {}
        self.dsem = None
        self.dcnt = 0


class Ctx:
    def __init__(self, nc):
        self.nc = nc
        self.E = {"pe": nc.tensor, "act": nc.scalar, "dve": nc.vector, "pool": nc.gpsimd, "sp": nc.sync}
        self.sem = {k: nc.alloc_semaphore("e_" + k) for k in self.E}
        self.cnt = {k: 0 for k in self.E}
        self.seen = {k: {} for k in self.E}
        self.dbufs = []
        self.slots = {}
        self.nalloc = 0
        self.cur = SBUF_BASE
        self.cur2 = 0
        self.lim2 = 0
        self.peak = 0

    def alloc(self, name, shape, dtype, reg=0):
        isz = 4 if dtype in (F32, I32) else 2
        n = isz
        for s in shape[1:]:
            n *= s
        n = (n + 63) // 64 * 64
        if reg == 0:
            off = self.cur
            self.cur += n
            self.peak = max(self.peak, self.cur)
            assert self.cur <= SBUF_BYTES, (name, self.cur)
        else:
            off = self.cur2
            self.cur2 += n
            assert self.cur2 <= self.lim2, (name, self.cur2, self.lim2)
        self.nalloc += 1
        return self.nc.alloc_sbuf_tensor_at(f"{name}_{self.nalloc}", list(shape), dtype, offset=off)

    def mark(self):
        return (self.cur, self.cur2)

    def release(self, m):
        self.barrier()
        self.cur, self.cur2 = m

    def _wait(self, eng, dep):
        kind, obj, val = dep
        key = obj if kind == "e" else id(obj)
        if self.seen[eng].get(key, 0) >= val:
            return
        sem = self.sem[obj] if kind == "e" else obj.dsem
        self.E[eng].wait_ge(sem, val)
        self.seen[eng][key] = val

    def _deps(self, eng, reads, writes):
        for b in reads:
            if b.writer is not None:
                self._wait(eng, b.writer)
        for b in writes:
            w = b.writer
            if w is not None and not (w[0] == "e" and w[1] == eng and eng == "pe"):
                self._wait(eng, w)
            for r in b.readers.values():
                if not (r[0] == "e" and r[1] == eng and eng == "pe"):
                    self._wait(eng, r)

    def op(self, eng, fn, reads=(), writes=()):
        self._deps(eng, reads, writes)
        inst = fn(self.E[eng])
        inst.then_inc(self.sem[eng], 1)
        self.cnt[eng] += 1
        tag = ("e", eng, self.cnt[eng])
        for b in reads:
            b.readers[eng] = tag
        for b in writes:
            b.writer = tag
            b.readers = {}

    def dma(self, q, out, in_, reads=(), writes=(), owner=None):
        self._deps(q, reads, writes)
        if owner is None:
            owner = writes[0] if writes else reads[0]
        slot = self.slots.get(owner.name)
        if slot is None:
            slot = Buf("slot_" + owner.name)
            slot.dsem = self.nc.alloc_semaphore("d_%d" % len(self.slots))
            self.slots[owner.name] = slot
            self.dbufs.append(slot)
        self.E[q].dma_start(out=out, in_=in_).then_inc(slot.dsem, 16)
        slot.dcnt += 16
        tag = ("d", slot, slot.dcnt)
        for b in reads:
            b.readers[("d", id(slot))] = tag
        for b in writes:
            b.writer = tag
            b.readers = {}

    def barrier(self):
        for e in self.E:
            for e2 in self.E:
                if e2 != e and self.cnt[e2] > 0:
                    self._wait(e, ("e", e2, self.cnt[e2]))
            for b in self.dbufs:
                if b.dcnt:
                    self._wait(e, ("d", b, b.dcnt))


class Rot:
    def __init__(self, items):
        self.items = items
        self.i = 0

    def get(self):
        it = self.items[self.i % len(self.items)]
        self.i += 1
        return it


class StopBuild(Exception):
    pass


def build(nc, nseq, layers, debug=False, stop=None):
    try:
        return _build(nc, nseq, layers, debug, stop)
    except StopBuild as ex:
        return nc, ex.args[0]


def _build(nc, nseq, layers, debug=False, stop=None):
    cx = Ctx(nc)
    op, dma, alloc = cx.op, cx.dma, cx.alloc

    def chk(name):
        if stop == name:
            cx.barrier()
            raise StopBuild(cx)

    x = nc.dram_tensor("x", [nseq, L, D], F32, kind="ExternalInput").ap()
    pos = nc.dram_tensor("positions", [nseq, L], I32, kind="ExternalInput").ap()
    w_in = nc.dram_tensor("w_in_x", [2, D, WX], F32, kind="ExternalInput").ap()
    g_up = nc.dram_tensor("gla_gate_up", [2, 16, 256], F32, kind="ExternalInput").ap()
    g_bias = nc.dram_tensor("gla_gate_bias", [2, 256], F32, kind="ExternalInput").ap()
    g_gain = nc.dram_tensor("gla_norm_gain", [2, 128], F32, kind="ExternalInput").ap()
    w_out = nc.dram_tensor("w_out", [2, D, D], F32, kind="ExternalInput").ap()
    ln1g = nc.dram_tensor("ln_mix_g", [2, D], F32, kind="ExternalInput").ap()
    ln1b = nc.dram_tensor("ln_mix_b", [2, D], F32, kind="ExternalInput").ap()
    r_w = nc.dram_tensor("router_w", [D, 16], F32, kind="ExternalInput").ap()
    r_b = nc.dram_tensor("router_bias", [16], F32, kind="ExternalInput").ap()
    w_eg = nc.dram_tensor("w_expert_gate", [2, 16, D, 512], F32, kind="ExternalInput").ap()
    w_eu = nc.dram_tensor("w_expert_up", [2, 16, D, 512], F32, kind="ExternalInput").ap()
    w_ed = nc.dram_tensor("w_expert_down", [2, 16, 512, D], F32, kind="ExternalInput").ap()
    ln2g = nc.dram_tensor("ln_ffn_g", [2, D], F32, kind="ExternalInput").ap()
    ln2b = nc.dram_tensor("ln_ffn_b", [2, D], F32, kind="ExternalInput").ap()
    cst_d = nc.dram_tensor("cst", [128, 1024], F32, kind="ExternalInput").ap()
    cst8_d = nc.dram_tensor("cst8", [8, 2568], F32, kind="ExternalInput").ap()
    out = nc.dram_tensor("out", [nseq, L, D], F32, kind="ExternalOutput").ap()
    hs = nc.dram_tensor("hspill", [L, D], F32, kind="Internal").ap()
    dbg = nc.dram_tensor("dbg", [128, 8, L], BF16, kind="ExternalOutput").ap() if debug else None

    cst = alloc("cst", [128, 1024], F32)
    c8b = alloc("c8b", [8, 2568], BF16)
    identB = alloc("identB", [128, 128], BF16)
    trinegB = alloc("trinegB", [128, 128], BF16)
    negidentB = alloc("negidentB", [128, 128], BF16)
    epsln = alloc("epsln", [128, 2], F32)
    zerosB = alloc("zerosB", [1, 512], BF16)
    HT = alloc("HT", [128, 8, L], BF16)
    Hoff = cx.cur
    H = alloc("H", [128, NT, D], F32)
    base_mark = cx.mark()

    b_cst = Buf("cst")
    Hb = [Buf(f"H{t}") for t in range(NT)]
    HTb = [Buf(f"HT{t}") for t in range(NT)]
    hsb = [Buf(f"hs{t}") for t in range(NT)]
    outb = Buf("out")

    identF = cst[:, 0:128]
    tri01 = cst[:, 128:256]
    trinegbig = cst[:, 384:512]
    invf = cst[:, 512:513]
    sinvf = cst[:, 513:514]
    onehotK = c8b[0:8, 0:2048]
    selw = c8b[0:8, 2048:2560]
    bpen = c8b[0:8, 2560:2568]

    PS = []
    for i in range(8):
        t = nc.alloc_psum_tensor(f"ps{i}", [128, 512], F32)
        PS.append((t, Buf(f"ps{i}")))

    dma("sp", cst[:], cst_d[:, :], writes=[b_cst])
    dma("pool", c8b[:], cst8_d[:, :], writes=[b_cst], owner=Buf("c8"))
    op("dve", lambda e: e.tensor_copy(out=identB[:], in_=cst[:, 0:128]), [b_cst], [b_cst])
    op("dve", lambda e: e.tensor_copy(out=trinegB[:], in_=cst[:, 256:384]), [b_cst], [b_cst])
    op("dve", lambda e: e.tensor_scalar(out=negidentB[:], in0=cst[:, 0:128], scalar1=-1.0, scalar2=None, op0=ALU.mult), [b_cst], [b_cst])
    op("dve", lambda e: e.memset(epsln[:, 0:1], 1e-5), [], [b_cst])
    op("dve", lambda e: e.memset(epsln[:, 1:2], 1e-6), [], [b_cst])
    op("dve", lambda e: e.memset(zerosB[:], 0.0), [], [b_cst])
    cx.barrier()

    def load_w(dst, src_rows_cols, buf, q="pool"):
        dma(q, dst, src_rows_cols.rearrange("(c p) f -> p c f", p=128), writes=[buf])

    def make_HT(t, pool):
        for half in range(2):
            pt, pb = pool.get()
            for c4 in range(4):
                c = half * 4 + c4
                op("pe", lambda e, c=c, c4=c4: e.transpose(out=pt[:, c4 * 128:(c4 + 1) * 128],
                                                           in_=H[:, t, c * 128:(c + 1) * 128], identity=identF),
                   [Hb[t], b_cst], [pb])
            op("act", lambda e: e.activation(out=HT[:, half * 4:(half + 1) * 4, t * 128:(t + 1) * 128],
                                             in_=pt[:, :].rearrange("p (a b) -> p a b", a=4), func=AF.Copy),
               [pb], [HTb[t]])

    def layer_norm_tile(t, gbc, bbc, b_par, stat, b_stat):
        hv = H[:, t, :]
        op("dve", lambda e: e.bn_stats(out=stat[:, 0:6], in_=H[:, t, 0:512]), [Hb[t]], [b_stat])
        op("dve", lambda e: e.bn_stats(out=stat[:, 6:12], in_=H[:, t, 512:1024]), [Hb[t]], [b_stat])
        op("dve", lambda e: e.bn_aggr(out=stat[:, 12:14], in_=stat[:, 0:12]), [b_stat], [b_stat])
        op("act", lambda e: e.activation(out=stat[:, 14:15], in_=stat[:, 13:14], func=AF.Sqrt, bias=epsln[:, 0:1], scale=1.0),
           [b_stat, b_cst], [b_stat])
        op("dve", lambda e: e.reciprocal(out=stat[:, 15:16], in_=stat[:, 14:15]), [b_stat], [b_stat])
        op("dve", lambda e: e.tensor_scalar(out=hv, in0=hv, scalar1=stat[:, 12:13], scalar2=stat[:, 15:16],
                                            op0=ALU.subtract, op1=ALU.mult), [Hb[t], b_stat], [Hb[t]])
        op("dve", lambda e: e.tensor_tensor(out=hv, in0=hv, in1=gbc[:], op=ALU.mult), [Hb[t], b_par], [Hb[t]])
        op("dve", lambda e: e.tensor_tensor(out=hv, in0=hv, in1=bbc[:], op=ALU.add), [Hb[t], b_par], [Hb[t]])

    def rope_tables(seq, cosT, sinT, b_tab):
        m = cx.mark()
        posf = alloc("posf", [128, L], F32)
        tmp = alloc("rtmp", [128, L], F32)
        tmi = alloc("rtmi", [128, L], I32)
        b_t = Buf("ropetmp")
        dma("pool", posf[:], pos[seq, :].partition_broadcast(128), writes=[b_t])
        for (tab, fcol, shift) in ((sinT, sinvf, 0.0), (cosT, invf, 0.25)):
            op("dve", lambda e, fcol=fcol, shift=shift: e.tensor_scalar(out=tmp[:], in0=posf[:], scalar1=fcol, scalar2=shift,
                                                                         op0=ALU.mult, op1=ALU.add), [b_t, b_cst], [b_t])
            op("dve", lambda e: e.tensor_copy(out=tmi[:], in_=tmp[:]), [b_t], [b_t])
            op("dve", lambda e: e.tensor_copy(out=tab[:], in_=tmi[:]), [b_t], [b_tab])
            op("dve", lambda e: e.tensor_tensor(out=tmp[:], in0=tmp[:], in1=tab[:], op=ALU.subtract), [b_t, b_tab], [b_t])
            op("dve", lambda e: e.tensor_scalar(out=tab[:], in0=tmp[:], scalar1=0.5, scalar2=None, op0=ALU.is_gt), [b_t], [b_tab])
            op("dve", lambda e: e.tensor_tensor(out=tmp[:], in0=tmp[:], in1=tab[:], op=ALU.subtract), [b_t, b_tab], [b_t])
            op("dve", lambda e: e.tensor_scalar(out=tab[:], in0=tmp[:], scalar1=-0.5, scalar2=None, op0=ALU.is_lt), [b_t], [b_tab])
            op("dve", lambda e: e.tensor_tensor(out=tmp[:], in0=tmp[:], in1=tab[:], op=ALU.add), [b_t, b_tab], [b_t])
            op("act", lambda e: e.activation(out=tab[:], in_=tmp[:], func=AF.Sin, scale=2 * np.pi), [b_t], [b_tab])
        cx.release(m)

    def proj_fm(wt, col0, M, tc, pt, pb, wbuf, pbase=0):
        for c in range(8):
            op("pe", lambda e, c=c: e.matmul(pt[pbase:pbase + M, 0:512], lhsT=wt[:, c, col0:col0 + M],
                                             rhs=HT[:, c, tc * 512:(tc + 1) * 512], start=(c == 0), stop=(c == 7)),
               [wbuf] + HTb[tc * 4:(tc + 1) * 4], [pb])

    def proj_tm(wt, col0, N, t, pt, pb, wbuf, ocol=0):
        for c in range(8):
            op("pe", lambda e, c=c: e.matmul(pt[:, ocol:ocol + N], lhsT=HT[:, c, t * 128:(t + 1) * 128],
                                             rhs=wt[:, c, col0:col0 + N], start=(c == 0), stop=(c == 7)),
               [wbuf, HTb[t]], [pb])

    def roped_proj(wt, wbuf, col, colr, dstT, b_dst, cosT, sinT, b_tab, tmps, pspool, ksum=None):
        ntile = dstT.shape[1]
        (t1, t2, b_tmp) = tmps
        for ti in range(ntile):
            for tc in range(4):
                py, pyb = pspool.get()
                pr, prb = pspool.get()
                proj_fm(wt, col + ti * 128, 128, tc, py, pyb, wbuf)
                proj_fm(wt, colr + ti * 128, 128, tc, pr, prb, wbuf)
                sl = slice(tc * 512, (tc + 1) * 512)
                op("dve", lambda e: e.tensor_tensor(out=t1[:], in0=pr[:, :], in1=sinT[:, sl], op=ALU.mult), [prb, b_tab], [b_tmp])
                op("dve", lambda e: e.tensor_tensor(out=t2[:], in0=py[:, :], in1=cosT[:, sl], op=ALU.mult), [pyb, b_tab], [b_tmp])
                if ksum is not None:
                    op("pool", lambda e: e.tensor_tensor(out=t2[:], in0=t1[:], in1=t2[:], op=ALU.add), [b_tmp], [b_tmp])
                    op("act", lambda e: e.activation(out=dstT[:, ti, sl], in_=t2[:], func=AF.Copy), [b_tmp], [b_dst])
                    (km, b_km) = ksum
                    op("dve", lambda e: e.tensor_reduce(out=km[:, ti, tc * 2:(tc + 1) * 2], in_=t2[:, :].rearrange("p (a b) -> p a b", a=2),
                                                        axis=AX.X, op=ALU.add), [b_tmp], [b_km])
                else:
                    op("pool", lambda e: e.tensor_tensor(out=dstT[:, ti, sl], in0=t1[:], in1=t2[:], op=ALU.add), [b_tmp], [b_dst])

    def v_aug_proj(wt, wbuf, col, vaug, b_v, pspool):
        op("dve", lambda e: e.memset(vaug[:, :, :, 64:65], 1.0), [], [b_v])
        for t in range(NT):
            pt, pb = pspool.get()
            proj_tm(wt, col, 256, t, pt, pb, wbuf)
            op("act", lambda e: e.activation(out=vaug[:, t, :, 0:64], in_=pt[:, 0:256].rearrange("p (a b) -> p a b", a=4), func=AF.Copy),
               [pb], [b_v])

    def attention(qT, kT, b_qk, vaug, b_v, maskfn, mix_chunk0, pools, otm, b_otm, pTs, rs, b_rs, mixT, mixb, qc_hook=None):
        (ps_s, ps_acc, ps_tr) = pools
        if qc_hook is not None:
            qc_hook(0)
        for qc in range(4):
            if qc_hook is not None and qc + 1 < 4:
                qc_hook(qc + 1)
            for h in range(4):
                ptile, bp = h // 2, (h % 2) * 64
                acc, accb = ps_acc.get()
                op("pe", lambda e: e.matmul(acc[:, 0:260], lhsT=zerosB[0:1, 0:128], rhs=zerosB[0:1, 0:260], start=True, stop=False,
                                            skip_group_check=True), [b_cst], [accb])
                for j in range(4 * qc + 4):
                    t0 = max(j, 4 * qc)
                    q0, q1 = t0 * 128, (4 * qc + 4) * 128
                    wq = q1 - q0
                    st, sb = ps_s.get()
                    op("pe", lambda e: e.matmul(st[:, 0:wq], lhsT=kT[bp:bp + 64, ptile, j * 128:(j + 1) * 128],
                                                rhs=qT[bp:bp + 64, ptile, q0:q1], start=True, stop=False, skip_group_check=True),
                       [b_qk], [sb])
                    maskfn(h, qc, j, t0, st, sb, wq)
                    pT, pTb = pTs.get()
                    op("act", lambda e: e.activation(out=pT[:, 0:wq], in_=st[:, 0:wq], func=AF.Exp, scale=0.125), [sb], [pTb])
                    for t in range(t0, 4 * qc + 4):
                        i = t - 4 * qc
                        op("pe", lambda e, t=t, i=i: e.matmul(acc[:, i * 65:(i + 1) * 65], lhsT=pT[:, (t - t0) * 128:(t - t0 + 1) * 128],
                                                              rhs=vaug[:, j, h, :], start=False, stop=(j == t), skip_group_check=True),
                           [pTb, b_v], [accb])
                acc3 = acc[:, 0:260].rearrange("p (a b) -> p a b", a=4)
                op("dve", lambda e: e.reciprocal(out=rs[:, :], in_=acc3[:, :, 64]), [accb], [b_rs])
                for i in range(4):
                    op("dve", lambda e, i=i: e.tensor_scalar(out=otm[:, i, h * 64:(h + 1) * 64], in0=acc3[:, i, 0:64],
                                                             scalar1=rs[:, i:i + 1], scalar2=None, op0=ALU.mult), [accb, b_rs], [b_otm])
            for i in range(4):
                t = 4 * qc + i
                pt, pb = ps_tr.get()
                for f in range(2):
                    op("pe", lambda e, f=f: e.transpose(out=pt[:, f * 128:(f + 1) * 128], in_=otm[:, i, f * 128:(f + 1) * 128], identity=identF),
                       [b_otm, b_cst], [pb])
                op("act", lambda e: e.activation(out=mixT[:, mix_chunk0:mix_chunk0 + 2, t * 128:(t + 1) * 128],
                                                 in_=pt[:, 0:256].rearrange("p (a b) -> p a b", a=2), func=AF.Copy), [pb], [mixb[t]])

    for seq in range(nseq):
        pool_tr = Rot(PS[0:4])
        for t in range(NT):
            dma("sp", H[:, t, :], x[seq, t * 128:(t + 1) * 128, :], writes=[Hb[t]])
        for t in range(NT):
            make_HT(t, pool_tr)

        for layer in layers:
            for t in range(NT):
                dma("sp", hs[t * 128:(t + 1) * 128, :], H[:, t, :], reads=[Hb[t]], writes=[hsb[t]], owner=Hb[t])
            cx.barrier()
            cx.cur, cx.cur2 = base_mark
            cx.cur2, cx.lim2 = Hoff, Hoff + 65536
            mixT = alloc("mixT", [128, 8, L], BF16)
            mixb = [Buf(f"mix{t}") for t in range(NT)]

            if stop == 'ht':
                cx.barrier()
                return nc, cx
            for p in range(2):
                m0 = cx.mark()
                Wg = alloc("Wg", [128, 8, 784], BF16); b_Wg = Buf("Wg")
                gu = alloc("gu", [16, 128], BF16)
                nbias = alloc("nbias", [128, 1], F32)
                gain = alloc("gain", [128, 2, 128], F32)
                b_gp = Buf("gpar")
                rankT = alloc("rankT", [16, L], BF16); b_rank = Buf("rank")
                la = alloc("la", [128, L], F32, 1); b_la = Buf("la")
                bc = alloc("bc", [128, L], F32, 1); b_bc = Buf("bc")
                nbl = alloc("nbl", [128, 16], F32); dec = alloc("dec", [128, 16], F32); b_nbl = Buf("nbl")
                qdT = alloc("qdT", [128, L], BF16, 1); kiT = alloc("kiT", [128, L], BF16, 1); b_qk = Buf("gqk")
                kd = alloc("kd", [128, NT, 128], BF16, 1); b_kd = Buf("kd")
                vtm = alloc("vtm", [128, NT, 256], BF16, 1); b_v = Buf("gv")
                Sall = alloc("Sall", [128, NT, 128], BF16, 1); b_Sall = Buf("Sall")
                Sst = alloc("Sst", [128, 128], F32); b_S = Buf("S")
                e1 = alloc("e1", [128, 512], F32); e2 = alloc("e2", [128, 512], F32); e3 = alloc("e3", [128, 512], F32)
                b_e = [Buf("e1"), Buf("e2"), Buf("e3")]
                kdT = alloc("kdT", [128, 512], F32); b_kdT = Buf("kdT")
                AT = [alloc("AT0", [128, 128], BF16), alloc("AT1", [128, 128], BF16)]; b_AT = [Buf("AT0"), Buf("AT1")]
                sg = alloc("sg", [128, 256], F32); b_sg = Buf("sg")
                otm = alloc("otmg", [128, 256], F32); b_otm = Buf("otmg")
                sq = alloc("sq", [128, 256], F32); ssq = alloc("ssq", [128, 4], F32); b_ssq = Buf("ssq")

                load_w(Wg[:], w_in[layer][:, COFF[f"g{p}"]:COFF[f"g{p}"] + 784], b_Wg)
                dma("pool", gu[:], g_up[layer][:, p * 128:(p + 1) * 128], writes=[b_gp])
                dma("sp", nbias[:], g_bias[layer, p * 128:(p + 1) * 128].rearrange("(p o) -> p o", o=1), writes=[b_gp], owner=Buf("nb"))
                for hh in range(2):
                    dma("sp", gain[:, hh, :], g_gain[layer, :].partition_broadcast(128), writes=[b_gp], owner=Buf("gn"))
                op("dve", lambda e: e.tensor_scalar(out=nbias[:], in0=nbias[:], scalar1=-1.0, scalar2=None, op0=ALU.mult), [b_gp], [b_gp])
                chk('g1')
                pp = Rot(PS[0:4])
                for tc in range(4):
                    pt, pb = pp.get()
                    for c in range(8):
                        op("pe", lambda e, c=c: e.matmul(pt[0:16, 0:512], lhsT=Wg[:, c, 768:784], rhs=HT[:, c, tc * 512:(tc + 1) * 512],
                                                         start=(c == 0), stop=(c == 7)), [b_Wg] + HTb[tc * 4:tc * 4 + 4], [pb])
                    op("act", lambda e: e.activation(out=rankT[:, tc * 512:(tc + 1) * 512], in_=pt[0:16, 0:512], func=AF.Copy), [pb], [b_rank])
                for tc in range(4):
                    pt, pb = pp.get()
                    sl = slice(tc * 512, (tc + 1) * 512)
                    op("pe", lambda e: e.matmul(pt[:, 0:512], lhsT=gu[0:16, :], rhs=rankT[0:16, sl], start=True, stop=True), [b_gp, b_rank], [pb])
                    op("act", lambda e: e.activation(out=la[:, sl], in_=pt[:, 0:512], func=AF.Exp, bias=nbias[:, 0:1], scale=-1.0), [pb, b_gp], [b_la])
                op("act", lambda e: e.activation(out=la[:], in_=la[:], func=AF.Ln, bias=1.0, scale=1.0), [b_la], [b_la])
                chk('g2')
                for n in range(NT):
                    sl = slice(n * 128, (n + 1) * 128)
                    op("dve", lambda e: e.tensor_tensor_scan(out=bc[:, sl], data0=la[:, sl], data1=cst[:, 640:768], initial=0.0,
                                                             op0=ALU.add, op1=ALU.add), [b_la, b_cst], [b_bc])
                chk('g3')
                cx_chk = None
                op("dve", lambda e: e.tensor_scalar(out=nbl[:], in0=bc[:, :].rearrange("p (n c) -> p n c", c=128)[:, :, 127],
                                                    scalar1=-1.0 / 16.0, scalar2=None, op0=ALU.mult), [b_bc], [b_nbl])
                op("act", lambda e: e.activation(out=dec[:], in_=nbl[:], func=AF.Exp), [b_nbl], [b_nbl])
                chk('g3b')
                ptr = Rot(PS[4:6])
                for tc in range(4):
                    sl = slice(tc * 512, (tc + 1) * 512)
                    pq, pqb = pp.get()
                    pk, pkb = pp.get()
                    proj_fm(Wg, 0, 128, tc, pq, pqb, b_Wg)
                    proj_fm(Wg, 128, 128, tc, pk, pkb, b_Wg)
                    op("act", lambda e: e.activation(out=e1[:], in_=bc[:, sl], func=AF.Exp, scale=-1.0 / 16.0), [b_bc], [b_e[0]])
                    op("dve", lambda e: e.scalar_tensor_tensor(out=qdT[:, sl], in0=pq[:, :], scalar=0.125, in1=e1[:], op0=ALU.mult, op1=ALU.mult),
                       [pqb, b_e[0]], [b_qk])
                    op("act", lambda e: e.activation(out=e2[:], in_=bc[:, sl], func=AF.Exp, scale=1.0 / 16.0), [b_bc], [b_e[1]])
                    op("dve", lambda e: e.tensor_tensor(out=kiT[:, sl], in0=pk[:, :], in1=e2[:], op=ALU.mult), [pkb, b_e[1]], [b_qk])
                    for n4 in range(4):
                        n = tc * 4 + n4
                        op("act", lambda e, n=n, n4=n4: e.activation(out=e3[:, n4 * 128:(n4 + 1) * 128], in_=bc[:, n * 128:(n + 1) * 128], func=AF.Exp,
                                                                     bias=nbl[:, n:n + 1], scale=1.0 / 16.0), [b_bc, b_nbl], [b_e[2]])
                    op("dve", lambda e: e.tensor_tensor(out=kdT[:], in0=pk[:, :], in1=e3[:], op=ALU.mult), [pkb, b_e[2]], [b_kdT])
                    pt, pb = ptr.get()
                    for n4 in range(4):
                        op("pe", lambda e, n4=n4: e.transpose(out=pt[:, n4 * 128:(n4 + 1) * 128], in_=kdT[:, n4 * 128:(n4 + 1) * 128], identity=identF),
                           [b_kdT, b_cst], [pb])
                    op("act", lambda e: e.activation(out=kd[:, tc * 4:(tc + 1) * 4, :], in_=pt[:, :].rearrange("p (a b) -> p a b", a=4), func=AF.Copy),
                       [pb], [b_kd])
                chk('g4')
                for t in range(NT):
                    pt, pb = pp.get()
                    proj_tm(Wg, 256, 256, t, pt, pb, b_Wg)
                    op("act", lambda e: e.activation(out=vtm[:, t, :], in_=pt[:, 0:256], func=AF.Copy), [pb], [b_v])
                chk('g5')
                op("dve", lambda e: e.memset(Sst[:], 0.0), [], [b_S])
                op("dve", lambda e: e.memset(Sall[:, 0, :], 0.0), [], [b_Sall])
                for n in range(NT - 1):
                    pt, pb = pp.get()
                    for hh in range(2):
                        op("pe", lambda e, hh=hh: e.matmul(pt[hh * 64:(hh + 1) * 64, 0:128], lhsT=kd[:, n, hh * 64:(hh + 1) * 64],
                                                           rhs=vtm[:, n, hh * 128:(hh + 1) * 128], start=True, stop=True), [b_kd, b_v], [pb])
                    op("dve", lambda e: e.scalar_tensor_tensor(out=Sst[:], in0=Sst[:], scalar=dec[:, n:n + 1], in1=pt[:, 0:128],
                                                               op0=ALU.mult, op1=ALU.add), [b_S, b_nbl, pb], [b_S])
                    op("act", lambda e: e.activation(out=Sall[:, n + 1, :], in_=Sst[:], func=AF.Copy), [b_S], [b_Sall])
                chk('g6')
                ps_o = Rot(PS[4:6]); ps_g = Rot(PS[6:8])
                o_all = alloc("o_all", [128, NT, 256], F32); b_oall = [Buf(f"oall{n}") for n in range(NT)]
                sg_all = alloc("sg_all", [128, NT, 256], F32); b_sgall = [Buf(f"sgall{n}") for n in range(NT)]
                ssq_all = alloc("ssq_all", [128, NT, 2], F32); rstd_all = alloc("rstd_all", [128, NT, 2], F32); b_ssqa = Buf("ssqa")
                otms = [(alloc(f"otmg{i}", [128, 256], F32), Buf(f"otmg{i}")) for i in range(2)]
                for n in range(NT):
                    sl = slice(n * 128, (n + 1) * 128)
                    po, pob = ps_o.get()
                    for hh in range(2):
                        bp = hh * 64
                        pa, pab = pp.get()
                        op("pe", lambda e: e.matmul(pa[:, 0:128], lhsT=kiT[bp:bp + 64, sl], rhs=qdT[bp:bp + 64, sl], start=True, stop=True), [b_qk], [pab])
                        op("dve", lambda e: e.tensor_tensor(out=AT[hh][:], in0=pa[:, 0:128], in1=tri01, op=ALU.mult), [pab, b_cst], [b_AT[hh]])
                        op("pe", lambda e: e.matmul(po[:, hh * 128:(hh + 1) * 128], lhsT=AT[hh][:], rhs=vtm[:, n, hh * 128:(hh + 1) * 128],
                                                    start=True, stop=False, skip_group_check=True), [b_AT[hh], b_v], [pob])
                        op("pe", lambda e: e.matmul(po[:, hh * 128:(hh + 1) * 128], lhsT=qdT[bp:bp + 64, sl], rhs=Sall[bp:bp + 64, n, :],
                                                    start=False, stop=True, skip_group_check=True), [b_qk, b_Sall], [pob])
                    for hh in range(2):
                        op("act", lambda e, hh=hh: e.activation(out=sq[:, hh * 128:(hh + 1) * 128], in_=po[:, hh * 128:(hh + 1) * 128], func=AF.Square,
                                                                accum_out=ssq_all[:, n, hh:hh + 1]), [pob], [b_ssqa])
                    op("act", lambda e: e.activation(out=o_all[:, n, :], in_=po[:, 0:256], func=AF.Copy), [pob], [b_oall[n]])
                op("act", lambda e: e.activation(out=rstd_all[:, :, :], in_=ssq_all[:, :, :], func=AF.Sqrt, bias=epsln[:, 1:2], scale=1.0 / 128.0), [b_ssqa, b_cst], [b_ssqa])
                op("dve", lambda e: e.reciprocal(out=rstd_all[:, :, :], in_=rstd_all[:, :, :]), [b_ssqa], [b_ssqa])
                for n in range(NT):
                    pg, pgb = ps_g.get()
                    proj_tm(Wg, 512, 256, n, pg, pgb, b_Wg)
                    op("act", lambda e: e.activation(out=sg_all[:, n, :], in_=pg[:, 0:256], func=AF.Silu), [pgb], [b_sgall[n]])
                    op("pool", lambda e: e.tensor_tensor(out=sg_all[:, n, :], in0=sg_all[:, n, :], in1=gain[:, :, :].rearrange("p a b -> p (a b)"), op=ALU.mult),
                       [b_sgall[n], b_gp], [b_sgall[n]])
                for n in range(NT):
                    sl = slice(n * 128, (n + 1) * 128)
                    otm, b_otm = otms[n % 2]
                    for hh in range(2):
                        op("dve", lambda e, hh=hh: e.scalar_tensor_tensor(out=otm[:, hh * 128:(hh + 1) * 128], in0=o_all[:, n, hh * 128:(hh + 1) * 128],
                                                                          scalar=rstd_all[:, n, hh:hh + 1], in1=sg_all[:, n, hh * 128:(hh + 1) * 128],
                                                                          op0=ALU.mult, op1=ALU.mult), [b_oall[n], b_ssqa, b_sgall[n]], [b_otm])
                    pt, pb = ps_o.get()
                    for hh in range(2):
                        op("pe", lambda e, hh=hh: e.transpose(out=pt[:, hh * 128:(hh + 1) * 128], in_=otm[:, hh * 128:(hh + 1) * 128], identity=identF),
                           [b_otm, b_cst], [pb])
                    op("act", lambda e: e.activation(out=mixT[:, 2 * p:2 * p + 2, sl], in_=pt[:, 0:256].rearrange("p (a b) -> p a b", a=2), func=AF.Copy),
                       [pb], [mixb[n]])
                cx.release(m0)
                chk('p0')

            if stop == 'gla':
                cx.barrier()
                return nc, cx
            def attn_common():
                qT = alloc("qT", [128, 2, L], BF16, 1); kT = alloc("kT", [128, 2, L], BF16, 1)
                vaug = alloc("vaug", [128, NT, 4, 65], BF16, 1)
                return qT, kT, vaug, Buf("aqk"), Buf("av")

            m0 = cx.mark()
            qT, kT, vaug, b_qk, b_v = attn_common()
            kmT = alloc("kmT", [128, 2, 8], F32); kmB = alloc("kmB", [128, 2, 8], BF16); b_km = Buf("km")
            mbT = alloc("mbT", [8, 4, L], BF16); b_mb = Buf("mbT")
            m1 = cx.mark()
            cosT = alloc("cosT", [128, L], F32); sinT = alloc("sinT", [128, L], F32); b_tab = Buf("tab")
            rope_tables(seq, cosT, sinT, b_tab)
            chk('m1')
            Wa = alloc("Wa", [128, 8, 512], BF16); b_Wa = Buf("Wa")
            Wb = alloc("Wb", [128, 8, 512], BF16); b_Wb = Buf("Wb")
            t1 = alloc("t1", [128, 512], F32); t2 = alloc("t2", [128, 512], F32); b_tmp = Buf("rt")
            pp = Rot(PS[0:6])
            load_w(Wa[:], w_in[layer][:, COFF["mq"]:COFF["mq"] + 512], b_Wa)
            load_w(Wb[:], w_in[layer][:, COFF["mk"]:COFF["mk"] + 512], b_Wb)
            roped_proj(Wa, b_Wa, 0, 256, qT, b_qk, cosT, sinT, b_tab, (t1, t2, b_tmp), pp)
            roped_proj(Wb, b_Wb, 0, 256, kT, b_qk, cosT, sinT, b_tab, (t1, t2, b_tmp), pp, ksum=(kmT, b_km))
            chk('m2a')
            load_w(Wa[:, :, 0:256], w_in[layer][:, COFF["mv"]:COFF["mv"] + 256], b_Wa)
            v_aug_proj(Wa, b_Wa, 0, vaug, b_v, pp)
            op("dve", lambda e: e.tensor_copy(out=kmB[:], in_=kmT[:]), [b_km], [b_km])
            cx.release(m1)
            chk('m2')
            Z = alloc("Z", [128, 4, 8], F32); gm = alloc("gm", [128, 4, 8], F32); top8 = alloc("top8", [128, 4, 8], F32); b_Z = Buf("Z")
            otm = alloc("otm", [128, 4, 256], F32); b_otm = Buf("otm")
            pTl = [(alloc(f"pT{i}", [128, 512], BF16), Buf(f"pT{i}")) for i in range(3)]
            rs = alloc("rs", [128, 4], F32); b_rs = Buf("rs")
            op("pool", lambda e: e.memset(mbT[:], 0.0), [], [b_mb])
            pg = Rot(PS[0:2])
            for t in range(8, NT):
                pt, pb = pg.get()
                for h in range(4):
                    ptile, bp = h // 2, (h % 2) * 64
                    op("pe", lambda e, h=h: e.matmul(pt[:, h * 8:(h + 1) * 8], lhsT=qT[bp:bp + 64, ptile, t * 128:(t + 1) * 128],
                                                     rhs=kmB[bp:bp + 64, ptile, :], start=True, stop=False, skip_group_check=True), [b_qk, b_km], [pb])
                    op("pe", lambda e, h=h: e.matmul(pt[:, h * 8:(h + 1) * 8], lhsT=onehotK[0:8, t * 128:(t + 1) * 128],
                                                     rhs=bpen, start=False, stop=True, skip_group_check=True), [b_cst], [pb])
                op("dve", lambda e: e.tensor_copy(out=gm[:], in_=pt[:, 0:32].rearrange("p (a b) -> p a b", a=4)), [pb], [b_Z])
                for h in range(4):
                    op("dve", lambda e, h=h: e.max(out=top8[:, h, :], in_=gm[:, h, :]), [b_Z], [b_Z])
                for h in range(4):
                    op("dve", lambda e, h=h: e.tensor_scalar(out=Z[:, h, :], in0=gm[:, h, :], scalar1=top8[:, h, 3:4], scalar2=NEG,
                                                             op0=ALU.is_lt, op1=ALU.mult), [b_Z], [b_Z])
                pt2, pb2 = pg.get()
                for h in range(4):
                    op("pe", lambda e, h=h: e.transpose(out=pt2[0:8, h * 128:(h + 1) * 128], in_=Z[:, h, :], identity=identF), [b_Z, b_cst], [pb2])
                op("dve", lambda e: e.tensor_copy(out=mbT[0:8, :, t * 128:(t + 1) * 128], in_=pt2[0:8, :].rearrange("p (a b) -> p a b", a=4)),
                   [pb2], [b_mb])
            chk('m3')

            def moba_mask(h, qc, j, t0, st, sb, wq):
                q0 = t0 * 128
                diag = (j >= 4 * qc)
                op("pe", lambda e: e.matmul(st[:, 0:wq], lhsT=onehotK[0:8, j * 128:(j + 1) * 128], rhs=mbT[0:8, h, q0:q0 + wq],
                                            start=False, stop=(not diag), skip_group_check=True), [b_cst, b_mb], [sb])
                if diag:
                    op("pe", lambda e: e.matmul(st[:, 0:128], lhsT=trinegB[:], rhs=identB[:], start=False, stop=True, skip_group_check=True), [b_cst], [sb])

            attention(qT, kT, b_qk, vaug, b_v, moba_mask, 4, (Rot(PS[2:5]), Rot(PS[5:7]), Rot(PS[7:8])), otm, b_otm, Rot(pTl), rs, b_rs, mixT, mixb)
            cx.release(m0)

            if stop == 'moba':
                cx.barrier()
                return nc, cx
            m0 = cx.mark()
            qT, kT, vaug, b_qk, b_v = attn_common()
            kiT = alloc("kiT", [128, 1, L], BF16, 1); b_ki = Buf("ki")
            wT = alloc("wT", [8, L], BF16, 1); b_wT = Buf("wT")
            qiT = alloc("qiT", [128, 4, L], BF16, 1); b_qi = Buf("qi")
            m1 = cx.mark()
            cosT = alloc("cosT", [128, L], F32); sinT = alloc("sinT", [128, L], F32); b_tab = Buf("tab")
            rope_tables(seq, cosT, sinT, b_tab)
            Wa = alloc("Wa", [128, 8, 512], BF16); b_Wa = Buf("Wa")
            Wb = alloc("Wb", [128, 8, 512], BF16); b_Wb = Buf("Wb")
            t1 = alloc("t1", [128, 512], F32); t2 = alloc("t2", [128, 512], F32); b_tmp = Buf("rt")
            pp = Rot(PS[0:6])
            tm3 = (t1, t2, b_tmp)
            load_w(Wa[:], w_in[layer][:, COFF["sq"]:COFF["sq"] + 512], b_Wa)
            load_w(Wb[:], w_in[layer][:, COFF["sk"]:COFF["sk"] + 512], b_Wb)
            roped_proj(Wa, b_Wa, 0, 256, qT, b_qk, cosT, sinT, b_tab, tm3, pp)
            roped_proj(Wb, b_Wb, 0, 256, kT, b_qk, cosT, sinT, b_tab, tm3, pp)
            load_w(Wa[:], w_in[layer][:, COFF["iq"]:COFF["iq"] + 512], b_Wa)
            load_w(Wb[:], w_in[layer][:, COFF["iqr"]:COFF["iqr"] + 512], b_Wb)
            for ti in range(4):
                for tc in range(4):
                    py, pyb = pp.get(); pr, prb = pp.get()
                    proj_fm(Wa, ti * 128, 128, tc, py, pyb, b_Wa)
                    proj_fm(Wb, ti * 128, 128, tc, pr, prb, b_Wb)
                    sl = slice(tc * 512, (tc + 1) * 512)
                    op("dve", lambda e: e.tensor_tensor(out=t1[:], in0=pr[:, :], in1=sinT[:, sl], op=ALU.mult), [prb, b_tab], [b_tmp])
                    op("dve", lambda e: e.tensor_tensor(out=t2[:], in0=py[:, :], in1=cosT[:, sl], op=ALU.mult), [pyb, b_tab], [b_tmp])
                    op("pool", lambda e: e.tensor_tensor(out=qiT[:, ti, sl], in0=t1[:], in1=t2[:], op=ALU.add), [b_tmp], [b_qi])
            load_w(Wa[:, :, 0:264], w_in[layer][:, COFF["ik"]:COFF["ik"] + 264], b_Wa)
            load_w(Wb[:, :, 0:256], w_in[layer][:, COFF["sv"]:COFF["sv"] + 256], b_Wb)
            roped_proj(Wa, b_Wa, 0, 128, kiT, b_ki, cosT, sinT, b_tab, tm3, pp)
            for tc in range(4):
                pt, pb = pp.get()
                for c in range(8):
                    op("pe", lambda e, c=c: e.matmul(pt[0:8, 0:512], lhsT=Wa[:, c, 256:264], rhs=HT[:, c, tc * 512:(tc + 1) * 512],
                                                     start=(c == 0), stop=(c == 7)), [b_Wa] + HTb[tc * 4:tc * 4 + 4], [pb])
                op("act", lambda e: e.activation(out=wT[:, tc * 512:(tc + 1) * 512], in_=pt[0:8, 0:512], func=AF.Copy), [pb], [b_wT])
            v_aug_proj(Wb, b_Wb, 0, vaug, b_v, pp)
            cx.release(m1)
            mbs = [HT[:, 0:4, :], alloc("mbB", [128, 4, L], BF16)]
            b_mb2 = [[Buf(f"mb{a}_{i}") for i in range(4)] for a in range(2)]
            isc = [HT[:, 4:6, :].bitcast(F32).rearrange("p a b -> p (a b)"), HT[:, 6:8, :].bitcast(F32).rearrange("p a b -> p (a b)"),
                   alloc("isc2", [128, L], F32), alloc("isc3", [128, L], F32)]
            b_isc = [Buf(f"isc{i}") for i in range(4)]
            qs = alloc("qs", [128, 4, 2, 512], BF16); b_qs = Buf("qs")
            otm = alloc("otm", [128, 4, 256], F32); b_otm = Buf("otm")
            pTl = [(alloc(f"pT{i}", [128, 512], BF16), Buf(f"pT{i}")) for i in range(3)]
            rs = alloc("rs", [128, 4], F32); b_rs = Buf("rs")
            bsA = alloc("bsA", [128, 32], F32); bsi = alloc("bsi", [128, 4], I32)
            lo = bsA[:, 0:4]; w0 = bsA[:, 4:8]; mid = bsA[:, 8:12]; nmid = bsA[:, 12:16]; cntv = bsA[:, 16:20]; thc = bsA[:, 20:24]; mx = bsA[:, 24:28]
            b_lo = Buf("lo"); b_mid = Buf("mid"); b_cnt = [Buf(f"cnt{i}") for i in range(4)]; b_msk = Buf("msk")
            junkD = alloc("junkD", [128, L], BF16); b_junkD = Buf("junkD")
            junkA = alloc("junkA", [128, L], BF16); b_junkA = Buf("junkA")
            ps_i = Rot(PS[0:3])
            rls = Rot([(alloc(f"rl{i}", [128, 512], BF16), Buf(f"rl{i}")) for i in range(4)])

            def dsa_qc(qc):
                mb = mbs[qc % 2]; b_mbq = b_mb2[qc % 2]
                qsl = slice(qc * 512, (qc + 1) * 512)
                for pr_ in range(4):
                    pt, pb = ps_i.get()
                    op("pe", lambda e: e.matmul(pt[:, 0:512], lhsT=selw[0:8, pr_ * 128:(pr_ + 1) * 128], rhs=wT[0:8, qsl], start=True, stop=True),
                       [b_cst, b_wT], [pb])
                    op("dve", lambda e: e.scalar_tensor_tensor(out=qs[:, pr_, 0, :], in0=pt[:, 0:512], scalar=0.0, in1=qiT[:, pr_, qsl],
                                                               op0=ALU.max, op1=ALU.mult), [pb, b_qi], [b_qs])
                    op("dve", lambda e: e.scalar_tensor_tensor(out=qs[:, pr_, 1, :], in0=pt[:, 0:512], scalar=0.0, in1=qiT[:, pr_, qsl],
                                                               op0=ALU.min, op1=ALU.mult), [pb, b_qi], [b_qs])
                act_tiles = []
                for i in range(4):
                    t = 4 * qc + i
                    S = (t + 1) * 128
                    if t < 2:
                        if t > 0:
                            op("pool", lambda e: e.memset(mb[:, i, 0:t * 128], 0.0), [], [b_mbq[i]])
                        op("pool", lambda e: e.tensor_copy(out=mb[:, i, t * 128:S], in_=trinegB[:]), [b_cst], [b_mbq[i]])
                        continue
                    act_tiles.append(i)
                    ic, icb = isc[i], b_isc[i]
                    for kc in range((S + 511) // 512):
                        w = min(512, S - kc * 512)
                        ksl = slice(kc * 512, kc * 512 + w)
                        first = True
                        nsum = 0
                        p2, p2b = PS[3]
                        pend = []

                        def flush_one():
                            nonlocal nsum
                            (rl_, rlb_, sg_) = pend.pop(0)
                            op("pe", lambda e: e.matmul(p2[:, 0:w], lhsT=(identB[:] if sg_ == 0 else negidentB[:]), rhs=rl_[:, 0:w],
                                                        start=(nsum == 0), stop=(nsum == 7)), [rlb_, b_cst], [p2b])
                            nsum += 1

                        for k in range(16):
                            hd, sgn = k // 2, k % 2
                            pr_, bp = hd // 2, (hd % 2) * 64
                            pt, pb = ps_i.get()
                            op("pe", lambda e: e.matmul(pt[:, 0:w], lhsT=qs[bp:bp + 64, pr_, sgn, i * 128:(i + 1) * 128], rhs=kiT[bp:bp + 64, 0, ksl],
                                                        start=True, stop=True), [b_qs, b_ki], [pb])
                            o = ALU.max if sgn == 0 else ALU.min
                            if hd % 2 == 1:
                                rl, rlb = rls.get()
                                op("act", lambda e: e.activation(out=rl[:, 0:w], in_=pt[:, 0:w], func=AF.Relu, scale=(1.0 if sgn == 0 else -1.0)), [pb], [rlb])
                                pend.append((rl, rlb, sgn))
                                if len(pend) > 2:
                                    flush_one()
                            elif first:
                                op("dve", lambda e: e.tensor_scalar(out=ic[:, ksl], in0=pt[:, 0:w], scalar1=0.0, scalar2=None, op0=o), [pb], [icb])
                                first = False
                            else:
                                op("dve", lambda e: e.scalar_tensor_tensor(out=ic[:, ksl], in0=pt[:, 0:w], scalar=0.0, in1=ic[:, ksl], op0=o, op1=ALU.add),
                                   [pb, icb], [icb])
                        while pend:
                            flush_one()
                        op("dve", lambda e: e.tensor_tensor(out=ic[:, ksl], in0=p2[:, 0:w], in1=ic[:, ksl], op=ALU.add), [p2b, icb], [icb])
                    op("dve", lambda e: e.tensor_reduce(out=lo[:, i:i + 1], in_=ic[:, 0:S], axis=AX.X, op=ALU.min), [icb], [b_lo])
                    op("dve", lambda e: e.tensor_reduce(out=mx[:, i:i + 1], in_=ic[:, 0:S], axis=AX.X, op=ALU.max), [icb], [b_lo])
                    op("dve", lambda e: e.tensor_tensor(out=ic[:, t * 128:S], in0=ic[:, t * 128:S], in1=trinegbig, op=ALU.add), [icb, b_cst], [icb])
                    on_act = (i % 2 == 1)
                    op("dve", lambda e: e.memset(thc[:, i:i + 1], (S - 511.0) if on_act else (S - 255.5)), [], [b_lo])
                a0, a1 = act_tiles[0], act_tiles[-1] + 1
                asl = slice(a0, a1)
                op("dve", lambda e: e.scalar_tensor_tensor(out=w0[:, asl], in0=mx[:, asl], scalar=1.0, in1=lo[:, asl], op0=ALU.add, op1=ALU.subtract),
                   [b_lo], [b_lo])
                for it in range(N_BISECT):
                    ck = 0.5 ** (it + 1)
                    op("dve", lambda e, ck=ck: e.scalar_tensor_tensor(out=mid[:, asl], in0=w0[:, asl], scalar=ck, in1=lo[:, asl], op0=ALU.mult, op1=ALU.add),
                       [b_lo], [b_mid])
                    for i in act_tiles:
                        S = (4 * qc + i + 1) * 128
                        ic, icb = isc[i], b_isc[i]
                        if i % 2 == 1:
                            op("act", lambda e, i=i, S=S, ic=ic: e.activation(out=junkA[:, 0:S], in_=ic[:, 0:S], func=AF.Sign, bias=mid[:, i:i + 1], scale=-1.0,
                                                                              accum_out=cntv[:, i:i + 1]), [icb, b_mid], [b_cnt[i], b_junkA])
                        else:
                            op("dve", lambda e, i=i, S=S, ic=ic: e.tensor_scalar(out=junkD[:, 0:S], in0=ic[:, 0:S], scalar1=mid[:, i:i + 1], scalar2=None,
                                                                                 op0=ALU.is_lt, op1=ALU.add, accum_out=cntv[:, i:i + 1]), [icb, b_mid], [b_cnt[i], b_junkD])
                    op("dve", lambda e: e.tensor_tensor(out=bsi[:, asl], in0=cntv[:, asl], in1=thc[:, asl], op=ALU.is_le), [b_cnt[i] for i in act_tiles] + [b_lo], [b_msk])
                    op("dve", lambda e: e.copy_predicated(out=lo[:, asl], mask=bsi[:, asl], data=mid[:, asl]), [b_msk, b_mid], [b_lo])
                for i in act_tiles:
                    S = (4 * qc + i + 1) * 128
                    ic, icb = isc[i], b_isc[i]
                    op("dve", lambda e, i=i, S=S, ic=ic: e.tensor_scalar(out=mb[:, i, 0:S], in0=ic[:, 0:S], scalar1=lo[:, i:i + 1], scalar2=NEG, op0=ALU.is_lt, op1=ALU.mult),
                       [icb, b_lo], [b_mbq[i]])

            def dsa_mask(h, qc, j, t0, st, sb, wq):
                last = 4 * qc + 3
                mb = mbs[qc % 2]; b_mbq = b_mb2[qc % 2]
                for t in range(t0, last + 1):
                    i = t - 4 * qc
                    op("pe", lambda e, t=t, i=i: e.matmul(st[:, (t - t0) * 128:(t - t0 + 1) * 128], lhsT=mb[:, i, j * 128:(j + 1) * 128], rhs=identB[:],
                                                          start=False, stop=(t == last), skip_group_check=True), [b_mbq[i], b_cst], [sb])

            attention(qT, kT, b_qk, vaug, b_v, dsa_mask, 6, (Rot(PS[4:6]), Rot(PS[6:8]), Rot(PS[3:4])), otm, b_otm, Rot(pTl), rs, b_rs, mixT, mixb,
                      qc_hook=dsa_qc)
            cx.release(m0)

            if stop == 'dsa':
                cx.barrier()
                return nc, cx
            if debug and seq == 0 and layer == layers[0]:
                dma("sp", dbg[:, :, :], mixT[:, :, :], reads=mixb, owner=Buf("dbg"))

            cx.cur2 = Hoff + 65536
            m0 = cx.mark()
            Wo = alloc("Wo", [128, 8, D], BF16); b_Wo = Buf("Wo")
            gbc = alloc("gbc", [128, D], F32); bbc = alloc("bbc", [128, D], F32); b_par = Buf("lnpar")
            hres = [alloc("hres0", [128, D], F32), alloc("hres1", [128, D], F32)]; b_hres = [Buf("hres0"), Buf("hres1")]
            stat = alloc("stat", [128, 16], F32); b_stat = Buf("stat")
            rw = alloc("rw", [128, 8, 16], F32); rwh = alloc("rwh", [128, 8, 16], BF16); rwl = alloc("rwl", [128, 8, 16], BF16); b_rw = Buf("rw")
            rbb = alloc("rbb", [128, 16], F32)
            HTlo = alloc("HTlo", [128, 8, 128], BF16); b_lo = Buf("HTlo")
            comb = alloc("comb", [128, NT, 16], F32); b_comb = Buf("comb")
            rt = alloc("rt", [128, 8, 16], F32); b_rt = Buf("rt")
            load_w(Wo[:], w_out[layer][:, :], b_Wo)
            dma("sp", gbc[:], ln1g[layer, :].partition_broadcast(128), writes=[b_par])
            dma("sp", bbc[:], ln1b[layer, :].partition_broadcast(128), writes=[b_par], owner=Buf("lnb"))
            dma("sp", rw[:], r_w.rearrange("(c p) e -> p c e", p=128), writes=[b_rw])
            dma("sp", rbb[:], r_b.partition_broadcast(128), writes=[b_rw], owner=Buf("rbb"))
            op("dve", lambda e: e.tensor_copy(out=rwh[:], in_=rw[:]), [b_rw], [b_rw])
            op("dve", lambda e: e.tensor_tensor(out=rw[:], in0=rw[:], in1=rwh[:], op=ALU.subtract), [b_rw], [b_rw])
            op("dve", lambda e: e.tensor_copy(out=rwl[:], in_=rw[:]), [b_rw], [b_rw])
            ps_o = Rot(PS[0:4]); ps_t = Rot(PS[4:7]); ps_r = Rot(PS[7:8])
            def ln1_front(t):
                hr, hrb = hres[t % 2], b_hres[t % 2]
                dma("sp", hr[:], hs[t * 128:(t + 1) * 128, :], reads=[hsb[t]], writes=[hrb])
                for half in range(2):
                    po, pob = ps_o.get()
                    for c in range(8):
                        op("pe", lambda e, c=c: e.matmul(po[:, 0:512], lhsT=mixT[:, c, t * 128:(t + 1) * 128], rhs=Wo[:, c, half * 512:(half + 1) * 512],
                                                         start=(c == 0), stop=(c == 7)), [mixb[t], b_Wo], [pob])
                    op("dve", lambda e: e.scalar_tensor_tensor(out=H[:, t, half * 512:(half + 1) * 512], in0=hr[:, half * 512:(half + 1) * 512], scalar=ALPHA,
                                                               in1=po[:, 0:512], op0=ALU.mult, op1=ALU.add), [hrb, pob], [Hb[t]])
                layer_norm_tile(t, gbc, bbc, b_par, stat, b_stat)

            def ln1_back(t):
                for half in range(2):
                    pt, pb = ps_t.get()
                    for c4 in range(4):
                        c = half * 4 + c4
                        op("pe", lambda e, c=c, c4=c4: e.transpose(out=pt[:, c4 * 128:(c4 + 1) * 128], in_=H[:, t, c * 128:(c + 1) * 128], identity=identF),
                           [Hb[t], b_cst], [pb])
                    pv = pt[:, :].rearrange("p (a b) -> p a b", a=4)
                    op("act", lambda e: e.activation(out=HT[:, half * 4:(half + 1) * 4, t * 128:(t + 1) * 128], in_=pv, func=AF.Copy), [pb], [HTb[t]])
                    op("dve", lambda e: e.tensor_tensor(out=HTlo[:, half * 4:(half + 1) * 4, :], in0=pv, in1=HT[:, half * 4:(half + 1) * 4, t * 128:(t + 1) * 128],
                                                        op=ALU.subtract), [pb, HTb[t]], [b_lo])
                pr_, prb = ps_r.get()
                k = 0
                for (A_, Ab, W_) in ((HT[:, :, t * 128:(t + 1) * 128], HTb[t], rwh), (HTlo[:, :, :], b_lo, rwh), (HT[:, :, t * 128:(t + 1) * 128], HTb[t], rwl)):
                    for c in range(8):
                        op("pe", lambda e, c=c, A_=A_, W_=W_, k=k: e.matmul(pr_[:, 0:16], lhsT=A_[:, c, :], rhs=W_[:, c, :], start=(k == 0), stop=(k == 23)),
                           [Ab, b_rw], [prb])
                        k += 1
                sc = rt[:, 0, :]; sel = rt[:, 1, :]; tmpa = rt[:, 2, :]; tmpb = rt[:, 3, :]; ch = rt[:, 4, :]
                m1 = rt[:, 5, 0:4]; m2 = rt[:, 5, 4:8]; gs = rt[:, 5, 8:12]; ing = rt[:, 5, 12:16]
                gmax = rt[:, 6, 0:1]; a1 = rt[:, 6, 1:2]; a2 = rt[:, 6, 2:3]; ssum = rt[:, 6, 3:4]
                v3 = lambda a: a.rearrange("p (g e) -> p g e", g=4)
                R = [b_rt]
                op("act", lambda e: e.activation(out=sc, in_=pr_[:, 0:16], func=AF.Sigmoid), [prb], R)
                op("dve", lambda e: e.tensor_tensor(out=sel, in0=sc, in1=rbb[:], op=ALU.add), R + [b_rw], R)
                op("dve", lambda e: e.tensor_reduce(out=m1, in_=v3(sel), axis=AX.X, op=ALU.max), R, R)
                op("dve", lambda e: e.tensor_tensor(out=v3(tmpa), in0=v3(sel), in1=m1[:, :, None].to_broadcast([128, 4, 4]), op=ALU.is_equal), R, R)
                op("dve", lambda e: e.scalar_tensor_tensor(out=tmpb, in0=tmpa, scalar=-1.0e9, in1=sel, op0=ALU.mult, op1=ALU.add), R, R)
                op("dve", lambda e: e.tensor_reduce(out=m2, in_=v3(tmpb), axis=AX.X, op=ALU.max), R, R)
                op("dve", lambda e: e.tensor_tensor(out=gs, in0=m1, in1=m2, op=ALU.add), R, R)
                op("dve", lambda e: e.tensor_reduce(out=gmax, in_=gs, axis=AX.X, op=ALU.max), R, R)
                op("dve", lambda e: e.tensor_scalar(out=ing, in0=gs, scalar1=gmax, scalar2=-1.0e9, op0=ALU.is_lt, op1=ALU.mult), R, R)
                op("dve", lambda e: e.tensor_tensor(out=v3(tmpa), in0=v3(sel), in1=ing[:, :, None].to_broadcast([128, 4, 4]), op=ALU.add), R, R)
                op("dve", lambda e: e.tensor_reduce(out=a1, in_=tmpa, axis=AX.X, op=ALU.max), R, R)
                op("dve", lambda e: e.tensor_scalar(out=ch, in0=tmpa, scalar1=a1, scalar2=None, op0=ALU.is_ge), R, R)
                op("dve", lambda e: e.scalar_tensor_tensor(out=tmpb, in0=ch, scalar=-1.0e9, in1=tmpa, op0=ALU.mult, op1=ALU.add), R, R)
                op("dve", lambda e: e.tensor_reduce(out=a2, in_=tmpb, axis=AX.X, op=ALU.max), R, R)
                op("dve", lambda e: e.tensor_scalar(out=tmpa, in0=tmpb, scalar1=a2, scalar2=None, op0=ALU.is_ge), R, R)
                op("dve", lambda e: e.tensor_tensor(out=ch, in0=ch, in1=tmpa, op=ALU.add), R, R)
                op("dve", lambda e: e.tensor_tensor(out=tmpb, in0=ch, in1=sc, op=ALU.mult), R, R)
                op("dve", lambda e: e.tensor_reduce(out=ssum, in_=tmpb, axis=AX.X, op=ALU.add), R, R)
                op("dve", lambda e: e.reciprocal(out=ssum, in_=ssum), R, R)
                op("dve", lambda e: e.tensor_scalar(out=comb[:, t, :], in0=tmpb, scalar1=ssum, scalar2=None, op0=ALU.mult), R, [b_comb])
                op("act", lambda e: e.activation(out=H[:, t, :], in_=H[:, t, :], func=AF.Copy, scale=ALPHA), [Hb[t]], [Hb[t]])

            ln1_front(0)
            for t in range(NT):
                if t + 1 < NT:
                    ln1_front(t + 1)
                ln1_back(t)
            cx.barrier()
            cx.cur, cx.cur2 = base_mark
            cx.cur2 = Hoff + 65536
            comb2 = alloc("comb2", [128, NT, 16], F32); b_comb2 = Buf("comb2")
            op("dve", lambda e: e.tensor_copy(out=comb2[:], in_=comb[:]), [b_comb], [b_comb2])
            cx.barrier()

            if stop == 'ln1':
                cx.barrier()
                return nc, cx
            m0 = cx.mark()
            Wg_ = [alloc(f"weg{i}", [128, 8, 512], BF16) for i in range(2)]; b_weg = [Buf("weg0"), Buf("weg1")]
            Wu_ = [alloc(f"weu{i}", [128, 8, 512], BF16) for i in range(2)]; b_weu = [Buf("weu0"), Buf("weu1")]
            Wd_ = [alloc(f"wed{i}", [128, 4, D], BF16) for i in range(2)]; b_wed = [Buf("wed0"), Buf("wed1")]
            actT = alloc("actT", [128, 4, L], BF16); b_act = [[Buf(f"act{f}_{tc}") for tc in range(4)] for f in range(4)]
            sgb = [(alloc(f"sgm{i}", [128, 512], F32), Buf(f"sgm{i}")) for i in range(3)]
            sgr = Rot(sgb)
            ps_gu = Rot(PS[0:4]); ps_d = Rot(PS[4:8])
            for ex in range(16):
                wg, wu, wd = Wg_[ex % 2], Wu_[ex % 2], Wd_[ex % 2]
                bg, bu, bd = b_weg[ex % 2], b_weu[ex % 2], b_wed[ex % 2]
                load_w(wg[:], w_eg[layer, ex], bg)
                load_w(wu[:], w_eu[layer, ex], bu)
                dma("pool", wd[:], w_ed[layer, ex].rearrange("(c p) f -> p c f", p=128), writes=[bd])
                for tc in range(4):
                    for f in range(4):
                        pg_, pgb = ps_gu.get(); pu_, pub = ps_gu.get()
                        for c in range(8):
                            op("pe", lambda e, c=c: e.matmul(pg_[:, 0:512], lhsT=wg[:, c, f * 128:(f + 1) * 128], rhs=HT[:, c, tc * 512:(tc + 1) * 512],
                                                             start=(c == 0), stop=(c == 7)), [bg] + HTb[tc * 4:tc * 4 + 4], [pgb])
                        for c in range(8):
                            op("pe", lambda e, c=c: e.matmul(pu_[:, 0:512], lhsT=wu[:, c, f * 128:(f + 1) * 128], rhs=HT[:, c, tc * 512:(tc + 1) * 512],
                                                             start=(c == 0), stop=(c == 7)), [bu] + HTb[tc * 4:tc * 4 + 4], [pub])
                        sgt, sgtb = sgr.get()
                        op("act", lambda e: e.activation(out=sgt[:], in_=pg_[:, 0:512], func=AF.Silu), [pgb], [sgtb])
                        op("dve", lambda e: e.tensor_tensor(out=actT[:, f, tc * 512:(tc + 1) * 512], in0=pu_[:, 0:512], in1=sgt[:], op=ALU.mult),
                           [pub, sgtb], [b_act[f][tc]])
                    for t4 in range(4):
                        t = tc * 4 + t4
                        for half in range(2):
                            pd_, pdb = ps_d.get()
                            for f in range(4):
                                op("pe", lambda e, f=f: e.matmul(pd_[:, 0:512], lhsT=actT[:, f, t * 128:(t + 1) * 128], rhs=wd[:, f, half * 512:(half + 1) * 512],
                                                                 start=(f == 0), stop=(f == 3)), [b_act[f][tc], bd], [pdb])
                            op("dve", lambda e: e.scalar_tensor_tensor(out=H[:, t, half * 512:(half + 1) * 512], in0=pd_[:, 0:512], scalar=comb2[:, t, ex:ex + 1],
                                                                       in1=H[:, t, half * 512:(half + 1) * 512], op0=ALU.mult, op1=ALU.add),
                               [pdb, b_comb2, Hb[t]], [Hb[t]])
            cx.release(m0)

            if stop == 'moe':
                cx.barrier()
                return nc, cx
            m0 = cx.mark()
            gbc = alloc("gbc", [128, D], F32); bbc = alloc("bbc", [128, D], F32); b_par = Buf("lnpar")
            stat = alloc("stat", [128, 16], F32); b_stat = Buf("stat")
            dma("sp", gbc[:], ln2g[layer, :].partition_broadcast(128), writes=[b_par])
            dma("sp", bbc[:], ln2b[layer, :].partition_broadcast(128), writes=[b_par], owner=Buf("lnb"))
            pool_tr = Rot(PS[0:4])
            last = (layer == layers[-1])
            for t in range(NT):
                layer_norm_tile(t, gbc, bbc, b_par, stat, b_stat)
                if last:
                    dma("sp", out[seq, t * 128:(t + 1) * 128, :], H[:, t, :], reads=[Hb[t]], owner=Hb[t])
                else:
                    make_HT(t, pool_tr)
            cx.release(m0)
            cx.cur, cx.cur2 = base_mark

    cx.barrier()
    return nc, cx


_CACHE = {}


def _prep_inputs(inputs):
    cst, cst8 = _consts()
    w_in_x = np.ascontiguousarray(np.asarray(inputs["w_in"], np.float32)[:, :, COLMAP])
    shared = {"w_in_x": w_in_x, "cst": cst, "cst8": cst8}
    for k in ("gla_gate_up", "gla_gate_bias", "gla_norm_gain", "w_out", "ln_mix_g", "ln_mix_b", "router_w", "router_bias",
              "w_expert_gate", "w_expert_up", "w_expert_down", "ln_ffn_g", "ln_ffn_b"):
        shared[k] = np.ascontiguousarray(np.asarray(inputs[k], np.float32))
    return shared


def kernel(**inputs):
    x = np.ascontiguousarray(np.asarray(inputs["x"], np.float32))
    positions = np.ascontiguousarray(np.asarray(inputs["positions"], np.int32))
    shared = _prep_inputs(inputs)
    ncores, nseq = 8, 2
    nc = bass.Bass("TRN2", target_bir_lowering=False)
    build(nc, nseq, [0, 1])
    in_maps = []
    for c in range(ncores):
        m = dict(shared)
        m["x"] = x[c * nseq:(c + 1) * nseq]
        m["positions"] = positions[c * nseq:(c + 1) * nseq]
        in_maps.append(m)
    res = run_bass_kernel_spmd(nc, in_maps, core_ids=list(range(ncores)))
    return np.concatenate([r["out"] for r in res.results], axis=0)
```

```python
import numpy as np
import concourse.bass as bass
import concourse.mybir as mybir
from concourse.bass_utils import run_bass_kernel_spmd

F32 = mybir.dt.float32
BF16 = mybir.dt.bfloat16
I32 = mybir.dt.int32
ALU = mybir.AluOpType
AF = mybir.ActivationFunctionType
AX = mybir.AxisListType

L = 2048
D = 1024
NT = 16
NEG = -30000.0
BIG = 1.0e30
ALPHA = 4.0 ** 0.25
N_BISECT = 18
SBUF_BASE = 16512
SBUF_BYTES = 229000

IN_WIDTHS = (256, 256, 512, 16, 512, 256, 256, 256, 256, 256, 256, 512, 64, 8)
IN_OFF = np.concatenate([[0], np.cumsum(IN_WIDTHS)]).astype(int)
(O_GQ, O_GK, O_GV, O_GR, O_GG, O_MQ, O_MK, O_MV, O_SQ, O_SK, O_SV, O_IQ, O_IK, O_IW) = [int(v) for v in IN_OFF[:-1]]


def _rot_cols(base, nheads):
    cols = []
    for h in range(nheads):
        for d in range(64):
            if d < 8:
                pd = d + 8
            elif d < 16:
                pd = d - 8
            else:
                pd = d
            cols.append(base + h * 64 + pd)
    return cols


def _build_colmap():
    cols = []
    off = {}

    def add(name, c):
        off[name] = len(cols)
        cols.extend(c)

    for p in range(2):
        add(f"g{p}", list(range(O_GQ + p * 128, O_GQ + p * 128 + 128)) + list(range(O_GK + p * 128, O_GK + p * 128 + 128))
            + list(range(O_GV + p * 256, O_GV + p * 256 + 256)) + list(range(O_GG + p * 256, O_GG + p * 256 + 256))
            + list(range(O_GR, O_GR + 16)))
    add("mq", list(range(O_MQ, O_MQ + 256)))
    add("mqr", _rot_cols(O_MQ, 4))
    add("mk", list(range(O_MK, O_MK + 256)))
    add("mkr", _rot_cols(O_MK, 4))
    add("mv", list(range(O_MV, O_MV + 256)))
    add("sq", list(range(O_SQ, O_SQ + 256)))
    add("sqr", _rot_cols(O_SQ, 4))
    add("sk", list(range(O_SK, O_SK + 256)))
    add("skr", _rot_cols(O_SK, 4))
    add("sv", list(range(O_SV, O_SV + 256)))
    add("iq", list(range(O_IQ, O_IQ + 512)))
    add("iqr", _rot_cols(O_IQ, 8))
    add("ik", list(range(O_IK, O_IK + 64)) * 2)
    add("ikr", _rot_cols(O_IK, 1) * 2)
    add("iw", list(range(O_IW, O_IW + 8)))
    return np.array(cols, dtype=np.int64), off


COLMAP, COFF = _build_colmap()
WX = len(COLMAP)


def _consts():
    c = np.zeros((128, 1024), np.float32)
    c[:, 0:128] = np.eye(128, dtype=np.float32)
    j = np.arange(128)[:, None]
    i = np.arange(128)[None, :]
    c[:, 128:256] = (i >= j).astype(np.float32)
    c[:, 256:384] = np.where(i <= j, 0.0, NEG)
    c[:, 384:512] = np.where(i <= j, 0.0, -BIG)
    p = np.arange(128)
    d = p % 64
    invf = np.where(d < 16, 500000.0 ** (-(d % 8) / 8.0), 0.0)
    c[:, 512] = invf / (2 * np.pi)
    c[:, 513] = np.where(d < 8, -invf, invf) / (2 * np.pi)
    c8 = np.zeros((8, 2048 + 512 + 8), np.float32)
    for k in range(8):
        for n in range(8):
            c8[k, 2560 + n] = 0.0 if n < k else (BIG if n == k else -BIG)
    for n in range(8):
        c8[n, n * 256:(n + 1) * 256] = 1.0
    for k in range(8):
        pr, hh = k // 2, k % 2
        c8[k, 2048 + pr * 128 + hh * 64: 2048 + pr * 128 + hh * 64 + 64] = 1.0
    return c, c8


class Buf:
    __slots__ = ("name", "writer", "readers", "dsem", "dcnt")

    def __init__(self, name):
        self.name = name
        self.writer = None
        self.readers = {}
        self.dsem = None
        self.dcnt = 0


class Ctx:
    def __init__(self, nc):
        self.nc = nc
        self.E = {"pe": nc.tensor, "act": nc.scalar, "dve": nc.vector, "pool": nc.gpsimd, "sp": nc.sync}
        self.sem = {k: nc.alloc_semaphore("e_" + k) for k in self.E}
        self.cnt = {k: 0 for k in self.E}
        self.seen = {k: {} for k in self.E}
        self.dbufs = []
        self.slots = {}
        self.nalloc = 0
        self.cur = SBUF_BASE
        self.cur2 = 0
        self.lim2 = 0
        self.peak = 0

    def alloc(self, name, shape, dtype, reg=0):
        isz = 4 if dtype in (F32, I32) else 2
        n = isz
        for s in shape[1:]:
            n *= s
        n = (n + 63) // 64 * 64
        if reg == 0:
            off = self.cur
            self.cur += n
            self.peak = max(self.peak, self.cur)
            assert self.cur <= SBUF_BYTES, (name, self.cur)
        else:
            off = self.cur2
            self.cur2 += n
            assert self.cur2 <= self.lim2, (name, self.cur2, self.lim2)
        self.nalloc += 1
        return self.nc.alloc_sbuf_tensor_at(f"{name}_{self.nalloc}", list(shape), dtype, offset=off)

    def mark(self):
        return (self.cur, self.cur2)

    def release(self, m):
        self.barrier()
        self.cur, self.cur2 = m

    def _wait(self, eng, dep):
        kind, obj, val = dep
        key = obj if kind == "e" else id(obj)
        if self.seen[eng].get(key, 0) >= val:
            return
        sem = self.sem[obj] if kind == "e" else obj.dsem
        self.E[eng].wait_ge(sem, val)
        self.seen[eng][key] = val

    def _deps(self, eng, reads, writes):
        for b in reads:
            if b.writer is not None:
                self._wait(eng, b.writer)
        for b in writes:
            w = b.writer
            if w is not None and not (w[0] == "e" and w[1] == eng and eng == "pe"):
                self._wait(eng, w)
            for r in b.readers.values():
                if not (r[0] == "e" and r[1] == eng and eng == "pe"):
                    self._wait(eng, r)

    def op(self, eng, fn, reads=(), writes=()):
        self._deps(eng, reads, writes)
        inst = fn(self.E[eng])
        inst.then_inc(self.sem[eng], 1)
        self.cnt[eng] += 1
        tag = ("e", eng, self.cnt[eng])
        for b in reads:
            b.readers[eng] = tag
        for b in writes:
            b.writer = tag
            b.readers = {}

    def dma(self, q, out, in_, reads=(), writes=(), owner=None):
        self._deps(q, reads, writes)
        if owner is None:
            owner = writes[0] if writes else reads[0]
        slot = self.slots.get(owner.name)
        if slot is None:
            slot = Buf("slot_" + owner.name)
            slot.dsem = self.nc.alloc_semaphore("d_%d" % len(self.slots))
            self.slots[owner.name] = slot
            self.dbufs.append(slot)
        self.E[q].dma_start(out=out, in_=in_).then_inc(slot.dsem, 16)
        slot.dcnt += 16
        tag = ("d", slot, slot.dcnt)
        for b in reads:
            b.readers[("d", id(slot))] = tag
        for b in writes:
            b.writer = tag
            b.readers = {}

    def barrier(self):
        for e in self.E:
            for e2 in self.E:
                if e2 != e and self.cnt[e2] > 0:
                    self._wait(e, ("e", e2, self.cnt[e2]))
            for b in self.dbufs:
                if b.dcnt:
                    self._wait(e, ("d", b, b.dcnt))


class Rot:
    def __init__(self, items):
        self.items = items
        self.i = 0

    def get(self):
        it = self.items[self.i % len(self.items)]
        self.i += 1
        return it


class StopBuild(Exception):
    pass


def build(nc, nseq, layers, debug=False, stop=None):
    try:
        return _build(nc, nseq, layers, debug, stop)
    except StopBuild as ex:
        return nc, ex.args[0]


def _build(nc, nseq, layers, debug=False, stop=None):
    cx = Ctx(nc)
    op, dma, alloc = cx.op, cx.dma, cx.alloc

    def chk(name):
        if stop == name:
            cx.barrier()
            raise StopBuild(cx)

    x = nc.dram_tensor("x", [nseq, L, D], F32, kind="ExternalInput").ap()
    pos = nc.dram_tensor("positions", [nseq, L], I32, kind="ExternalInput").ap()
    w_in = nc.dram_tensor("w_in_x", [2, D, WX], F32, kind="ExternalInput").ap()
    g_up = nc.dram_tensor("gla_gate_up", [2, 16, 256], F32, kind="ExternalInput").ap()
    g_bias = nc.dram_tensor("gla_gate_bias", [2, 256], F32, kind="ExternalInput").ap()
    g_gain = nc.dram_tensor("gla_norm_gain", [2, 128], F32, kind="ExternalInput").ap()
    w_out = nc.dram_tensor("w_out", [2, D, D], F32, kind="ExternalInput").ap()
    ln1g = nc.dram_tensor("ln_mix_g", [2, D], F32, kind="ExternalInput").ap()
    ln1b = nc.dram_tensor("ln_mix_b", [2, D], F32, kind="ExternalInput").ap()
    r_w = nc.dram_tensor("router_w", [D, 16], F32, kind="ExternalInput").ap()
    r_b = nc.dram_tensor("router_bias", [16], F32, kind="ExternalInput").ap()
    w_eg = nc.dram_tensor("w_expert_gate", [2, 16, D, 512], F32, kind="ExternalInput").ap()
    w_eu = nc.dram_tensor("w_expert_up", [2, 16, D, 512], F32, kind="ExternalInput").ap()
    w_ed = nc.dram_tensor("w_expert_down", [2, 16, 512, D], F32, kind="ExternalInput").ap()
    ln2g = nc.dram_tensor("ln_ffn_g", [2, D], F32, kind="ExternalInput").ap()
    ln2b = nc.dram_tensor("ln_ffn_b", [2, D], F32, kind="ExternalInput").ap()
    cst_d = nc.dram_tensor("cst", [128, 1024], F32, kind="ExternalInput").ap()
    cst8_d = nc.dram_tensor("cst8", [8, 2568], F32, kind="ExternalInput").ap()
    out = nc.dram_tensor("out", [nseq, L, D], F32, kind="ExternalOutput").ap()
    hs = nc.dram_tensor("hspill", [L, D], F32, kind="Internal").ap()
    dbg = nc.dram_tensor("dbg", [128, 8, L], BF16, kind="ExternalOutput").ap() if debug else None

    cst = alloc("cst", [128, 1024], F32)
    c8b = alloc("c8b", [8, 2568], BF16)
    identB = alloc("identB", [128, 128], BF16)
    trinegB = alloc("trinegB", [128, 128], BF16)
    negidentB = alloc("negidentB", [128, 128], BF16)
    epsln = alloc("epsln", [128, 2], F32)
    zerosB = alloc("zerosB", [1, 512], BF16)
    HT = alloc("HT", [128, 8, L], BF16)
    Hoff = cx.cur
    H = alloc("H", [128, NT, D], F32)
    base_mark = cx.mark()

    b_cst = Buf("cst")
    Hb = [Buf(f"H{t}") for t in range(NT)]
    HTb = [Buf(f"HT{t}") for t in range(NT)]
    hsb = [Buf(f"hs{t}") for t in range(NT)]
    outb = Buf("out")

    identF = cst[:, 0:128]
    tri01 = cst[:, 128:256]
    trinegbig = cst[:, 384:512]
    invf = cst[:, 512:513]
    sinvf = cst[:, 513:514]
    onehotK = c8b[0:8, 0:2048]
    selw = c8b[0:8, 2048:2560]
    bpen = c8b[0:8, 2560:2568]

    PS = []
    for i in range(8):
        t = nc.alloc_psum_tensor(f"ps{i}", [128, 512], F32)
        PS.append((t, Buf(f"ps{i}")))

    dma("sp", cst[:], cst_d[:, :], writes=[b_cst])
    dma("pool", c8b[:], cst8_d[:, :], writes=[b_cst], owner=Buf("c8"))
    op("dve", lambda e: e.tensor_copy(out=identB[:], in_=cst[:, 0:128]), [b_cst], [b_cst])
    op("dve", lambda e: e.tensor_copy(out=trinegB[:], in_=cst[:, 256:384]), [b_cst], [b_cst])
    op("dve", lambda e: e.tensor_scalar(out=negidentB[:], in0=cst[:, 0:128], scalar1=-1.0, scalar2=None, op0=ALU.mult), [b_cst], [b_cst])
    op("dve", lambda e: e.memset(epsln[:, 0:1], 1e-5), [], [b_cst])
    op("dve", lambda e: e.memset(epsln[:, 1:2], 1e-6), [], [b_cst])
    op("dve", lambda e: e.memset(zerosB[:], 0.0), [], [b_cst])
    cx.barrier()

    def load_w(dst, src_rows_cols, buf, q="pool"):
        dma(q, dst, src_rows_cols.rearrange("(c p) f -> p c f", p=128), writes=[buf])

    def make_HT(t, pool):
        for half in range(2):
            pt, pb = pool.get()
            for c4 in range(4):
                c = half * 4 + c4
                op("pe", lambda e, c=c, c4=c4: e.transpose(out=pt[:, c4 * 128:(c4 + 1) * 128],
                                                           in_=H[:, t, c * 128:(c + 1) * 128], identity=identF),
                   [Hb[t], b_cst], [pb])
            op("act", lambda e: e.activation(out=HT[:, half * 4:(half + 1) * 4, t * 128:(t + 1) * 128],
                                             in_=pt[:, :].rearrange("p (a b) -> p a b", a=4), func=AF.Copy),
               [pb], [HTb[t]])

    def layer_norm_tile(t, gbc, bbc, b_par, stat, b_stat):
        hv = H[:, t, :]
        op("dve", lambda e: e.bn_stats(out=stat[:, 0:6], in_=H[:, t, 0:512]), [Hb[t]], [b_stat])
        op("dve", lambda e: e.bn_stats(out=stat[:, 6:12], in_=H[:, t, 512:1024]), [Hb[t]], [b_stat])
        op("dve", lambda e: e.bn_aggr(out=stat[:, 12:14], in_=stat[:, 0:12]), [b_stat], [b_stat])
        op("act", lambda e: e.activation(out=stat[:, 14:15], in_=stat[:, 13:14], func=AF.Sqrt, bias=epsln[:, 0:1], scale=1.0),
           [b_stat, b_cst], [b_stat])
        op("dve", lambda e: e.reciprocal(out=stat[:, 15:16], in_=stat[:, 14:15]), [b_stat], [b_stat])
        op("dve", lambda e: e.tensor_scalar(out=hv, in0=hv, scalar1=stat[:, 12:13], scalar2=stat[:, 15:16],
                                            op0=ALU.subtract, op1=ALU.mult), [Hb[t], b_stat], [Hb[t]])
        op("dve", lambda e: e.tensor_tensor(out=hv, in0=hv, in1=gbc[:], op=ALU.mult), [Hb[t], b_par], [Hb[t]])
        op("dve", lambda e: e.tensor_tensor(out=hv, in0=hv, in1=bbc[:], op=ALU.add), [Hb[t], b_par], [Hb[t]])

    def rope_tables(seq, cosT, sinT, b_tab):
        m = cx.mark()
        posf = alloc("posf", [128, L], F32)
        tmp = alloc("rtmp", [128, L], F32)
        tmi = alloc("rtmi", [128, L], I32)
        b_t = Buf("ropetmp")
        dma("pool", posf[:], pos[seq, :].partition_broadcast(128), writes=[b_t])
        for (tab, fcol, shift) in ((sinT, sinvf, 0.0), (cosT, invf, 0.25)):
            op("dve", lambda e, fcol=fcol, shift=shift: e.tensor_scalar(out=tmp[:], in0=posf[:], scalar1=fcol, scalar2=shift,
                                                                         op0=ALU.mult, op1=ALU.add), [b_t, b_cst], [b_t])
            op("dve", lambda e: e.tensor_copy(out=tmi[:], in_=tmp[:]), [b_t], [b_t])
            op("dve", lambda e: e.tensor_copy(out=tab[:], in_=tmi[:]), [b_t], [b_tab])
            op("dve", lambda e: e.tensor_tensor(out=tmp[:], in0=tmp[:], in1=tab[:], op=ALU.subtract), [b_t, b_tab], [b_t])
            op("dve", lambda e: e.tensor_scalar(out=tab[:], in0=tmp[:], scalar1=0.5, scalar2=None, op0=ALU.is_gt), [b_t], [b_tab])
            op("dve", lambda e: e.tensor_tensor(out=tmp[:], in0=tmp[:], in1=tab[:], op=ALU.subtract), [b_t, b_tab], [b_t])
            op("dve", lambda e: e.tensor_scalar(out=tab[:], in0=tmp[:], scalar1=-0.5, scalar2=None, op0=ALU.is_lt), [b_t], [b_tab])
            op("dve", lambda e: e.tensor_tensor(out=tmp[:], in0=tmp[:], in1=tab[:], op=ALU.add), [b_t, b_tab], [b_t])
            op("act", lambda e: e.activation(out=tab[:], in_=tmp[:], func=AF.Sin, scale=2 * np.pi), [b_t], [b_tab])
        cx.release(m)

    def proj_fm(wt, col0, M, tc, pt, pb, wbuf, pbase=0):
        for c in range(8):
            op("pe", lambda e, c=c: e.matmul(pt[pbase:pbase + M, 0:512], lhsT=wt[:, c, col0:col0 + M],
                                             rhs=HT[:, c, tc * 512:(tc + 1) * 512], start=(c == 0), stop=(c == 7)),
               [wbuf] + HTb[tc * 4:(tc + 1) * 4], [pb])

    def proj_tm(wt, col0, N, t, pt, pb, wbuf, ocol=0):
        for c in range(8):
            op("pe", lambda e, c=c: e.matmul(pt[:, ocol:ocol + N], lhsT=HT[:, c, t * 128:(t + 1) * 128],
                                             rhs=wt[:, c, col0:col0 + N], start=(c == 0), stop=(c == 7)),
               [wbuf, HTb[t]], [pb])

    def roped_proj(wt, wbuf, col, colr, dstT, b_dst, cosT, sinT, b_tab, tmps, pspool, ksum=None):
        ntile = dstT.shape[1]
        (t1, t2, b_tmp) = tmps
        for ti in range(ntile):
            for tc in range(4):
                py, pyb = pspool.get()
                pr, prb = pspool.get()
                proj_fm(wt, col + ti * 128, 128, tc, py, pyb, wbuf)
                proj_fm(wt, colr + ti * 128, 128, tc, pr, prb, wbuf)
                sl = slice(tc * 512, (tc + 1) * 512)
                op("dve", lambda e: e.tensor_tensor(out=t1[:], in0=pr[:, :], in1=sinT[:, sl], op=ALU.mult), [prb, b_tab], [b_tmp])
                op("dve", lambda e: e.tensor_tensor(out=t2[:], in0=py[:, :], in1=cosT[:, sl], op=ALU.mult), [pyb, b_tab], [b_tmp])
                if ksum is not None:
                    op("pool", lambda e: e.tensor_tensor(out=t2[:], in0=t1[:], in1=t2[:], op=ALU.add), [b_tmp], [b_tmp])
                    op("act", lambda e: e.activation(out=dstT[:, ti, sl], in_=t2[:], func=AF.Copy), [b_tmp], [b_dst])
                    (km, b_km) = ksum
                    op("dve", lambda e: e.tensor_reduce(out=km[:, ti, tc * 2:(tc + 1) * 2], in_=t2[:, :].rearrange("p (a b) -> p a b", a=2),
                                                        axis=AX.X, op=ALU.add), [b_tmp], [b_km])
                else:
                    op("pool", lambda e: e.tensor_tensor(out=dstT[:, ti, sl], in0=t1[:], in1=t2[:], op=ALU.add), [b_tmp], [b_dst])

    def v_aug_proj(wt, wbuf, col, vaug, b_v, pspool):
        op("dve", lambda e: e.memset(vaug[:, :, :, 64:65], 1.0), [], [b_v])
        for t in range(NT):
            pt, pb = pspool.get()
            proj_tm(wt, col, 256, t, pt, pb, wbuf)
            op("act", lambda e: e.activation(out=vaug[:, t, :, 0:64], in_=pt[:, 0:256].rearrange("p (a b) -> p a b", a=4), func=AF.Copy),
               [pb], [b_v])

    def attention(qT, kT, b_qk, vaug, b_v, maskfn, mix_chunk0, pools, otm, b_otm, pTs, rs, b_rs, mixT, mixb, qc_hook=None):
        (ps_s, ps_acc, ps_tr) = pools
        if qc_hook is not None:
            qc_hook(0)
        for qc in range(4):
            if qc_hook is not None and qc + 1 < 4:
                qc_hook(qc + 1)
            for h in range(4):
                ptile, bp = h // 2, (h % 2) * 64
                acc, accb = ps_acc.get()
                op("pe", lambda e: e.matmul(acc[:, 0:260], lhsT=zerosB[0:1, 0:128], rhs=zerosB[0:1, 0:260], start=True, stop=False,
                                            skip_group_check=True), [b_cst], [accb])
                prev = None

                def emit_pv(args):
                    (j_, t0_, pT_, pTb_) = args
                    for t in range(t0_, 4 * qc + 4):
                        i = t - 4 * qc
                        op("pe", lambda e, t=t, i=i: e.matmul(acc[:, i * 65:(i + 1) * 65], lhsT=pT_[:, (t - t0_) * 128:(t - t0_ + 1) * 128],
                                                              rhs=vaug[:, j_, h, :], start=False, stop=(j_ == t), skip_group_check=True),
                           [pTb_, b_v], [accb])

                for j in range(4 * qc + 4):
                    t0 = max(j, 4 * qc)
                    q0, q1 = t0 * 128, (4 * qc + 4) * 128
                    wq = q1 - q0
                    st, sb = ps_s.get()
                    op("pe", lambda e: e.matmul(st[:, 0:wq], lhsT=kT[bp:bp + 64, ptile, j * 128:(j + 1) * 128],
                                                rhs=qT[bp:bp + 64, ptile, q0:q1], start=True, stop=False, skip_group_check=True),
                       [b_qk], [sb])
                    maskfn(h, qc, j, t0, st, sb, wq)
                    pT, pTb = pTs.get()
                    op("act", lambda e: e.activation(out=pT[:, 0:wq], in_=st[:, 0:wq], func=AF.Exp, scale=0.125), [sb], [pTb])
                    if prev is not None:
                        emit_pv(prev)
                    prev = (j, t0, pT, pTb)
                emit_pv(prev)
                acc3 = acc[:, 0:260].rearrange("p (a b) -> p a b", a=4)
                op("dve", lambda e: e.reciprocal(out=rs[:, :], in_=acc3[:, :, 64]), [accb], [b_rs])
                for i in range(4):
                    op("dve", lambda e, i=i: e.tensor_scalar(out=otm[:, i, h * 64:(h + 1) * 64], in0=acc3[:, i, 0:64],
                                                             scalar1=rs[:, i:i + 1], scalar2=None, op0=ALU.mult), [accb, b_rs], [b_otm])
            for i in range(4):
                t = 4 * qc + i
                pt, pb = ps_tr.get()
                for f in range(2):
                    op("pe", lambda e, f=f: e.transpose(out=pt[:, f * 128:(f + 1) * 128], in_=otm[:, i, f * 128:(f + 1) * 128], identity=identF),
                       [b_otm, b_cst], [pb])
                op("act", lambda e: e.activation(out=mixT[:, mix_chunk0:mix_chunk0 + 2, t * 128:(t + 1) * 128],
                                                 in_=pt[:, 0:256].rearrange("p (a b) -> p a b", a=2), func=AF.Copy), [pb], [mixb[t]])

    for seq in range(nseq):
        pool_tr = Rot(PS[0:4])
        for t in range(NT):
            dma("sp", H[:, t, :], x[seq, t * 128:(t + 1) * 128, :], writes=[Hb[t]])
        for t in range(NT):
            make_HT(t, pool_tr)

        for layer in layers:
            for t in range(NT):
                dma("sp", hs[t * 128:(t + 1) * 128, :], H[:, t, :], reads=[Hb[t]], writes=[hsb[t]], owner=Hb[t])
            cx.barrier()
            cx.cur, cx.cur2 = base_mark
            cx.cur2, cx.lim2 = Hoff, Hoff + 65536
            mixT = alloc("mixT", [128, 8, L], BF16)
            mixb = [Buf(f"mix{t}") for t in range(NT)]

            if stop == 'ht':
                cx.barrier()
                return nc, cx
            for p in range(2):
                m0 = cx.mark()
                Wg = alloc("Wg", [128, 8, 784], BF16); b_Wg = Buf("Wg")
                gu = alloc("gu", [16, 128], BF16)
                nbias = alloc("nbias", [128, 1], F32)
                gain = alloc("gain", [128, 2, 128], F32)
                b_gp = Buf("gpar")
                rankT = alloc("rankT", [16, L], BF16); b_rank = Buf("rank")
                la = alloc("la", [128, L], F32, 1); b_la = Buf("la")
                bc = alloc("bc", [128, L], F32, 1); b_bc = Buf("bc")
                nbl = alloc("nbl", [128, 16], F32); dec = alloc("dec", [128, 16], F32); b_nbl = Buf("nbl")
                qdT = alloc("qdT", [128, L], BF16, 1); kiT = alloc("kiT", [128, L], BF16, 1); b_qk = Buf("gqk")
                kd = alloc("kd", [128, NT, 128], BF16, 1); b_kd = Buf("kd")
                vtm = alloc("vtm", [128, NT, 256], BF16, 1); b_v = Buf("gv")
                Sall = alloc("Sall", [128, NT, 128], BF16, 1); b_Sall = Buf("Sall")
                Sst = alloc("Sst", [128, 128], F32); b_S = Buf("S")
                e1 = alloc("e1", [128, 512], F32); e2 = alloc("e2", [128, 512], F32); e3 = alloc("e3", [128, 512], F32)
                b_e = [Buf("e1"), Buf("e2"), Buf("e3")]
                kdT = alloc("kdT", [128, 512], F32); b_kdT = Buf("kdT")
                AT = [alloc("AT0", [128, 128], BF16), alloc("AT1", [128, 128], BF16)]; b_AT = [Buf("AT0"), Buf("AT1")]
                sg = alloc("sg", [128, 256], F32); b_sg = Buf("sg")
                otm = alloc("otmg", [128, 256], F32); b_otm = Buf("otmg")
                sq = alloc("sq", [128, 256], F32); ssq = alloc("ssq", [128, 4], F32); b_ssq = Buf("ssq")

                load_w(Wg[:], w_in[layer][:, COFF[f"g{p}"]:COFF[f"g{p}"] + 784], b_Wg)
                dma("pool", gu[:], g_up[layer][:, p * 128:(p + 1) * 128], writes=[b_gp])
                dma("sp", nbias[:], g_bias[layer, p * 128:(p + 1) * 128].rearrange("(p o) -> p o", o=1), writes=[b_gp], owner=Buf("nb"))
                for hh in range(2):
                    dma("sp", gain[:, hh, :], g_gain[layer, :].partition_broadcast(128), writes=[b_gp], owner=Buf("gn"))
                op("dve", lambda e: e.tensor_scalar(out=nbias[:], in0=nbias[:], scalar1=-1.0, scalar2=None, op0=ALU.mult), [b_gp], [b_gp])
                chk('g1')
                pp = Rot(PS[0:4])
                for tc in range(4):
                    pt, pb = pp.get()
                    for c in range(8):
                        op("pe", lambda e, c=c: e.matmul(pt[0:16, 0:512], lhsT=Wg[:, c, 768:784], rhs=HT[:, c, tc * 512:(tc + 1) * 512],
                                                         start=(c == 0), stop=(c == 7)), [b_Wg] + HTb[tc * 4:tc * 4 + 4], [pb])
                    op("act", lambda e: e.activation(out=rankT[:, tc * 512:(tc + 1) * 512], in_=pt[0:16, 0:512], func=AF.Copy), [pb], [b_rank])
                for tc in range(4):
                    pt, pb = pp.get()
                    sl = slice(tc * 512, (tc + 1) * 512)
                    op("pe", lambda e: e.matmul(pt[:, 0:512], lhsT=gu[0:16, :], rhs=rankT[0:16, sl], start=True, stop=True), [b_gp, b_rank], [pb])
                    op("act", lambda e: e.activation(out=la[:, sl], in_=pt[:, 0:512], func=AF.Exp, bias=nbias[:, 0:1], scale=-1.0), [pb, b_gp], [b_la])
                op("act", lambda e: e.activation(out=la[:], in_=la[:], func=AF.Ln, bias=1.0, scale=1.0), [b_la], [b_la])
                chk('g2')
                for n in range(NT):
                    sl = slice(n * 128, (n + 1) * 128)
                    op("dve", lambda e: e.tensor_tensor_scan(out=bc[:, sl], data0=la[:, sl], data1=cst[:, 640:768], initial=0.0,
                                                             op0=ALU.add, op1=ALU.add), [b_la, b_cst], [b_bc])
                chk('g3')
                cx_chk = None
                op("dve", lambda e: e.tensor_scalar(out=nbl[:], in0=bc[:, :].rearrange("p (n c) -> p n c", c=128)[:, :, 127],
                                                    scalar1=-1.0 / 16.0, scalar2=None, op0=ALU.mult), [b_bc], [b_nbl])
                op("act", lambda e: e.activation(out=dec[:], in_=nbl[:], func=AF.Exp), [b_nbl], [b_nbl])
                chk('g3b')
                ptr = Rot(PS[4:6])
                for tc in range(4):
                    sl = slice(tc * 512, (tc + 1) * 512)
                    pq, pqb = pp.get()
                    pk, pkb = pp.get()
                    proj_fm(Wg, 0, 128, tc, pq, pqb, b_Wg)
                    proj_fm(Wg, 128, 128, tc, pk, pkb, b_Wg)
                    op("act", lambda e: e.activation(out=e1[:], in_=bc[:, sl], func=AF.Exp, scale=-1.0 / 16.0), [b_bc], [b_e[0]])
                    op("dve", lambda e: e.scalar_tensor_tensor(out=qdT[:, sl], in0=pq[:, :], scalar=0.125, in1=e1[:], op0=ALU.mult, op1=ALU.mult),
                       [pqb, b_e[0]], [b_qk])
                    op("act", lambda e: e.activation(out=e2[:], in_=bc[:, sl], func=AF.Exp, scale=1.0 / 16.0), [b_bc], [b_e[1]])
                    op("dve", lambda e: e.tensor_tensor(out=kiT[:, sl], in0=pk[:, :], in1=e2[:], op=ALU.mult), [pkb, b_e[1]], [b_qk])
                    for n4 in range(4):
                        n = tc * 4 + n4
                        op("act", lambda e, n=n, n4=n4: e.activation(out=e3[:, n4 * 128:(n4 + 1) * 128], in_=bc[:, n * 128:(n + 1) * 128], func=AF.Exp,
                                                                     bias=nbl[:, n:n + 1], scale=1.0 / 16.0), [b_bc, b_nbl], [b_e[2]])
                    op("dve", lambda e: e.tensor_tensor(out=kdT[:], in0=pk[:, :], in1=e3[:], op=ALU.mult), [pkb, b_e[2]], [b_kdT])
                    pt, pb = ptr.get()
                    for n4 in range(4):
                        op("pe", lambda e, n4=n4: e.transpose(out=pt[:, n4 * 128:(n4 + 1) * 128], in_=kdT[:, n4 * 128:(n4 + 1) * 128], identity=identF),
                           [b_kdT, b_cst], [pb])
                    op("act", lambda e: e.activation(out=kd[:, tc * 4:(tc + 1) * 4, :], in_=pt[:, :].rearrange("p (a b) -> p a b", a=4), func=AF.Copy),
                       [pb], [b_kd])
                chk('g4')
                for t in range(NT):
                    pt, pb = pp.get()
                    proj_tm(Wg, 256, 256, t, pt, pb, b_Wg)
                    op("act", lambda e: e.activation(out=vtm[:, t, :], in_=pt[:, 0:256], func=AF.Copy), [pb], [b_v])
                chk('g5')
                op("dve", lambda e: e.memset(Sst[:], 0.0), [], [b_S])
                op("dve", lambda e: e.memset(Sall[:, 0, :], 0.0), [], [b_Sall])
                for n in range(NT - 1):
                    pt, pb = pp.get()
                    for hh in range(2):
                        op("pe", lambda e, hh=hh: e.matmul(pt[hh * 64:(hh + 1) * 64, 0:128], lhsT=kd[:, n, hh * 64:(hh + 1) * 64],
                                                           rhs=vtm[:, n, hh * 128:(hh + 1) * 128], start=True, stop=True), [b_kd, b_v], [pb])
                    op("dve", lambda e: e.scalar_tensor_tensor(out=Sst[:], in0=Sst[:], scalar=dec[:, n:n + 1], in1=pt[:, 0:128],
                                                               op0=ALU.mult, op1=ALU.add), [b_S, b_nbl, pb], [b_S])
                    op("act", lambda e: e.activation(out=Sall[:, n + 1, :], in_=Sst[:], func=AF.Copy), [b_S], [b_Sall])
                chk('g6')
                ps_o = Rot(PS[4:6]); ps_g = Rot(PS[6:8])
                o_all = alloc("o_all", [128, NT, 256], F32); b_oall = [Buf(f"oall{n}") for n in range(NT)]
                sg_all = alloc("sg_all", [128, NT, 256], F32); b_sgall = [Buf(f"sgall{n}") for n in range(NT)]
                ssq_all = alloc("ssq_all", [128, NT, 2], F32); rstd_all = alloc("rstd_all", [128, NT, 2], F32); b_ssqa = Buf("ssqa")
                otms = [(alloc(f"otmg{i}", [128, 256], F32), Buf(f"otmg{i}")) for i in range(2)]
                for n in range(NT):
                    sl = slice(n * 128, (n + 1) * 128)
                    po, pob = ps_o.get()
                    for hh in range(2):
                        bp = hh * 64
                        pa, pab = pp.get()
                        op("pe", lambda e: e.matmul(pa[:, 0:128], lhsT=kiT[bp:bp + 64, sl], rhs=qdT[bp:bp + 64, sl], start=True, stop=True), [b_qk], [pab])
                        op("dve", lambda e: e.tensor_tensor(out=AT[hh][:], in0=pa[:, 0:128], in1=tri01, op=ALU.mult), [pab, b_cst], [b_AT[hh]])
                        op("pe", lambda e: e.matmul(po[:, hh * 128:(hh + 1) * 128], lhsT=AT[hh][:], rhs=vtm[:, n, hh * 128:(hh + 1) * 128],
                                                    start=True, stop=False, skip_group_check=True), [b_AT[hh], b_v], [pob])
                        op("pe", lambda e: e.matmul(po[:, hh * 128:(hh + 1) * 128], lhsT=qdT[bp:bp + 64, sl], rhs=Sall[bp:bp + 64, n, :],
                                                    start=False, stop=True, skip_group_check=True), [b_qk, b_Sall], [pob])
                    for hh in range(2):
                        op("act", lambda e, hh=hh: e.activation(out=sq[:, hh * 128:(hh + 1) * 128], in_=po[:, hh * 128:(hh + 1) * 128], func=AF.Square,
                                                                accum_out=ssq_all[:, n, hh:hh + 1]), [pob], [b_ssqa])
                    op("act", lambda e: e.activation(out=o_all[:, n, :], in_=po[:, 0:256], func=AF.Copy), [pob], [b_oall[n]])
                op("act", lambda e: e.activation(out=rstd_all[:, :, :], in_=ssq_all[:, :, :], func=AF.Sqrt, bias=epsln[:, 1:2], scale=1.0 / 128.0), [b_ssqa, b_cst], [b_ssqa])
                op("dve", lambda e: e.reciprocal(out=rstd_all[:, :, :], in_=rstd_all[:, :, :]), [b_ssqa], [b_ssqa])
                for n in range(NT):
                    pg, pgb = ps_g.get()
                    proj_tm(Wg, 512, 256, n, pg, pgb, b_Wg)
                    op("act", lambda e: e.activation(out=sg_all[:, n, :], in_=pg[:, 0:256], func=AF.Silu), [pgb], [b_sgall[n]])
                    op("pool", lambda e: e.tensor_tensor(out=sg_all[:, n, :], in0=sg_all[:, n, :], in1=gain[:, :, :].rearrange("p a b -> p (a b)"), op=ALU.mult),
                       [b_sgall[n], b_gp], [b_sgall[n]])
                for n in range(NT):
                    sl = slice(n * 128, (n + 1) * 128)
                    otm, b_otm = otms[n % 2]
                    for hh in range(2):
                        op("dve", lambda e, hh=hh: e.scalar_tensor_tensor(out=otm[:, hh * 128:(hh + 1) * 128], in0=o_all[:, n, hh * 128:(hh + 1) * 128],
                                                                          scalar=rstd_all[:, n, hh:hh + 1], in1=sg_all[:, n, hh * 128:(hh + 1) * 128],
                                                                          op0=ALU.mult, op1=ALU.mult), [b_oall[n], b_ssqa, b_sgall[n]], [b_otm])
                    pt, pb = ps_o.get()
                    for hh in range(2):
                        op("pe", lambda e, hh=hh: e.transpose(out=pt[:, hh * 128:(hh + 1) * 128], in_=otm[:, hh * 128:(hh + 1) * 128], identity=identF),
                           [b_otm, b_cst], [pb])
                    op("act", lambda e: e.activation(out=mixT[:, 2 * p:2 * p + 2, sl], in_=pt[:, 0:256].rearrange("p (a b) -> p a b", a=2), func=AF.Copy),
                       [pb], [mixb[n]])
                cx.release(m0)
                chk('p0')

            if stop == 'gla':
                cx.barrier()
                return nc, cx
            def attn_common():
                qT = alloc("qT", [128, 2, L], BF16, 1); kT = alloc("kT", [128, 2, L], BF16, 1)
                vaug = alloc("vaug", [128, NT, 4, 65], BF16, 1)
                return qT, kT, vaug, Buf("aqk"), Buf("av")

            m0 = cx.mark()
            qT, kT, vaug, b_qk, b_v = attn_common()
            kmT = alloc("kmT", [128, 2, 8], F32); kmB = alloc("kmB", [128, 2, 8], BF16); b_km = Buf("km")
            mbT = alloc("mbT", [8, 4, L], BF16); b_mb = Buf("mbT")
            m1 = cx.mark()
            cosT = alloc("cosT", [128, L], F32); sinT = alloc("sinT", [128, L], F32); b_tab = Buf("tab")
            rope_tables(seq, cosT, sinT, b_tab)
            chk('m1')
            Wa = alloc("Wa", [128, 8, 512], BF16); b_Wa = Buf("Wa")
            Wb = alloc("Wb", [128, 8, 512], BF16); b_Wb = Buf("Wb")
            t1 = alloc("t1", [128, 512], F32); t2 = alloc("t2", [128, 512], F32); b_tmp = Buf("rt")
            pp = Rot(PS[0:6])
            load_w(Wa[:], w_in[layer][:, COFF["mq"]:COFF["mq"] + 512], b_Wa)
            load_w(Wb[:], w_in[layer][:, COFF["mk"]:COFF["mk"] + 512], b_Wb)
            roped_proj(Wa, b_Wa, 0, 256, qT, b_qk, cosT, sinT, b_tab, (t1, t2, b_tmp), pp)
            roped_proj(Wb, b_Wb, 0, 256, kT, b_qk, cosT, sinT, b_tab, (t1, t2, b_tmp), pp, ksum=(kmT, b_km))
            chk('m2a')
            load_w(Wa[:, :, 0:256], w_in[layer][:, COFF["mv"]:COFF["mv"] + 256], b_Wa)
            v_aug_proj(Wa, b_Wa, 0, vaug, b_v, pp)
            op("dve", lambda e: e.tensor_copy(out=kmB[:], in_=kmT[:]), [b_km], [b_km])
            cx.release(m1)
            chk('m2')
            Z = alloc("Z", [128, 4, 8], F32); gm = alloc("gm", [128, 4, 8], F32); top8 = alloc("top8", [128, 4, 8], F32); b_Z = Buf("Z")
            otm = alloc("otm", [128, 4, 256], F32); b_otm = Buf("otm")
            pTl = [(alloc(f"pT{i}", [128, 512], BF16), Buf(f"pT{i}")) for i in range(3)]
            rs = alloc("rs", [128, 4], F32); b_rs = Buf("rs")
            op("pool", lambda e: e.memset(mbT[:], 0.0), [], [b_mb])
            pg = Rot(PS[0:2])
            for t in range(8, NT):
                pt, pb = pg.get()
                for h in range(4):
                    ptile, bp = h // 2, (h % 2) * 64
                    op("pe", lambda e, h=h: e.matmul(pt[:, h * 8:(h + 1) * 8], lhsT=qT[bp:bp + 64, ptile, t * 128:(t + 1) * 128],
                                                     rhs=kmB[bp:bp + 64, ptile, :], start=True, stop=False, skip_group_check=True), [b_qk, b_km], [pb])
                    op("pe", lambda e, h=h: e.matmul(pt[:, h * 8:(h + 1) * 8], lhsT=onehotK[0:8, t * 128:(t + 1) * 128],
                                                     rhs=bpen, start=False, stop=True, skip_group_check=True), [b_cst], [pb])
                op("dve", lambda e: e.tensor_copy(out=gm[:], in_=pt[:, 0:32].rearrange("p (a b) -> p a b", a=4)), [pb], [b_Z])
                for h in range(4):
                    op("dve", lambda e, h=h: e.max(out=top8[:, h, :], in_=gm[:, h, :]), [b_Z], [b_Z])
                for h in range(4):
                    op("dve", lambda e, h=h: e.tensor_scalar(out=Z[:, h, :], in0=gm[:, h, :], scalar1=top8[:, h, 3:4], scalar2=NEG,
                                                             op0=ALU.is_lt, op1=ALU.mult), [b_Z], [b_Z])
                pt2, pb2 = pg.get()
                for h in range(4):
                    op("pe", lambda e, h=h: e.transpose(out=pt2[0:8, h * 128:(h + 1) * 128], in_=Z[:, h, :], identity=identF), [b_Z, b_cst], [pb2])
                op("dve", lambda e: e.tensor_copy(out=mbT[0:8, :, t * 128:(t + 1) * 128], in_=pt2[0:8, :].rearrange("p (a b) -> p a b", a=4)),
                   [pb2], [b_mb])
            chk('m3')

            def moba_mask(h, qc, j, t0, st, sb, wq):
                q0 = t0 * 128
                diag = (j >= 4 * qc)
                op("pe", lambda e: e.matmul(st[:, 0:wq], lhsT=onehotK[0:8, j * 128:(j + 1) * 128], rhs=mbT[0:8, h, q0:q0 + wq],
                                            start=False, stop=(not diag), skip_group_check=True), [b_cst, b_mb], [sb])
                if diag:
                    op("pe", lambda e: e.matmul(st[:, 0:128], lhsT=trinegB[:], rhs=identB[:], start=False, stop=True, skip_group_check=True), [b_cst], [sb])

            attention(qT, kT, b_qk, vaug, b_v, moba_mask, 4, (Rot(PS[2:5]), Rot(PS[5:7]), Rot(PS[7:8])), otm, b_otm, Rot(pTl), rs, b_rs, mixT, mixb)
            cx.release(m0)

            if stop == 'moba':
                cx.barrier()
                return nc, cx
            m0 = cx.mark()
            qT, kT, vaug, b_qk, b_v = attn_common()
            kiT = alloc("kiT", [128, 1, L], BF16, 1); b_ki = Buf("ki")
            wT = alloc("wT", [8, L], BF16, 1); b_wT = Buf("wT")
            qiT = alloc("qiT", [128, 4, L], BF16, 1); b_qi = Buf("qi")
            m1 = cx.mark()
            cosT = alloc("cosT", [128, L], F32); sinT = alloc("sinT", [128, L], F32); b_tab = Buf("tab")
            rope_tables(seq, cosT, sinT, b_tab)
            Wa = alloc("Wa", [128, 8, 512], BF16); b_Wa = Buf("Wa")
            Wb = alloc("Wb", [128, 8, 512], BF16); b_Wb = Buf("Wb")
            t1 = alloc("t1", [128, 512], F32); t2 = alloc("t2", [128, 512], F32); b_tmp = Buf("rt")
            pp = Rot(PS[0:6])
            tm3 = (t1, t2, b_tmp)
            load_w(Wa[:], w_in[layer][:, COFF["sq"]:COFF["sq"] + 512], b_Wa)
            load_w(Wb[:], w_in[layer][:, COFF["sk"]:COFF["sk"] + 512], b_Wb)
            roped_proj(Wa, b_Wa, 0, 256, qT, b_qk, cosT, sinT, b_tab, tm3, pp)
            roped_proj(Wb, b_Wb, 0, 256, kT, b_qk, cosT, sinT, b_tab, tm3, pp)
            load_w(Wa[:], w_in[layer][:, COFF["iq"]:COFF["iq"] + 512], b_Wa)
            load_w(Wb[:], w_in[layer][:, COFF["iqr"]:COFF["iqr"] + 512], b_Wb)
            for ti in range(4):
                for tc in range(4):
                    py, pyb = pp.get(); pr, prb = pp.get()
                    proj_fm(Wa, ti * 128, 128, tc, py, pyb, b_Wa)
                    proj_fm(Wb, ti * 128, 128, tc, pr, prb, b_Wb)
                    sl = slice(tc * 512, (tc + 1) * 512)
                    op("dve", lambda e: e.tensor_tensor(out=t1[:], in0=pr[:, :], in1=sinT[:, sl], op=ALU.mult), [prb, b_tab], [b_tmp])
                    op("dve", lambda e: e.tensor_tensor(out=t2[:], in0=py[:, :], in1=cosT[:, sl], op=ALU.mult), [pyb, b_tab], [b_tmp])
                    op("pool", lambda e: e.tensor_tensor(out=qiT[:, ti, sl], in0=t1[:], in1=t2[:], op=ALU.add), [b_tmp], [b_qi])
            load_w(Wa[:, :, 0:264], w_in[layer][:, COFF["ik"]:COFF["ik"] + 264], b_Wa)
            load_w(Wb[:, :, 0:256], w_in[layer][:, COFF["sv"]:COFF["sv"] + 256], b_Wb)
            roped_proj(Wa, b_Wa, 0, 128, kiT, b_ki, cosT, sinT, b_tab, tm3, pp)
            for tc in range(4):
                pt, pb = pp.get()
                for c in range(8):
                    op("pe", lambda e, c=c: e.matmul(pt[0:8, 0:512], lhsT=Wa[:, c, 256:264], rhs=HT[:, c, tc * 512:(tc + 1) * 512],
                                                     start=(c == 0), stop=(c == 7)), [b_Wa] + HTb[tc * 4:tc * 4 + 4], [pb])
                op("act", lambda e: e.activation(out=wT[:, tc * 512:(tc + 1) * 512], in_=pt[0:8, 0:512], func=AF.Copy), [pb], [b_wT])
            v_aug_proj(Wb, b_Wb, 0, vaug, b_v, pp)
            cx.release(m1)
            mbs = [HT[:, 0:4, :], alloc("mbB", [128, 4, L], BF16)]
            b_mb2 = [[Buf(f"mb{a}_{i}") for i in range(4)] for a in range(2)]
            isc = [HT[:, 4:6, :].bitcast(F32).rearrange("p a b -> p (a b)"), HT[:, 6:8, :].bitcast(F32).rearrange("p a b -> p (a b)"),
                   alloc("isc2", [128, L], F32), alloc("isc3", [128, L], F32)]
            b_isc = [Buf(f"isc{i}") for i in range(4)]
            qs = alloc("qs", [128, 4, 2, 512], BF16); b_qs = Buf("qs")
            otm = alloc("otm", [128, 4, 256], F32); b_otm = Buf("otm")
            pTl = [(alloc(f"pT{i}", [128, 512], BF16), Buf(f"pT{i}")) for i in range(3)]
            rs = alloc("rs", [128, 4], F32); b_rs = Buf("rs")
            bsA = alloc("bsA", [128, 32], F32); bsi = alloc("bsi", [128, 4], I32)
            lo = bsA[:, 0:4]; w0 = bsA[:, 4:8]; mid = bsA[:, 8:12]; nmid = bsA[:, 12:16]; cntv = bsA[:, 16:20]; thc = bsA[:, 20:24]; mx = bsA[:, 24:28]
            b_lo = Buf("lo"); b_mid = Buf("mid"); b_cnt = [Buf(f"cnt{i}") for i in range(4)]; b_msk = Buf("msk")
            junkD = alloc("junkD", [128, L], BF16); b_junkD = Buf("junkD")
            junkA = alloc("junkA", [128, L], BF16); b_junkA = Buf("junkA")
            ps_i = Rot(PS[0:3])
            rls = Rot([(alloc(f"rl{i}", [128, 512], BF16), Buf(f"rl{i}")) for i in range(4)])

            def dsa_qc(qc):
                mb = mbs[qc % 2]; b_mbq = b_mb2[qc % 2]
                qsl = slice(qc * 512, (qc + 1) * 512)
                for pr_ in range(4):
                    pt, pb = ps_i.get()
                    op("pe", lambda e: e.matmul(pt[:, 0:512], lhsT=selw[0:8, pr_ * 128:(pr_ + 1) * 128], rhs=wT[0:8, qsl], start=True, stop=True),
                       [b_cst, b_wT], [pb])
                    op("dve", lambda e: e.scalar_tensor_tensor(out=qs[:, pr_, 0, :], in0=pt[:, 0:512], scalar=0.0, in1=qiT[:, pr_, qsl],
                                                               op0=ALU.max, op1=ALU.mult), [pb, b_qi], [b_qs])
                    op("dve", lambda e: e.scalar_tensor_tensor(out=qs[:, pr_, 1, :], in0=pt[:, 0:512], scalar=0.0, in1=qiT[:, pr_, qsl],
                                                               op0=ALU.min, op1=ALU.mult), [pb, b_qi], [b_qs])
                act_tiles = []
                for i in range(4):
                    t = 4 * qc + i
                    S = (t + 1) * 128
                    if t < 2:
                        if t > 0:
                            op("pool", lambda e: e.memset(mb[:, i, 0:t * 128], 0.0), [], [b_mbq[i]])
                        op("pool", lambda e: e.tensor_copy(out=mb[:, i, t * 128:S], in_=trinegB[:]), [b_cst], [b_mbq[i]])
                        continue
                    act_tiles.append(i)
                    ic, icb = isc[i], b_isc[i]
                    for kc in range((S + 511) // 512):
                        w = min(512, S - kc * 512)
                        ksl = slice(kc * 512, kc * 512 + w)
                        first = True
                        nsum = 0
                        p2, p2b = PS[3]
                        pend = []

                        def flush_one():
                            nonlocal nsum
                            (rl_, rlb_, sg_) = pend.pop(0)
                            op("pe", lambda e: e.matmul(p2[:, 0:w], lhsT=(identB[:] if sg_ == 0 else negidentB[:]), rhs=rl_[:, 0:w],
                                                        start=(nsum == 0), stop=(nsum == 7)), [rlb_, b_cst], [p2b])
                            nsum += 1

                        for k in range(16):
                            hd, sgn = k // 2, k % 2
                            pr_, bp = hd // 2, (hd % 2) * 64
                            pt, pb = ps_i.get()
                            op("pe", lambda e: e.matmul(pt[:, 0:w], lhsT=qs[bp:bp + 64, pr_, sgn, i * 128:(i + 1) * 128], rhs=kiT[bp:bp + 64, 0, ksl],
                                                        start=True, stop=True), [b_qs, b_ki], [pb])
                            o = ALU.max if sgn == 0 else ALU.min
                            if hd % 2 == 1:
                                rl, rlb = rls.get()
                                op("act", lambda e: e.activation(out=rl[:, 0:w], in_=pt[:, 0:w], func=AF.Relu, scale=(1.0 if sgn == 0 else -1.0)), [pb], [rlb])
                                pend.append((rl, rlb, sgn))
                                if len(pend) > 2:
                                    flush_one()
                            elif first:
                                op("dve", lambda e: e.tensor_scalar(out=ic[:, ksl], in0=pt[:, 0:w], scalar1=0.0, scalar2=None, op0=o), [pb], [icb])
                                first = False
                            else:
                                op("dve", lambda e: e.scalar_tensor_tensor(out=ic[:, ksl], in0=pt[:, 0:w], scalar=0.0, in1=ic[:, ksl], op0=o, op1=ALU.add),
                                   [pb, icb], [icb])
                        while pend:
                            flush_one()
                        op("dve", lambda e: e.tensor_tensor(out=ic[:, ksl], in0=p2[:, 0:w], in1=ic[:, ksl], op=ALU.add), [p2b, icb], [icb])
                    op("dve", lambda e: e.tensor_reduce(out=lo[:, i:i + 1], in_=ic[:, 0:S], axis=AX.X, op=ALU.min), [icb], [b_lo])
                    op("dve", lambda e: e.tensor_reduce(out=mx[:, i:i + 1], in_=ic[:, 0:S], axis=AX.X, op=ALU.max), [icb], [b_lo])
                    op("dve", lambda e: e.tensor_tensor(out=ic[:, t * 128:S], in0=ic[:, t * 128:S], in1=trinegbig, op=ALU.add), [icb, b_cst], [icb])
                    on_act = (i % 2 == 1)
                    op("dve", lambda e: e.memset(thc[:, i:i + 1], (S - 511.0) if on_act else (S - 255.5)), [], [b_lo])
                a0, a1 = act_tiles[0], act_tiles[-1] + 1
                asl = slice(a0, a1)
                op("dve", lambda e: e.scalar_tensor_tensor(out=w0[:, asl], in0=mx[:, asl], scalar=1.0, in1=lo[:, asl], op0=ALU.add, op1=ALU.subtract),
                   [b_lo], [b_lo])
                for it in range(N_BISECT):
                    ck = 0.5 ** (it + 1)
                    op("dve", lambda e, ck=ck: e.scalar_tensor_tensor(out=mid[:, asl], in0=w0[:, asl], scalar=ck, in1=lo[:, asl], op0=ALU.mult, op1=ALU.add),
                       [b_lo], [b_mid])
                    for i in act_tiles:
                        S = (4 * qc + i + 1) * 128
                        ic, icb = isc[i], b_isc[i]
                        if i % 2 == 1:
                            op("act", lambda e, i=i, S=S, ic=ic: e.activation(out=junkA[:, 0:S], in_=ic[:, 0:S], func=AF.Sign, bias=mid[:, i:i + 1], scale=-1.0,
                                                                              accum_out=cntv[:, i:i + 1]), [icb, b_mid], [b_cnt[i], b_junkA])
                        else:
                            op("dve", lambda e, i=i, S=S, ic=ic: e.tensor_scalar(out=junkD[:, 0:S], in0=ic[:, 0:S], scalar1=mid[:, i:i + 1], scalar2=None,
                                                                                 op0=ALU.is_lt, op1=ALU.add, accum_out=cntv[:, i:i + 1]), [icb, b_mid], [b_cnt[i], b_junkD])
                    op("dve", lambda e: e.tensor_tensor(out=bsi[:, asl], in0=cntv[:, asl], in1=thc[:, asl], op=ALU.is_le), [b_cnt[i] for i in act_tiles] + [b_lo], [b_msk])
                    op("dve", lambda e: e.copy_predicated(out=lo[:, asl], mask=bsi[:, asl], data=mid[:, asl]), [b_msk, b_mid], [b_lo])
                for i in act_tiles:
                    S = (4 * qc + i + 1) * 128
                    ic, icb = isc[i], b_isc[i]
                    op("dve", lambda e, i=i, S=S, ic=ic: e.tensor_scalar(out=mb[:, i, 0:S], in0=ic[:, 0:S], scalar1=lo[:, i:i + 1], scalar2=NEG, op0=ALU.is_lt, op1=ALU.mult),
                       [icb, b_lo], [b_mbq[i]])

            def dsa_mask(h, qc, j, t0, st, sb, wq):
                last = 4 * qc + 3
                mb = mbs[qc % 2]; b_mbq = b_mb2[qc % 2]
                for t in range(t0, last + 1):
                    i = t - 4 * qc
                    op("pe", lambda e, t=t, i=i: e.matmul(st[:, (t - t0) * 128:(t - t0 + 1) * 128], lhsT=mb[:, i, j * 128:(j + 1) * 128], rhs=identB[:],
                                                          start=False, stop=(t == last), skip_group_check=True), [b_mbq[i], b_cst], [sb])

            attention(qT, kT, b_qk, vaug, b_v, dsa_mask, 6, (Rot(PS[4:6]), Rot(PS[6:8]), Rot(PS[3:4])), otm, b_otm, Rot(pTl), rs, b_rs, mixT, mixb,
                      qc_hook=dsa_qc)
            cx.release(m0)

            if stop == 'dsa':
                cx.barrier()
                return nc, cx
            if debug and seq == 0 and layer == layers[0]:
                dma("sp", dbg[:, :, :], mixT[:, :, :], reads=mixb, owner=Buf("dbg"))

            cx.cur2 = Hoff + 65536
            m0 = cx.mark()
            Wo = alloc("Wo", [128, 8, D], BF16); b_Wo = Buf("Wo")
            gbc = alloc("gbc", [128, D], F32); bbc = alloc("bbc", [128, D], F32); b_par = Buf("lnpar")
            hres = [alloc("hres0", [128, D], F32), alloc("hres1", [128, D], F32)]; b_hres = [Buf("hres0"), Buf("hres1")]
            stat = alloc("stat", [128, 16], F32); b_stat = Buf("stat")
            rw = alloc("rw", [128, 8, 16], F32); rwh = alloc("rwh", [128, 8, 16], BF16); rwl = alloc("rwl", [128, 8, 16], BF16); b_rw = Buf("rw")
            rbb = alloc("rbb", [128, 16], F32)
            HTlo = alloc("HTlo", [128, 8, 128], BF16); b_lo = Buf("HTlo")
            comb = alloc("comb", [128, NT, 16], F32); b_comb = Buf("comb")
            rt = alloc("rt", [128, 8, 16], F32); b_rt = Buf("rt")
            load_w(Wo[:], w_out[layer][:, :], b_Wo)
            dma("sp", gbc[:], ln1g[layer, :].partition_broadcast(128), writes=[b_par])
            dma("sp", bbc[:], ln1b[layer, :].partition_broadcast(128), writes=[b_par], owner=Buf("lnb"))
            dma("sp", rw[:], r_w.rearrange("(c p) e -> p c e", p=128), writes=[b_rw])
            dma("sp", rbb[:], r_b.partition_broadcast(128), writes=[b_rw], owner=Buf("rbb"))
            op("dve", lambda e: e.tensor_copy(out=rwh[:], in_=rw[:]), [b_rw], [b_rw])
            op("dve", lambda e: e.tensor_tensor(out=rw[:], in0=rw[:], in1=rwh[:], op=ALU.subtract), [b_rw], [b_rw])
            op("dve", lambda e: e.tensor_copy(out=rwl[:], in_=rw[:]), [b_rw], [b_rw])
            ps_o = Rot(PS[0:4]); ps_t = Rot(PS[4:7]); ps_r = Rot(PS[7:8])
            def ln1_front(t):
                hr, hrb = hres[t % 2], b_hres[t % 2]
                dma("sp", hr[:], hs[t * 128:(t + 1) * 128, :], reads=[hsb[t]], writes=[hrb])
                for half in range(2):
                    po, pob = ps_o.get()
                    for c in range(8):
                        op("pe", lambda e, c=c: e.matmul(po[:, 0:512], lhsT=mixT[:, c, t * 128:(t + 1) * 128], rhs=Wo[:, c, half * 512:(half + 1) * 512],
                                                         start=(c == 0), stop=(c == 7)), [mixb[t], b_Wo], [pob])
                    op("dve", lambda e: e.scalar_tensor_tensor(out=H[:, t, half * 512:(half + 1) * 512], in0=hr[:, half * 512:(half + 1) * 512], scalar=ALPHA,
                                                               in1=po[:, 0:512], op0=ALU.mult, op1=ALU.add), [hrb, pob], [Hb[t]])
                layer_norm_tile(t, gbc, bbc, b_par, stat, b_stat)

            def ln1_back(t):
                for half in range(2):
                    pt, pb = ps_t.get()
                    for c4 in range(4):
                        c = half * 4 + c4
                        op("pe", lambda e, c=c, c4=c4: e.transpose(out=pt[:, c4 * 128:(c4 + 1) * 128], in_=H[:, t, c * 128:(c + 1) * 128], identity=identF),
                           [Hb[t], b_cst], [pb])
                    pv = pt[:, :].rearrange("p (a b) -> p a b", a=4)
                    op("act", lambda e: e.activation(out=HT[:, half * 4:(half + 1) * 4, t * 128:(t + 1) * 128], in_=pv, func=AF.Copy), [pb], [HTb[t]])
                    op("dve", lambda e: e.tensor_tensor(out=HTlo[:, half * 4:(half + 1) * 4, :], in0=pv, in1=HT[:, half * 4:(half + 1) * 4, t * 128:(t + 1) * 128],
                                                        op=ALU.subtract), [pb, HTb[t]], [b_lo])
                pr_, prb = ps_r.get()
                k = 0
                for (A_, Ab, W_) in ((HT[:, :, t * 128:(t + 1) * 128], HTb[t], rwh), (HTlo[:, :, :], b_lo, rwh), (HT[:, :, t * 128:(t + 1) * 128], HTb[t], rwl)):
                    for c in range(8):
                        op("pe", lambda e, c=c, A_=A_, W_=W_, k=k: e.matmul(pr_[:, 0:16], lhsT=A_[:, c, :], rhs=W_[:, c, :], start=(k == 0), stop=(k == 23)),
                           [Ab, b_rw], [prb])
                        k += 1
                sc = rt[:, 0, :]; sel = rt[:, 1, :]; tmpa = rt[:, 2, :]; tmpb = rt[:, 3, :]; ch = rt[:, 4, :]
                m1 = rt[:, 5, 0:4]; m2 = rt[:, 5, 4:8]; gs = rt[:, 5, 8:12]; ing = rt[:, 5, 12:16]
                gmax = rt[:, 6, 0:1]; a1 = rt[:, 6, 1:2]; a2 = rt[:, 6, 2:3]; ssum = rt[:, 6, 3:4]
                v3 = lambda a: a.rearrange("p (g e) -> p g e", g=4)
                R = [b_rt]
                op("act", lambda e: e.activation(out=sc, in_=pr_[:, 0:16], func=AF.Sigmoid), [prb], R)
                op("dve", lambda e: e.tensor_tensor(out=sel, in0=sc, in1=rbb[:], op=ALU.add), R + [b_rw], R)
                op("dve", lambda e: e.tensor_reduce(out=m1, in_=v3(sel), axis=AX.X, op=ALU.max), R, R)
                op("dve", lambda e: e.tensor_tensor(out=v3(tmpa), in0=v3(sel), in1=m1[:, :, None].to_broadcast([128, 4, 4]), op=ALU.is_equal), R, R)
                op("dve", lambda e: e.scalar_tensor_tensor(out=tmpb, in0=tmpa, scalar=-1.0e9, in1=sel, op0=ALU.mult, op1=ALU.add), R, R)
                op("dve", lambda e: e.tensor_reduce(out=m2, in_=v3(tmpb), axis=AX.X, op=ALU.max), R, R)
                op("dve", lambda e: e.tensor_tensor(out=gs, in0=m1, in1=m2, op=ALU.add), R, R)
                op("dve", lambda e: e.tensor_reduce(out=gmax, in_=gs, axis=AX.X, op=ALU.max), R, R)
                op("dve", lambda e: e.tensor_scalar(out=ing, in0=gs, scalar1=gmax, scalar2=-1.0e9, op0=ALU.is_lt, op1=ALU.mult), R, R)
                op("dve", lambda e: e.tensor_tensor(out=v3(tmpa), in0=v3(sel), in1=ing[:, :, None].to_broadcast([128, 4, 4]), op=ALU.add), R, R)
                op("dve", lambda e: e.tensor_reduce(out=a1, in_=tmpa, axis=AX.X, op=ALU.max), R, R)
                op("dve", lambda e: e.tensor_scalar(out=ch, in0=tmpa, scalar1=a1, scalar2=None, op0=ALU.is_ge), R, R)
                op("dve", lambda e: e.scalar_tensor_tensor(out=tmpb, in0=ch, scalar=-1.0e9, in1=tmpa, op0=ALU.mult, op1=ALU.add), R, R)
                op("dve", lambda e: e.tensor_reduce(out=a2, in_=tmpb, axis=AX.X, op=ALU.max), R, R)
                op("dve", lambda e: e.tensor_scalar(out=tmpa, in0=tmpb, scalar1=a2, scalar2=None, op0=ALU.is_ge), R, R)
                op("dve", lambda e: e.tensor_tensor(out=ch, in0=ch, in1=tmpa, op=ALU.add), R, R)
                op("dve", lambda e: e.tensor_tensor(out=tmpb, in0=ch, in1=sc, op=ALU.mult), R, R)
                op("dve", lambda e: e.tensor_reduce(out=ssum, in_=tmpb, axis=AX.X, op=ALU.add), R, R)
                op("dve", lambda e: e.reciprocal(out=ssum, in_=ssum), R, R)
                op("dve", lambda e: e.tensor_scalar(out=comb[:, t, :], in0=tmpb, scalar1=ssum, scalar2=None, op0=ALU.mult), R, [b_comb])
                op("act", lambda e: e.activation(out=H[:, t, :], in_=H[:, t, :], func=AF.Copy, scale=ALPHA), [Hb[t]], [Hb[t]])

            ln1_front(0)
            for t in range(NT):
                if t + 1 < NT:
                    ln1_front(t + 1)
                ln1_back(t)
            cx.barrier()
            cx.cur, cx.cur2 = base_mark
            cx.cur2 = Hoff + 65536
            comb2 = alloc("comb2", [128, NT, 16], F32); b_comb2 = Buf("comb2")
            op("dve", lambda e: e.tensor_copy(out=comb2[:], in_=comb[:]), [b_comb], [b_comb2])
            cx.barrier()

            if stop == 'ln1':
                cx.barrier()
                return nc, cx
            m0 = cx.mark()
            Wg_ = [alloc(f"weg{i}", [128, 8, 512], BF16) for i in range(2)]; b_weg = [Buf("weg0"), Buf("weg1")]
            Wu_ = [alloc(f"weu{i}", [128, 8, 512], BF16) for i in range(2)]; b_weu = [Buf("weu0"), Buf("weu1")]
            Wd_ = [alloc(f"wed{i}", [128, 4, D], BF16) for i in range(2)]; b_wed = [Buf("wed0"), Buf("wed1")]
            actT = alloc("actT", [128, 4, L], BF16); b_act = [[Buf(f"act{f}_{tc}") for tc in range(4)] for f in range(4)]
            sgb = [(alloc(f"sgm{i}", [128, 512], F32), Buf(f"sgm{i}")) for i in range(3)]
            sgr = Rot(sgb)
            ps_gu = Rot(PS[0:4]); ps_d = Rot(PS[4:8])
            for ex in range(16):
                wg, wu, wd = Wg_[ex % 2], Wu_[ex % 2], Wd_[ex % 2]
                bg, bu, bd = b_weg[ex % 2], b_weu[ex % 2], b_wed[ex % 2]
                load_w(wg[:], w_eg[layer, ex], bg)
                load_w(wu[:], w_eu[layer, ex], bu)
                dma("pool", wd[:], w_ed[layer, ex].rearrange("(c p) f -> p c f", p=128), writes=[bd])
                for tc in range(4):
                    for f in range(4):
                        pg_, pgb = ps_gu.get(); pu_, pub = ps_gu.get()
                        for c in range(8):
                            op("pe", lambda e, c=c: e.matmul(pg_[:, 0:512], lhsT=wg[:, c, f * 128:(f + 1) * 128], rhs=HT[:, c, tc * 512:(tc + 1) * 512],
                                                             start=(c == 0), stop=(c == 7)), [bg] + HTb[tc * 4:tc * 4 + 4], [pgb])
                        for c in range(8):
                            op("pe", lambda e, c=c: e.matmul(pu_[:, 0:512], lhsT=wu[:, c, f * 128:(f + 1) * 128], rhs=HT[:, c, tc * 512:(tc + 1) * 512],
                                                             start=(c == 0), stop=(c == 7)), [bu] + HTb[tc * 4:tc * 4 + 4], [pub])
                        sgt, sgtb = sgr.get()
                        op("act", lambda e: e.activation(out=sgt[:], in_=pg_[:, 0:512], func=AF.Silu), [pgb], [sgtb])
                        op("dve", lambda e: e.tensor_tensor(out=actT[:, f, tc * 512:(tc + 1) * 512], in0=pu_[:, 0:512], in1=sgt[:], op=ALU.mult),
                           [pub, sgtb], [b_act[f][tc]])
                    for t4 in range(4):
                        t = tc * 4 + t4
                        for half in range(2):
                            pd_, pdb = ps_d.get()
                            for f in range(4):
                                op("pe", lambda e, f=f: e.matmul(pd_[:, 0:512], lhsT=actT[:, f, t * 128:(t + 1) * 128], rhs=wd[:, f, half * 512:(half + 1) * 512],
                                                                 start=(f == 0), stop=(f == 3)), [b_act[f][tc], bd], [pdb])
                            op("dve", lambda e: e.scalar_tensor_tensor(out=H[:, t, half * 512:(half + 1) * 512], in0=pd_[:, 0:512], scalar=comb2[:, t, ex:ex + 1],
                                                                       in1=H[:, t, half * 512:(half + 1) * 512], op0=ALU.mult, op1=ALU.add),
                               [pdb, b_comb2, Hb[t]], [Hb[t]])
            cx.release(m0)

            if stop == 'moe':
                cx.barrier()
                return nc, cx
            m0 = cx.mark()
            gbc = alloc("gbc", [128, D], F32); bbc = alloc("bbc", [128, D], F32); b_par = Buf("lnpar")
            stat = alloc("stat", [128, 16], F32); b_stat = Buf("stat")
            dma("sp", gbc[:], ln2g[layer, :].partition_broadcast(128), writes=[b_par])
            dma("sp", bbc[:], ln2b[layer, :].partition_broadcast(128), writes=[b_par], owner=Buf("lnb"))
            pool_tr = Rot(PS[0:4])
            last = (layer == layers[-1])
            for t in range(NT):
                layer_norm_tile(t, gbc, bbc, b_par, stat, b_stat)
                if last:
                    dma("sp", out[seq, t * 128:(t + 1) * 128, :], H[:, t, :], reads=[Hb[t]], owner=Hb[t])
                else:
                    make_HT(t, pool_tr)
            cx.release(m0)
            cx.cur, cx.cur2 = base_mark

    cx.barrier()
    return nc, cx


_CACHE = {}


def _prep_inputs(inputs):
    cst, cst8 = _consts()
    w_in_x = np.ascontiguousarray(np.asarray(inputs["w_in"], np.float32)[:, :, COLMAP])
    shared = {"w_in_x": w_in_x, "cst": cst, "cst8": cst8}
    for k in ("gla_gate_up", "gla_gate_bias", "gla_norm_gain", "w_out", "ln_mix_g", "ln_mix_b", "router_w", "router_bias",
              "w_expert_gate", "w_expert_up", "w_expert_down", "ln_ffn_g", "ln_ffn_b"):
        shared[k] = np.ascontiguousarray(np.asarray(inputs[k], np.float32))
    return shared


def kernel(**inputs):
    x = np.ascontiguousarray(np.asarray(inputs["x"], np.float32))
    positions = np.ascontiguousarray(np.asarray(inputs["positions"], np.int32))
    shared = _prep_inputs(inputs)
    ncores, nseq = 8, 2
    nc = bass.Bass("TRN2", target_bir_lowering=False)
    build(nc, nseq, [0, 1])
    in_maps = []
    for c in range(ncores):
        m = dict(shared)
        m["x"] = x[c * nseq:(c + 1) * nseq]
        m["positions"] = positions[c * nseq:(c + 1) * nseq]
        in_maps.append(m)
    res = run_bass_kernel_spmd(nc, in_maps, core_ids=list(range(ncores)))
    return np.concatenate([r["out"] for r in res.results], axis=0)
```

```python
import numpy as np
import concourse.bass as bass
import concourse.mybir as mybir
from concourse.bass_utils import run_bass_kernel_spmd

F32 = mybir.dt.float32
BF16 = mybir.dt.bfloat16
I32 = mybir.dt.int32
ALU = mybir.AluOpType
AF = mybir.ActivationFunctionType
AX = mybir.AxisListType

L = 2048
D = 1024
NT = 16
NEG = -30000.0
BIG = 1.0e30
ALPHA = 4.0 ** 0.25
N_BISECT = 18
SBUF_BASE = 16512
SBUF_BYTES = 229000

IN_WIDTHS = (256, 256, 512, 16, 512, 256, 256, 256, 256, 256, 256, 512, 64, 8)
IN_OFF = np.concatenate([[0], np.cumsum(IN_WIDTHS)]).astype(int)
(O_GQ, O_GK, O_GV, O_GR, O_GG, O_MQ, O_MK, O_MV, O_SQ, O_SK, O_SV, O_IQ, O_IK, O_IW) = [int(v) for v in IN_OFF[:-1]]


def _rot_cols(base, nheads):
    cols = []
    for h in range(nheads):
        for d in range(64):
            if d < 8:
                pd = d + 8
            elif d < 16:
                pd = d - 8
            else:
                pd = d
            cols.append(base + h * 64 + pd)
    return cols


def _build_colmap():
    cols = []
    off = {}

    def add(name, c):
        off[name] = len(cols)
        cols.extend(c)

    for p in range(2):
        add(f"g{p}", list(range(O_GQ + p * 128, O_GQ + p * 128 + 128)) + list(range(O_GK + p * 128, O_GK + p * 128 + 128))
            + list(range(O_GV + p * 256, O_GV + p * 256 + 256)) + list(range(O_GG + p * 256, O_GG + p * 256 + 256))
            + list(range(O_GR, O_GR + 16)))
    add("mq", list(range(O_MQ, O_MQ + 256)))
    add("mqr", _rot_cols(O_MQ, 4))
    add("mk", list(range(O_MK, O_MK + 256)))
    add("mkr", _rot_cols(O_MK, 4))
    add("mv", list(range(O_MV, O_MV + 256)))
    add("sq", list(range(O_SQ, O_SQ + 256)))
    add("sqr", _rot_cols(O_SQ, 4))
    add("sk", list(range(O_SK, O_SK + 256)))
    add("skr", _rot_cols(O_SK, 4))
    add("sv", list(range(O_SV, O_SV + 256)))
    add("iq", list(range(O_IQ, O_IQ + 512)))
    add("iqr", _rot_cols(O_IQ, 8))
    add("ik", list(range(O_IK, O_IK + 64)) * 2)
    add("ikr", _rot_cols(O_IK, 1) * 2)
    add("iw", list(range(O_IW, O_IW + 8)))
    return np.array(cols, dtype=np.int64), off


COLMAP, COFF = _build_colmap()
WX = len(COLMAP)


def _consts():
    c = np.zeros((128, 1024), np.float32)
    c[:, 0:128] = np.eye(128, dtype=np.float32)
    j = np.arange(128)[:, None]
    i = np.arange(128)[None, :]
    c[:, 128:256] = (i >= j).astype(np.float32)
    c[:, 256:384] = np.where(i <= j, 0.0, NEG)
    c[:, 384:512] = np.where(i <= j, 0.0, -BIG)
    p = np.arange(128)
    d = p % 64
    invf = np.where(d < 16, 500000.0 ** (-(d % 8) / 8.0), 0.0)
    c[:, 512] = invf / (2 * np.pi)
    c[:, 513] = np.where(d < 8, -invf, invf) / (2 * np.pi)
    c8 = np.zeros((8, 2048 + 512 + 8), np.float32)
    for k in range(8):
        for n in range(8):
            c8[k, 2560 + n] = 0.0 if n < k else (BIG if n == k else -BIG)
    for n in range(8):
        c8[n, n * 256:(n + 1) * 256] = 1.0
    for k in range(8):
        pr, hh = k // 2, k % 2
        c8[k, 2048 + pr * 128 + hh * 64: 2048 + pr * 128 + hh * 64 + 64] = 1.0
    return c, c8


class Buf:
    __slots__ = ("name", "writer", "readers", "dsem", "dcnt")

    def __init__(self, name):
        self.name = name
        self.writer = None
        self.readers = {}
        self.dsem = None
        self.dcnt = 0


class Ctx:
    def __init__(self, nc):
        self.nc = nc
        self.E = {"pe": nc.tensor, "act": nc.scalar, "dve": nc.vector, "pool": nc.gpsimd, "sp": nc.sync}
        self.sem = {k: nc.alloc_semaphore("e_" + k) for k in self.E}
        self.cnt = {k: 0 for k in self.E}
        self.seen = {k: {} for k in self.E}
        self.dbufs = []
        self.slots = {}
        self.nalloc = 0
        self.cur = SBUF_BASE
        self.cur2 = 0
        self.lim2 = 0
        self.peak = 0

    def alloc(self, name, shape, dtype, reg=0):
        isz = 4 if dtype in (F32, I32) else 2
        n = isz
        for s in shape[1:]:
            n *= s
        n = (n + 63) // 64 * 64
        if reg == 0:
            off = self.cur
            self.cur += n
            self.peak = max(self.peak, self.cur)
            assert self.cur <= SBUF_BYTES, (name, self.cur)
        else:
            off = self.cur2
            self.cur2 += n
            assert self.cur2 <= self.lim2, (name, self.cur2, self.lim2)
        self.nalloc += 1
        return self.nc.alloc_sbuf_tensor_at(f"{name}_{self.nalloc}", list(shape), dtype, offset=off)

    def mark(self):
        return (self.cur, self.cur2)

    def release(self, m):
        self.barrier()
        self.cur, self.cur2 = m

    def _wait(self, eng, dep):
        kind, obj, val = dep
        key = obj if kind == "e" else id(obj)
        if self.seen[eng].get(key, 0) >= val:
            return
        sem = self.sem[obj] if kind == "e" else obj.dsem
        self.E[eng].wait_ge(sem, val)
        self.seen[eng][key] = val

    def _deps(self, eng, reads, writes):
        for b in reads:
            if b.writer is not None:
                self._wait(eng, b.writer)
        for b in writes:
            w = b.writer
            if w is not None and not (w[0] == "e" and w[1] == eng and eng == "pe"):
                self._wait(eng, w)
            for r in b.readers.values():
                if not (r[0] == "e" and r[1] == eng and eng == "pe"):
                    self._wait(eng, r)

    def op(self, eng, fn, reads=(), writes=()):
        self._deps(eng, reads, writes)
        inst = fn(self.E[eng])
        inst.then_inc(self.sem[eng], 1)
        self.cnt[eng] += 1
        tag = ("e", eng, self.cnt[eng])
        for b in reads:
            b.readers[eng] = tag
        for b in writes:
            b.writer = tag
            b.readers = {}

    def dma(self, q, out, in_, reads=(), writes=(), owner=None):
        self._deps(q, reads, writes)
        if owner is None:
            owner = writes[0] if writes else reads[0]
        slot = self.slots.get(owner.name)
        if slot is None:
            slot = Buf("slot_" + owner.name)
            slot.dsem = self.nc.alloc_semaphore("d_%d" % len(self.slots))
            self.slots[owner.name] = slot
            self.dbufs.append(slot)
        self.E[q].dma_start(out=out, in_=in_).then_inc(slot.dsem, 16)
        slot.dcnt += 16
        tag = ("d", slot, slot.dcnt)
        for b in reads:
            b.readers[("d", id(slot))] = tag
        for b in writes:
            b.writer = tag
            b.readers = {}

    def barrier(self):
        for e in self.E:
            for e2 in self.E:
                if e2 != e and self.cnt[e2] > 0:
                    self._wait(e, ("e", e2, self.cnt[e2]))
            for b in self.dbufs:
                if b.dcnt:
                    self._wait(e, ("d", b, b.dcnt))


class Rot:
    def __init__(self, items):
        self.items = items
        self.i = 0

    def get(self):
        it = self.items[self.i % len(self.items)]
        self.i += 1
        return it


class StopBuild(Exception):
    pass


def build(nc, nseq, layers, debug=False, stop=None):
    try:
        return _build(nc, nseq, layers, debug, stop)
    except StopBuild as ex:
        return nc, ex.args[0]


def _build(nc, nseq, layers, debug=False, stop=None):
    cx = Ctx(nc)
    op, dma, alloc = cx.op, cx.dma, cx.alloc

    def chk(name):
        if stop == name:
            cx.barrier()
            raise StopBuild(cx)

    x = nc.dram_tensor("x", [nseq, L, D], F32, kind="ExternalInput").ap()
    pos = nc.dram_tensor("positions", [nseq, L], I32, kind="ExternalInput").ap()
    w_in = nc.dram_tensor("w_in_x", [2, D, WX], F32, kind="ExternalInput").ap()
    g_up = nc.dram_tensor("gla_gate_up", [2, 16, 256], F32, kind="ExternalInput").ap()
    g_bias = nc.dram_tensor("gla_gate_bias", [2, 256], F32, kind="ExternalInput").ap()
    g_gain = nc.dram_tensor("gla_norm_gain", [2, 128], F32, kind="ExternalInput").ap()
    w_out = nc.dram_tensor("w_out", [2, D, D], F32, kind="ExternalInput").ap()
    ln1g = nc.dram_tensor("ln_mix_g", [2, D], F32, kind="ExternalInput").ap()
    ln1b = nc.dram_tensor("ln_mix_b", [2, D], F32, kind="ExternalInput").ap()
    r_w = nc.dram_tensor("router_w", [D, 16], F32, kind="ExternalInput").ap()
    r_b = nc.dram_tensor("router_bias", [16], F32, kind="ExternalInput").ap()
    w_eg = nc.dram_tensor("w_expert_gate", [2, 16, D, 512], F32, kind="ExternalInput").ap()
    w_eu = nc.dram_tensor("w_expert_up", [2, 16, D, 512], F32, kind="ExternalInput").ap()
    w_ed = nc.dram_tensor("w_expert_down", [2, 16, 512, D], F32, kind="ExternalInput").ap()
    ln2g = nc.dram_tensor("ln_ffn_g", [2, D], F32, kind="ExternalInput").ap()
    ln2b = nc.dram_tensor("ln_ffn_b", [2, D], F32, kind="ExternalInput").ap()
    cst_d = nc.dram_tensor("cst", [128, 1024], F32, kind="ExternalInput").ap()
    cst8_d = nc.dram_tensor("cst8", [8, 2568], F32, kind="ExternalInput").ap()
    out = nc.dram_tensor("out", [nseq, L, D], F32, kind="ExternalOutput").ap()
    hs = nc.dram_tensor("hspill", [L, D], F32, kind="Internal").ap()
    dbg = nc.dram_tensor("dbg", [128, 8, L], BF16, kind="ExternalOutput").ap() if debug else None

    cst = alloc("cst", [128, 1024], F32)
    c8b = alloc("c8b", [8, 2568], BF16)
    identB = alloc("identB", [128, 128], BF16)
    trinegB = alloc("trinegB", [128, 128], BF16)
    negidentB = alloc("negidentB", [128, 128], BF16)
    epsln = alloc("epsln", [128, 2], F32)
    zerosB = alloc("zerosB", [1, 512], BF16)
    HT = alloc("HT", [128, 8, L], BF16)
    Hoff = cx.cur
    H = alloc("H", [128, NT, D], F32)
    base_mark = cx.mark()

    b_cst = Buf("cst")
    Hb = [Buf(f"H{t}") for t in range(NT)]
    HTb = [Buf(f"HT{t}") for t in range(NT)]
    hsb = [Buf(f"hs{t}") for t in range(NT)]
    outb = Buf("out")

    identF = cst[:, 0:128]
    tri01 = cst[:, 128:256]
    trinegbig = cst[:, 384:512]
    invf = cst[:, 512:513]
    sinvf = cst[:, 513:514]
    onehotK = c8b[0:8, 0:2048]
    selw = c8b[0:8, 2048:2560]
    bpen = c8b[0:8, 2560:2568]

    PS = []
    for i in range(8):
        t = nc.alloc_psum_tensor(f"ps{i}", [128, 512], F32)
        PS.append((t, Buf(f"ps{i}")))

    dma("sp", cst[:], cst_d[:, :], writes=[b_cst])
    dma("pool", c8b[:], cst8_d[:, :], writes=[b_cst], owner=Buf("c8"))
    op("dve", lambda e: e.tensor_copy(out=identB[:], in_=cst[:, 0:128]), [b_cst], [b_cst])
    op("dve", lambda e: e.tensor_copy(out=trinegB[:], in_=cst[:, 256:384]), [b_cst], [b_cst])
    op("dve", lambda e: e.tensor_scalar(out=negidentB[:], in0=cst[:, 0:128], scalar1=-1.0, scalar2=None, op0=ALU.mult), [b_cst], [b_cst])
    op("dve", lambda e: e.memset(epsln[:, 0:1], 1e-5), [], [b_cst])
    op("dve", lambda e: e.memset(epsln[:, 1:2], 1e-6), [], [b_cst])
    op("dve", lambda e: e.memset(zerosB[:], 0.0), [], [b_cst])
    cx.barrier()

    def load_w(dst, src_rows_cols, buf, q="pool"):
        dma(q, dst, src_rows_cols.rearrange("(c p) f -> p c f", p=128), writes=[buf])

    def make_HT(t, pool):
        for half in range(2):
            pt, pb = pool.get()
            for c4 in range(4):
                c = half * 4 + c4
                op("pe", lambda e, c=c, c4=c4: e.transpose(out=pt[:, c4 * 128:(c4 + 1) * 128],
                                                           in_=H[:, t, c * 128:(c + 1) * 128], identity=identF),
                   [Hb[t], b_cst], [pb])
            op("act", lambda e: e.activation(out=HT[:, half * 4:(half + 1) * 4, t * 128:(t + 1) * 128],
                                             in_=pt[:, :].rearrange("p (a b) -> p a b", a=4), func=AF.Copy),
               [pb], [HTb[t]])

    def ln_stats(t, stat, b_stat):
        st = stat[:, t, :]
        bs_ = [b_stat[t]]
        op("dve", lambda e: e.bn_stats(out=st[:, 0:6], in_=H[:, t, 0:512]), [Hb[t]], bs_)
        op("dve", lambda e: e.bn_stats(out=st[:, 6:12], in_=H[:, t, 512:1024]), [Hb[t]], bs_)
        op("dve", lambda e: e.bn_aggr(out=st[:, 12:14], in_=st[:, 0:12]), bs_, bs_)
        op("act", lambda e: e.activation(out=st[:, 14:15], in_=st[:, 13:14], func=AF.Sqrt, bias=epsln[:, 0:1], scale=1.0),
           bs_ + [b_cst], bs_)
        op("dve", lambda e: e.reciprocal(out=st[:, 15:16], in_=st[:, 14:15]), bs_, bs_)
        op("dve", lambda e: e.scalar_tensor_tensor(out=st[:, 16:17], in0=st[:, 12:13], scalar=-1.0, in1=st[:, 15:16], op0=ALU.mult, op1=ALU.mult), bs_, bs_)

    def ln_apply(t, gbc, bbc, b_par, stat, b_stat, between=None):
        hv = H[:, t, :]
        st = stat[:, t, :]
        op("act", lambda e: e.activation(out=hv, in_=hv, func=AF.Identity, bias=st[:, 16:17], scale=st[:, 15:16]), [Hb[t], b_stat[t]], [Hb[t]])
        if between is not None:
            between()
        op("dve", lambda e: e.tensor_tensor(out=hv, in0=hv, in1=gbc[:], op=ALU.mult), [Hb[t], b_par], [Hb[t]])
        op("dve", lambda e: e.tensor_tensor(out=hv, in0=hv, in1=bbc[:], op=ALU.add), [Hb[t], b_par], [Hb[t]])

    def rope_tables(seq, cosT, sinT, b_tab):
        m = cx.mark()
        posf = alloc("posf", [128, L], F32)
        tmp = alloc("rtmp", [128, L], F32)
        tmi = alloc("rtmi", [128, L], I32)
        b_t = Buf("ropetmp")
        dma("pool", posf[:], pos[seq, :].partition_broadcast(128), writes=[b_t])
        for (tab, fcol, shift) in ((sinT, sinvf, 0.0), (cosT, invf, 0.25)):
            op("dve", lambda e, fcol=fcol, shift=shift: e.tensor_scalar(out=tmp[:], in0=posf[:], scalar1=fcol, scalar2=shift,
                                                                         op0=ALU.mult, op1=ALU.add), [b_t, b_cst], [b_t])
            op("dve", lambda e: e.tensor_copy(out=tmi[:], in_=tmp[:]), [b_t], [b_t])
            op("dve", lambda e: e.tensor_copy(out=tab[:], in_=tmi[:]), [b_t], [b_tab])
            op("dve", lambda e: e.tensor_tensor(out=tmp[:], in0=tmp[:], in1=tab[:], op=ALU.subtract), [b_t, b_tab], [b_t])
            op("dve", lambda e: e.tensor_scalar(out=tab[:], in0=tmp[:], scalar1=0.5, scalar2=None, op0=ALU.is_gt), [b_t], [b_tab])
            op("dve", lambda e: e.tensor_tensor(out=tmp[:], in0=tmp[:], in1=tab[:], op=ALU.subtract), [b_t, b_tab], [b_t])
            op("dve", lambda e: e.tensor_scalar(out=tab[:], in0=tmp[:], scalar1=-0.5, scalar2=None, op0=ALU.is_lt), [b_t], [b_tab])
            op("dve", lambda e: e.tensor_tensor(out=tmp[:], in0=tmp[:], in1=tab[:], op=ALU.add), [b_t, b_tab], [b_t])
            op("act", lambda e: e.activation(out=tab[:], in_=tmp[:], func=AF.Sin, scale=2 * np.pi), [b_t], [b_tab])
        cx.release(m)

    def proj_fm(wt, col0, M, tc, pt, pb, wbuf, pbase=0):
        for c in range(8):
            op("pe", lambda e, c=c: e.matmul(pt[pbase:pbase + M, 0:512], lhsT=wt[:, c, col0:col0 + M],
                                             rhs=HT[:, c, tc * 512:(tc + 1) * 512], start=(c == 0), stop=(c == 7)),
               [wbuf] + HTb[tc * 4:(tc + 1) * 4], [pb])

    def proj_tm(wt, col0, N, t, pt, pb, wbuf, ocol=0):
        for c in range(8):
            op("pe", lambda e, c=c: e.matmul(pt[:, ocol:ocol + N], lhsT=HT[:, c, t * 128:(t + 1) * 128],
                                             rhs=wt[:, c, col0:col0 + N], start=(c == 0), stop=(c == 7)),
               [wbuf, HTb[t]], [pb])

    def roped_proj(wt, wbuf, col, colr, dstT, b_dst, cosT, sinT, b_tab, tmps, pspool, ksum=None):
        ntile = dstT.shape[1]
        (t1, t2, b_tmp) = tmps
        for ti in range(ntile):
            for tc in range(4):
                py, pyb = pspool.get()
                pr, prb = pspool.get()
                proj_fm(wt, col + ti * 128, 128, tc, py, pyb, wbuf)
                proj_fm(wt, colr + ti * 128, 128, tc, pr, prb, wbuf)
                sl = slice(tc * 512, (tc + 1) * 512)
                op("dve", lambda e: e.tensor_tensor(out=t1[:], in0=pr[:, :], in1=sinT[:, sl], op=ALU.mult), [prb, b_tab], [b_tmp])
                op("dve", lambda e: e.tensor_tensor(out=t2[:], in0=py[:, :], in1=cosT[:, sl], op=ALU.mult), [pyb, b_tab], [b_tmp])
                if ksum is not None:
                    op("pool", lambda e: e.tensor_tensor(out=t2[:], in0=t1[:], in1=t2[:], op=ALU.add), [b_tmp], [b_tmp])
                    op("act", lambda e: e.activation(out=dstT[:, ti, sl], in_=t2[:], func=AF.Copy), [b_tmp], [b_dst])
                    (km, b_km) = ksum
                    op("dve", lambda e: e.tensor_reduce(out=km[:, ti, tc * 2:(tc + 1) * 2], in_=t2[:, :].rearrange("p (a b) -> p a b", a=2),
                                                        axis=AX.X, op=ALU.add), [b_tmp], [b_km])
                else:
                    op("pool", lambda e: e.tensor_tensor(out=dstT[:, ti, sl], in0=t1[:], in1=t2[:], op=ALU.add), [b_tmp], [b_dst])

    def v_aug_proj(wt, wbuf, col, vaug, b_v, pspool):
        op("dve", lambda e: e.memset(vaug[:, :, :, 64:65], 1.0), [], [b_v])
        for t in range(NT):
            pt, pb = pspool.get()
            proj_tm(wt, col, 256, t, pt, pb, wbuf)
            op("act", lambda e: e.activation(out=vaug[:, t, :, 0:64], in_=pt[:, 0:256].rearrange("p (a b) -> p a b", a=4), func=AF.Copy),
               [pb], [b_v])

    def attention(qT, kT, b_qk, vaug, b_v, maskfn, mix_chunk0, pools, otm, b_otm, pTs, rs, b_rs, mixT, mixb, qc_hook=None):
        (ps_s, ps_acc, ps_tr) = pools
        if qc_hook is not None:
            qc_hook(0)
        for qc in range(4):
            if qc_hook is not None and qc + 1 < 4:
                qc_hook(qc + 1)
            for h in range(4):
                ptile, bp = h // 2, (h % 2) * 64
                acc, accb = ps_acc.get()
                op("pe", lambda e: e.matmul(acc[:, 0:260], lhsT=zerosB[0:1, 0:128], rhs=zerosB[0:1, 0:260], start=True, stop=False,
                                            skip_group_check=True), [b_cst], [accb])
                prev = None

                def emit_pv(args):
                    (j_, t0_, pT_, pTb_) = args
                    for t in range(t0_, 4 * qc + 4):
                        i = t - 4 * qc
                        op("pe", lambda e, t=t, i=i: e.matmul(acc[:, i * 65:(i + 1) * 65], lhsT=pT_[:, (t - t0_) * 128:(t - t0_ + 1) * 128],
                                                              rhs=vaug[:, j_, h, :], start=False, stop=(j_ == t), skip_group_check=True),
                           [pTb_, b_v], [accb])

                for j in range(4 * qc + 4):
                    t0 = max(j, 4 * qc)
                    q0, q1 = t0 * 128, (4 * qc + 4) * 128
                    wq = q1 - q0
                    st, sb = ps_s.get()
                    op("pe", lambda e: e.matmul(st[:, 0:wq], lhsT=kT[bp:bp + 64, ptile, j * 128:(j + 1) * 128],
                                                rhs=qT[bp:bp + 64, ptile, q0:q1], start=True, stop=False, skip_group_check=True),
                       [b_qk], [sb])
                    maskfn(h, qc, j, t0, st, sb, wq)
                    pT, pTb = pTs.get()
                    op("act", lambda e: e.activation(out=pT[:, 0:wq], in_=st[:, 0:wq], func=AF.Exp, scale=0.125), [sb], [pTb])
                    if prev is not None:
                        emit_pv(prev)
                    prev = (j, t0, pT, pTb)
                emit_pv(prev)
                acc3 = acc[:, 0:260].rearrange("p (a b) -> p a b", a=4)
                op("dve", lambda e: e.reciprocal(out=rs[:, :], in_=acc3[:, :, 64]), [accb], [b_rs])
                for i in range(4):
                    op("dve", lambda e, i=i: e.tensor_scalar(out=otm[:, i, h * 64:(h + 1) * 64], in0=acc3[:, i, 0:64],
                                                             scalar1=rs[:, i:i + 1], scalar2=None, op0=ALU.mult), [accb, b_rs], [b_otm])
            for i in range(4):
                t = 4 * qc + i
                pt, pb = ps_tr.get()
                for f in range(2):
                    op("pe", lambda e, f=f: e.transpose(out=pt[:, f * 128:(f + 1) * 128], in_=otm[:, i, f * 128:(f + 1) * 128], identity=identF),
                       [b_otm, b_cst], [pb])
                op("act", lambda e: e.activation(out=mixT[:, mix_chunk0:mix_chunk0 + 2, t * 128:(t + 1) * 128],
                                                 in_=pt[:, 0:256].rearrange("p (a b) -> p a b", a=2), func=AF.Copy), [pb], [mixb[t]])

    for seq in range(nseq):
        pool_tr = Rot(PS[0:4])
        for t in range(NT):
            dma("sp", H[:, t, :], x[seq, t * 128:(t + 1) * 128, :], writes=[Hb[t]])
        for t in range(NT):
            make_HT(t, pool_tr)

        for layer in layers:
            for t in range(NT):
                dma("sp", hs[t * 128:(t + 1) * 128, :], H[:, t, :], reads=[Hb[t]], writes=[hsb[t]], owner=Hb[t])
            cx.barrier()
            cx.cur, cx.cur2 = base_mark
            cx.cur2, cx.lim2 = Hoff, Hoff + 65536
            mixT = alloc("mixT", [128, 8, L], BF16)
            mixb = [Buf(f"mix{t}") for t in range(NT)]

            if stop == 'ht':
                cx.barrier()
                return nc, cx
            for p in range(2):
                m0 = cx.mark()
                Wg = alloc("Wg", [128, 8, 784], BF16); b_Wg = Buf("Wg")
                gu = alloc("gu", [16, 128], BF16)
                nbias = alloc("nbias", [128, 1], F32)
                gain = alloc("gain", [128, 2, 128], F32)
                b_gp = Buf("gpar")
                rankT = alloc("rankT", [16, L], BF16); b_rank = Buf("rank")
                la = alloc("la", [128, L], F32, 1); b_la = Buf("la")
                bc = alloc("bc", [128, L], F32, 1); b_bc = Buf("bc")
                nbl = alloc("nbl", [128, 16], F32); dec = alloc("dec", [128, 16], F32); b_nbl = Buf("nbl")
                qdT = alloc("qdT", [128, L], BF16, 1); kiT = alloc("kiT", [128, L], BF16, 1); b_qk = Buf("gqk")
                kd = alloc("kd", [128, NT, 128], BF16, 1); b_kd = Buf("kd")
                vtm = alloc("vtm", [128, NT, 256], BF16, 1); b_v = Buf("gv")
                Sall = alloc("Sall", [128, NT, 128], BF16, 1); b_Sall = Buf("Sall")
                Sst = alloc("Sst", [128, 128], F32); b_S = Buf("S")
                e1 = alloc("e1", [128, 512], F32); e2 = alloc("e2", [128, 512], F32); e3 = alloc("e3", [128, 512], F32)
                b_e = [Buf("e1"), Buf("e2"), Buf("e3")]
                kdT = alloc("kdT", [128, 512], F32); b_kdT = Buf("kdT")
                AT = [alloc("AT0", [128, 128], BF16), alloc("AT1", [128, 128], BF16)]; b_AT = [Buf("AT0"), Buf("AT1")]
                sg = alloc("sg", [128, 256], F32); b_sg = Buf("sg")
                otm = alloc("otmg", [128, 256], F32); b_otm = Buf("otmg")
                sq = alloc("sq", [128, 256], F32); ssq = alloc("ssq", [128, 4], F32); b_ssq = Buf("ssq")

                load_w(Wg[:], w_in[layer][:, COFF[f"g{p}"]:COFF[f"g{p}"] + 784], b_Wg)
                dma("pool", gu[:], g_up[layer][:, p * 128:(p + 1) * 128], writes=[b_gp])
                dma("sp", nbias[:], g_bias[layer, p * 128:(p + 1) * 128].rearrange("(p o) -> p o", o=1), writes=[b_gp], owner=Buf("nb"))
                for hh in range(2):
                    dma("sp", gain[:, hh, :], g_gain[layer, :].partition_broadcast(128), writes=[b_gp], owner=Buf("gn"))
                op("dve", lambda e: e.tensor_scalar(out=nbias[:], in0=nbias[:], scalar1=-1.0, scalar2=None, op0=ALU.mult), [b_gp], [b_gp])
                chk('g1')
                pp = Rot(PS[0:4])
                for tc in range(4):
                    pt, pb = pp.get()
                    for c in range(8):
                        op("pe", lambda e, c=c: e.matmul(pt[0:16, 0:512], lhsT=Wg[:, c, 768:784], rhs=HT[:, c, tc * 512:(tc + 1) * 512],
                                                         start=(c == 0), stop=(c == 7)), [b_Wg] + HTb[tc * 4:tc * 4 + 4], [pb])
                    op("act", lambda e: e.activation(out=rankT[:, tc * 512:(tc + 1) * 512], in_=pt[0:16, 0:512], func=AF.Copy), [pb], [b_rank])
                for tc in range(4):
                    pt, pb = pp.get()
                    sl = slice(tc * 512, (tc + 1) * 512)
                    op("pe", lambda e: e.matmul(pt[:, 0:512], lhsT=gu[0:16, :], rhs=rankT[0:16, sl], start=True, stop=True), [b_gp, b_rank], [pb])
                    op("act", lambda e: e.activation(out=la[:, sl], in_=pt[:, 0:512], func=AF.Exp, bias=nbias[:, 0:1], scale=-1.0), [pb, b_gp], [b_la])
                op("act", lambda e: e.activation(out=la[:], in_=la[:], func=AF.Ln, bias=1.0, scale=1.0), [b_la], [b_la])
                chk('g2')
                for n in range(NT):
                    sl = slice(n * 128, (n + 1) * 128)
                    op("dve", lambda e: e.tensor_tensor_scan(out=bc[:, sl], data0=la[:, sl], data1=cst[:, 640:768], initial=0.0,
                                                             op0=ALU.add, op1=ALU.add), [b_la, b_cst], [b_bc])
                chk('g3')
                cx_chk = None
                op("dve", lambda e: e.tensor_scalar(out=nbl[:], in0=bc[:, :].rearrange("p (n c) -> p n c", c=128)[:, :, 127],
                                                    scalar1=-1.0 / 16.0, scalar2=None, op0=ALU.mult), [b_bc], [b_nbl])
                op("act", lambda e: e.activation(out=dec[:], in_=nbl[:], func=AF.Exp), [b_nbl], [b_nbl])
                chk('g3b')
                ptr = Rot(PS[4:6])
                for tc in range(4):
                    sl = slice(tc * 512, (tc + 1) * 512)
                    pq, pqb = pp.get()
                    pk, pkb = pp.get()
                    proj_fm(Wg, 0, 128, tc, pq, pqb, b_Wg)
                    proj_fm(Wg, 128, 128, tc, pk, pkb, b_Wg)
                    op("act", lambda e: e.activation(out=e1[:], in_=bc[:, sl], func=AF.Exp, scale=-1.0 / 16.0), [b_bc], [b_e[0]])
                    op("dve", lambda e: e.scalar_tensor_tensor(out=qdT[:, sl], in0=pq[:, :], scalar=0.125, in1=e1[:], op0=ALU.mult, op1=ALU.mult),
                       [pqb, b_e[0]], [b_qk])
                    op("act", lambda e: e.activation(out=e2[:], in_=bc[:, sl], func=AF.Exp, scale=1.0 / 16.0), [b_bc], [b_e[1]])
                    op("dve", lambda e: e.tensor_tensor(out=kiT[:, sl], in0=pk[:, :], in1=e2[:], op=ALU.mult), [pkb, b_e[1]], [b_qk])
                    for n4 in range(4):
                        n = tc * 4 + n4
                        op("act", lambda e, n=n, n4=n4: e.activation(out=e3[:, n4 * 128:(n4 + 1) * 128], in_=bc[:, n * 128:(n + 1) * 128], func=AF.Exp,
                                                                     bias=nbl[:, n:n + 1], scale=1.0 / 16.0), [b_bc, b_nbl], [b_e[2]])
                    op("dve", lambda e: e.tensor_tensor(out=kdT[:], in0=pk[:, :], in1=e3[:], op=ALU.mult), [pkb, b_e[2]], [b_kdT])
                    pt, pb = ptr.get()
                    for n4 in range(4):
                        op("pe", lambda e, n4=n4: e.transpose(out=pt[:, n4 * 128:(n4 + 1) * 128], in_=kdT[:, n4 * 128:(n4 + 1) * 128], identity=identF),
                           [b_kdT, b_cst], [pb])
                    op("act", lambda e: e.activation(out=kd[:, tc * 4:(tc + 1) * 4, :], in_=pt[:, :].rearrange("p (a b) -> p a b", a=4), func=AF.Copy),
                       [pb], [b_kd])
                chk('g4')
                for t in range(NT):
                    pt, pb = pp.get()
                    proj_tm(Wg, 256, 256, t, pt, pb, b_Wg)
                    op("act", lambda e: e.activation(out=vtm[:, t, :], in_=pt[:, 0:256], func=AF.Copy), [pb], [b_v])
                chk('g5')
                op("dve", lambda e: e.memset(Sst[:], 0.0), [], [b_S])
                op("dve", lambda e: e.memset(Sall[:, 0, :], 0.0), [], [b_Sall])
                for n in range(NT - 1):
                    pt, pb = pp.get()
                    for hh in range(2):
                        op("pe", lambda e, hh=hh: e.matmul(pt[hh * 64:(hh + 1) * 64, 0:128], lhsT=kd[:, n, hh * 64:(hh + 1) * 64],
                                                           rhs=vtm[:, n, hh * 128:(hh + 1) * 128], start=True, stop=True), [b_kd, b_v], [pb])
                    op("dve", lambda e: e.scalar_tensor_tensor(out=Sst[:], in0=Sst[:], scalar=dec[:, n:n + 1], in1=pt[:, 0:128],
                                                               op0=ALU.mult, op1=ALU.add), [b_S, b_nbl, pb], [b_S])
                    op("act", lambda e: e.activation(out=Sall[:, n + 1, :], in_=Sst[:], func=AF.Copy), [b_S], [b_Sall])
                chk('g6')
                ps_o = Rot(PS[4:6]); ps_g = Rot(PS[6:8])
                o_all = alloc("o_all", [128, NT, 256], F32); b_oall = [Buf(f"oall{n}") for n in range(NT)]
                sg_all = alloc("sg_all", [128, NT, 256], F32); b_sgall = [Buf(f"sgall{n}") for n in range(NT)]
                ssq_all = alloc("ssq_all", [128, NT, 2], F32); rstd_all = alloc("rstd_all", [128, NT, 2], F32); b_ssqa = Buf("ssqa")
                otms = [(alloc(f"otmg{i}", [128, 256], F32), Buf(f"otmg{i}")) for i in range(2)]
                for n in range(NT):
                    sl = slice(n * 128, (n + 1) * 128)
                    po, pob = ps_o.get()
                    for hh in range(2):
                        bp = hh * 64
                        pa, pab = pp.get()
                        op("pe", lambda e: e.matmul(pa[:, 0:128], lhsT=kiT[bp:bp + 64, sl], rhs=qdT[bp:bp + 64, sl], start=True, stop=True), [b_qk], [pab])
                        op("dve", lambda e: e.tensor_tensor(out=AT[hh][:], in0=pa[:, 0:128], in1=tri01, op=ALU.mult), [pab, b_cst], [b_AT[hh]])
                        op("pe", lambda e: e.matmul(po[:, hh * 128:(hh + 1) * 128], lhsT=AT[hh][:], rhs=vtm[:, n, hh * 128:(hh + 1) * 128],
                                                    start=True, stop=False, skip_group_check=True), [b_AT[hh], b_v], [pob])
                        op("pe", lambda e: e.matmul(po[:, hh * 128:(hh + 1) * 128], lhsT=qdT[bp:bp + 64, sl], rhs=Sall[bp:bp + 64, n, :],
                                                    start=False, stop=True, skip_group_check=True), [b_qk, b_Sall], [pob])
                    for hh in range(2):
                        op("act", lambda e, hh=hh: e.activation(out=sq[:, hh * 128:(hh + 1) * 128], in_=po[:, hh * 128:(hh + 1) * 128], func=AF.Square,
                                                                accum_out=ssq_all[:, n, hh:hh + 1]), [pob], [b_ssqa])
                    op("act", lambda e: e.activation(out=o_all[:, n, :], in_=po[:, 0:256], func=AF.Copy), [pob], [b_oall[n]])
                op("act", lambda e: e.activation(out=rstd_all[:, :, :], in_=ssq_all[:, :, :], func=AF.Sqrt, bias=epsln[:, 1:2], scale=1.0 / 128.0), [b_ssqa, b_cst], [b_ssqa])
                op("dve", lambda e: e.reciprocal(out=rstd_all[:, :, :], in_=rstd_all[:, :, :]), [b_ssqa], [b_ssqa])
                for n in range(NT):
                    pg, pgb = ps_g.get()
                    proj_tm(Wg, 512, 256, n, pg, pgb, b_Wg)
                    op("act", lambda e: e.activation(out=sg_all[:, n, :], in_=pg[:, 0:256], func=AF.Silu), [pgb], [b_sgall[n]])
                    op("pool", lambda e: e.tensor_tensor(out=sg_all[:, n, :], in0=sg_all[:, n, :], in1=gain[:, :, :].rearrange("p a b -> p (a b)"), op=ALU.mult),
                       [b_sgall[n], b_gp], [b_sgall[n]])
                for n in range(NT):
                    sl = slice(n * 128, (n + 1) * 128)
                    otm, b_otm = otms[n % 2]
                    for hh in range(2):
                        op("dve", lambda e, hh=hh: e.scalar_tensor_tensor(out=otm[:, hh * 128:(hh + 1) * 128], in0=o_all[:, n, hh * 128:(hh + 1) * 128],
                                                                          scalar=rstd_all[:, n, hh:hh + 1], in1=sg_all[:, n, hh * 128:(hh + 1) * 128],
                                                                          op0=ALU.mult, op1=ALU.mult), [b_oall[n], b_ssqa, b_sgall[n]], [b_otm])
                    pt, pb = ps_o.get()
                    for hh in range(2):
                        op("pe", lambda e, hh=hh: e.transpose(out=pt[:, hh * 128:(hh + 1) * 128], in_=otm[:, hh * 128:(hh + 1) * 128], identity=identF),
                           [b_otm, b_cst], [pb])
                    op("act", lambda e: e.activation(out=mixT[:, 2 * p:2 * p + 2, sl], in_=pt[:, 0:256].rearrange("p (a b) -> p a b", a=2), func=AF.Copy),
                       [pb], [mixb[n]])
                cx.release(m0)
                chk('p0')

            if stop == 'gla':
                cx.barrier()
                return nc, cx
            def attn_common():
                qT = alloc("qT", [128, 2, L], BF16, 1); kT = alloc("kT", [128, 2, L], BF16, 1)
                vaug = alloc("vaug", [128, NT, 4, 65], BF16, 1)
                return qT, kT, vaug, Buf("aqk"), Buf("av")

            m0 = cx.mark()
            qT, kT, vaug, b_qk, b_v = attn_common()
            kmT = alloc("kmT", [128, 2, 8], F32); kmB = alloc("kmB", [128, 2, 8], BF16); b_km = Buf("km")
            mbT = alloc("mbT", [8, 4, L], BF16); b_mb = Buf("mbT")
            m1 = cx.mark()
            cosT = alloc("cosT", [128, L], F32); sinT = alloc("sinT", [128, L], F32); b_tab = Buf("tab")
            rope_tables(seq, cosT, sinT, b_tab)
            chk('m1')
            Wa = alloc("Wa", [128, 8, 512], BF16); b_Wa = Buf("Wa")
            Wb = alloc("Wb", [128, 8, 512], BF16); b_Wb = Buf("Wb")
            t1 = alloc("t1", [128, 512], F32); t2 = alloc("t2", [128, 512], F32); b_tmp = Buf("rt")
            pp = Rot(PS[0:6])
            load_w(Wa[:], w_in[layer][:, COFF["mq"]:COFF["mq"] + 512], b_Wa)
            load_w(Wb[:], w_in[layer][:, COFF["mk"]:COFF["mk"] + 512], b_Wb)
            roped_proj(Wa, b_Wa, 0, 256, qT, b_qk, cosT, sinT, b_tab, (t1, t2, b_tmp), pp)
            roped_proj(Wb, b_Wb, 0, 256, kT, b_qk, cosT, sinT, b_tab, (t1, t2, b_tmp), pp, ksum=(kmT, b_km))
            chk('m2a')
            load_w(Wa[:, :, 0:256], w_in[layer][:, COFF["mv"]:COFF["mv"] + 256], b_Wa)
            v_aug_proj(Wa, b_Wa, 0, vaug, b_v, pp)
            op("dve", lambda e: e.tensor_copy(out=kmB[:], in_=kmT[:]), [b_km], [b_km])
            cx.release(m1)
            chk('m2')
            Z = alloc("Z", [128, 4, 8], F32); gm = alloc("gm", [128, 4, 8], F32); top8 = alloc("top8", [128, 4, 8], F32); b_Z = Buf("Z")
            otm = alloc("otm", [128, 4, 256], F32); b_otm = Buf("otm")
            pTl = [(alloc(f"pT{i}", [128, 512], BF16), Buf(f"pT{i}")) for i in range(3)]
            rs = alloc("rs", [128, 4], F32); b_rs = Buf("rs")
            op("pool", lambda e: e.memset(mbT[:], 0.0), [], [b_mb])
            pg = Rot(PS[0:2])
            for t in range(8, NT):
                pt, pb = pg.get()
                for h in range(4):
                    ptile, bp = h // 2, (h % 2) * 64
                    op("pe", lambda e, h=h: e.matmul(pt[:, h * 8:(h + 1) * 8], lhsT=qT[bp:bp + 64, ptile, t * 128:(t + 1) * 128],
                                                     rhs=kmB[bp:bp + 64, ptile, :], start=True, stop=False, skip_group_check=True), [b_qk, b_km], [pb])
                    op("pe", lambda e, h=h: e.matmul(pt[:, h * 8:(h + 1) * 8], lhsT=onehotK[0:8, t * 128:(t + 1) * 128],
                                                     rhs=bpen, start=False, stop=True, skip_group_check=True), [b_cst], [pb])
                op("dve", lambda e: e.tensor_copy(out=gm[:], in_=pt[:, 0:32].rearrange("p (a b) -> p a b", a=4)), [pb], [b_Z])
                for h in range(4):
                    op("dve", lambda e, h=h: e.max(out=top8[:, h, :], in_=gm[:, h, :]), [b_Z], [b_Z])
                for h in range(4):
                    op("dve", lambda e, h=h: e.tensor_scalar(out=Z[:, h, :], in0=gm[:, h, :], scalar1=top8[:, h, 3:4], scalar2=NEG,
                                                             op0=ALU.is_lt, op1=ALU.mult), [b_Z], [b_Z])
                pt2, pb2 = pg.get()
                for h in range(4):
                    op("pe", lambda e, h=h: e.transpose(out=pt2[0:8, h * 128:(h + 1) * 128], in_=Z[:, h, :], identity=identF), [b_Z, b_cst], [pb2])
                op("dve", lambda e: e.tensor_copy(out=mbT[0:8, :, t * 128:(t + 1) * 128], in_=pt2[0:8, :].rearrange("p (a b) -> p a b", a=4)),
                   [pb2], [b_mb])
            chk('m3')

            def moba_mask(h, qc, j, t0, st, sb, wq):
                q0 = t0 * 128
                diag = (j >= 4 * qc)
                op("pe", lambda e: e.matmul(st[:, 0:wq], lhsT=onehotK[0:8, j * 128:(j + 1) * 128], rhs=mbT[0:8, h, q0:q0 + wq],
                                            start=False, stop=(not diag), skip_group_check=True), [b_cst, b_mb], [sb])
                if diag:
                    op("pe", lambda e: e.matmul(st[:, 0:128], lhsT=trinegB[:], rhs=identB[:], start=False, stop=True, skip_group_check=True), [b_cst], [sb])

            attention(qT, kT, b_qk, vaug, b_v, moba_mask, 4, (Rot(PS[2:5]), Rot(PS[5:7]), Rot(PS[7:8])), otm, b_otm, Rot(pTl), rs, b_rs, mixT, mixb)
            cx.release(m0)

            if stop == 'moba':
                cx.barrier()
                return nc, cx
            m0 = cx.mark()
            qT, kT, vaug, b_qk, b_v = attn_common()
            kiT = alloc("kiT", [128, 1, L], BF16, 1); b_ki = Buf("ki")
            wT = alloc("wT", [8, L], BF16, 1); b_wT = Buf("wT")
            qiT = alloc("qiT", [128, 4, L], BF16, 1); b_qi = Buf("qi")
            m1 = cx.mark()
            cosT = alloc("cosT", [128, L], F32); sinT = alloc("sinT", [128, L], F32); b_tab = Buf("tab")
            rope_tables(seq, cosT, sinT, b_tab)
            Wa = alloc("Wa", [128, 8, 512], BF16); b_Wa = Buf("Wa")
            Wb = alloc("Wb", [128, 8, 512], BF16); b_Wb = Buf("Wb")
            t1 = alloc("t1", [128, 512], F32); t2 = alloc("t2", [128, 512], F32); b_tmp = Buf("rt")
            pp = Rot(PS[0:6])
            tm3 = (t1, t2, b_tmp)
            load_w(Wa[:], w_in[layer][:, COFF["sq"]:COFF["sq"] + 512], b_Wa)
            load_w(Wb[:], w_in[layer][:, COFF["sk"]:COFF["sk"] + 512], b_Wb)
            roped_proj(Wa, b_Wa, 0, 256, qT, b_qk, cosT, sinT, b_tab, tm3, pp)
            roped_proj(Wb, b_Wb, 0, 256, kT, b_qk, cosT, sinT, b_tab, tm3, pp)
            load_w(Wa[:], w_in[layer][:, COFF["iq"]:COFF["iq"] + 512], b_Wa)
            load_w(Wb[:], w_in[layer][:, COFF["iqr"]:COFF["iqr"] + 512], b_Wb)
            for ti in range(4):
                for tc in range(4):
                    py, pyb = pp.get(); pr, prb = pp.get()
                    proj_fm(Wa, ti * 128, 128, tc, py, pyb, b_Wa)
                    proj_fm(Wb, ti * 128, 128, tc, pr, prb, b_Wb)
                    sl = slice(tc * 512, (tc + 1) * 512)
                    op("dve", lambda e: e.tensor_tensor(out=t1[:], in0=pr[:, :], in1=sinT[:, sl], op=ALU.mult), [prb, b_tab], [b_tmp])
                    op("dve", lambda e: e.tensor_tensor(out=t2[:], in0=py[:, :], in1=cosT[:, sl], op=ALU.mult), [pyb, b_tab], [b_tmp])
                    op("pool", lambda e: e.tensor_tensor(out=qiT[:, ti, sl], in0=t1[:], in1=t2[:], op=ALU.add), [b_tmp], [b_qi])
            load_w(Wa[:, :, 0:264], w_in[layer][:, COFF["ik"]:COFF["ik"] + 264], b_Wa)
            load_w(Wb[:, :, 0:256], w_in[layer][:, COFF["sv"]:COFF["sv"] + 256], b_Wb)
            roped_proj(Wa, b_Wa, 0, 128, kiT, b_ki, cosT, sinT, b_tab, tm3, pp)
            for tc in range(4):
                pt, pb = pp.get()
                for c in range(8):
                    op("pe", lambda e, c=c: e.matmul(pt[0:8, 0:512], lhsT=Wa[:, c, 256:264], rhs=HT[:, c, tc * 512:(tc + 1) * 512],
                                                     start=(c == 0), stop=(c == 7)), [b_Wa] + HTb[tc * 4:tc * 4 + 4], [pb])
                op("act", lambda e: e.activation(out=wT[:, tc * 512:(tc + 1) * 512], in_=pt[0:8, 0:512], func=AF.Copy), [pb], [b_wT])
            v_aug_proj(Wb, b_Wb, 0, vaug, b_v, pp)
            cx.release(m1)
            mbs = [HT[:, 0:4, :], alloc("mbB", [128, 4, L], BF16)]
            b_mb2 = [[Buf(f"mb{a}_{i}") for i in range(4)] for a in range(2)]
            isc = [HT[:, 4:6, :].bitcast(F32).rearrange("p a b -> p (a b)"), HT[:, 6:8, :].bitcast(F32).rearrange("p a b -> p (a b)"),
                   alloc("isc2", [128, L], F32), alloc("isc3", [128, L], F32)]
            b_isc = [Buf(f"isc{i}") for i in range(4)]
            qs = alloc("qs", [128, 4, 2, 512], BF16); b_qs = Buf("qs")
            otm = alloc("otm", [128, 4, 256], F32); b_otm = Buf("otm")
            pTl = [(alloc(f"pT{i}", [128, 512], BF16), Buf(f"pT{i}")) for i in range(3)]
            rs = alloc("rs", [128, 4], F32); b_rs = Buf("rs")
            bsA = alloc("bsA", [128, 32], F32); bsi = alloc("bsi", [128, 4], I32)
            lo = bsA[:, 0:4]; w0 = bsA[:, 4:8]; mid = bsA[:, 8:12]; nmid = bsA[:, 12:16]; cntv = bsA[:, 16:20]; thc = bsA[:, 20:24]; mx = bsA[:, 24:28]
            b_lo = Buf("lo"); b_mid = Buf("mid"); b_cnt = [Buf(f"cnt{i}") for i in range(4)]; b_msk = Buf("msk")
            junkD = alloc("junkD", [128, L], BF16); b_junkD = Buf("junkD")
            junkA = alloc("junkA", [128, L], BF16); b_junkA = Buf("junkA")
            ps_i = Rot(PS[0:3])
            rls = Rot([(alloc(f"rl{i}", [128, 512], BF16), Buf(f"rl{i}")) for i in range(4)])

            def dsa_qc(qc):
                mb = mbs[qc % 2]; b_mbq = b_mb2[qc % 2]
                qsl = slice(qc * 512, (qc + 1) * 512)
                for pr_ in range(4):
                    pt, pb = ps_i.get()
                    op("pe", lambda e: e.matmul(pt[:, 0:512], lhsT=selw[0:8, pr_ * 128:(pr_ + 1) * 128], rhs=wT[0:8, qsl], start=True, stop=True),
                       [b_cst, b_wT], [pb])
                    op("dve", lambda e: e.scalar_tensor_tensor(out=qs[:, pr_, 0, :], in0=pt[:, 0:512], scalar=0.0, in1=qiT[:, pr_, qsl],
                                                               op0=ALU.max, op1=ALU.mult), [pb, b_qi], [b_qs])
                    op("dve", lambda e: e.scalar_tensor_tensor(out=qs[:, pr_, 1, :], in0=pt[:, 0:512], scalar=0.0, in1=qiT[:, pr_, qsl],
                                                               op0=ALU.min, op1=ALU.mult), [pb, b_qi], [b_qs])
                act_tiles = []
                for i in range(4):
                    t = 4 * qc + i
                    S = (t + 1) * 128
                    if t < 2:
                        if t > 0:
                            op("pool", lambda e: e.memset(mb[:, i, 0:t * 128], 0.0), [], [b_mbq[i]])
                        op("pool", lambda e: e.tensor_copy(out=mb[:, i, t * 128:S], in_=trinegB[:]), [b_cst], [b_mbq[i]])
                        continue
                    act_tiles.append(i)
                    ic, icb = isc[i], b_isc[i]
                    for kc in range((S + 511) // 512):
                        w = min(512, S - kc * 512)
                        ksl = slice(kc * 512, kc * 512 + w)
                        first = True
                        nsum = 0
                        p2, p2b = PS[3]
                        pend = []

                        def flush_one():
                            nonlocal nsum
                            (rl_, rlb_, sg_) = pend.pop(0)
                            op("pe", lambda e: e.matmul(p2[:, 0:w], lhsT=(identB[:] if sg_ == 0 else negidentB[:]), rhs=rl_[:, 0:w],
                                                        start=(nsum == 0), stop=(nsum == 7)), [rlb_, b_cst], [p2b])
                            nsum += 1

                        for k in range(16):
                            hd, sgn = k // 2, k % 2
                            pr_, bp = hd // 2, (hd % 2) * 64
                            pt, pb = ps_i.get()
                            op("pe", lambda e: e.matmul(pt[:, 0:w], lhsT=qs[bp:bp + 64, pr_, sgn, i * 128:(i + 1) * 128], rhs=kiT[bp:bp + 64, 0, ksl],
                                                        start=True, stop=True), [b_qs, b_ki], [pb])
                            o = ALU.max if sgn == 0 else ALU.min
                            if hd % 2 == 1:
                                rl, rlb = rls.get()
                                op("act", lambda e: e.activation(out=rl[:, 0:w], in_=pt[:, 0:w], func=AF.Relu, scale=(1.0 if sgn == 0 else -1.0)), [pb], [rlb])
                                pend.append((rl, rlb, sgn))
                                if len(pend) > 2:
                                    flush_one()
                            elif first:
                                op("dve", lambda e: e.tensor_scalar(out=ic[:, ksl], in0=pt[:, 0:w], scalar1=0.0, scalar2=None, op0=o), [pb], [icb])
                                first = False
                            else:
                                op("dve", lambda e: e.scalar_tensor_tensor(out=ic[:, ksl], in0=pt[:, 0:w], scalar=0.0, in1=ic[:, ksl], op0=o, op1=ALU.add),
                                   [pb, icb], [icb])
                        while pend:
                            flush_one()
                        op("dve", lambda e: e.tensor_tensor(out=ic[:, ksl], in0=p2[:, 0:w], in1=ic[:, ksl], op=ALU.add), [p2b, icb], [icb])
                    op("dve", lambda e: e.tensor_reduce(out=lo[:, i:i + 1], in_=ic[:, 0:S], axis=AX.X, op=ALU.min), [icb], [b_lo])
                    op("dve", lambda e: e.tensor_reduce(out=mx[:, i:i + 1], in_=ic[:, 0:S], axis=AX.X, op=ALU.max), [icb], [b_lo])
                    op("dve", lambda e: e.tensor_tensor(out=ic[:, t * 128:S], in0=ic[:, t * 128:S], in1=trinegbig, op=ALU.add), [icb, b_cst], [icb])
                    on_act = (i % 2 == 1)
                    op("dve", lambda e: e.memset(thc[:, i:i + 1], (S - 511.0) if on_act else (S - 255.5)), [], [b_lo])
                a0, a1 = act_tiles[0], act_tiles[-1] + 1
                asl = slice(a0, a1)
                op("dve", lambda e: e.scalar_tensor_tensor(out=w0[:, asl], in0=mx[:, asl], scalar=1.0, in1=lo[:, asl], op0=ALU.add, op1=ALU.subtract),
                   [b_lo], [b_lo])
                for it in range(N_BISECT):
                    ck = 0.5 ** (it + 1)
                    op("dve", lambda e, ck=ck: e.scalar_tensor_tensor(out=mid[:, asl], in0=w0[:, asl], scalar=ck, in1=lo[:, asl], op0=ALU.mult, op1=ALU.add),
                       [b_lo], [b_mid])
                    for i in act_tiles:
                        S = (4 * qc + i + 1) * 128
                        ic, icb = isc[i], b_isc[i]
                        if i % 2 == 1:
                            op("act", lambda e, i=i, S=S, ic=ic: e.activation(out=junkA[:, 0:S], in_=ic[:, 0:S], func=AF.Sign, bias=mid[:, i:i + 1], scale=-1.0,
                                                                              accum_out=cntv[:, i:i + 1]), [icb, b_mid], [b_cnt[i], b_junkA])
                        else:
                            op("dve", lambda e, i=i, S=S, ic=ic: e.tensor_scalar(out=junkD[:, 0:S], in0=ic[:, 0:S], scalar1=mid[:, i:i + 1], scalar2=None,
                                                                                 op0=ALU.is_lt, op1=ALU.add, accum_out=cntv[:, i:i + 1]), [icb, b_mid], [b_cnt[i], b_junkD])
                    op("dve", lambda e: e.tensor_tensor(out=bsi[:, asl], in0=cntv[:, asl], in1=thc[:, asl], op=ALU.is_le), [b_cnt[i] for i in act_tiles] + [b_lo], [b_msk])
                    op("dve", lambda e: e.copy_predicated(out=lo[:, asl], mask=bsi[:, asl], data=mid[:, asl]), [b_msk, b_mid], [b_lo])
                for i in act_tiles:
                    S = (4 * qc + i + 1) * 128
                    ic, icb = isc[i], b_isc[i]
                    op("dve", lambda e, i=i, S=S, ic=ic: e.tensor_scalar(out=mb[:, i, 0:S], in0=ic[:, 0:S], scalar1=lo[:, i:i + 1], scalar2=NEG, op0=ALU.is_lt, op1=ALU.mult),
                       [icb, b_lo], [b_mbq[i]])

            def dsa_mask(h, qc, j, t0, st, sb, wq):
                last = 4 * qc + 3
                mb = mbs[qc % 2]; b_mbq = b_mb2[qc % 2]
                for t in range(t0, last + 1):
                    i = t - 4 * qc
                    op("pe", lambda e, t=t, i=i: e.matmul(st[:, (t - t0) * 128:(t - t0 + 1) * 128], lhsT=mb[:, i, j * 128:(j + 1) * 128], rhs=identB[:],
                                                          start=False, stop=(t == last), skip_group_check=True), [b_mbq[i], b_cst], [sb])

            attention(qT, kT, b_qk, vaug, b_v, dsa_mask, 6, (Rot(PS[4:6]), Rot(PS[6:8]), Rot(PS[3:4])), otm, b_otm, Rot(pTl), rs, b_rs, mixT, mixb,
                      qc_hook=dsa_qc)
            cx.release(m0)

            if stop == 'dsa':
                cx.barrier()
                return nc, cx
            if debug and seq == 0 and layer == layers[0]:
                dma("sp", dbg[:, :, :], mixT[:, :, :], reads=mixb, owner=Buf("dbg"))

            cx.cur2 = Hoff + 65536
            m0 = cx.mark()
            Wo = alloc("Wo", [128, 8, D], BF16); b_Wo = Buf("Wo")
            gbc = alloc("gbc", [128, D], F32); bbc = alloc("bbc", [128, D], F32); b_par = Buf("lnpar")
            hres = [alloc("hres0", [128, D], F32), alloc("hres1", [128, D], F32)]; b_hres = [Buf("hres0"), Buf("hres1")]
            stat = alloc("stat", [128, NT, 20], F32); b_stat = [Buf(f"stat{t}") for t in range(NT)]
            rw = alloc("rw", [128, 8, 16], F32); rwh = alloc("rwh", [128, 8, 16], BF16); rwl = alloc("rwl", [128, 8, 16], BF16); b_rw = Buf("rw")
            rbb = alloc("rbb", [128, 16], F32)
            HTlo = alloc("HTlo", [128, 8, 128], BF16); b_lo = Buf("HTlo")
            comb = alloc("comb", [128, NT, 16], F32); b_comb = Buf("comb")
            rt = alloc("rt", [128, 8, NT * 16], F32); b_rt = Buf("rt")
            lg_all = alloc("lg_all", [128, NT, 16], F32); b_lg = Buf("lg")
            load_w(Wo[:], w_out[layer][:, :], b_Wo)
            dma("sp", gbc[:], ln1g[layer, :].partition_broadcast(128), writes=[b_par])
            dma("sp", bbc[:], ln1b[layer, :].partition_broadcast(128), writes=[b_par], owner=Buf("lnb"))
            dma("sp", rw[:], r_w.rearrange("(c p) e -> p c e", p=128), writes=[b_rw])
            dma("sp", rbb[:], r_b.partition_broadcast(128), writes=[b_rw], owner=Buf("rbb"))
            op("dve", lambda e: e.tensor_copy(out=rwh[:], in_=rw[:]), [b_rw], [b_rw])
            op("dve", lambda e: e.tensor_tensor(out=rw[:], in0=rw[:], in1=rwh[:], op=ALU.subtract), [b_rw], [b_rw])
            op("dve", lambda e: e.tensor_copy(out=rwl[:], in_=rw[:]), [b_rw], [b_rw])
            ps_o = Rot(PS[0:4]); ps_t = Rot(PS[4:7]); ps_r = Rot(PS[7:8])
            def ln1_front(t):
                hr, hrb = hres[t % 2], b_hres[t % 2]
                dma("sp", hr[:], hs[t * 128:(t + 1) * 128, :], reads=[hsb[t]], writes=[hrb])
                for half in range(2):
                    po, pob = ps_o.get()
                    for c in range(8):
                        op("pe", lambda e, c=c: e.matmul(po[:, 0:512], lhsT=mixT[:, c, t * 128:(t + 1) * 128], rhs=Wo[:, c, half * 512:(half + 1) * 512],
                                                         start=(c == 0), stop=(c == 7)), [mixb[t], b_Wo], [pob])
                    op("dve", lambda e: e.scalar_tensor_tensor(out=H[:, t, half * 512:(half + 1) * 512], in0=hr[:, half * 512:(half + 1) * 512], scalar=ALPHA,
                                                               in1=po[:, 0:512], op0=ALU.mult, op1=ALU.add), [hrb, pob], [Hb[t]])
                ln_stats(t, stat, b_stat)

            def ln1_back(t):
                ln_apply(t, gbc, bbc, b_par, stat, b_stat)
                for half in range(2):
                    pt, pb = ps_t.get()
                    for c4 in range(4):
                        c = half * 4 + c4
                        op("pe", lambda e, c=c, c4=c4: e.transpose(out=pt[:, c4 * 128:(c4 + 1) * 128], in_=H[:, t, c * 128:(c + 1) * 128], identity=identF),
                           [Hb[t], b_cst], [pb])
                    pv = pt[:, :].rearrange("p (a b) -> p a b", a=4)
                    op("act", lambda e: e.activation(out=HT[:, half * 4:(half + 1) * 4, t * 128:(t + 1) * 128], in_=pv, func=AF.Copy), [pb], [HTb[t]])
                    op("dve", lambda e: e.tensor_tensor(out=HTlo[:, half * 4:(half + 1) * 4, :], in0=pv, in1=HT[:, half * 4:(half + 1) * 4, t * 128:(t + 1) * 128],
                                                        op=ALU.subtract), [pb, HTb[t]], [b_lo])
                pr_, prb = ps_r.get()
                k = 0
                for (A_, Ab, W_) in ((HT[:, :, t * 128:(t + 1) * 128], HTb[t], rwh), (HTlo[:, :, :], b_lo, rwh), (HT[:, :, t * 128:(t + 1) * 128], HTb[t], rwl)):
                    for c in range(8):
                        op("pe", lambda e, c=c, A_=A_, W_=W_, k=k: e.matmul(pr_[:, 0:16], lhsT=A_[:, c, :], rhs=W_[:, c, :], start=(k == 0), stop=(k == 23)),
                           [Ab, b_rw], [prb])
                        k += 1
                op("act", lambda e: e.activation(out=lg_all[:, t, :], in_=pr_[:, 0:16], func=AF.Copy), [prb], [b_lg])
                op("act", lambda e: e.activation(out=H[:, t, :], in_=H[:, t, :], func=AF.Copy, scale=ALPHA), [Hb[t]], [Hb[t]])

            ln1_front(0)
            for t in range(NT):
                if t + 1 < NT:
                    ln1_front(t + 1)
                ln1_back(t)
            T_ = NT
            sc = rt[:, 0, :]; sel = rt[:, 1, :]; tmpa = rt[:, 2, :]; tmpb = rt[:, 3, :]; ch = rt[:, 4, :]
            m1 = rt[:, 5, 0:T_ * 4]; m2 = rt[:, 5, T_ * 4:T_ * 8]; gs = rt[:, 5, T_ * 8:T_ * 12]; ing = rt[:, 5, T_ * 12:T_ * 16]
            gmax = rt[:, 6, 0:T_]; a1 = rt[:, 6, T_:2 * T_]; a2 = rt[:, 6, 2 * T_:3 * T_]; ssum = rt[:, 6, 3 * T_:4 * T_]
            v3 = lambda a: a.rearrange("p (t e) -> p t e", t=T_)
            v4 = lambda a: a.rearrange("p (t g e) -> p t g e", t=T_, g=4)
            g3 = lambda a: a.rearrange("p (t g) -> p t g", t=T_)
            R = [b_rt]
            op("act", lambda e: e.activation(out=sc, in_=lg_all[:, :, :].rearrange("p t e -> p (t e)"), func=AF.Sigmoid), [b_lg], R)
            op("dve", lambda e: e.tensor_tensor(out=v3(sel), in0=v3(sc), in1=rbb[:, None, :].to_broadcast([128, T_, 16]), op=ALU.add), R + [b_rw], R)
            op("dve", lambda e: e.tensor_reduce(out=g3(m1), in_=v4(sel), axis=AX.X, op=ALU.max), R, R)
            op("dve", lambda e: e.tensor_tensor(out=v4(tmpa), in0=v4(sel), in1=g3(m1)[:, :, :, None].to_broadcast([128, T_, 4, 4]), op=ALU.is_equal), R, R)
            op("dve", lambda e: e.scalar_tensor_tensor(out=tmpb, in0=tmpa, scalar=-1.0e9, in1=sel, op0=ALU.mult, op1=ALU.add), R, R)
            op("dve", lambda e: e.tensor_reduce(out=g3(m2), in_=v4(tmpb), axis=AX.X, op=ALU.max), R, R)
            op("dve", lambda e: e.tensor_tensor(out=gs, in0=m1, in1=m2, op=ALU.add), R, R)
            op("dve", lambda e: e.tensor_reduce(out=gmax, in_=g3(gs), axis=AX.X, op=ALU.max), R, R)
            op("dve", lambda e: e.tensor_tensor(out=g3(ing), in0=g3(gs), in1=gmax[:, :, None].to_broadcast([128, T_, 4]), op=ALU.is_lt), R, R)
            op("dve", lambda e: e.tensor_scalar(out=ing, in0=ing, scalar1=-1.0e9, scalar2=None, op0=ALU.mult), R, R)
            op("dve", lambda e: e.tensor_tensor(out=v4(tmpa), in0=v4(sel), in1=g3(ing)[:, :, :, None].to_broadcast([128, T_, 4, 4]), op=ALU.add), R, R)
            op("dve", lambda e: e.tensor_reduce(out=a1, in_=v3(tmpa), axis=AX.X, op=ALU.max), R, R)
            op("dve", lambda e: e.tensor_tensor(out=v3(ch), in0=v3(tmpa), in1=a1[:, :, None].to_broadcast([128, T_, 16]), op=ALU.is_ge), R, R)
            op("dve", lambda e: e.scalar_tensor_tensor(out=tmpb, in0=ch, scalar=-1.0e9, in1=tmpa, op0=ALU.mult, op1=ALU.add), R, R)
            op("dve", lambda e: e.tensor_reduce(out=a2, in_=v3(tmpb), axis=AX.X, op=ALU.max), R, R)
            op("dve", lambda e: e.tensor_tensor(out=v3(tmpa), in0=v3(tmpb), in1=a2[:, :, None].to_broadcast([128, T_, 16]), op=ALU.is_ge), R, R)
            op("dve", lambda e: e.tensor_tensor(out=ch, in0=ch, in1=tmpa, op=ALU.add), R, R)
            op("dve", lambda e: e.tensor_tensor(out=tmpb, in0=ch, in1=sc, op=ALU.mult), R, R)
            op("dve", lambda e: e.tensor_reduce(out=ssum, in_=v3(tmpb), axis=AX.X, op=ALU.add), R, R)
            op("dve", lambda e: e.reciprocal(out=ssum, in_=ssum), R, R)
            op("dve", lambda e: e.tensor_tensor(out=comb[:, :, :], in0=v3(tmpb), in1=ssum[:, :, None].to_broadcast([128, T_, 16]), op=ALU.mult), R, [b_comb])
            cx.barrier()
            cx.cur, cx.cur2 = base_mark
            cx.cur2 = Hoff + 65536
            comb2 = alloc("comb2", [128, NT, 16], F32); b_comb2 = Buf("comb2")
            op("dve", lambda e: e.tensor_copy(out=comb2[:], in_=comb[:]), [b_comb], [b_comb2])
            cx.barrier()

            if stop == 'ln1':
                cx.barrier()
                return nc, cx
            m0 = cx.mark()
            Wg_ = [alloc(f"weg{i}", [128, 8, 512], BF16) for i in range(2)]; b_weg = [Buf("weg0"), Buf("weg1")]
            Wu_ = [alloc(f"weu{i}", [128, 8, 512], BF16) for i in range(2)]; b_weu = [Buf("weu0"), Buf("weu1")]
            Wd_ = [alloc(f"wed{i}", [128, 4, D], BF16) for i in range(2)]; b_wed = [Buf("wed0"), Buf("wed1")]
            actT = alloc("actT", [128, 4, L], BF16); b_act = [[Buf(f"act{f}_{tc}") for tc in range(4)] for f in range(4)]
            sgb = [(alloc(f"sgm{i}", [128, 512], F32), Buf(f"sgm{i}")) for i in range(3)]
            sgr = Rot(sgb)
            ps_gu = Rot(PS[0:4]); ps_d = Rot(PS[4:8])
            for ex in range(16):
                wg, wu, wd = Wg_[ex % 2], Wu_[ex % 2], Wd_[ex % 2]
                bg, bu, bd = b_weg[ex % 2], b_weu[ex % 2], b_wed[ex % 2]
                load_w(wg[:], w_eg[layer, ex], bg)
                load_w(wu[:], w_eu[layer, ex], bu)
                dma("pool", wd[:], w_ed[layer, ex].rearrange("(c p) f -> p c f", p=128), writes=[bd])
                for tc in range(4):
                    for f in range(4):
                        pg_, pgb = ps_gu.get(); pu_, pub = ps_gu.get()
                        for c in range(8):
                            op("pe", lambda e, c=c: e.matmul(pg_[:, 0:512], lhsT=wg[:, c, f * 128:(f + 1) * 128], rhs=HT[:, c, tc * 512:(tc + 1) * 512],
                                                             start=(c == 0), stop=(c == 7)), [bg] + HTb[tc * 4:tc * 4 + 4], [pgb])
                        for c in range(8):
                            op("pe", lambda e, c=c: e.matmul(pu_[:, 0:512], lhsT=wu[:, c, f * 128:(f + 1) * 128], rhs=HT[:, c, tc * 512:(tc + 1) * 512],
                                                             start=(c == 0), stop=(c == 7)), [bu] + HTb[tc * 4:tc * 4 + 4], [pub])
                        sgt, sgtb = sgr.get()
                        op("act", lambda e: e.activation(out=sgt[:], in_=pg_[:, 0:512], func=AF.Silu), [pgb], [sgtb])
                        op("dve", lambda e: e.tensor_tensor(out=actT[:, f, tc * 512:(tc + 1) * 512], in0=pu_[:, 0:512], in1=sgt[:], op=ALU.mult),
                           [pub, sgtb], [b_act[f][tc]])
                    for t4 in range(4):
                        t = tc * 4 + t4
                        for half in range(2):
                            pd_, pdb = ps_d.get()
                            for f in range(4):
                                op("pe", lambda e, f=f: e.matmul(pd_[:, 0:512], lhsT=actT[:, f, t * 128:(t + 1) * 128], rhs=wd[:, f, half * 512:(half + 1) * 512],
                                                                 start=(f == 0), stop=(f == 3)), [b_act[f][tc], bd], [pdb])
                            op("dve", lambda e: e.scalar_tensor_tensor(out=H[:, t, half * 512:(half + 1) * 512], in0=pd_[:, 0:512], scalar=comb2[:, t, ex:ex + 1],
                                                                       in1=H[:, t, half * 512:(half + 1) * 512], op0=ALU.mult, op1=ALU.add),
                               [pdb, b_comb2, Hb[t]], [Hb[t]])
            cx.release(m0)

            if stop == 'moe':
                cx.barrier()
                return nc, cx
            m0 = cx.mark()
            gbc = alloc("gbc", [128, D], F32); bbc = alloc("bbc", [128, D], F32); b_par = Buf("lnpar")
            stat = alloc("stat", [128, NT, 20], F32); b_stat = [Buf(f"stat{t}") for t in range(NT)]
            dma("sp", gbc[:], ln2g[layer, :].partition_broadcast(128), writes=[b_par])
            dma("sp", bbc[:], ln2b[layer, :].partition_broadcast(128), writes=[b_par], owner=Buf("lnb"))
            pool_tr = Rot(PS[0:4])
            last = (layer == layers[-1])
            ln_stats(0, stat, b_stat)
            for t in range(NT):
                ln_apply(t, gbc, bbc, b_par, stat, b_stat, between=((lambda t=t: ln_stats(t + 1, stat, b_stat)) if t + 1 < NT else None))
                if last:
                    dma("sp", out[seq, t * 128:(t + 1) * 128, :], H[:, t, :], reads=[Hb[t]], owner=Hb[t])
                else:
                    make_HT(t, pool_tr)
            cx.release(m0)
            cx.cur, cx.cur2 = base_mark

    cx.barrier()
    return nc, cx


_CACHE = {}


def _prep_inputs(inputs):
    cst, cst8 = _consts()
    w_in_x = np.ascontiguousarray(np.asarray(inputs["w_in"], np.float32)[:, :, COLMAP])
    shared = {"w_in_x": w_in_x, "cst": cst, "cst8": cst8}
    for k in ("gla_gate_up", "gla_gate_bias", "gla_norm_gain", "w_out", "ln_mix_g", "ln_mix_b", "router_w", "router_bias",
              "w_expert_gate", "w_expert_up", "w_expert_down", "ln_ffn_g", "ln_ffn_b"):
        shared[k] = np.ascontiguousarray(np.asarray(inputs[k], np.float32))
    return shared


def kernel(**inputs):
    x = np.ascontiguousarray(np.asarray(inputs["x"], np.float32))
    positions = np.ascontiguousarray(np.asarray(inputs["positions"], np.int32))
    shared = _prep_inputs(inputs)
    ncores, nseq = 8, 2
    nc = bass.Bass("TRN2", target_bir_lowering=False)
    build(nc, nseq, [0, 1])
    in_maps = []
    for c in range(ncores):
        m = dict(shared)
        m["x"] = x[c * nseq:(c + 1) * nseq]
        m["positions"] = positions[c * nseq:(c + 1) * nseq]
        in_maps.append(m)
    res = run_bass_kernel_spmd(nc, in_maps, core_ids=list(range(ncores)))
    return np.concatenate([r["out"] for r in res.results], axis=0)
```

```python
import numpy as np
import concourse.bass as bass
import concourse.mybir as mybir
from concourse.bass_utils import run_bass_kernel_spmd

F32 = mybir.dt.float32
BF16 = mybir.dt.bfloat16
I32 = mybir.dt.int32
ALU = mybir.AluOpType
AF = mybir.ActivationFunctionType
AX = mybir.AxisListType

L = 2048
D = 1024
NT = 16
NEG = -30000.0
BIG = 1.0e30
ALPHA = 4.0 ** 0.25
N_BISECT = 18
SBUF_BASE = 16512
SBUF_BYTES = 229000

IN_WIDTHS = (256, 256, 512, 16, 512, 256, 256, 256, 256, 256, 256, 512, 64, 8)
IN_OFF = np.concatenate([[0], np.cumsum(IN_WIDTHS)]).astype(int)
(O_GQ, O_GK, O_GV, O_GR, O_GG, O_MQ, O_MK, O_MV, O_SQ, O_SK, O_SV, O_IQ, O_IK, O_IW) = [int(v) for v in IN_OFF[:-1]]


def _rot_cols(base, nheads):
    cols = []
    for h in range(nheads):
        for d in range(64):
            if d < 8:
                pd = d + 8
            elif d < 16:
                pd = d - 8
            else:
                pd = d
            cols.append(base + h * 64 + pd)
    return cols


def _build_colmap():
    cols = []
    off = {}

    def add(name, c):
        off[name] = len(cols)
        cols.extend(c)

    for p in range(2):
        add(f"g{p}", list(range(O_GQ + p * 128, O_GQ + p * 128 + 128)) + list(range(O_GK + p * 128, O_GK + p * 128 + 128))
            + list(range(O_GV + p * 256, O_GV + p * 256 + 256)) + list(range(O_GG + p * 256, O_GG + p * 256 + 256))
            + list(range(O_GR, O_GR + 16)))
    add("mq", list(range(O_MQ, O_MQ + 256)))
    add("mqr", _rot_cols(O_MQ, 4))
    add("mk", list(range(O_MK, O_MK + 256)))
    add("mkr", _rot_cols(O_MK, 4))
    add("mv", list(range(O_MV, O_MV + 256)))
    add("sq", list(range(O_SQ, O_SQ + 256)))
    add("sqr", _rot_cols(O_SQ, 4))
    add("sk", list(range(O_SK, O_SK + 256)))
    add("skr", _rot_cols(O_SK, 4))
    add("sv", list(range(O_SV, O_SV + 256)))
    add("iq", list(range(O_IQ, O_IQ + 512)))
    add("iqr", _rot_cols(O_IQ, 8))
    add("ik", list(range(O_IK, O_IK + 64)) * 2)
    add("ikr", _rot_cols(O_IK, 1) * 2)
    add("iw", list(range(O_IW, O_IW + 8)))
    return np.array(cols, dtype=np.int64), off


COLMAP, COFF = _build_colmap()
WX = len(COLMAP)


def _consts():
    c = np.zeros((128, 1024), np.float32)
    c[:, 0:128] = np.eye(128, dtype=np.float32)
    j = np.arange(128)[:, None]
    i = np.arange(128)[None, :]
    c[:, 128:256] = (i >= j).astype(np.float32)
    c[:, 256:384] = np.where(i <= j, 0.0, NEG)
    c[:, 384:512] = np.where(i <= j, 0.0, -BIG)
    p = np.arange(128)
    d = p % 64
    invf = np.where(d < 16, 500000.0 ** (-(d % 8) / 8.0), 0.0)
    c[:, 512] = invf / (2 * np.pi)
    c[:, 513] = np.where(d < 8, -invf, invf) / (2 * np.pi)
    c8 = np.zeros((8, 2048 + 512 + 8), np.float32)
    for k in range(8):
        for n in range(8):
            c8[k, 2560 + n] = 0.0 if n < k else (BIG if n == k else -BIG)
    for n in range(8):
        c8[n, n * 256:(n + 1) * 256] = 1.0
    for k in range(8):
        pr, hh = k // 2, k % 2
        c8[k, 2048 + pr * 128 + hh * 64: 2048 + pr * 128 + hh * 64 + 64] = 1.0
    return c, c8


class Buf:
    __slots__ = ("name", "writer", "readers", "dsem", "dcnt")

    def __init__(self, name):
        self.name = name
        self.writer = None
        self.readers = {}
        self.dsem = None
        self.dcnt = 0


class Ctx:
    def __init__(self, nc):
        self.nc = nc
        self.E = {"pe": nc.tensor, "act": nc.scalar, "dve": nc.vector, "pool": nc.gpsimd, "sp": nc.sync}
        self.sem = {k: nc.alloc_semaphore("e_" + k) for k in self.E}
        self.cnt = {k: 0 for k in self.E}
        self.seen = {k: {} for k in self.E}
        self.dbufs = []
        self.slots = {}
        self.nalloc = 0
        self.cur = SBUF_BASE
        self.cur2 = 0
        self.lim2 = 0
        self.peak = 0

    def alloc(self, name, shape, dtype, reg=0):
        isz = 4 if dtype in (F32, I32) else 2
        n = isz
        for s in shape[1:]:
            n *= s
        n = (n + 63) // 64 * 64
        if reg == 0:
            off = self.cur
            self.cur += n
            self.peak = max(self.peak, self.cur)
            assert self.cur <= SBUF_BYTES, (name, self.cur)
        else:
            off = self.cur2
            self.cur2 += n
            assert self.cur2 <= self.lim2, (name, self.cur2, self.lim2)
        self.nalloc += 1
        return self.nc.alloc_sbuf_tensor_at(f"{name}_{self.nalloc}", list(shape), dtype, offset=off)

    def mark(self):
        return (self.cur, self.cur2)

    def release(self, m):
        self.barrier()
        self.cur, self.cur2 = m

    def _wait(self, eng, dep):
        kind, obj, val = dep
        key = obj if kind == "e" else id(obj)
        if self.seen[eng].get(key, 0) >= val:
            return
        sem = self.sem[obj] if kind == "e" else obj.dsem
        self.E[eng].wait_ge(sem, val)
        self.seen[eng][key] = val

    def _deps(self, eng, reads, writes):
        for b in reads:
            if b.writer is not None:
                self._wait(eng, b.writer)
        for b in writes:
            w = b.writer
            if w is not None and not (w[0] == "e" and w[1] == eng and eng == "pe"):
                self._wait(eng, w)
            for r in b.readers.values():
                if not (r[0] == "e" and r[1] == eng and eng == "pe"):
                    self._wait(eng, r)

    def op(self, eng, fn, reads=(), writes=()):
        self._deps(eng, reads, writes)
        inst = fn(self.E[eng])
        inst.then_inc(self.sem[eng], 1)
        self.cnt[eng] += 1
        tag = ("e", eng, self.cnt[eng])
        for b in reads:
            b.readers[eng] = tag
        for b in writes:
            b.writer = tag
            b.readers = {}

    def dma(self, q, out, in_, reads=(), writes=(), owner=None):
        self._deps(q, reads, writes)
        if owner is None:
            owner = writes[0] if writes else reads[0]
        slot = self.slots.get(owner.name)
        if slot is None:
            slot = Buf("slot_" + owner.name)
            slot.dsem = self.nc.alloc_semaphore("d_%d" % len(self.slots))
            self.slots[owner.name] = slot
            self.dbufs.append(slot)
        self.E[q].dma_start(out=out, in_=in_).then_inc(slot.dsem, 16)
        slot.dcnt += 16
        tag = ("d", slot, slot.dcnt)
        for b in reads:
            b.readers[("d", id(slot))] = tag
        for b in writes:
            b.writer = tag
            b.readers = {}

    def barrier(self):
        for e in self.E:
            for e2 in self.E:
                if e2 != e and self.cnt[e2] > 0:
                    self._wait(e, ("e", e2, self.cnt[e2]))
            for b in self.dbufs:
                if b.dcnt:
                    self._wait(e, ("d", b, b.dcnt))


class Rot:
    def __init__(self, items):
        self.items = items
        self.i = 0

    def get(self):
        it = self.items[self.i % len(self.items)]
        self.i += 1
        return it


class StopBuild(Exception):
    pass


def build(nc, nseq, layers, debug=False, stop=None):
    try:
        return _build(nc, nseq, layers, debug, stop)
    except StopBuild as ex:
        return nc, ex.args[0]


def _build(nc, nseq, layers, debug=False, stop=None):
    cx = Ctx(nc)
    op, dma, alloc = cx.op, cx.dma, cx.alloc

    def chk(name):
        if stop == name:
            cx.barrier()
            raise StopBuild(cx)

    x = nc.dram_tensor("x", [nseq, L, D], F32, kind="ExternalInput").ap()
    pos = nc.dram_tensor("positions", [nseq, L], I32, kind="ExternalInput").ap()
    w_in = nc.dram_tensor("w_in_x", [2, D, WX], F32, kind="ExternalInput").ap()
    g_up = nc.dram_tensor("gla_gate_up", [2, 16, 256], F32, kind="ExternalInput").ap()
    g_bias = nc.dram_tensor("gla_gate_bias", [2, 256], F32, kind="ExternalInput").ap()
    g_gain = nc.dram_tensor("gla_norm_gain", [2, 128], F32, kind="ExternalInput").ap()
    w_out = nc.dram_tensor("w_out", [2, D, D], F32, kind="ExternalInput").ap()
    ln1g = nc.dram_tensor("ln_mix_g", [2, D], F32, kind="ExternalInput").ap()
    ln1b = nc.dram_tensor("ln_mix_b", [2, D], F32, kind="ExternalInput").ap()
    r_w = nc.dram_tensor("router_w", [D, 16], F32, kind="ExternalInput").ap()
    r_b = nc.dram_tensor("router_bias", [16], F32, kind="ExternalInput").ap()
    w_eg = nc.dram_tensor("w_expert_gate", [2, 16, D, 512], F32, kind="ExternalInput").ap()
    w_eu = nc.dram_tensor("w_expert_up", [2, 16, D, 512], F32, kind="ExternalInput").ap()
    w_ed = nc.dram_tensor("w_expert_down", [2, 16, 512, D], F32, kind="ExternalInput").ap()
    ln2g = nc.dram_tensor("ln_ffn_g", [2, D], F32, kind="ExternalInput").ap()
    ln2b = nc.dram_tensor("ln_ffn_b", [2, D], F32, kind="ExternalInput").ap()
    cst_d = nc.dram_tensor("cst", [128, 1024], F32, kind="ExternalInput").ap()
    cst8_d = nc.dram_tensor("cst8", [8, 2568], F32, kind="ExternalInput").ap()
    out = nc.dram_tensor("out", [nseq, L, D], F32, kind="ExternalOutput").ap()
    hs = nc.dram_tensor("hspill", [L, D], F32, kind="Internal").ap()
    dbg = nc.dram_tensor("dbg", [128, 8, L], BF16, kind="ExternalOutput").ap() if debug else None

    cst = alloc("cst", [128, 1024], F32)
    c8b = alloc("c8b", [8, 2568], BF16)
    identB = alloc("identB", [128, 128], BF16)
    trinegB = alloc("trinegB", [128, 128], BF16)
    negidentB = alloc("negidentB", [128, 128], BF16)
    epsln = alloc("epsln", [128, 2], F32)
    zerosB = alloc("zerosB", [1, 512], BF16)
    HT = alloc("HT", [128, 8, L], BF16)
    Hoff = cx.cur
    H = alloc("H", [128, NT, D], F32)
    base_mark = cx.mark()

    b_cst = Buf("cst")
    Hb = [Buf(f"H{t}") for t in range(NT)]
    HTb = [Buf(f"HT{t}") for t in range(NT)]
    hsb = [Buf(f"hs{t}") for t in range(NT)]
    outb = Buf("out")

    identF = cst[:, 0:128]
    tri01 = cst[:, 128:256]
    trinegbig = cst[:, 384:512]
    invf = cst[:, 512:513]
    sinvf = cst[:, 513:514]
    onehotK = c8b[0:8, 0:2048]
    selw = c8b[0:8, 2048:2560]
    bpen = c8b[0:8, 2560:2568]

    PS = []
    for i in range(8):
        t = nc.alloc_psum_tensor(f"ps{i}", [128, 512], F32)
        PS.append((t, Buf(f"ps{i}")))

    dma("sp", cst[:], cst_d[:, :], writes=[b_cst])
    dma("pool", c8b[:], cst8_d[:, :], writes=[b_cst], owner=Buf("c8"))
    op("dve", lambda e: e.tensor_copy(out=identB[:], in_=cst[:, 0:128]), [b_cst], [b_cst])
    op("dve", lambda e: e.tensor_copy(out=trinegB[:], in_=cst[:, 256:384]), [b_cst], [b_cst])
    op("dve", lambda e: e.tensor_scalar(out=negidentB[:], in0=cst[:, 0:128], scalar1=-1.0, scalar2=None, op0=ALU.mult), [b_cst], [b_cst])
    op("dve", lambda e: e.memset(epsln[:, 0:1], 1e-5), [], [b_cst])
    op("dve", lambda e: e.memset(epsln[:, 1:2], 1e-6), [], [b_cst])
    op("dve", lambda e: e.memset(zerosB[:], 0.0), [], [b_cst])
    cx.barrier()

    def load_w(dst, src_rows_cols, buf, q="pool"):
        dma(q, dst, src_rows_cols.rearrange("(c p) f -> p c f", p=128), writes=[buf])

    def make_HT(t, pool):
        for half in range(2):
            pt, pb = pool.get()
            for c4 in range(4):
                c = half * 4 + c4
                op("pe", lambda e, c=c, c4=c4: e.transpose(out=pt[:, c4 * 128:(c4 + 1) * 128],
                                                           in_=H[:, t, c * 128:(c + 1) * 128], identity=identF),
                   [Hb[t], b_cst], [pb])
            op("act", lambda e: e.activation(out=HT[:, half * 4:(half + 1) * 4, t * 128:(t + 1) * 128],
                                             in_=pt[:, :].rearrange("p (a b) -> p a b", a=4), func=AF.Copy),
               [pb], [HTb[t]])

    def ln_stats(t, stat, b_stat):
        st = stat[:, t, :]
        bs_ = [b_stat[t]]
        op("dve", lambda e: e.bn_stats(out=st[:, 0:6], in_=H[:, t, 0:512]), [Hb[t]], bs_)
        op("dve", lambda e: e.bn_stats(out=st[:, 6:12], in_=H[:, t, 512:1024]), [Hb[t]], bs_)
        op("dve", lambda e: e.bn_aggr(out=st[:, 12:14], in_=st[:, 0:12]), bs_, bs_)
        op("act", lambda e: e.activation(out=st[:, 14:15], in_=st[:, 13:14], func=AF.Sqrt, bias=epsln[:, 0:1], scale=1.0),
           bs_ + [b_cst], bs_)
        op("dve", lambda e: e.reciprocal(out=st[:, 15:16], in_=st[:, 14:15]), bs_, bs_)
        op("dve", lambda e: e.scalar_tensor_tensor(out=st[:, 16:17], in0=st[:, 12:13], scalar=-1.0, in1=st[:, 15:16], op0=ALU.mult, op1=ALU.mult), bs_, bs_)

    def ln_apply(t, gbc, bbc, b_par, stat, b_stat, between=None):
        hv = H[:, t, :]
        st = stat[:, t, :]
        op("act", lambda e: e.activation(out=hv, in_=hv, func=AF.Identity, bias=st[:, 16:17], scale=st[:, 15:16]), [Hb[t], b_stat[t]], [Hb[t]])
        if between is not None:
            between()
        op("dve", lambda e: e.tensor_tensor(out=hv, in0=hv, in1=gbc[:], op=ALU.mult), [Hb[t], b_par], [Hb[t]])
        op("dve", lambda e: e.tensor_tensor(out=hv, in0=hv, in1=bbc[:], op=ALU.add), [Hb[t], b_par], [Hb[t]])

    def rope_tables(seq, cosT, sinT, b_tab):
        m = cx.mark()
        posf = alloc("posf", [128, L], F32)
        tmp = alloc("rtmp", [128, L], F32)
        tmi = alloc("rtmi", [128, L], I32)
        b_t = Buf("ropetmp")
        dma("pool", posf[:], pos[seq, :].partition_broadcast(128), writes=[b_t])
        for (tab, fcol, shift) in ((sinT, sinvf, 0.0), (cosT, invf, 0.25)):
            op("dve", lambda e, fcol=fcol, shift=shift: e.tensor_scalar(out=tmp[:], in0=posf[:], scalar1=fcol, scalar2=shift,
                                                                         op0=ALU.mult, op1=ALU.add), [b_t, b_cst], [b_t])
            op("dve", lambda e: e.tensor_copy(out=tmi[:], in_=tmp[:]), [b_t], [b_t])
            op("dve", lambda e: e.tensor_copy(out=tab[:], in_=tmi[:]), [b_t], [b_tab])
            op("dve", lambda e: e.tensor_tensor(out=tmp[:], in0=tmp[:], in1=tab[:], op=ALU.subtract), [b_t, b_tab], [b_t])
            op("dve", lambda e: e.tensor_scalar(out=tab[:], in0=tmp[:], scalar1=0.5, scalar2=None, op0=ALU.is_gt), [b_t], [b_tab])
            op("dve", lambda e: e.tensor_tensor(out=tmp[:], in0=tmp[:], in1=tab[:], op=ALU.subtract), [b_t, b_tab], [b_t])
            op("dve", lambda e: e.tensor_scalar(out=tab[:], in0=tmp[:], scalar1=-0.5, scalar2=None, op0=ALU.is_lt), [b_t], [b_tab])
            op("dve", lambda e: e.tensor_tensor(out=tmp[:], in0=tmp[:], in1=tab[:], op=ALU.add), [b_t, b_tab], [b_t])
            op("act", lambda e: e.activation(out=tab[:], in_=tmp[:], func=AF.Sin, scale=2 * np.pi), [b_t], [b_tab])
        cx.release(m)

    def proj_fm(wt, col0, M, tc, pt, pb, wbuf, pbase=0):
        for c in range(8):
            op("pe", lambda e, c=c: e.matmul(pt[pbase:pbase + M, 0:512], lhsT=wt[:, c, col0:col0 + M],
                                             rhs=HT[:, c, tc * 512:(tc + 1) * 512], start=(c == 0), stop=(c == 7)),
               [wbuf] + HTb[tc * 4:(tc + 1) * 4], [pb])

    def proj_tm(wt, col0, N, t, pt, pb, wbuf, ocol=0):
        for c in range(8):
            op("pe", lambda e, c=c: e.matmul(pt[:, ocol:ocol + N], lhsT=HT[:, c, t * 128:(t + 1) * 128],
                                             rhs=wt[:, c, col0:col0 + N], start=(c == 0), stop=(c == 7)),
               [wbuf, HTb[t]], [pb])

    def roped_proj(wt, wbuf, col, colr, dstT, b_dst, cosT, sinT, b_tab, tmps, pspool, ksum=None):
        ntile = dstT.shape[1]
        (t1, t2, b_tmp) = tmps
        for ti in range(ntile):
            for tc in range(4):
                py, pyb = pspool.get()
                pr, prb = pspool.get()
                proj_fm(wt, col + ti * 128, 128, tc, py, pyb, wbuf)
                proj_fm(wt, colr + ti * 128, 128, tc, pr, prb, wbuf)
                sl = slice(tc * 512, (tc + 1) * 512)
                op("dve", lambda e: e.tensor_tensor(out=t1[:], in0=pr[:, :], in1=sinT[:, sl], op=ALU.mult), [prb, b_tab], [b_tmp])
                op("dve", lambda e: e.tensor_tensor(out=t2[:], in0=py[:, :], in1=cosT[:, sl], op=ALU.mult), [pyb, b_tab], [b_tmp])
                if ksum is not None:
                    op("pool", lambda e: e.tensor_tensor(out=t2[:], in0=t1[:], in1=t2[:], op=ALU.add), [b_tmp], [b_tmp])
                    op("act", lambda e: e.activation(out=dstT[:, ti, sl], in_=t2[:], func=AF.Copy), [b_tmp], [b_dst])
                    (km, b_km) = ksum
                    op("dve", lambda e: e.tensor_reduce(out=km[:, ti, tc * 2:(tc + 1) * 2], in_=t2[:, :].rearrange("p (a b) -> p a b", a=2),
                                                        axis=AX.X, op=ALU.add), [b_tmp], [b_km])
                else:
                    op("pool", lambda e: e.tensor_tensor(out=dstT[:, ti, sl], in0=t1[:], in1=t2[:], op=ALU.add), [b_tmp], [b_dst])

    def v_aug_proj(wt, wbuf, col, vaug, b_v, pspool):
        op("dve", lambda e: e.memset(vaug[:, :, :, 64:65], 1.0), [], [b_v])
        for t in range(NT):
            pt, pb = pspool.get()
            proj_tm(wt, col, 256, t, pt, pb, wbuf)
            op("act", lambda e: e.activation(out=vaug[:, t, :, 0:64], in_=pt[:, 0:256].rearrange("p (a b) -> p a b", a=4), func=AF.Copy),
               [pb], [b_v])

    def attention(qT, kT, b_qk, vaug, b_v, maskfn, mix_chunk0, pools, otm, b_otm, pTs, rs, b_rs, mixT, mixb, qc_hook=None):
        (ps_s, ps_acc, ps_tr) = pools

        def att_gen(qc):
                for h in range(4):
                    ptile, bp = h // 2, (h % 2) * 64
                    acc, accb = ps_acc.get()
                    op("pe", lambda e: e.matmul(acc[:, 0:260], lhsT=zerosB[0:1, 0:128], rhs=zerosB[0:1, 0:260], start=True, stop=False,
                                                skip_group_check=True), [b_cst], [accb])
                    prev = None

                    def emit_pv(args):
                        (j_, t0_, pT_, pTb_) = args
                        for t in range(t0_, 4 * qc + 4):
                            i = t - 4 * qc
                            op("pe", lambda e, t=t, i=i: e.matmul(acc[:, i * 65:(i + 1) * 65], lhsT=pT_[:, (t - t0_) * 128:(t - t0_ + 1) * 128],
                                                                  rhs=vaug[:, j_, h, :], start=False, stop=(j_ == t), skip_group_check=True),
                               [pTb_, b_v], [accb])

                    for j in range(4 * qc + 4):
                        t0 = max(j, 4 * qc)
                        q0, q1 = t0 * 128, (4 * qc + 4) * 128
                        wq = q1 - q0
                        st, sb = ps_s.get()
                        op("pe", lambda e: e.matmul(st[:, 0:wq], lhsT=kT[bp:bp + 64, ptile, j * 128:(j + 1) * 128],
                                                    rhs=qT[bp:bp + 64, ptile, q0:q1], start=True, stop=False, skip_group_check=True),
                           [b_qk], [sb])
                        maskfn(h, qc, j, t0, st, sb, wq)
                        pT, pTb = pTs.get()
                        op("act", lambda e: e.activation(out=pT[:, 0:wq], in_=st[:, 0:wq], func=AF.Exp, scale=0.125), [sb], [pTb])
                        if prev is not None:
                            emit_pv(prev)
                        prev = (j, t0, pT, pTb)
                        yield
                    emit_pv(prev)
                    acc3 = acc[:, 0:260].rearrange("p (a b) -> p a b", a=4)
                    op("dve", lambda e: e.reciprocal(out=rs[:, :], in_=acc3[:, :, 64]), [accb], [b_rs])
                    for i in range(4):
                        op("dve", lambda e, i=i: e.tensor_scalar(out=otm[:, i, h * 64:(h + 1) * 64], in0=acc3[:, i, 0:64],
                                                                 scalar1=rs[:, i:i + 1], scalar2=None, op0=ALU.mult), [accb, b_rs], [b_otm])
                    yield
                for i in range(4):
                    t = 4 * qc + i
                    pt, pb = ps_tr.get()
                    for f in range(2):
                        op("pe", lambda e, f=f: e.transpose(out=pt[:, f * 128:(f + 1) * 128], in_=otm[:, i, f * 128:(f + 1) * 128], identity=identF),
                           [b_otm, b_cst], [pb])
                    op("act", lambda e: e.activation(out=mixT[:, mix_chunk0:mix_chunk0 + 2, t * 128:(t + 1) * 128],
                                                     in_=pt[:, 0:256].rearrange("p (a b) -> p a b", a=2), func=AF.Copy), [pb], [mixb[t]])
                    yield

        def drain(g):
            for _ in g:
                pass

        def merge(ga, na, gb, nb):
            ia = ib = 0
            da = db = False
            while not (da and db):
                if not da and (db or ia * nb <= ib * na):
                    try:
                        next(ga)
                    except StopIteration:
                        da = True
                    ia += 1
                else:
                    try:
                        next(gb)
                    except StopIteration:
                        db = True
                    ib += 1

        if qc_hook is not None:
            drain(qc_hook(0))
        for qc in range(4):
            ga = att_gen(qc)
            if qc_hook is not None and qc + 1 < 4:
                n_att = 4 * (4 * qc + 4) + 8
                n_hook = 4 + sum(((4 * (qc + 1) + i + 1) * 128 + 511) // 512 for i in range(4)) + N_BISECT + 2
                merge(ga, n_att, qc_hook(qc + 1), n_hook)
            else:
                drain(ga)

    for seq in range(nseq):
        pool_tr = Rot(PS[0:4])
        for t in range(NT):
            dma("sp", H[:, t, :], x[seq, t * 128:(t + 1) * 128, :], writes=[Hb[t]])
        for t in range(NT):
            make_HT(t, pool_tr)

        for layer in layers:
            for t in range(NT):
                dma("sp", hs[t * 128:(t + 1) * 128, :], H[:, t, :], reads=[Hb[t]], writes=[hsb[t]], owner=Hb[t])
            cx.barrier()
            cx.cur, cx.cur2 = base_mark
            cx.cur2, cx.lim2 = Hoff, Hoff + 65536
            mixT = alloc("mixT", [128, 8, L], BF16)
            mixb = [Buf(f"mix{t}") for t in range(NT)]

            if stop == 'ht':
                cx.barrier()
                return nc, cx
            for p in range(2):
                m0 = cx.mark()
                Wg = alloc("Wg", [128, 8, 784], BF16); b_Wg = Buf("Wg")
                gu = alloc("gu", [16, 128], BF16)
                nbias = alloc("nbias", [128, 1], F32)
                gain = alloc("gain", [128, 2, 128], F32)
                b_gp = Buf("gpar")
                rankT = alloc("rankT", [16, L], BF16); b_rank = Buf("rank")
                la = alloc("la", [128, L], F32, 1); b_la = Buf("la")
                bc = alloc("bc", [128, L], F32, 1); b_bc = Buf("bc")
                nbl = alloc("nbl", [128, 16], F32); dec = alloc("dec", [128, 16], F32); b_nbl = Buf("nbl")
                qdT = alloc("qdT", [128, L], BF16, 1); kiT = alloc("kiT", [128, L], BF16, 1); b_qk = Buf("gqk")
                kd = alloc("kd", [128, NT, 128], BF16, 1); b_kd = Buf("kd")
                vtm = alloc("vtm", [128, NT, 256], BF16, 1); b_v = Buf("gv")
                Sall = alloc("Sall", [128, NT, 128], BF16, 1); b_Sall = Buf("Sall")
                Sst = alloc("Sst", [128, 128], F32); b_S = Buf("S")
                e1 = alloc("e1", [128, 512], F32); e2 = alloc("e2", [128, 512], F32); e3 = alloc("e3", [128, 512], F32)
                b_e = [Buf("e1"), Buf("e2"), Buf("e3")]
                kdT = alloc("kdT", [128, 512], F32); b_kdT = Buf("kdT")
                AT = [alloc("AT0", [128, 128], BF16), alloc("AT1", [128, 128], BF16)]; b_AT = [Buf("AT0"), Buf("AT1")]
                sg = alloc("sg", [128, 256], F32); b_sg = Buf("sg")
                otm = alloc("otmg", [128, 256], F32); b_otm = Buf("otmg")
                sq = alloc("sq", [128, 256], F32); ssq = alloc("ssq", [128, 4], F32); b_ssq = Buf("ssq")

                load_w(Wg[:], w_in[layer][:, COFF[f"g{p}"]:COFF[f"g{p}"] + 784], b_Wg)
                dma("pool", gu[:], g_up[layer][:, p * 128:(p + 1) * 128], writes=[b_gp])
                dma("sp", nbias[:], g_bias[layer, p * 128:(p + 1) * 128].rearrange("(p o) -> p o", o=1), writes=[b_gp], owner=Buf("nb"))
                for hh in range(2):
                    dma("sp", gain[:, hh, :], g_gain[layer, :].partition_broadcast(128), writes=[b_gp], owner=Buf("gn"))
                op("dve", lambda e: e.tensor_scalar(out=nbias[:], in0=nbias[:], scalar1=-1.0, scalar2=None, op0=ALU.mult), [b_gp], [b_gp])
                chk('g1')
                pp = Rot(PS[0:4])
                for tc in range(4):
                    pt, pb = pp.get()
                    for c in range(8):
                        op("pe", lambda e, c=c: e.matmul(pt[0:16, 0:512], lhsT=Wg[:, c, 768:784], rhs=HT[:, c, tc * 512:(tc + 1) * 512],
                                                         start=(c == 0), stop=(c == 7)), [b_Wg] + HTb[tc * 4:tc * 4 + 4], [pb])
                    op("act", lambda e: e.activation(out=rankT[:, tc * 512:(tc + 1) * 512], in_=pt[0:16, 0:512], func=AF.Copy), [pb], [b_rank])
                for tc in range(4):
                    pt, pb = pp.get()
                    sl = slice(tc * 512, (tc + 1) * 512)
                    op("pe", lambda e: e.matmul(pt[:, 0:512], lhsT=gu[0:16, :], rhs=rankT[0:16, sl], start=True, stop=True), [b_gp, b_rank], [pb])
                    op("act", lambda e: e.activation(out=la[:, sl], in_=pt[:, 0:512], func=AF.Exp, bias=nbias[:, 0:1], scale=-1.0), [pb, b_gp], [b_la])
                op("act", lambda e: e.activation(out=la[:], in_=la[:], func=AF.Ln, bias=1.0, scale=1.0), [b_la], [b_la])
                chk('g2')
                for n in range(NT):
                    sl = slice(n * 128, (n + 1) * 128)
                    op("dve", lambda e: e.tensor_tensor_scan(out=bc[:, sl], data0=la[:, sl], data1=cst[:, 640:768], initial=0.0,
                                                             op0=ALU.add, op1=ALU.add), [b_la, b_cst], [b_bc])
                chk('g3')
                cx_chk = None
                op("dve", lambda e: e.tensor_scalar(out=nbl[:], in0=bc[:, :].rearrange("p (n c) -> p n c", c=128)[:, :, 127],
                                                    scalar1=-1.0 / 16.0, scalar2=None, op0=ALU.mult), [b_bc], [b_nbl])
                op("act", lambda e: e.activation(out=dec[:], in_=nbl[:], func=AF.Exp), [b_nbl], [b_nbl])
                chk('g3b')
                ptr = Rot(PS[4:6])
                for tc in range(4):
                    sl = slice(tc * 512, (tc + 1) * 512)
                    pq, pqb = pp.get()
                    pk, pkb = pp.get()
                    proj_fm(Wg, 0, 128, tc, pq, pqb, b_Wg)
                    proj_fm(Wg, 128, 128, tc, pk, pkb, b_Wg)
                    op("act", lambda e: e.activation(out=e1[:], in_=bc[:, sl], func=AF.Exp, scale=-1.0 / 16.0), [b_bc], [b_e[0]])
                    op("dve", lambda e: e.scalar_tensor_tensor(out=qdT[:, sl], in0=pq[:, :], scalar=0.125, in1=e1[:], op0=ALU.mult, op1=ALU.mult),
                       [pqb, b_e[0]], [b_qk])
                    op("act", lambda e: e.activation(out=e2[:], in_=bc[:, sl], func=AF.Exp, scale=1.0 / 16.0), [b_bc], [b_e[1]])
                    op("dve", lambda e: e.tensor_tensor(out=kiT[:, sl], in0=pk[:, :], in1=e2[:], op=ALU.mult), [pkb, b_e[1]], [b_qk])
                    for n4 in range(4):
                        n = tc * 4 + n4
                        op("act", lambda e, n=n, n4=n4: e.activation(out=e3[:, n4 * 128:(n4 + 1) * 128], in_=bc[:, n * 128:(n + 1) * 128], func=AF.Exp,
                                                                     bias=nbl[:, n:n + 1], scale=1.0 / 16.0), [b_bc, b_nbl], [b_e[2]])
                    op("dve", lambda e: e.tensor_tensor(out=kdT[:], in0=pk[:, :], in1=e3[:], op=ALU.mult), [pkb, b_e[2]], [b_kdT])
                    pt, pb = ptr.get()
                    for n4 in range(4):
                        op("pe", lambda e, n4=n4: e.transpose(out=pt[:, n4 * 128:(n4 + 1) * 128], in_=kdT[:, n4 * 128:(n4 + 1) * 128], identity=identF),
                           [b_kdT, b_cst], [pb])
                    op("act", lambda e: e.activation(out=kd[:, tc * 4:(tc + 1) * 4, :], in_=pt[:, :].rearrange("p (a b) -> p a b", a=4), func=AF.Copy),
                       [pb], [b_kd])
                chk('g4')
                for t in range(NT):
                    pt, pb = pp.get()
                    proj_tm(Wg, 256, 256, t, pt, pb, b_Wg)
                    op("act", lambda e: e.activation(out=vtm[:, t, :], in_=pt[:, 0:256], func=AF.Copy), [pb], [b_v])
                chk('g5')
                op("dve", lambda e: e.memset(Sst[:], 0.0), [], [b_S])
                op("dve", lambda e: e.memset(Sall[:, 0, :], 0.0), [], [b_Sall])
                for n in range(NT - 1):
                    pt, pb = pp.get()
                    for hh in range(2):
                        op("pe", lambda e, hh=hh: e.matmul(pt[hh * 64:(hh + 1) * 64, 0:128], lhsT=kd[:, n, hh * 64:(hh + 1) * 64],
                                                           rhs=vtm[:, n, hh * 128:(hh + 1) * 128], start=True, stop=True), [b_kd, b_v], [pb])
                    op("dve", lambda e: e.scalar_tensor_tensor(out=Sst[:], in0=Sst[:], scalar=dec[:, n:n + 1], in1=pt[:, 0:128],
                                                               op0=ALU.mult, op1=ALU.add), [b_S, b_nbl, pb], [b_S])
                    op("act", lambda e: e.activation(out=Sall[:, n + 1, :], in_=Sst[:], func=AF.Copy), [b_S], [b_Sall])
                chk('g6')
                ps_o = Rot(PS[4:6]); ps_g = Rot(PS[6:8])
                o_all = alloc("o_all", [128, NT, 256], F32); b_oall = [Buf(f"oall{n}") for n in range(NT)]
                sg_all = alloc("sg_all", [128, NT, 256], F32); b_sgall = [Buf(f"sgall{n}") for n in range(NT)]
                ssq_all = alloc("ssq_all", [128, NT, 2], F32); rstd_all = alloc("rstd_all", [128, NT, 2], F32); b_ssqa = Buf("ssqa")
                otms = [(alloc(f"otmg{i}", [128, 256], F32), Buf(f"otmg{i}")) for i in range(2)]
                for n in range(NT):
                    sl = slice(n * 128, (n + 1) * 128)
                    po, pob = ps_o.get()
                    for hh in range(2):
                        bp = hh * 64
                        pa, pab = pp.get()
                        op("pe", lambda e: e.matmul(pa[:, 0:128], lhsT=kiT[bp:bp + 64, sl], rhs=qdT[bp:bp + 64, sl], start=True, stop=True), [b_qk], [pab])
                        op("dve", lambda e: e.tensor_tensor(out=AT[hh][:], in0=pa[:, 0:128], in1=tri01, op=ALU.mult), [pab, b_cst], [b_AT[hh]])
                        op("pe", lambda e: e.matmul(po[:, hh * 128:(hh + 1) * 128], lhsT=AT[hh][:], rhs=vtm[:, n, hh * 128:(hh + 1) * 128],
                                                    start=True, stop=False, skip_group_check=True), [b_AT[hh], b_v], [pob])
                        op("pe", lambda e: e.matmul(po[:, hh * 128:(hh + 1) * 128], lhsT=qdT[bp:bp + 64, sl], rhs=Sall[bp:bp + 64, n, :],
                                                    start=False, stop=True, skip_group_check=True), [b_qk, b_Sall], [pob])
                    for hh in range(2):
                        op("act", lambda e, hh=hh: e.activation(out=sq[:, hh * 128:(hh + 1) * 128], in_=po[:, hh * 128:(hh + 1) * 128], func=AF.Square,
                                                                accum_out=ssq_all[:, n, hh:hh + 1]), [pob], [b_ssqa])
                    op("act", lambda e: e.activation(out=o_all[:, n, :], in_=po[:, 0:256], func=AF.Copy), [pob], [b_oall[n]])
                op("act", lambda e: e.activation(out=rstd_all[:, :, :], in_=ssq_all[:, :, :], func=AF.Sqrt, bias=epsln[:, 1:2], scale=1.0 / 128.0), [b_ssqa, b_cst], [b_ssqa])
                op("dve", lambda e: e.reciprocal(out=rstd_all[:, :, :], in_=rstd_all[:, :, :]), [b_ssqa], [b_ssqa])
                for n in range(NT):
                    pg, pgb = ps_g.get()
                    proj_tm(Wg, 512, 256, n, pg, pgb, b_Wg)
                    op("act", lambda e: e.activation(out=sg_all[:, n, :], in_=pg[:, 0:256], func=AF.Silu), [pgb], [b_sgall[n]])
                    op("pool", lambda e: e.tensor_tensor(out=sg_all[:, n, :], in0=sg_all[:, n, :], in1=gain[:, :, :].rearrange("p a b -> p (a b)"), op=ALU.mult),
                       [b_sgall[n], b_gp], [b_sgall[n]])
                for n in range(NT):
                    sl = slice(n * 128, (n + 1) * 128)
                    otm, b_otm = otms[n % 2]
                    for hh in range(2):
                        op("dve", lambda e, hh=hh: e.scalar_tensor_tensor(out=otm[:, hh * 128:(hh + 1) * 128], in0=o_all[:, n, hh * 128:(hh + 1) * 128],
                                                                          scalar=rstd_all[:, n, hh:hh + 1], in1=sg_all[:, n, hh * 128:(hh + 1) * 128],
                                                                          op0=ALU.mult, op1=ALU.mult), [b_oall[n], b_ssqa, b_sgall[n]], [b_otm])
                    pt, pb = ps_o.get()
                    for hh in range(2):
                        op("pe", lambda e, hh=hh: e.transpose(out=pt[:, hh * 128:(hh + 1) * 128], in_=otm[:, hh * 128:(hh + 1) * 128], identity=identF),
                           [b_otm, b_cst], [pb])
                    op("act", lambda e: e.activation(out=mixT[:, 2 * p:2 * p + 2, sl], in_=pt[:, 0:256].rearrange("p (a b) -> p a b", a=2), func=AF.Copy),
                       [pb], [mixb[n]])
                cx.release(m0)
                chk('p0')

            if stop == 'gla':
                cx.barrier()
                return nc, cx
            def attn_common():
                qT = alloc("qT", [128, 2, L], BF16, 1); kT = alloc("kT", [128, 2, L], BF16, 1)
                vaug = alloc("vaug", [128, NT, 4, 65], BF16, 1)
                return qT, kT, vaug, Buf("aqk"), Buf("av")

            m0 = cx.mark()
            qT, kT, vaug, b_qk, b_v = attn_common()
            kmT = alloc("kmT", [128, 2, 8], F32); kmB = alloc("kmB", [128, 2, 8], BF16); b_km = Buf("km")
            mbT = alloc("mbT", [8, 4, L], BF16); b_mb = Buf("mbT")
            m1 = cx.mark()
            cosT = alloc("cosT", [128, L], F32); sinT = alloc("sinT", [128, L], F32); b_tab = Buf("tab")
            rope_tables(seq, cosT, sinT, b_tab)
            chk('m1')
            Wa = alloc("Wa", [128, 8, 512], BF16); b_Wa = Buf("Wa")
            Wb = alloc("Wb", [128, 8, 512], BF16); b_Wb = Buf("Wb")
            t1 = alloc("t1", [128, 512], F32); t2 = alloc("t2", [128, 512], F32); b_tmp = Buf("rt")
            pp = Rot(PS[0:6])
            load_w(Wa[:], w_in[layer][:, COFF["mq"]:COFF["mq"] + 512], b_Wa)
            load_w(Wb[:], w_in[layer][:, COFF["mk"]:COFF["mk"] + 512], b_Wb)
            roped_proj(Wa, b_Wa, 0, 256, qT, b_qk, cosT, sinT, b_tab, (t1, t2, b_tmp), pp)
            roped_proj(Wb, b_Wb, 0, 256, kT, b_qk, cosT, sinT, b_tab, (t1, t2, b_tmp), pp, ksum=(kmT, b_km))
            chk('m2a')
            load_w(Wa[:, :, 0:256], w_in[layer][:, COFF["mv"]:COFF["mv"] + 256], b_Wa)
            v_aug_proj(Wa, b_Wa, 0, vaug, b_v, pp)
            op("dve", lambda e: e.tensor_copy(out=kmB[:], in_=kmT[:]), [b_km], [b_km])
            cx.release(m1)
            chk('m2')
            Z = alloc("Z", [128, 4, 8], F32); gm = alloc("gm", [128, 4, 8], F32); top8 = alloc("top8", [128, 4, 8], F32); b_Z = Buf("Z")
            otm = alloc("otm", [128, 4, 256], F32); b_otm = Buf("otm")
            pTl = [(alloc(f"pT{i}", [128, 512], BF16), Buf(f"pT{i}")) for i in range(3)]
            rs = alloc("rs", [128, 4], F32); b_rs = Buf("rs")
            op("pool", lambda e: e.memset(mbT[:], 0.0), [], [b_mb])
            pg = Rot(PS[0:2])
            for t in range(8, NT):
                pt, pb = pg.get()
                for h in range(4):
                    ptile, bp = h // 2, (h % 2) * 64
                    op("pe", lambda e, h=h: e.matmul(pt[:, h * 8:(h + 1) * 8], lhsT=qT[bp:bp + 64, ptile, t * 128:(t + 1) * 128],
                                                     rhs=kmB[bp:bp + 64, ptile, :], start=True, stop=False, skip_group_check=True), [b_qk, b_km], [pb])
                    op("pe", lambda e, h=h: e.matmul(pt[:, h * 8:(h + 1) * 8], lhsT=onehotK[0:8, t * 128:(t + 1) * 128],
                                                     rhs=bpen, start=False, stop=True, skip_group_check=True), [b_cst], [pb])
                op("dve", lambda e: e.tensor_copy(out=gm[:], in_=pt[:, 0:32].rearrange("p (a b) -> p a b", a=4)), [pb], [b_Z])
                for h in range(4):
                    op("dve", lambda e, h=h: e.max(out=top8[:, h, :], in_=gm[:, h, :]), [b_Z], [b_Z])
                for h in range(4):
                    op("dve", lambda e, h=h: e.tensor_scalar(out=Z[:, h, :], in0=gm[:, h, :], scalar1=top8[:, h, 3:4], scalar2=NEG,
                                                             op0=ALU.is_lt, op1=ALU.mult), [b_Z], [b_Z])
                pt2, pb2 = pg.get()
                for h in range(4):
                    op("pe", lambda e, h=h: e.transpose(out=pt2[0:8, h * 128:(h + 1) * 128], in_=Z[:, h, :], identity=identF), [b_Z, b_cst], [pb2])
                op("dve", lambda e: e.tensor_copy(out=mbT[0:8, :, t * 128:(t + 1) * 128], in_=pt2[0:8, :].rearrange("p (a b) -> p a b", a=4)),
                   [pb2], [b_mb])
            chk('m3')

            def moba_mask(h, qc, j, t0, st, sb, wq):
                q0 = t0 * 128
                diag = (j >= 4 * qc)
                op("pe", lambda e: e.matmul(st[:, 0:wq], lhsT=onehotK[0:8, j * 128:(j + 1) * 128], rhs=mbT[0:8, h, q0:q0 + wq],
                                            start=False, stop=(not diag), skip_group_check=True), [b_cst, b_mb], [sb])
                if diag:
                    op("pe", lambda e: e.matmul(st[:, 0:128], lhsT=trinegB[:], rhs=identB[:], start=False, stop=True, skip_group_check=True), [b_cst], [sb])

            attention(qT, kT, b_qk, vaug, b_v, moba_mask, 4, (Rot(PS[2:5]), Rot(PS[5:7]), Rot(PS[7:8])), otm, b_otm, Rot(pTl), rs, b_rs, mixT, mixb)
            cx.release(m0)

            if stop == 'moba':
                cx.barrier()
                return nc, cx
            m0 = cx.mark()
            qT, kT, vaug, b_qk, b_v = attn_common()
            kiT = alloc("kiT", [128, 1, L], BF16, 1); b_ki = Buf("ki")
            wT = alloc("wT", [8, L], BF16, 1); b_wT = Buf("wT")
            qiT = alloc("qiT", [128, 4, L], BF16, 1); b_qi = Buf("qi")
            m1 = cx.mark()
            cosT = alloc("cosT", [128, L], F32); sinT = alloc("sinT", [128, L], F32); b_tab = Buf("tab")
            rope_tables(seq, cosT, sinT, b_tab)
            Wa = alloc("Wa", [128, 8, 512], BF16); b_Wa = Buf("Wa")
            Wb = alloc("Wb", [128, 8, 512], BF16); b_Wb = Buf("Wb")
            t1 = alloc("t1", [128, 512], F32); t2 = alloc("t2", [128, 512], F32); b_tmp = Buf("rt")
            pp = Rot(PS[0:6])
            tm3 = (t1, t2, b_tmp)
            load_w(Wa[:], w_in[layer][:, COFF["sq"]:COFF["sq"] + 512], b_Wa)
            load_w(Wb[:], w_in[layer][:, COFF["sk"]:COFF["sk"] + 512], b_Wb)
            roped_proj(Wa, b_Wa, 0, 256, qT, b_qk, cosT, sinT, b_tab, tm3, pp)
            roped_proj(Wb, b_Wb, 0, 256, kT, b_qk, cosT, sinT, b_tab, tm3, pp)
            load_w(Wa[:], w_in[layer][:, COFF["iq"]:COFF["iq"] + 512], b_Wa)
            load_w(Wb[:], w_in[layer][:, COFF["iqr"]:COFF["iqr"] + 512], b_Wb)
            for ti in range(4):
                for tc in range(4):
                    py, pyb = pp.get(); pr, prb = pp.get()
                    proj_fm(Wa, ti * 128, 128, tc, py, pyb, b_Wa)
                    proj_fm(Wb, ti * 128, 128, tc, pr, prb, b_Wb)
                    sl = slice(tc * 512, (tc + 1) * 512)
                    op("dve", lambda e: e.tensor_tensor(out=t1[:], in0=pr[:, :], in1=sinT[:, sl], op=ALU.mult), [prb, b_tab], [b_tmp])
                    op("dve", lambda e: e.tensor_tensor(out=t2[:], in0=py[:, :], in1=cosT[:, sl], op=ALU.mult), [pyb, b_tab], [b_tmp])
                    op("pool", lambda e: e.tensor_tensor(out=qiT[:, ti, sl], in0=t1[:], in1=t2[:], op=ALU.add), [b_tmp], [b_qi])
            load_w(Wa[:, :, 0:264], w_in[layer][:, COFF["ik"]:COFF["ik"] + 264], b_Wa)
            load_w(Wb[:, :, 0:256], w_in[layer][:, COFF["sv"]:COFF["sv"] + 256], b_Wb)
            roped_proj(Wa, b_Wa, 0, 128, kiT, b_ki, cosT, sinT, b_tab, tm3, pp)
            for tc in range(4):
                pt, pb = pp.get()
                for c in range(8):
                    op("pe", lambda e, c=c: e.matmul(pt[0:8, 0:512], lhsT=Wa[:, c, 256:264], rhs=HT[:, c, tc * 512:(tc + 1) * 512],
                                                     start=(c == 0), stop=(c == 7)), [b_Wa] + HTb[tc * 4:tc * 4 + 4], [pb])
                op("act", lambda e: e.activation(out=wT[:, tc * 512:(tc + 1) * 512], in_=pt[0:8, 0:512], func=AF.Copy), [pb], [b_wT])
            v_aug_proj(Wb, b_Wb, 0, vaug, b_v, pp)
            cx.release(m1)
            mbs = [HT[:, 0:4, :], alloc("mbB", [128, 4, L], BF16)]
            b_mb2 = [[Buf(f"mb{a}_{i}") for i in range(4)] for a in range(2)]
            isc = [HT[:, 4:6, :].bitcast(F32).rearrange("p a b -> p (a b)"), HT[:, 6:8, :].bitcast(F32).rearrange("p a b -> p (a b)"),
                   alloc("isc2", [128, L], F32), alloc("isc3", [128, L], F32)]
            b_isc = [Buf(f"isc{i}") for i in range(4)]
            qs = alloc("qs", [128, 4, 2, 512], BF16); b_qs = Buf("qs")
            otm = alloc("otm", [128, 4, 256], F32); b_otm = Buf("otm")
            pTl = [(alloc(f"pT{i}", [128, 512], BF16), Buf(f"pT{i}")) for i in range(3)]
            rs = alloc("rs", [128, 4], F32); b_rs = Buf("rs")
            bsA = alloc("bsA", [128, 32], F32); bsi = alloc("bsi", [128, 4], I32)
            lo = bsA[:, 0:4]; w0 = bsA[:, 4:8]; mid = bsA[:, 8:12]; nmid = bsA[:, 12:16]; cntv = bsA[:, 16:20]; thc = bsA[:, 20:24]; mx = bsA[:, 24:28]
            b_lo = Buf("lo"); b_mid = Buf("mid"); b_cnt = [Buf(f"cnt{i}") for i in range(4)]; b_msk = Buf("msk")
            junkD = alloc("junkD", [128, L], BF16); b_junkD = Buf("junkD")
            junkA = alloc("junkA", [128, L], BF16); b_junkA = Buf("junkA")
            ps_i = Rot(PS[0:3])
            rls = Rot([(alloc(f"rl{i}", [128, 512], BF16), Buf(f"rl{i}")) for i in range(4)])

            def dsa_qc(qc):
                mb = mbs[qc % 2]; b_mbq = b_mb2[qc % 2]
                qsl = slice(qc * 512, (qc + 1) * 512)
                for pr_ in range(4):
                    pt, pb = ps_i.get()
                    op("pe", lambda e: e.matmul(pt[:, 0:512], lhsT=selw[0:8, pr_ * 128:(pr_ + 1) * 128], rhs=wT[0:8, qsl], start=True, stop=True),
                       [b_cst, b_wT], [pb])
                    op("dve", lambda e: e.scalar_tensor_tensor(out=qs[:, pr_, 0, :], in0=pt[:, 0:512], scalar=0.0, in1=qiT[:, pr_, qsl],
                                                               op0=ALU.max, op1=ALU.mult), [pb, b_qi], [b_qs])
                    op("dve", lambda e: e.scalar_tensor_tensor(out=qs[:, pr_, 1, :], in0=pt[:, 0:512], scalar=0.0, in1=qiT[:, pr_, qsl],
                                                               op0=ALU.min, op1=ALU.mult), [pb, b_qi], [b_qs])
                yield
                act_tiles = []
                for i in range(4):
                    t = 4 * qc + i
                    S = (t + 1) * 128
                    if t < 2:
                        if t > 0:
                            op("pool", lambda e: e.memset(mb[:, i, 0:t * 128], 0.0), [], [b_mbq[i]])
                        op("pool", lambda e: e.tensor_copy(out=mb[:, i, t * 128:S], in_=trinegB[:]), [b_cst], [b_mbq[i]])
                        continue
                    act_tiles.append(i)
                    ic, icb = isc[i], b_isc[i]
                    for kc in range((S + 511) // 512):
                        w = min(512, S - kc * 512)
                        ksl = slice(kc * 512, kc * 512 + w)
                        first = True
                        nsum = 0
                        p2, p2b = PS[3]
                        pend = []

                        def flush_one():
                            nonlocal nsum
                            (rl_, rlb_, sg_) = pend.pop(0)
                            op("pe", lambda e: e.matmul(p2[:, 0:w], lhsT=(identB[:] if sg_ == 0 else negidentB[:]), rhs=rl_[:, 0:w],
                                                        start=(nsum == 0), stop=(nsum == 7)), [rlb_, b_cst], [p2b])
                            nsum += 1

                        for k in range(16):
                            hd, sgn = k // 2, k % 2
                            pr_, bp = hd // 2, (hd % 2) * 64
                            pt, pb = ps_i.get()
                            op("pe", lambda e: e.matmul(pt[:, 0:w], lhsT=qs[bp:bp + 64, pr_, sgn, i * 128:(i + 1) * 128], rhs=kiT[bp:bp + 64, 0, ksl],
                                                        start=True, stop=True), [b_qs, b_ki], [pb])
                            o = ALU.max if sgn == 0 else ALU.min
                            if hd % 2 == 1:
                                rl, rlb = rls.get()
                                op("act", lambda e: e.activation(out=rl[:, 0:w], in_=pt[:, 0:w], func=AF.Relu, scale=(1.0 if sgn == 0 else -1.0)), [pb], [rlb])
                                pend.append((rl, rlb, sgn))
                                if len(pend) > 2:
                                    flush_one()
                            elif first:
                                op("dve", lambda e: e.tensor_scalar(out=ic[:, ksl], in0=pt[:, 0:w], scalar1=0.0, scalar2=None, op0=o), [pb], [icb])
                                first = False
                            else:
                                op("dve", lambda e: e.scalar_tensor_tensor(out=ic[:, ksl], in0=pt[:, 0:w], scalar=0.0, in1=ic[:, ksl], op0=o, op1=ALU.add),
                                   [pb, icb], [icb])
                        while pend:
                            flush_one()
                        op("dve", lambda e: e.tensor_tensor(out=ic[:, ksl], in0=p2[:, 0:w], in1=ic[:, ksl], op=ALU.add), [p2b, icb], [icb])
                        yield
                    op("dve", lambda e: e.tensor_reduce(out=lo[:, i:i + 1], in_=ic[:, 0:S], axis=AX.X, op=ALU.min), [icb], [b_lo])
                    op("dve", lambda e: e.tensor_reduce(out=mx[:, i:i + 1], in_=ic[:, 0:S], axis=AX.X, op=ALU.max), [icb], [b_lo])
                    op("dve", lambda e: e.tensor_tensor(out=ic[:, t * 128:S], in0=ic[:, t * 128:S], in1=trinegbig, op=ALU.add), [icb, b_cst], [icb])
                    on_act = (i % 2 == 1)
                    op("dve", lambda e: e.memset(thc[:, i:i + 1], (S - 511.0) if on_act else (S - 255.5)), [], [b_lo])
                a0, a1 = act_tiles[0], act_tiles[-1] + 1
                asl = slice(a0, a1)
                op("dve", lambda e: e.scalar_tensor_tensor(out=w0[:, asl], in0=mx[:, asl], scalar=1.0, in1=lo[:, asl], op0=ALU.add, op1=ALU.subtract),
                   [b_lo], [b_lo])
                for it in range(N_BISECT):
                    ck = 0.5 ** (it + 1)
                    op("dve", lambda e, ck=ck: e.scalar_tensor_tensor(out=mid[:, asl], in0=w0[:, asl], scalar=ck, in1=lo[:, asl], op0=ALU.mult, op1=ALU.add),
                       [b_lo], [b_mid])
                    for i in act_tiles:
                        S = (4 * qc + i + 1) * 128
                        ic, icb = isc[i], b_isc[i]
                        if i % 2 == 1:
                            op("act", lambda e, i=i, S=S, ic=ic: e.activation(out=junkA[:, 0:S], in_=ic[:, 0:S], func=AF.Sign, bias=mid[:, i:i + 1], scale=-1.0,
                                                                              accum_out=cntv[:, i:i + 1]), [icb, b_mid], [b_cnt[i], b_junkA])
                        else:
                            op("dve", lambda e, i=i, S=S, ic=ic: e.tensor_scalar(out=junkD[:, 0:S], in0=ic[:, 0:S], scalar1=mid[:, i:i + 1], scalar2=None,
                                                                                 op0=ALU.is_lt, op1=ALU.add, accum_out=cntv[:, i:i + 1]), [icb, b_mid], [b_cnt[i], b_junkD])
                    op("dve", lambda e: e.tensor_tensor(out=bsi[:, asl], in0=cntv[:, asl], in1=thc[:, asl], op=ALU.is_le), [b_cnt[i] for i in act_tiles] + [b_lo], [b_msk])
                    op("dve", lambda e: e.copy_predicated(out=lo[:, asl], mask=bsi[:, asl], data=mid[:, asl]), [b_msk, b_mid], [b_lo])
                    yield
                for i in act_tiles:
                    S = (4 * qc + i + 1) * 128
                    ic, icb = isc[i], b_isc[i]
                    op("dve", lambda e, i=i, S=S, ic=ic: e.tensor_scalar(out=mb[:, i, 0:S], in0=ic[:, 0:S], scalar1=lo[:, i:i + 1], scalar2=NEG, op0=ALU.is_lt, op1=ALU.mult),
                       [icb, b_lo], [b_mbq[i]])

            def dsa_mask(h, qc, j, t0, st, sb, wq):
                last = 4 * qc + 3
                mb = mbs[qc % 2]; b_mbq = b_mb2[qc % 2]
                for t in range(t0, last + 1):
                    i = t - 4 * qc
                    op("pe", lambda e, t=t, i=i: e.matmul(st[:, (t - t0) * 128:(t - t0 + 1) * 128], lhsT=mb[:, i, j * 128:(j + 1) * 128], rhs=identB[:],
                                                          start=False, stop=(t == last), skip_group_check=True), [b_mbq[i], b_cst], [sb])

            attention(qT, kT, b_qk, vaug, b_v, dsa_mask, 6, (Rot(PS[4:6]), Rot(PS[6:8]), Rot(PS[3:4])), otm, b_otm, Rot(pTl), rs, b_rs, mixT, mixb,
                      qc_hook=dsa_qc)
            cx.release(m0)

            if stop == 'dsa':
                cx.barrier()
                return nc, cx
            if debug and seq == 0 and layer == layers[0]:
                dma("sp", dbg[:, :, :], mixT[:, :, :], reads=mixb, owner=Buf("dbg"))

            cx.cur2 = Hoff + 65536
            m0 = cx.mark()
            Wo = alloc("Wo", [128, 8, D], BF16); b_Wo = Buf("Wo")
            gbc = alloc("gbc", [128, D], F32); bbc = alloc("bbc", [128, D], F32); b_par = Buf("lnpar")
            hres = [alloc("hres0", [128, D], F32), alloc("hres1", [128, D], F32)]; b_hres = [Buf("hres0"), Buf("hres1")]
            stat = alloc("stat", [128, NT, 20], F32); b_stat = [Buf(f"stat{t}") for t in range(NT)]
            rw = alloc("rw", [128, 8, 16], F32); rwh = alloc("rwh", [128, 8, 16], BF16); rwl = alloc("rwl", [128, 8, 16], BF16); b_rw = Buf("rw")
            rbb = alloc("rbb", [128, 16], F32)
            HTlo = alloc("HTlo", [128, 8, 128], BF16); b_lo = Buf("HTlo")
            comb = alloc("comb", [128, NT, 16], F32); b_comb = Buf("comb")
            rt = alloc("rt", [128, 8, NT * 16], F32); b_rt = Buf("rt")
            lg_all = alloc("lg_all", [128, NT, 16], F32); b_lg = Buf("lg")
            load_w(Wo[:], w_out[layer][:, :], b_Wo)
            dma("sp", gbc[:], ln1g[layer, :].partition_broadcast(128), writes=[b_par])
            dma("sp", bbc[:], ln1b[layer, :].partition_broadcast(128), writes=[b_par], owner=Buf("lnb"))
            dma("sp", rw[:], r_w.rearrange("(c p) e -> p c e", p=128), writes=[b_rw])
            dma("sp", rbb[:], r_b.partition_broadcast(128), writes=[b_rw], owner=Buf("rbb"))
            op("dve", lambda e: e.tensor_copy(out=rwh[:], in_=rw[:]), [b_rw], [b_rw])
            op("dve", lambda e: e.tensor_tensor(out=rw[:], in0=rw[:], in1=rwh[:], op=ALU.subtract), [b_rw], [b_rw])
            op("dve", lambda e: e.tensor_copy(out=rwl[:], in_=rw[:]), [b_rw], [b_rw])
            ps_o = Rot(PS[0:4]); ps_t = Rot(PS[4:7]); ps_r = Rot(PS[7:8])
            def ln1_front(t):
                hr, hrb = hres[t % 2], b_hres[t % 2]
                dma("sp", hr[:], hs[t * 128:(t + 1) * 128, :], reads=[hsb[t]], writes=[hrb])
                for half in range(2):
                    po, pob = ps_o.get()
                    for c in range(8):
                        op("pe", lambda e, c=c: e.matmul(po[:, 0:512], lhsT=mixT[:, c, t * 128:(t + 1) * 128], rhs=Wo[:, c, half * 512:(half + 1) * 512],
                                                         start=(c == 0), stop=(c == 7)), [mixb[t], b_Wo], [pob])
                    op("dve", lambda e: e.scalar_tensor_tensor(out=H[:, t, half * 512:(half + 1) * 512], in0=hr[:, half * 512:(half + 1) * 512], scalar=ALPHA,
                                                               in1=po[:, 0:512], op0=ALU.mult, op1=ALU.add), [hrb, pob], [Hb[t]])
                ln_stats(t, stat, b_stat)

            def ln1_back(t):
                ln_apply(t, gbc, bbc, b_par, stat, b_stat)
                for half in range(2):
                    pt, pb = ps_t.get()
                    for c4 in range(4):
                        c = half * 4 + c4
                        op("pe", lambda e, c=c, c4=c4: e.transpose(out=pt[:, c4 * 128:(c4 + 1) * 128], in_=H[:, t, c * 128:(c + 1) * 128], identity=identF),
                           [Hb[t], b_cst], [pb])
                    pv = pt[:, :].rearrange("p (a b) -> p a b", a=4)
                    op("act", lambda e: e.activation(out=HT[:, half * 4:(half + 1) * 4, t * 128:(t + 1) * 128], in_=pv, func=AF.Copy), [pb], [HTb[t]])
                    op("dve", lambda e: e.tensor_tensor(out=HTlo[:, half * 4:(half + 1) * 4, :], in0=pv, in1=HT[:, half * 4:(half + 1) * 4, t * 128:(t + 1) * 128],
                                                        op=ALU.subtract), [pb, HTb[t]], [b_lo])
                pr_, prb = ps_r.get()
                k = 0
                for (A_, Ab, W_) in ((HT[:, :, t * 128:(t + 1) * 128], HTb[t], rwh), (HTlo[:, :, :], b_lo, rwh), (HT[:, :, t * 128:(t + 1) * 128], HTb[t], rwl)):
                    for c in range(8):
                        op("pe", lambda e, c=c, A_=A_, W_=W_, k=k: e.matmul(pr_[:, 0:16], lhsT=A_[:, c, :], rhs=W_[:, c, :], start=(k == 0), stop=(k == 23)),
                           [Ab, b_rw], [prb])
                        k += 1
                op("act", lambda e: e.activation(out=lg_all[:, t, :], in_=pr_[:, 0:16], func=AF.Copy), [prb], [b_lg])
                op("act", lambda e: e.activation(out=H[:, t, :], in_=H[:, t, :], func=AF.Copy, scale=ALPHA), [Hb[t]], [Hb[t]])

            ln1_front(0)
            for t in range(NT):
                if t + 1 < NT:
                    ln1_front(t + 1)
                ln1_back(t)
            T_ = NT
            sc = rt[:, 0, :]; sel = rt[:, 1, :]; tmpa = rt[:, 2, :]; tmpb = rt[:, 3, :]; ch = rt[:, 4, :]
            m1 = rt[:, 5, 0:T_ * 4]; m2 = rt[:, 5, T_ * 4:T_ * 8]; gs = rt[:, 5, T_ * 8:T_ * 12]; ing = rt[:, 5, T_ * 12:T_ * 16]
            gmax = rt[:, 6, 0:T_]; a1 = rt[:, 6, T_:2 * T_]; a2 = rt[:, 6, 2 * T_:3 * T_]; ssum = rt[:, 6, 3 * T_:4 * T_]
            v3 = lambda a: a.rearrange("p (t e) -> p t e", t=T_)
            v4 = lambda a: a.rearrange("p (t g e) -> p t g e", t=T_, g=4)
            g3 = lambda a: a.rearrange("p (t g) -> p t g", t=T_)
            R = [b_rt]
            op("act", lambda e: e.activation(out=sc, in_=lg_all[:, :, :].rearrange("p t e -> p (t e)"), func=AF.Sigmoid), [b_lg], R)
            op("dve", lambda e: e.tensor_tensor(out=v3(sel), in0=v3(sc), in1=rbb[:, None, :].to_broadcast([128, T_, 16]), op=ALU.add), R + [b_rw], R)
            op("dve", lambda e: e.tensor_reduce(out=g3(m1), in_=v4(sel), axis=AX.X, op=ALU.max), R, R)
            op("dve", lambda e: e.tensor_tensor(out=v4(tmpa), in0=v4(sel), in1=g3(m1)[:, :, :, None].to_broadcast([128, T_, 4, 4]), op=ALU.is_equal), R, R)
            op("dve", lambda e: e.scalar_tensor_tensor(out=tmpb, in0=tmpa, scalar=-1.0e9, in1=sel, op0=ALU.mult, op1=ALU.add), R, R)
            op("dve", lambda e: e.tensor_reduce(out=g3(m2), in_=v4(tmpb), axis=AX.X, op=ALU.max), R, R)
            op("dve", lambda e: e.tensor_tensor(out=gs, in0=m1, in1=m2, op=ALU.add), R, R)
            op("dve", lambda e: e.tensor_reduce(out=gmax, in_=g3(gs), axis=AX.X, op=ALU.max), R, R)
            op("dve", lambda e: e.tensor_tensor(out=g3(ing), in0=g3(gs), in1=gmax[:, :, None].to_broadcast([128, T_, 4]), op=ALU.is_lt), R, R)
            op("dve", lambda e: e.tensor_scalar(out=ing, in0=ing, scalar1=-1.0e9, scalar2=None, op0=ALU.mult), R, R)
            op("dve", lambda e: e.tensor_tensor(out=v4(tmpa), in0=v4(sel), in1=g3(ing)[:, :, :, None].to_broadcast([128, T_, 4, 4]), op=ALU.add), R, R)
            op("dve", lambda e: e.tensor_reduce(out=a1, in_=v3(tmpa), axis=AX.X, op=ALU.max), R, R)
            op("dve", lambda e: e.tensor_tensor(out=v3(ch), in0=v3(tmpa), in1=a1[:, :, None].to_broadcast([128, T_, 16]), op=ALU.is_ge), R, R)
            op("dve", lambda e: e.scalar_tensor_tensor(out=tmpb, in0=ch, scalar=-1.0e9, in1=tmpa, op0=ALU.mult, op1=ALU.add), R, R)
            op("dve", lambda e: e.tensor_reduce(out=a2, in_=v3(tmpb), axis=AX.X, op=ALU.max), R, R)
            op("dve", lambda e: e.tensor_tensor(out=v3(tmpa), in0=v3(tmpb), in1=a2[:, :, None].to_broadcast([128, T_, 16]), op=ALU.is_ge), R, R)
            op("dve", lambda e: e.tensor_tensor(out=ch, in0=ch, in1=tmpa, op=ALU.add), R, R)
            op("dve", lambda e: e.tensor_tensor(out=tmpb, in0=ch, in1=sc, op=ALU.mult), R, R)
            op("dve", lambda e: e.tensor_reduce(out=ssum, in_=v3(tmpb), axis=AX.X, op=ALU.add), R, R)
            op("dve", lambda e: e.reciprocal(out=ssum, in_=ssum), R, R)
            op("dve", lambda e: e.tensor_tensor(out=comb[:, :, :], in0=v3(tmpb), in1=ssum[:, :, None].to_broadcast([128, T_, 16]), op=ALU.mult), R, [b_comb])
            cx.barrier()
            cx.cur, cx.cur2 = base_mark
            cx.cur2 = Hoff + 65536
            comb2 = alloc("comb2", [128, NT, 16], F32); b_comb2 = Buf("comb2")
            op("dve", lambda e: e.tensor_copy(out=comb2[:], in_=comb[:]), [b_comb], [b_comb2])
            cx.barrier()

            if stop == 'ln1':
                cx.barrier()
                return nc, cx
            m0 = cx.mark()
            Wg_ = [alloc(f"weg{i}", [128, 8, 512], BF16) for i in range(2)]; b_weg = [Buf("weg0"), Buf("weg1")]
            Wu_ = [alloc(f"weu{i}", [128, 8, 512], BF16) for i in range(2)]; b_weu = [Buf("weu0"), Buf("weu1")]
            Wd_ = [alloc(f"wed{i}", [128, 4, D], BF16) for i in range(2)]; b_wed = [Buf("wed0"), Buf("wed1")]
            actT = alloc("actT", [128, 4, L], BF16); b_act = [[Buf(f"act{f}_{tc}") for tc in range(4)] for f in range(4)]
            sgb = [(alloc(f"sgm{i}", [128, 512], F32), Buf(f"sgm{i}")) for i in range(3)]
            sgr = Rot(sgb)
            ps_gu = Rot(PS[0:4]); ps_d = Rot(PS[4:8])
            for ex in range(16):
                wg, wu, wd = Wg_[ex % 2], Wu_[ex % 2], Wd_[ex % 2]
                bg, bu, bd = b_weg[ex % 2], b_weu[ex % 2], b_wed[ex % 2]
                load_w(wg[:], w_eg[layer, ex], bg)
                load_w(wu[:], w_eu[layer, ex], bu)
                dma("pool", wd[:], w_ed[layer, ex].rearrange("(c p) f -> p c f", p=128), writes=[bd])
                for tc in range(4):
                    for f in range(4):
                        pg_, pgb = ps_gu.get(); pu_, pub = ps_gu.get()
                        for c in range(8):
                            op("pe", lambda e, c=c: e.matmul(pg_[:, 0:512], lhsT=wg[:, c, f * 128:(f + 1) * 128], rhs=HT[:, c, tc * 512:(tc + 1) * 512],
                                                             start=(c == 0), stop=(c == 7)), [bg] + HTb[tc * 4:tc * 4 + 4], [pgb])
                        for c in range(8):
                            op("pe", lambda e, c=c: e.matmul(pu_[:, 0:512], lhsT=wu[:, c, f * 128:(f + 1) * 128], rhs=HT[:, c, tc * 512:(tc + 1) * 512],
                                                             start=(c == 0), stop=(c == 7)), [bu] + HTb[tc * 4:tc * 4 + 4], [pub])
                        sgt, sgtb = sgr.get()
                        op("act", lambda e: e.activation(out=sgt[:], in_=pg_[:, 0:512], func=AF.Silu), [pgb], [sgtb])
                        op("dve", lambda e: e.tensor_tensor(out=actT[:, f, tc * 512:(tc + 1) * 512], in0=pu_[:, 0:512], in1=sgt[:], op=ALU.mult),
                           [pub, sgtb], [b_act[f][tc]])
                    for t4 in range(4):
                        t = tc * 4 + t4
                        for half in range(2):
                            pd_, pdb = ps_d.get()
                            for f in range(4):
                                op("pe", lambda e, f=f: e.matmul(pd_[:, 0:512], lhsT=actT[:, f, t * 128:(t + 1) * 128], rhs=wd[:, f, half * 512:(half + 1) * 512],
                                                                 start=(f == 0), stop=(f == 3)), [b_act[f][tc], bd], [pdb])
                            op("dve", lambda e: e.scalar_tensor_tensor(out=H[:, t, half * 512:(half + 1) * 512], in0=pd_[:, 0:512], scalar=comb2[:, t, ex:ex + 1],
                                                                       in1=H[:, t, half * 512:(half + 1) * 512], op0=ALU.mult, op1=ALU.add),
                               [pdb, b_comb2, Hb[t]], [Hb[t]])
            cx.release(m0)

            if stop == 'moe':
                cx.barrier()
                return nc, cx
            m0 = cx.mark()
            gbc = alloc("gbc", [128, D], F32); bbc = alloc("bbc", [128, D], F32); b_par = Buf("lnpar")
            stat = alloc("stat", [128, NT, 20], F32); b_stat = [Buf(f"stat{t}") for t in range(NT)]
            dma("sp", gbc[:], ln2g[layer, :].partition_broadcast(128), writes=[b_par])
            dma("sp", bbc[:], ln2b[layer, :].partition_broadcast(128), writes=[b_par], owner=Buf("lnb"))
            pool_tr = Rot(PS[0:4])
            last = (layer == layers[-1])
            ln_stats(0, stat, b_stat)
            for t in range(NT):
                ln_apply(t, gbc, bbc, b_par, stat, b_stat, between=((lambda t=t: ln_stats(t + 1, stat, b_stat)) if t + 1 < NT else None))
                if last:
                    dma("sp", out[seq, t * 128:(t + 1) * 128, :], H[:, t, :], reads=[Hb[t]], owner=Hb[t])
                else:
                    make_HT(t, pool_tr)
            cx.release(m0)
            cx.cur, cx.cur2 = base_mark

    cx.barrier()
    return nc, cx


_CACHE = {}


def _prep_inputs(inputs):
    cst, cst8 = _consts()
    w_in_x = np.ascontiguousarray(np.asarray(inputs["w_in"], np.float32)[:, :, COLMAP])
    shared = {"w_in_x": w_in_x, "cst": cst, "cst8": cst8}
    for k in ("gla_gate_up", "gla_gate_bias", "gla_norm_gain", "w_out", "ln_mix_g", "ln_mix_b", "router_w", "router_bias",
              "w_expert_gate", "w_expert_up", "w_expert_down", "ln_ffn_g", "ln_ffn_b"):
        shared[k] = np.ascontiguousarray(np.asarray(inputs[k], np.float32))
    return shared


def kernel(**inputs):
    x = np.ascontiguousarray(np.asarray(inputs["x"], np.float32))
    positions = np.ascontiguousarray(np.asarray(inputs["positions"], np.int32))
    shared = _prep_inputs(inputs)
    ncores, nseq = 8, 2
    nc = bass.Bass("TRN2", target_bir_lowering=False)
    build(nc, nseq, [0, 1])
    in_maps = []
    for c in range(ncores):
        m = dict(shared)
        m["x"] = x[c * nseq:(c + 1) * nseq]
        m["positions"] = positions[c * nseq:(c + 1) * nseq]
        in_maps.append(m)
    res = run_bass_kernel_spmd(nc, in_maps, core_ids=list(range(ncores)))
    return np.concatenate([r["out"] for r in res.results], axis=0)
```

```python
import numpy as np
import concourse.bass as bass
import concourse.mybir as mybir
from concourse.bass_utils import run_bass_kernel_spmd

F32 = mybir.dt.float32
BF16 = mybir.dt.bfloat16
I32 = mybir.dt.int32
ALU = mybir.AluOpType
AF = mybir.ActivationFunctionType
AX = mybir.AxisListType

L = 2048
D = 1024
NT = 16
NEG = -30000.0
BIG = 1.0e30
ALPHA = 4.0 ** 0.25
N_BISECT = 18
SBUF_BASE = 16512
SBUF_BYTES = 229000

IN_WIDTHS = (256, 256, 512, 16, 512, 256, 256, 256, 256, 256, 256, 512, 64, 8)
IN_OFF = np.concatenate([[0], np.cumsum(IN_WIDTHS)]).astype(int)
(O_GQ, O_GK, O_GV, O_GR, O_GG, O_MQ, O_MK, O_MV, O_SQ, O_SK, O_SV, O_IQ, O_IK, O_IW) = [int(v) for v in IN_OFF[:-1]]


def _rot_cols(base, nheads):
    cols = []
    for h in range(nheads):
        for d in range(64):
            if d < 8:
                pd = d + 8
            elif d < 16:
                pd = d - 8
            else:
                pd = d
            cols.append(base + h * 64 + pd)
    return cols


def _build_colmap():
    cols = []
    off = {}

    def add(name, c):
        off[name] = len(cols)
        cols.extend(c)

    for p in range(2):
        add(f"g{p}", list(range(O_GQ + p * 128, O_GQ + p * 128 + 128)) + list(range(O_GK + p * 128, O_GK + p * 128 + 128))
            + list(range(O_GV + p * 256, O_GV + p * 256 + 256)) + list(range(O_GG + p * 256, O_GG + p * 256 + 256))
            + list(range(O_GR, O_GR + 16)))
    add("mq", list(range(O_MQ, O_MQ + 256)))
    add("mqr", _rot_cols(O_MQ, 4))
    add("mk", list(range(O_MK, O_MK + 256)))
    add("mkr", _rot_cols(O_MK, 4))
    add("mv", list(range(O_MV, O_MV + 256)))
    add("sq", list(range(O_SQ, O_SQ + 256)))
    add("sqr", _rot_cols(O_SQ, 4))
    add("sk", list(range(O_SK, O_SK + 256)))
    add("skr", _rot_cols(O_SK, 4))
    add("sv", list(range(O_SV, O_SV + 256)))
    add("iq", list(range(O_IQ, O_IQ + 512)))
    add("iqr", _rot_cols(O_IQ, 8))
    add("ik", list(range(O_IK, O_IK + 64)) * 2)
    add("ikr", _rot_cols(O_IK, 1) * 2)
    add("iw", list(range(O_IW, O_IW + 8)))
    return np.array(cols, dtype=np.int64), off


COLMAP, COFF = _build_colmap()
WX = len(COLMAP)


def _consts():
    c = np.zeros((128, 1024), np.float32)
    c[:, 0:128] = np.eye(128, dtype=np.float32)
    j = np.arange(128)[:, None]
    i = np.arange(128)[None, :]
    c[:, 128:256] = (i >= j).astype(np.float32)
    c[:, 256:384] = np.where(i <= j, 0.0, NEG)
    c[:, 384:512] = np.where(i <= j, 0.0, -BIG)
    p = np.arange(128)
    d = p % 64
    invf = np.where(d < 16, 500000.0 ** (-(d % 8) / 8.0), 0.0)
    c[:, 512] = invf / (2 * np.pi)
    c[:, 513] = np.where(d < 8, -invf, invf) / (2 * np.pi)
    c8 = np.zeros((8, 2048 + 512 + 8), np.float32)
    for k in range(8):
        for n in range(8):
            c8[k, 2560 + n] = 0.0 if n < k else (BIG if n == k else -BIG)
    for n in range(8):
        c8[n, n * 256:(n + 1) * 256] = 1.0
    for k in range(8):
        pr, hh = k // 2, k % 2
        c8[k, 2048 + pr * 128 + hh * 64: 2048 + pr * 128 + hh * 64 + 64] = 1.0
    return c, c8


class Buf:
    __slots__ = ("name", "writer", "readers", "dsem", "dcnt")

    def __init__(self, name):
        self.name = name
        self.writer = None
        self.readers = {}
        self.dsem = None
        self.dcnt = 0


class Ctx:
    def __init__(self, nc):
        self.nc = nc
        self.E = {"pe": nc.tensor, "act": nc.scalar, "dve": nc.vector, "pool": nc.gpsimd, "sp": nc.sync}
        self.sem = {k: nc.alloc_semaphore("e_" + k) for k in self.E}
        self.cnt = {k: 0 for k in self.E}
        self.seen = {k: {} for k in self.E}
        self.dbufs = []
        self.slots = {}
        self.nalloc = 0
        self.cur = SBUF_BASE
        self.cur2 = 0
        self.lim2 = 0
        self.peak = 0

    def alloc(self, name, shape, dtype, reg=0):
        isz = 4 if dtype in (F32, I32) else 2
        n = isz
        for s in shape[1:]:
            n *= s
        n = (n + 63) // 64 * 64
        if reg == 0:
            off = self.cur
            self.cur += n
            self.peak = max(self.peak, self.cur)
            assert self.cur <= SBUF_BYTES, (name, self.cur)
        else:
            off = self.cur2
            self.cur2 += n
            assert self.cur2 <= self.lim2, (name, self.cur2, self.lim2)
        self.nalloc += 1
        return self.nc.alloc_sbuf_tensor_at(f"{name}_{self.nalloc}", list(shape), dtype, offset=off)

    def mark(self):
        return (self.cur, self.cur2)

    def release(self, m):
        self.barrier()
        self.cur, self.cur2 = m

    def _wait(self, eng, dep):
        kind, obj, val = dep
        key = obj if kind == "e" else id(obj)
        if self.seen[eng].get(key, 0) >= val:
            return
        sem = self.sem[obj] if kind == "e" else obj.dsem
        self.E[eng].wait_ge(sem, val)
        self.seen[eng][key] = val

    def _deps(self, eng, reads, writes):
        for b in reads:
            if b.writer is not None:
                self._wait(eng, b.writer)
        for b in writes:
            w = b.writer
            if w is not None and not (w[0] == "e" and w[1] == eng and eng == "pe"):
                self._wait(eng, w)
            for r in b.readers.values():
                if not (r[0] == "e" and r[1] == eng and eng == "pe"):
                    self._wait(eng, r)

    def op(self, eng, fn, reads=(), writes=()):
        self._deps(eng, reads, writes)
        inst = fn(self.E[eng])
        inst.then_inc(self.sem[eng], 1)
        self.cnt[eng] += 1
        tag = ("e", eng, self.cnt[eng])
        for b in reads:
            b.readers[eng] = tag
        for b in writes:
            b.writer = tag
            b.readers = {}

    def dma(self, q, out, in_, reads=(), writes=(), owner=None):
        self._deps(q, reads, writes)
        if owner is None:
            owner = writes[0] if writes else reads[0]
        slot = self.slots.get(owner.name)
        if slot is None:
            slot = Buf("slot_" + owner.name)
            slot.dsem = self.nc.alloc_semaphore("d_%d" % len(self.slots))
            self.slots[owner.name] = slot
            self.dbufs.append(slot)
        self.E[q].dma_start(out=out, in_=in_).then_inc(slot.dsem, 16)
        slot.dcnt += 16
        tag = ("d", slot, slot.dcnt)
        for b in reads:
            b.readers[("d", id(slot))] = tag
        for b in writes:
            b.writer = tag
            b.readers = {}

    def barrier(self):
        for e in self.E:
            for e2 in self.E:
                if e2 != e and self.cnt[e2] > 0:
                    self._wait(e, ("e", e2, self.cnt[e2]))
            for b in self.dbufs:
                if b.dcnt:
                    self._wait(e, ("d", b, b.dcnt))


class Rot:
    def __init__(self, items):
        self.items = items
        self.i = 0

    def get(self):
        it = self.items[self.i % len(self.items)]
        self.i += 1
        return it


class StopBuild(Exception):
    pass


def build(nc, nseq, layers, debug=False, stop=None):
    try:
        return _build(nc, nseq, layers, debug, stop)
    except StopBuild as ex:
        return nc, ex.args[0]


def _build(nc, nseq, layers, debug=False, stop=None):
    cx = Ctx(nc)
    op, dma, alloc = cx.op, cx.dma, cx.alloc

    def chk(name):
        if stop == name:
            cx.barrier()
            raise StopBuild(cx)

    x = nc.dram_tensor("x", [nseq, L, D], F32, kind="ExternalInput").ap()
    pos = nc.dram_tensor("positions", [nseq, L], I32, kind="ExternalInput").ap()
    w_in = nc.dram_tensor("w_in_x", [2, D, WX], F32, kind="ExternalInput").ap()
    g_up = nc.dram_tensor("gla_gate_up", [2, 16, 256], F32, kind="ExternalInput").ap()
    g_bias = nc.dram_tensor("gla_gate_bias", [2, 256], F32, kind="ExternalInput").ap()
    g_gain = nc.dram_tensor("gla_norm_gain", [2, 128], F32, kind="ExternalInput").ap()
    w_out = nc.dram_tensor("w_out", [2, D, D], F32, kind="ExternalInput").ap()
    ln1g = nc.dram_tensor("ln_mix_g", [2, D], F32, kind="ExternalInput").ap()
    ln1b = nc.dram_tensor("ln_mix_b", [2, D], F32, kind="ExternalInput").ap()
    r_w = nc.dram_tensor("router_w", [D, 16], F32, kind="ExternalInput").ap()
    r_b = nc.dram_tensor("router_bias", [16], F32, kind="ExternalInput").ap()
    w_eg = nc.dram_tensor("w_expert_gate", [2, 16, D, 512], F32, kind="ExternalInput").ap()
    w_eu = nc.dram_tensor("w_expert_up", [2, 16, D, 512], F32, kind="ExternalInput").ap()
    w_ed = nc.dram_tensor("w_expert_down", [2, 16, 512, D], F32, kind="ExternalInput").ap()
    ln2g = nc.dram_tensor("ln_ffn_g", [2, D], F32, kind="ExternalInput").ap()
    ln2b = nc.dram_tensor("ln_ffn_b", [2, D], F32, kind="ExternalInput").ap()
    cst_d = nc.dram_tensor("cst", [128, 1024], F32, kind="ExternalInput").ap()
    cst8_d = nc.dram_tensor("cst8", [8, 2568], F32, kind="ExternalInput").ap()
    out = nc.dram_tensor("out", [nseq, L, D], F32, kind="ExternalOutput").ap()
    hs = nc.dram_tensor("hspill", [L, D], F32, kind="Internal").ap()
    dbg = nc.dram_tensor("dbg", [128, 8, L], BF16, kind="ExternalOutput").ap() if debug else None

    cst = alloc("cst", [128, 1024], F32)
    c8b = alloc("c8b", [8, 2568], BF16)
    identB = alloc("identB", [128, 128], BF16)
    trinegB = alloc("trinegB", [128, 128], BF16)
    negidentB = alloc("negidentB", [128, 128], BF16)
    epsln = alloc("epsln", [128, 2], F32)
    zerosB = alloc("zerosB", [1, 512], BF16)
    HT = alloc("HT", [128, 8, L], BF16)
    Hoff = cx.cur
    H = alloc("H", [128, NT, D], F32)
    cosT = alloc("cosT", [128, L], F32)
    sinT = alloc("sinT", [128, L], F32)
    b_tab = Buf("tab")
    base_mark = cx.mark()

    b_cst = Buf("cst")
    Hb = [Buf(f"H{t}") for t in range(NT)]
    HTb = [Buf(f"HT{t}") for t in range(NT)]
    hsb = [Buf(f"hs{t}") for t in range(NT)]
    outb = Buf("out")

    identF = cst[:, 0:128]
    tri01 = cst[:, 128:256]
    trinegbig = cst[:, 384:512]
    invf = cst[:, 512:513]
    sinvf = cst[:, 513:514]
    onehotK = c8b[0:8, 0:2048]
    selw = c8b[0:8, 2048:2560]
    bpen = c8b[0:8, 2560:2568]

    PS = []
    for i in range(8):
        t = nc.alloc_psum_tensor(f"ps{i}", [128, 512], F32)
        PS.append((t, Buf(f"ps{i}")))

    dma("sp", cst[:], cst_d[:, :], writes=[b_cst])
    dma("pool", c8b[:], cst8_d[:, :], writes=[b_cst], owner=Buf("c8"))
    op("dve", lambda e: e.tensor_copy(out=identB[:], in_=cst[:, 0:128]), [b_cst], [b_cst])
    op("dve", lambda e: e.tensor_copy(out=trinegB[:], in_=cst[:, 256:384]), [b_cst], [b_cst])
    op("dve", lambda e: e.tensor_scalar(out=negidentB[:], in0=cst[:, 0:128], scalar1=-1.0, scalar2=None, op0=ALU.mult), [b_cst], [b_cst])
    op("dve", lambda e: e.memset(epsln[:, 0:1], 1e-5), [], [b_cst])
    op("dve", lambda e: e.memset(epsln[:, 1:2], 1e-6), [], [b_cst])
    op("dve", lambda e: e.memset(zerosB[:], 0.0), [], [b_cst])
    cx.barrier()

    def load_w(dst, src_rows_cols, buf, q="pool"):
        dma(q, dst, src_rows_cols.rearrange("(c p) f -> p c f", p=128), writes=[buf])

    def make_HT(t, pool):
        for half in range(2):
            pt, pb = pool.get()
            for c4 in range(4):
                c = half * 4 + c4
                op("pe", lambda e, c=c, c4=c4: e.transpose(out=pt[:, c4 * 128:(c4 + 1) * 128],
                                                           in_=H[:, t, c * 128:(c + 1) * 128], identity=identF),
                   [Hb[t], b_cst], [pb])
            op("act", lambda e: e.activation(out=HT[:, half * 4:(half + 1) * 4, t * 128:(t + 1) * 128],
                                             in_=pt[:, :].rearrange("p (a b) -> p a b", a=4), func=AF.Copy),
               [pb], [HTb[t]])

    def ln_stats(t, stat, b_stat):
        st = stat[:, t, :]
        bs_ = [b_stat[t]]
        op("dve", lambda e: e.bn_stats(out=st[:, 0:6], in_=H[:, t, 0:512]), [Hb[t]], bs_)
        op("dve", lambda e: e.bn_stats(out=st[:, 6:12], in_=H[:, t, 512:1024]), [Hb[t]], bs_)
        op("dve", lambda e: e.bn_aggr(out=st[:, 12:14], in_=st[:, 0:12]), bs_, bs_)
        op("act", lambda e: e.activation(out=st[:, 14:15], in_=st[:, 13:14], func=AF.Sqrt, bias=epsln[:, 0:1], scale=1.0),
           bs_ + [b_cst], bs_)
        op("dve", lambda e: e.reciprocal(out=st[:, 15:16], in_=st[:, 14:15]), bs_, bs_)
        op("dve", lambda e: e.scalar_tensor_tensor(out=st[:, 16:17], in0=st[:, 12:13], scalar=-1.0, in1=st[:, 15:16], op0=ALU.mult, op1=ALU.mult), bs_, bs_)

    def ln_apply(t, gbc, bbc, b_par, stat, b_stat, between=None):
        hv = H[:, t, :]
        st = stat[:, t, :]
        op("act", lambda e: e.activation(out=hv, in_=hv, func=AF.Identity, bias=st[:, 16:17], scale=st[:, 15:16]), [Hb[t], b_stat[t]], [Hb[t]])
        if between is not None:
            between()
        op("dve", lambda e: e.tensor_tensor(out=hv, in0=hv, in1=gbc[:], op=ALU.mult), [Hb[t], b_par], [Hb[t]])
        op("dve", lambda e: e.tensor_tensor(out=hv, in0=hv, in1=bbc[:], op=ALU.add), [Hb[t], b_par], [Hb[t]])

    def rope_tables(seq, cosT, sinT, b_tab):
        m = cx.mark()
        posf = alloc("posf", [128, L], F32)
        tmp = alloc("rtmp", [128, L], F32)
        tmi = alloc("rtmi", [128, L], I32)
        b_t = Buf("ropetmp")
        dma("pool", posf[:], pos[seq, :].partition_broadcast(128), writes=[b_t])
        for (tab, fcol, shift) in ((sinT, sinvf, 0.0), (cosT, invf, 0.25)):
            op("dve", lambda e, fcol=fcol, shift=shift: e.tensor_scalar(out=tmp[:], in0=posf[:], scalar1=fcol, scalar2=shift,
                                                                         op0=ALU.mult, op1=ALU.add), [b_t, b_cst], [b_t])
            op("dve", lambda e: e.tensor_copy(out=tmi[:], in_=tmp[:]), [b_t], [b_t])
            op("dve", lambda e: e.tensor_copy(out=tab[:], in_=tmi[:]), [b_t], [b_tab])
            op("dve", lambda e: e.tensor_tensor(out=tmp[:], in0=tmp[:], in1=tab[:], op=ALU.subtract), [b_t, b_tab], [b_t])
            op("dve", lambda e: e.tensor_scalar(out=tab[:], in0=tmp[:], scalar1=0.5, scalar2=None, op0=ALU.is_gt), [b_t], [b_tab])
            op("dve", lambda e: e.tensor_tensor(out=tmp[:], in0=tmp[:], in1=tab[:], op=ALU.subtract), [b_t, b_tab], [b_t])
            op("dve", lambda e: e.tensor_scalar(out=tab[:], in0=tmp[:], scalar1=-0.5, scalar2=None, op0=ALU.is_lt), [b_t], [b_tab])
            op("dve", lambda e: e.tensor_tensor(out=tmp[:], in0=tmp[:], in1=tab[:], op=ALU.add), [b_t, b_tab], [b_t])
            op("act", lambda e: e.activation(out=tab[:], in_=tmp[:], func=AF.Sin, scale=2 * np.pi), [b_t], [b_tab])
        cx.release(m)

    def proj_fm(wt, col0, M, tc, pt, pb, wbuf, pbase=0):
        for c in range(8):
            op("pe", lambda e, c=c: e.matmul(pt[pbase:pbase + M, 0:512], lhsT=wt[:, c, col0:col0 + M],
                                             rhs=HT[:, c, tc * 512:(tc + 1) * 512], start=(c == 0), stop=(c == 7)),
               [wbuf] + HTb[tc * 4:(tc + 1) * 4], [pb])

    def proj_tm(wt, col0, N, t, pt, pb, wbuf, ocol=0):
        for c in range(8):
            op("pe", lambda e, c=c: e.matmul(pt[:, ocol:ocol + N], lhsT=HT[:, c, t * 128:(t + 1) * 128],
                                             rhs=wt[:, c, col0:col0 + N], start=(c == 0), stop=(c == 7)),
               [wbuf, HTb[t]], [pb])

    def roped_proj(wt, wbuf, col, colr, dstT, b_dst, cosT, sinT, b_tab, tmps, pspool, ksum=None):
        ntile = dstT.shape[1]
        (t1, t2, b_tmp) = tmps
        for ti in range(ntile):
            for tc in range(4):
                py, pyb = pspool.get()
                pr, prb = pspool.get()
                proj_fm(wt, col + ti * 128, 128, tc, py, pyb, wbuf)
                proj_fm(wt, colr + ti * 128, 128, tc, pr, prb, wbuf)
                sl = slice(tc * 512, (tc + 1) * 512)
                op("dve", lambda e: e.tensor_tensor(out=t1[:], in0=pr[:, :], in1=sinT[:, sl], op=ALU.mult), [prb, b_tab], [b_tmp])
                op("dve", lambda e: e.tensor_tensor(out=t2[:], in0=py[:, :], in1=cosT[:, sl], op=ALU.mult), [pyb, b_tab], [b_tmp])
                if ksum is not None:
                    op("pool", lambda e: e.tensor_tensor(out=t2[:], in0=t1[:], in1=t2[:], op=ALU.add), [b_tmp], [b_tmp])
                    op("act", lambda e: e.activation(out=dstT[:, ti, sl], in_=t2[:], func=AF.Copy), [b_tmp], [b_dst])
                    (km, b_km) = ksum
                    op("dve", lambda e: e.tensor_reduce(out=km[:, ti, tc * 2:(tc + 1) * 2], in_=t2[:, :].rearrange("p (a b) -> p a b", a=2),
                                                        axis=AX.X, op=ALU.add), [b_tmp], [b_km])
                else:
                    op("pool", lambda e: e.tensor_tensor(out=dstT[:, ti, sl], in0=t1[:], in1=t2[:], op=ALU.add), [b_tmp], [b_dst])

    def v_aug_proj(wt, wbuf, col, vaug, b_v, pspool):
        op("dve", lambda e: e.memset(vaug[:, :, :, 64:65], 1.0), [], [b_v])
        for t in range(NT):
            pt, pb = pspool.get()
            proj_tm(wt, col, 256, t, pt, pb, wbuf)
            op("act", lambda e: e.activation(out=vaug[:, t, :, 0:64], in_=pt[:, 0:256].rearrange("p (a b) -> p a b", a=4), func=AF.Copy),
               [pb], [b_v])

    def attention(qT, kT, b_qk, vaug, b_v, maskfn, mix_chunk0, pools, otm, b_otm, pTs, rs, b_rs, mixT, mixb, qc_hook=None):
        (ps_s, ps_acc, ps_tr) = pools

        def att_gen(qc):
                for h in range(4):
                    ptile, bp = h // 2, (h % 2) * 64
                    acc, accb = ps_acc.get()
                    op("pe", lambda e: e.matmul(acc[:, 0:260], lhsT=zerosB[0:1, 0:128], rhs=zerosB[0:1, 0:260], start=True, stop=False,
                                                skip_group_check=True), [b_cst], [accb])
                    prev = None

                    def emit_pv(args):
                        (j_, t0_, pT_, pTb_) = args
                        for t in range(t0_, 4 * qc + 4):
                            i = t - 4 * qc
                            op("pe", lambda e, t=t, i=i: e.matmul(acc[:, i * 65:(i + 1) * 65], lhsT=pT_[:, (t - t0_) * 128:(t - t0_ + 1) * 128],
                                                                  rhs=vaug[:, j_, h, :], start=False, stop=(j_ == t), skip_group_check=True),
                               [pTb_, b_v], [accb])

                    for j in range(4 * qc + 4):
                        t0 = max(j, 4 * qc)
                        q0, q1 = t0 * 128, (4 * qc + 4) * 128
                        wq = q1 - q0
                        st, sb = ps_s.get()
                        op("pe", lambda e: e.matmul(st[:, 0:wq], lhsT=kT[bp:bp + 64, ptile, j * 128:(j + 1) * 128],
                                                    rhs=qT[bp:bp + 64, ptile, q0:q1], start=True, stop=False, skip_group_check=True),
                           [b_qk], [sb])
                        maskfn(h, qc, j, t0, st, sb, wq)
                        pT, pTb = pTs.get()
                        op("act", lambda e: e.activation(out=pT[:, 0:wq], in_=st[:, 0:wq], func=AF.Exp, scale=0.125), [sb], [pTb])
                        if prev is not None:
                            emit_pv(prev)
                        prev = (j, t0, pT, pTb)
                        yield
                    emit_pv(prev)
                    acc3 = acc[:, 0:260].rearrange("p (a b) -> p a b", a=4)
                    op("dve", lambda e: e.reciprocal(out=rs[:, :], in_=acc3[:, :, 64]), [accb], [b_rs])
                    for i in range(4):
                        op("dve", lambda e, i=i: e.tensor_scalar(out=otm[:, i, h * 64:(h + 1) * 64], in0=acc3[:, i, 0:64],
                                                                 scalar1=rs[:, i:i + 1], scalar2=None, op0=ALU.mult), [accb, b_rs], [b_otm])
                    yield
                for i in range(4):
                    t = 4 * qc + i
                    pt, pb = ps_tr.get()
                    for f in range(2):
                        op("pe", lambda e, f=f: e.transpose(out=pt[:, f * 128:(f + 1) * 128], in_=otm[:, i, f * 128:(f + 1) * 128], identity=identF),
                           [b_otm, b_cst], [pb])
                    op("act", lambda e: e.activation(out=mixT[:, mix_chunk0:mix_chunk0 + 2, t * 128:(t + 1) * 128],
                                                     in_=pt[:, 0:256].rearrange("p (a b) -> p a b", a=2), func=AF.Copy), [pb], [mixb[t]])
                    yield

        def drain(g):
            for _ in g:
                pass

        def merge(ga, na, gb, nb):
            ia = ib = 0
            da = db = False
            while not (da and db):
                if not da and (db or ia * nb <= ib * na):
                    try:
                        next(ga)
                    except StopIteration:
                        da = True
                    ia += 1
                else:
                    try:
                        next(gb)
                    except StopIteration:
                        db = True
                    ib += 1

        if qc_hook is not None:
            drain(qc_hook(0))
        for qc in range(4):
            ga = att_gen(qc)
            if qc_hook is not None and qc + 1 < 4:
                n_att = 4 * (4 * qc + 4) + 8
                n_hook = 4 + sum(((4 * (qc + 1) + i + 1) * 128 + 511) // 512 for i in range(4)) + N_BISECT + 2
                merge(ga, n_att, qc_hook(qc + 1), n_hook)
            else:
                drain(ga)

    for seq in range(nseq):
        pool_tr = Rot(PS[0:4])
        for t in range(NT):
            dma("sp", H[:, t, :], x[seq, t * 128:(t + 1) * 128, :], writes=[Hb[t]])
        for t in range(NT):
            make_HT(t, pool_tr)
        rope_tables(seq, cosT, sinT, b_tab)

        for layer in layers:
            for t in range(NT):
                dma("sp", hs[t * 128:(t + 1) * 128, :], H[:, t, :], reads=[Hb[t]], writes=[hsb[t]], owner=Hb[t])
            cx.barrier()
            cx.cur, cx.cur2 = base_mark
            cx.cur2, cx.lim2 = Hoff, Hoff + 65536
            mixT = alloc("mixT", [128, 8, L], BF16)
            mixb = [Buf(f"mix{t}") for t in range(NT)]

            if stop == 'ht':
                cx.barrier()
                return nc, cx
            for p in range(2):
                m0 = cx.mark()
                Wg = alloc("Wg", [128, 8, 784], BF16); b_Wg = Buf("Wg")
                gu = alloc("gu", [16, 128], BF16)
                nbias = alloc("nbias", [128, 1], F32)
                gain = alloc("gain", [128, 2, 128], F32)
                b_gp = Buf("gpar")
                rankT = alloc("rankT", [16, L], BF16); b_rank = Buf("rank")
                la = alloc("la", [128, L], F32, 1); b_la = Buf("la")
                bc = alloc("bc", [128, L], F32, 1); b_bc = Buf("bc")
                nbl = alloc("nbl", [128, 16], F32); dec = alloc("dec", [128, 16], F32); b_nbl = Buf("nbl")
                qdT = alloc("qdT", [128, L], BF16, 1); kiT = alloc("kiT", [128, L], BF16, 1); b_qk = Buf("gqk")
                kd = alloc("kd", [128, NT, 128], BF16, 1); b_kd = Buf("kd")
                vtm = alloc("vtm", [128, NT, 256], BF16, 1); b_v = Buf("gv")
                Sall = alloc("Sall", [128, NT, 128], BF16, 1); b_Sall = Buf("Sall")
                Sst = alloc("Sst", [128, 128], F32); b_S = Buf("S")
                e1 = alloc("e1", [128, 512], F32); e2 = alloc("e2", [128, 512], F32); e3 = alloc("e3", [128, 512], F32)
                b_e = [Buf("e1"), Buf("e2"), Buf("e3")]
                kdT = alloc("kdT", [128, 512], F32); b_kdT = Buf("kdT")
                AT = [alloc("AT0", [128, 128], BF16), alloc("AT1", [128, 128], BF16)]; b_AT = [Buf("AT0"), Buf("AT1")]
                sg = alloc("sg", [128, 256], F32); b_sg = Buf("sg")
                otm = alloc("otmg", [128, 256], F32); b_otm = Buf("otmg")
                sq = alloc("sq", [128, 256], F32); ssq = alloc("ssq", [128, 4], F32); b_ssq = Buf("ssq")

                load_w(Wg[:], w_in[layer][:, COFF[f"g{p}"]:COFF[f"g{p}"] + 784], b_Wg)
                dma("pool", gu[:], g_up[layer][:, p * 128:(p + 1) * 128], writes=[b_gp])
                dma("sp", nbias[:], g_bias[layer, p * 128:(p + 1) * 128].rearrange("(p o) -> p o", o=1), writes=[b_gp], owner=Buf("nb"))
                for hh in range(2):
                    dma("sp", gain[:, hh, :], g_gain[layer, :].partition_broadcast(128), writes=[b_gp], owner=Buf("gn"))
                op("dve", lambda e: e.tensor_scalar(out=nbias[:], in0=nbias[:], scalar1=-1.0, scalar2=None, op0=ALU.mult), [b_gp], [b_gp])
                chk('g1')
                pp = Rot(PS[0:4])
                for tc in range(4):
                    pt, pb = pp.get()
                    for c in range(8):
                        op("pe", lambda e, c=c: e.matmul(pt[0:16, 0:512], lhsT=Wg[:, c, 768:784], rhs=HT[:, c, tc * 512:(tc + 1) * 512],
                                                         start=(c == 0), stop=(c == 7)), [b_Wg] + HTb[tc * 4:tc * 4 + 4], [pb])
                    op("act", lambda e: e.activation(out=rankT[:, tc * 512:(tc + 1) * 512], in_=pt[0:16, 0:512], func=AF.Copy), [pb], [b_rank])
                for tc in range(4):
                    pt, pb = pp.get()
                    sl = slice(tc * 512, (tc + 1) * 512)
                    op("pe", lambda e: e.matmul(pt[:, 0:512], lhsT=gu[0:16, :], rhs=rankT[0:16, sl], start=True, stop=True), [b_gp, b_rank], [pb])
                    op("act", lambda e: e.activation(out=la[:, sl], in_=pt[:, 0:512], func=AF.Exp, bias=nbias[:, 0:1], scale=-1.0), [pb, b_gp], [b_la])
                op("act", lambda e: e.activation(out=la[:], in_=la[:], func=AF.Ln, bias=1.0, scale=1.0), [b_la], [b_la])
                chk('g2')
                for n in range(NT):
                    sl = slice(n * 128, (n + 1) * 128)
                    op("dve", lambda e: e.tensor_tensor_scan(out=bc[:, sl], data0=la[:, sl], data1=cst[:, 640:768], initial=0.0,
                                                             op0=ALU.add, op1=ALU.add), [b_la, b_cst], [b_bc])
                chk('g3')
                cx_chk = None
                op("dve", lambda e: e.tensor_scalar(out=nbl[:], in0=bc[:, :].rearrange("p (n c) -> p n c", c=128)[:, :, 127],
                                                    scalar1=-1.0 / 16.0, scalar2=None, op0=ALU.mult), [b_bc], [b_nbl])
                op("act", lambda e: e.activation(out=dec[:], in_=nbl[:], func=AF.Exp), [b_nbl], [b_nbl])
                chk('g3b')
                ptr = Rot(PS[4:6])
                for tc in range(4):
                    sl = slice(tc * 512, (tc + 1) * 512)
                    pq, pqb = pp.get()
                    pk, pkb = pp.get()
                    proj_fm(Wg, 0, 128, tc, pq, pqb, b_Wg)
                    proj_fm(Wg, 128, 128, tc, pk, pkb, b_Wg)
                    op("act", lambda e: e.activation(out=e1[:], in_=bc[:, sl], func=AF.Exp, scale=-1.0 / 16.0), [b_bc], [b_e[0]])
                    op("dve", lambda e: e.scalar_tensor_tensor(out=qdT[:, sl], in0=pq[:, :], scalar=0.125, in1=e1[:], op0=ALU.mult, op1=ALU.mult),
                       [pqb, b_e[0]], [b_qk])
                    op("act", lambda e: e.activation(out=e2[:], in_=bc[:, sl], func=AF.Exp, scale=1.0 / 16.0), [b_bc], [b_e[1]])
                    op("dve", lambda e: e.tensor_tensor(out=kiT[:, sl], in0=pk[:, :], in1=e2[:], op=ALU.mult), [pkb, b_e[1]], [b_qk])
                    for n4 in range(4):
                        n = tc * 4 + n4
                        op("act", lambda e, n=n, n4=n4: e.activation(out=e3[:, n4 * 128:(n4 + 1) * 128], in_=bc[:, n * 128:(n + 1) * 128], func=AF.Exp,
                                                                     bias=nbl[:, n:n + 1], scale=1.0 / 16.0), [b_bc, b_nbl], [b_e[2]])
                    op("dve", lambda e: e.tensor_tensor(out=kdT[:], in0=pk[:, :], in1=e3[:], op=ALU.mult), [pkb, b_e[2]], [b_kdT])
                    pt, pb = ptr.get()
                    for n4 in range(4):
                        op("pe", lambda e, n4=n4: e.transpose(out=pt[:, n4 * 128:(n4 + 1) * 128], in_=kdT[:, n4 * 128:(n4 + 1) * 128], identity=identF),
                           [b_kdT, b_cst], [pb])
                    op("act", lambda e: e.activation(out=kd[:, tc * 4:(tc + 1) * 4, :], in_=pt[:, :].rearrange("p (a b) -> p a b", a=4), func=AF.Copy),
                       [pb], [b_kd])
                chk('g4')
                for t in range(NT):
                    pt, pb = pp.get()
                    proj_tm(Wg, 256, 256, t, pt, pb, b_Wg)
                    op("act", lambda e: e.activation(out=vtm[:, t, :], in_=pt[:, 0:256], func=AF.Copy), [pb], [b_v])
                chk('g5')
                op("dve", lambda e: e.memset(Sst[:], 0.0), [], [b_S])
                op("dve", lambda e: e.memset(Sall[:, 0, :], 0.0), [], [b_Sall])
                for n in range(NT - 1):
                    pt, pb = pp.get()
                    for hh in range(2):
                        op("pe", lambda e, hh=hh: e.matmul(pt[hh * 64:(hh + 1) * 64, 0:128], lhsT=kd[:, n, hh * 64:(hh + 1) * 64],
                                                           rhs=vtm[:, n, hh * 128:(hh + 1) * 128], start=True, stop=True), [b_kd, b_v], [pb])
                    op("dve", lambda e: e.scalar_tensor_tensor(out=Sst[:], in0=Sst[:], scalar=dec[:, n:n + 1], in1=pt[:, 0:128],
                                                               op0=ALU.mult, op1=ALU.add), [b_S, b_nbl, pb], [b_S])
                    op("act", lambda e: e.activation(out=Sall[:, n + 1, :], in_=Sst[:], func=AF.Copy), [b_S], [b_Sall])
                chk('g6')
                ps_o = Rot(PS[4:6]); ps_g = Rot(PS[6:8])
                o_all = alloc("o_all", [128, NT, 256], F32, 1); b_oall = [Buf(f"oall{n}") for n in range(NT)]
                sg_all = alloc("sg_all", [128, NT, 256], F32); b_sgall = [Buf(f"sgall{n}") for n in range(NT)]
                ssq_all = alloc("ssq_all", [128, NT, 2], F32); rstd_all = alloc("rstd_all", [128, NT, 2], F32); b_ssqa = Buf("ssqa")
                otms = [(alloc(f"otmg{i}", [128, 256], F32), Buf(f"otmg{i}")) for i in range(2)]
                for n in range(NT):
                    sl = slice(n * 128, (n + 1) * 128)
                    po, pob = ps_o.get()
                    for hh in range(2):
                        bp = hh * 64
                        pa, pab = pp.get()
                        op("pe", lambda e: e.matmul(pa[:, 0:128], lhsT=kiT[bp:bp + 64, sl], rhs=qdT[bp:bp + 64, sl], start=True, stop=True), [b_qk], [pab])
                        op("dve", lambda e: e.tensor_tensor(out=AT[hh][:], in0=pa[:, 0:128], in1=tri01, op=ALU.mult), [pab, b_cst], [b_AT[hh]])
                        op("pe", lambda e: e.matmul(po[:, hh * 128:(hh + 1) * 128], lhsT=AT[hh][:], rhs=vtm[:, n, hh * 128:(hh + 1) * 128],
                                                    start=True, stop=False, skip_group_check=True), [b_AT[hh], b_v], [pob])
                        op("pe", lambda e: e.matmul(po[:, hh * 128:(hh + 1) * 128], lhsT=qdT[bp:bp + 64, sl], rhs=Sall[bp:bp + 64, n, :],
                                                    start=False, stop=True, skip_group_check=True), [b_qk, b_Sall], [pob])
                    for hh in range(2):
                        op("act", lambda e, hh=hh: e.activation(out=sq[:, hh * 128:(hh + 1) * 128], in_=po[:, hh * 128:(hh + 1) * 128], func=AF.Square,
                                                                accum_out=ssq_all[:, n, hh:hh + 1]), [pob], [b_ssqa])
                    op("act", lambda e: e.activation(out=o_all[:, n, :], in_=po[:, 0:256], func=AF.Copy), [pob], [b_oall[n]])
                op("act", lambda e: e.activation(out=rstd_all[:, :, :], in_=ssq_all[:, :, :], func=AF.Sqrt, bias=epsln[:, 1:2], scale=1.0 / 128.0), [b_ssqa, b_cst], [b_ssqa])
                op("dve", lambda e: e.reciprocal(out=rstd_all[:, :, :], in_=rstd_all[:, :, :]), [b_ssqa], [b_ssqa])
                for n in range(NT):
                    pg, pgb = ps_g.get()
                    proj_tm(Wg, 512, 256, n, pg, pgb, b_Wg)
                    op("act", lambda e: e.activation(out=sg_all[:, n, :], in_=pg[:, 0:256], func=AF.Silu), [pgb], [b_sgall[n]])
                    op("pool", lambda e: e.tensor_tensor(out=sg_all[:, n, :], in0=sg_all[:, n, :], in1=gain[:, :, :].rearrange("p a b -> p (a b)"), op=ALU.mult),
                       [b_sgall[n], b_gp], [b_sgall[n]])
                for n in range(NT):
                    sl = slice(n * 128, (n + 1) * 128)
                    otm, b_otm = otms[n % 2]
                    for hh in range(2):
                        op("dve", lambda e, hh=hh: e.scalar_tensor_tensor(out=otm[:, hh * 128:(hh + 1) * 128], in0=o_all[:, n, hh * 128:(hh + 1) * 128],
                                                                          scalar=rstd_all[:, n, hh:hh + 1], in1=sg_all[:, n, hh * 128:(hh + 1) * 128],
                                                                          op0=ALU.mult, op1=ALU.mult), [b_oall[n], b_ssqa, b_sgall[n]], [b_otm])
                    pt, pb = ps_o.get()
                    for hh in range(2):
                        op("pe", lambda e, hh=hh: e.transpose(out=pt[:, hh * 128:(hh + 1) * 128], in_=otm[:, hh * 128:(hh + 1) * 128], identity=identF),
                           [b_otm, b_cst], [pb])
                    op("act", lambda e: e.activation(out=mixT[:, 2 * p:2 * p + 2, sl], in_=pt[:, 0:256].rearrange("p (a b) -> p a b", a=2), func=AF.Copy),
                       [pb], [mixb[n]])
                cx.release(m0)
                chk('p0')

            if stop == 'gla':
                cx.barrier()
                return nc, cx
            def attn_common():
                qT = alloc("qT", [128, 2, L], BF16, 1); kT = alloc("kT", [128, 2, L], BF16, 1)
                vaug = alloc("vaug", [128, NT, 4, 65], BF16, 1)
                return qT, kT, vaug, Buf("aqk"), Buf("av")

            m0 = cx.mark()
            qT, kT, vaug, b_qk, b_v = attn_common()
            kmT = alloc("kmT", [128, 2, 8], F32); kmB = alloc("kmB", [128, 2, 8], BF16); b_km = Buf("km")
            mbT = alloc("mbT", [8, 4, L], BF16); b_mb = Buf("mbT")
            m1 = cx.mark()
            chk('m1')
            Wa = alloc("Wa", [128, 8, 512], BF16); b_Wa = Buf("Wa")
            Wb = alloc("Wb", [128, 8, 512], BF16); b_Wb = Buf("Wb")
            t1 = alloc("t1", [128, 512], F32); t2 = alloc("t2", [128, 512], F32); b_tmp = Buf("rt")
            pp = Rot(PS[0:6])
            load_w(Wa[:], w_in[layer][:, COFF["mq"]:COFF["mq"] + 512], b_Wa)
            load_w(Wb[:], w_in[layer][:, COFF["mk"]:COFF["mk"] + 512], b_Wb)
            roped_proj(Wa, b_Wa, 0, 256, qT, b_qk, cosT, sinT, b_tab, (t1, t2, b_tmp), pp)
            roped_proj(Wb, b_Wb, 0, 256, kT, b_qk, cosT, sinT, b_tab, (t1, t2, b_tmp), pp, ksum=(kmT, b_km))
            chk('m2a')
            load_w(Wa[:, :, 0:256], w_in[layer][:, COFF["mv"]:COFF["mv"] + 256], b_Wa)
            v_aug_proj(Wa, b_Wa, 0, vaug, b_v, pp)
            op("dve", lambda e: e.tensor_copy(out=kmB[:], in_=kmT[:]), [b_km], [b_km])
            cx.release(m1)
            chk('m2')
            Z = alloc("Z", [128, 4, 8], F32); gm = alloc("gm", [128, 4, 8], F32); top8 = alloc("top8", [128, 4, 8], F32); b_Z = Buf("Z")
            otm = alloc("otm", [128, 4, 256], F32); b_otm = Buf("otm")
            pTl = [(alloc(f"pT{i}", [128, 512], BF16), Buf(f"pT{i}")) for i in range(3)]
            rs = alloc("rs", [128, 4], F32); b_rs = Buf("rs")
            op("pool", lambda e: e.memset(mbT[:], 0.0), [], [b_mb])
            pg = Rot(PS[0:2])
            for t in range(8, NT):
                pt, pb = pg.get()
                for h in range(4):
                    ptile, bp = h // 2, (h % 2) * 64
                    op("pe", lambda e, h=h: e.matmul(pt[:, h * 8:(h + 1) * 8], lhsT=qT[bp:bp + 64, ptile, t * 128:(t + 1) * 128],
                                                     rhs=kmB[bp:bp + 64, ptile, :], start=True, stop=False, skip_group_check=True), [b_qk, b_km], [pb])
                    op("pe", lambda e, h=h: e.matmul(pt[:, h * 8:(h + 1) * 8], lhsT=onehotK[0:8, t * 128:(t + 1) * 128],
                                                     rhs=bpen, start=False, stop=True, skip_group_check=True), [b_cst], [pb])
                op("dve", lambda e: e.tensor_copy(out=gm[:], in_=pt[:, 0:32].rearrange("p (a b) -> p a b", a=4)), [pb], [b_Z])
                for h in range(4):
                    op("dve", lambda e, h=h: e.max(out=top8[:, h, :], in_=gm[:, h, :]), [b_Z], [b_Z])
                for h in range(4):
                    op("dve", lambda e, h=h: e.tensor_scalar(out=Z[:, h, :], in0=gm[:, h, :], scalar1=top8[:, h, 3:4], scalar2=NEG,
                                                             op0=ALU.is_lt, op1=ALU.mult), [b_Z], [b_Z])
                pt2, pb2 = pg.get()
                for h in range(4):
                    op("pe", lambda e, h=h: e.transpose(out=pt2[0:8, h * 128:(h + 1) * 128], in_=Z[:, h, :], identity=identF), [b_Z, b_cst], [pb2])
                op("dve", lambda e: e.tensor_copy(out=mbT[0:8, :, t * 128:(t + 1) * 128], in_=pt2[0:8, :].rearrange("p (a b) -> p a b", a=4)),
                   [pb2], [b_mb])
            chk('m3')

            def moba_mask(h, qc, j, t0, st, sb, wq):
                q0 = t0 * 128
                diag = (j >= 4 * qc)
                op("pe", lambda e: e.matmul(st[:, 0:wq], lhsT=onehotK[0:8, j * 128:(j + 1) * 128], rhs=mbT[0:8, h, q0:q0 + wq],
                                            start=False, stop=(not diag), skip_group_check=True), [b_cst, b_mb], [sb])
                if diag:
                    op("pe", lambda e: e.matmul(st[:, 0:128], lhsT=trinegB[:], rhs=identB[:], start=False, stop=True, skip_group_check=True), [b_cst], [sb])

            attention(qT, kT, b_qk, vaug, b_v, moba_mask, 4, (Rot(PS[2:5]), Rot(PS[5:7]), Rot(PS[7:8])), otm, b_otm, Rot(pTl), rs, b_rs, mixT, mixb)
            cx.release(m0)

            if stop == 'moba':
                cx.barrier()
                return nc, cx
            m0 = cx.mark()
            qT, kT, vaug, b_qk, b_v = attn_common()
            kiT = alloc("kiT", [128, 1, L], BF16, 1); b_ki = Buf("ki")
            wT = alloc("wT", [8, L], BF16, 1); b_wT = Buf("wT")
            qiT = alloc("qiT", [128, 4, L], BF16, 1); b_qi = Buf("qi")
            m1 = cx.mark()
            Wa = alloc("Wa", [128, 8, 512], BF16); b_Wa = Buf("Wa")
            Wb = alloc("Wb", [128, 8, 512], BF16); b_Wb = Buf("Wb")
            t1 = alloc("t1", [128, 512], F32); t2 = alloc("t2", [128, 512], F32); b_tmp = Buf("rt")
            pp = Rot(PS[0:6])
            tm3 = (t1, t2, b_tmp)
            load_w(Wa[:], w_in[layer][:, COFF["sq"]:COFF["sq"] + 512], b_Wa)
            load_w(Wb[:], w_in[layer][:, COFF["sk"]:COFF["sk"] + 512], b_Wb)
            roped_proj(Wa, b_Wa, 0, 256, qT, b_qk, cosT, sinT, b_tab, tm3, pp)
            roped_proj(Wb, b_Wb, 0, 256, kT, b_qk, cosT, sinT, b_tab, tm3, pp)
            load_w(Wa[:], w_in[layer][:, COFF["iq"]:COFF["iq"] + 512], b_Wa)
            load_w(Wb[:], w_in[layer][:, COFF["iqr"]:COFF["iqr"] + 512], b_Wb)
            for ti in range(4):
                for tc in range(4):
                    py, pyb = pp.get(); pr, prb = pp.get()
                    proj_fm(Wa, ti * 128, 128, tc, py, pyb, b_Wa)
                    proj_fm(Wb, ti * 128, 128, tc, pr, prb, b_Wb)
                    sl = slice(tc * 512, (tc + 1) * 512)
                    op("dve", lambda e: e.tensor_tensor(out=t1[:], in0=pr[:, :], in1=sinT[:, sl], op=ALU.mult), [prb, b_tab], [b_tmp])
                    op("dve", lambda e: e.tensor_tensor(out=t2[:], in0=py[:, :], in1=cosT[:, sl], op=ALU.mult), [pyb, b_tab], [b_tmp])
                    op("pool", lambda e: e.tensor_tensor(out=qiT[:, ti, sl], in0=t1[:], in1=t2[:], op=ALU.add), [b_tmp], [b_qi])
            load_w(Wa[:, :, 0:264], w_in[layer][:, COFF["ik"]:COFF["ik"] + 264], b_Wa)
            load_w(Wb[:, :, 0:256], w_in[layer][:, COFF["sv"]:COFF["sv"] + 256], b_Wb)
            roped_proj(Wa, b_Wa, 0, 128, kiT, b_ki, cosT, sinT, b_tab, tm3, pp)
            for tc in range(4):
                pt, pb = pp.get()
                for c in range(8):
                    op("pe", lambda e, c=c: e.matmul(pt[0:8, 0:512], lhsT=Wa[:, c, 256:264], rhs=HT[:, c, tc * 512:(tc + 1) * 512],
                                                     start=(c == 0), stop=(c == 7)), [b_Wa] + HTb[tc * 4:tc * 4 + 4], [pb])
                op("act", lambda e: e.activation(out=wT[:, tc * 512:(tc + 1) * 512], in_=pt[0:8, 0:512], func=AF.Copy), [pb], [b_wT])
            v_aug_proj(Wb, b_Wb, 0, vaug, b_v, pp)
            cx.release(m1)
            mbs = [HT[:, 0:4, :], alloc("mbB", [128, 4, L], BF16)]
            b_mb2 = [[Buf(f"mb{a}_{i}") for i in range(4)] for a in range(2)]
            isc = [HT[:, 4:6, :].bitcast(F32).rearrange("p a b -> p (a b)"), HT[:, 6:8, :].bitcast(F32).rearrange("p a b -> p (a b)"),
                   alloc("isc2", [128, L], F32), alloc("isc3", [128, L], F32)]
            b_isc = [Buf(f"isc{i}") for i in range(4)]
            qs = alloc("qs", [128, 4, 2, 512], BF16, 1); b_qs = Buf("qs")
            otm = alloc("otm", [128, 4, 256], F32); b_otm = Buf("otm")
            pTl = [(alloc(f"pT{i}", [128, 512], BF16), Buf(f"pT{i}")) for i in range(3)]
            rs = alloc("rs", [128, 4], F32); b_rs = Buf("rs")
            bsA = alloc("bsA", [128, 32], F32); bsi = alloc("bsi", [128, 4], I32)
            lo = bsA[:, 0:4]; w0 = bsA[:, 4:8]; mid = bsA[:, 8:12]; nmid = bsA[:, 12:16]; cntv = bsA[:, 16:20]; thc = bsA[:, 20:24]; mx = bsA[:, 24:28]
            b_lo = Buf("lo"); b_mid = Buf("mid"); b_cnt = [Buf(f"cnt{i}") for i in range(4)]; b_msk = Buf("msk")
            junkD = alloc("junkD", [128, L], BF16); b_junkD = Buf("junkD")
            junkA = alloc("junkA", [128, L], BF16); b_junkA = Buf("junkA")
            ps_i = Rot(PS[0:3])
            rls = Rot([(alloc(f"rl{i}", [128, 512], BF16), Buf(f"rl{i}")) for i in range(4)])

            def dsa_qc(qc):
                mb = mbs[qc % 2]; b_mbq = b_mb2[qc % 2]
                qsl = slice(qc * 512, (qc + 1) * 512)
                for pr_ in range(4):
                    pt, pb = ps_i.get()
                    op("pe", lambda e: e.matmul(pt[:, 0:512], lhsT=selw[0:8, pr_ * 128:(pr_ + 1) * 128], rhs=wT[0:8, qsl], start=True, stop=True),
                       [b_cst, b_wT], [pb])
                    op("dve", lambda e: e.scalar_tensor_tensor(out=qs[:, pr_, 0, :], in0=pt[:, 0:512], scalar=0.0, in1=qiT[:, pr_, qsl],
                                                               op0=ALU.max, op1=ALU.mult), [pb, b_qi], [b_qs])
                    op("dve", lambda e: e.scalar_tensor_tensor(out=qs[:, pr_, 1, :], in0=pt[:, 0:512], scalar=0.0, in1=qiT[:, pr_, qsl],
                                                               op0=ALU.min, op1=ALU.mult), [pb, b_qi], [b_qs])
                yield
                act_tiles = []
                for i in range(4):
                    t = 4 * qc + i
                    S = (t + 1) * 128
                    if t < 2:
                        if t > 0:
                            op("pool", lambda e: e.memset(mb[:, i, 0:t * 128], 0.0), [], [b_mbq[i]])
                        op("pool", lambda e: e.tensor_copy(out=mb[:, i, t * 128:S], in_=trinegB[:]), [b_cst], [b_mbq[i]])
                        continue
                    act_tiles.append(i)
                    ic, icb = isc[i], b_isc[i]
                    for kc in range((S + 511) // 512):
                        w = min(512, S - kc * 512)
                        ksl = slice(kc * 512, kc * 512 + w)
                        first = True
                        nsum = 0
                        p2, p2b = PS[3]
                        pend = []

                        def flush_one():
                            nonlocal nsum
                            (rl_, rlb_, sg_) = pend.pop(0)
                            op("pe", lambda e: e.matmul(p2[:, 0:w], lhsT=(identB[:] if sg_ == 0 else negidentB[:]), rhs=rl_[:, 0:w],
                                                        start=(nsum == 0), stop=(nsum == 7)), [rlb_, b_cst], [p2b])
                            nsum += 1

                        for k in range(16):
                            hd, sgn = k // 2, k % 2
                            pr_, bp = hd // 2, (hd % 2) * 64
                            pt, pb = ps_i.get()
                            op("pe", lambda e: e.matmul(pt[:, 0:w], lhsT=qs[bp:bp + 64, pr_, sgn, i * 128:(i + 1) * 128], rhs=kiT[bp:bp + 64, 0, ksl],
                                                        start=True, stop=True), [b_qs, b_ki], [pb])
                            o = ALU.max if sgn == 0 else ALU.min
                            if hd % 2 == 1:
                                rl, rlb = rls.get()
                                op("act", lambda e: e.activation(out=rl[:, 0:w], in_=pt[:, 0:w], func=AF.Relu, scale=(1.0 if sgn == 0 else -1.0)), [pb], [rlb])
                                pend.append((rl, rlb, sgn))
                                if len(pend) > 2:
                                    flush_one()
                            elif first:
                                op("dve", lambda e: e.tensor_scalar(out=ic[:, ksl], in0=pt[:, 0:w], scalar1=0.0, scalar2=None, op0=o), [pb], [icb])
                                first = False
                            else:
                                op("dve", lambda e: e.scalar_tensor_tensor(out=ic[:, ksl], in0=pt[:, 0:w], scalar=0.0, in1=ic[:, ksl], op0=o, op1=ALU.add),
                                   [pb, icb], [icb])
                        while pend:
                            flush_one()
                        op("dve", lambda e: e.tensor_tensor(out=ic[:, ksl], in0=p2[:, 0:w], in1=ic[:, ksl], op=ALU.add), [p2b, icb], [icb])
                        yield
                    op("dve", lambda e: e.tensor_reduce(out=lo[:, i:i + 1], in_=ic[:, 0:S], axis=AX.X, op=ALU.min), [icb], [b_lo])
                    op("dve", lambda e: e.tensor_reduce(out=mx[:, i:i + 1], in_=ic[:, 0:S], axis=AX.X, op=ALU.max), [icb], [b_lo])
                    op("dve", lambda e: e.tensor_tensor(out=ic[:, t * 128:S], in0=ic[:, t * 128:S], in1=trinegbig, op=ALU.add), [icb, b_cst], [icb])
                    on_act = (i % 2 == 1)
                    op("dve", lambda e: e.memset(thc[:, i:i + 1], (S - 511.0) if on_act else (S - 255.5)), [], [b_lo])
                a0, a1 = act_tiles[0], act_tiles[-1] + 1
                asl = slice(a0, a1)
                op("dve", lambda e: e.scalar_tensor_tensor(out=w0[:, asl], in0=mx[:, asl], scalar=1.0, in1=lo[:, asl], op0=ALU.add, op1=ALU.subtract),
                   [b_lo], [b_lo])
                for it in range(N_BISECT):
                    ck = 0.5 ** (it + 1)
                    op("dve", lambda e, ck=ck: e.scalar_tensor_tensor(out=mid[:, asl], in0=w0[:, asl], scalar=ck, in1=lo[:, asl], op0=ALU.mult, op1=ALU.add),
                       [b_lo], [b_mid])
                    for i in act_tiles:
                        S = (4 * qc + i + 1) * 128
                        ic, icb = isc[i], b_isc[i]
                        if i % 2 == 1:
                            op("act", lambda e, i=i, S=S, ic=ic: e.activation(out=junkA[:, 0:S], in_=ic[:, 0:S], func=AF.Sign, bias=mid[:, i:i + 1], scale=-1.0,
                                                                              accum_out=cntv[:, i:i + 1]), [icb, b_mid], [b_cnt[i], b_junkA])
                        else:
                            op("dve", lambda e, i=i, S=S, ic=ic: e.tensor_scalar(out=junkD[:, 0:S], in0=ic[:, 0:S], scalar1=mid[:, i:i + 1], scalar2=None,
                                                                                 op0=ALU.is_lt, op1=ALU.add, accum_out=cntv[:, i:i + 1]), [icb, b_mid], [b_cnt[i], b_junkD])
                    op("dve", lambda e: e.tensor_tensor(out=bsi[:, asl], in0=cntv[:, asl], in1=thc[:, asl], op=ALU.is_le), [b_cnt[i] for i in act_tiles] + [b_lo], [b_msk])
                    op("dve", lambda e: e.copy_predicated(out=lo[:, asl], mask=bsi[:, asl], data=mid[:, asl]), [b_msk, b_mid], [b_lo])
                    yield
                for i in act_tiles:
                    S = (4 * qc + i + 1) * 128
                    ic, icb = isc[i], b_isc[i]
                    op("dve", lambda e, i=i, S=S, ic=ic: e.tensor_scalar(out=mb[:, i, 0:S], in0=ic[:, 0:S], scalar1=lo[:, i:i + 1], scalar2=NEG, op0=ALU.is_lt, op1=ALU.mult),
                       [icb, b_lo], [b_mbq[i]])

            def dsa_mask(h, qc, j, t0, st, sb, wq):
                last = 4 * qc + 3
                mb = mbs[qc % 2]; b_mbq = b_mb2[qc % 2]
                for t in range(t0, last + 1):
                    i = t - 4 * qc
                    op("pe", lambda e, t=t, i=i: e.matmul(st[:, (t - t0) * 128:(t - t0 + 1) * 128], lhsT=mb[:, i, j * 128:(j + 1) * 128], rhs=identB[:],
                                                          start=False, stop=(t == last), skip_group_check=True), [b_mbq[i], b_cst], [sb])

            attention(qT, kT, b_qk, vaug, b_v, dsa_mask, 6, (Rot(PS[4:6]), Rot(PS[6:8]), Rot(PS[3:4])), otm, b_otm, Rot(pTl), rs, b_rs, mixT, mixb,
                      qc_hook=dsa_qc)
            cx.release(m0)

            if stop == 'dsa':
                cx.barrier()
                return nc, cx
            if debug and seq == 0 and layer == layers[0]:
                dma("sp", dbg[:, :, :], mixT[:, :, :], reads=mixb, owner=Buf("dbg"))

            cx.cur2 = Hoff + 65536
            m0 = cx.mark()
            Wo = alloc("Wo", [128, 8, D], BF16); b_Wo = Buf("Wo")
            gbc = alloc("gbc", [128, D], F32); bbc = alloc("bbc", [128, D], F32); b_par = Buf("lnpar")
            hres = [alloc("hres0", [128, D], F32), alloc("hres1", [128, D], F32)]; b_hres = [Buf("hres0"), Buf("hres1")]
            stat = alloc("stat", [128, NT, 20], F32); b_stat = [Buf(f"stat{t}") for t in range(NT)]
            rw = alloc("rw", [128, 8, 16], F32); rwh = alloc("rwh", [128, 8, 16], BF16); rwl = alloc("rwl", [128, 8, 16], BF16); b_rw = Buf("rw")
            rbb = alloc("rbb", [128, 16], F32)
            HTlo = alloc("HTlo", [128, 8, 128], BF16); b_lo = Buf("HTlo")
            comb = alloc("comb", [128, NT, 16], F32); b_comb = Buf("comb")
            rt = alloc("rt", [128, 8, NT * 16], F32); b_rt = Buf("rt")
            lg_all = alloc("lg_all", [128, NT, 16], F32); b_lg = Buf("lg")
            load_w(Wo[:], w_out[layer][:, :], b_Wo)
            dma("sp", gbc[:], ln1g[layer, :].partition_broadcast(128), writes=[b_par])
            dma("sp", bbc[:], ln1b[layer, :].partition_broadcast(128), writes=[b_par], owner=Buf("lnb"))
            dma("sp", rw[:], r_w.rearrange("(c p) e -> p c e", p=128), writes=[b_rw])
            dma("sp", rbb[:], r_b.partition_broadcast(128), writes=[b_rw], owner=Buf("rbb"))
            op("dve", lambda e: e.tensor_copy(out=rwh[:], in_=rw[:]), [b_rw], [b_rw])
            op("dve", lambda e: e.tensor_tensor(out=rw[:], in0=rw[:], in1=rwh[:], op=ALU.subtract), [b_rw], [b_rw])
            op("dve", lambda e: e.tensor_copy(out=rwl[:], in_=rw[:]), [b_rw], [b_rw])
            ps_o = Rot(PS[0:4]); ps_t = Rot(PS[4:7]); ps_r = Rot(PS[7:8])
            def ln1_front(t):
                hr, hrb = hres[t % 2], b_hres[t % 2]
                dma("sp", hr[:], hs[t * 128:(t + 1) * 128, :], reads=[hsb[t]], writes=[hrb])
                for half in range(2):
                    po, pob = ps_o.get()
                    for c in range(8):
                        op("pe", lambda e, c=c: e.matmul(po[:, 0:512], lhsT=mixT[:, c, t * 128:(t + 1) * 128], rhs=Wo[:, c, half * 512:(half + 1) * 512],
                                                         start=(c == 0), stop=(c == 7)), [mixb[t], b_Wo], [pob])
                    op("dve", lambda e: e.scalar_tensor_tensor(out=H[:, t, half * 512:(half + 1) * 512], in0=hr[:, half * 512:(half + 1) * 512], scalar=ALPHA,
                                                               in1=po[:, 0:512], op0=ALU.mult, op1=ALU.add), [hrb, pob], [Hb[t]])
                ln_stats(t, stat, b_stat)

            def ln1_back(t):
                ln_apply(t, gbc, bbc, b_par, stat, b_stat)
                for half in range(2):
                    pt, pb = ps_t.get()
                    for c4 in range(4):
                        c = half * 4 + c4
                        op("pe", lambda e, c=c, c4=c4: e.transpose(out=pt[:, c4 * 128:(c4 + 1) * 128], in_=H[:, t, c * 128:(c + 1) * 128], identity=identF),
                           [Hb[t], b_cst], [pb])
                    pv = pt[:, :].rearrange("p (a b) -> p a b", a=4)
                    op("act", lambda e: e.activation(out=HT[:, half * 4:(half + 1) * 4, t * 128:(t + 1) * 128], in_=pv, func=AF.Copy), [pb], [HTb[t]])
                    op("dve", lambda e: e.tensor_tensor(out=HTlo[:, half * 4:(half + 1) * 4, :], in0=pv, in1=HT[:, half * 4:(half + 1) * 4, t * 128:(t + 1) * 128],
                                                        op=ALU.subtract), [pb, HTb[t]], [b_lo])
                pr_, prb = ps_r.get()
                k = 0
                for (A_, Ab, W_) in ((HT[:, :, t * 128:(t + 1) * 128], HTb[t], rwh), (HTlo[:, :, :], b_lo, rwh), (HT[:, :, t * 128:(t + 1) * 128], HTb[t], rwl)):
                    for c in range(8):
                        op("pe", lambda e, c=c, A_=A_, W_=W_, k=k: e.matmul(pr_[:, 0:16], lhsT=A_[:, c, :], rhs=W_[:, c, :], start=(k == 0), stop=(k == 23)),
                           [Ab, b_rw], [prb])
                        k += 1
                op("act", lambda e: e.activation(out=lg_all[:, t, :], in_=pr_[:, 0:16], func=AF.Copy), [prb], [b_lg])
                op("act", lambda e: e.activation(out=H[:, t, :], in_=H[:, t, :], func=AF.Copy, scale=ALPHA), [Hb[t]], [Hb[t]])

            ln1_front(0)
            for t in range(NT):
                if t + 1 < NT:
                    ln1_front(t + 1)
                ln1_back(t)
            T_ = NT
            sc = rt[:, 0, :]; sel = rt[:, 1, :]; tmpa = rt[:, 2, :]; tmpb = rt[:, 3, :]; ch = rt[:, 4, :]
            m1 = rt[:, 5, 0:T_ * 4]; m2 = rt[:, 5, T_ * 4:T_ * 8]; gs = rt[:, 5, T_ * 8:T_ * 12]; ing = rt[:, 5, T_ * 12:T_ * 16]
            gmax = rt[:, 6, 0:T_]; a1 = rt[:, 6, T_:2 * T_]; a2 = rt[:, 6, 2 * T_:3 * T_]; ssum = rt[:, 6, 3 * T_:4 * T_]
            v3 = lambda a: a.rearrange("p (t e) -> p t e", t=T_)
            v4 = lambda a: a.rearrange("p (t g e) -> p t g e", t=T_, g=4)
            g3 = lambda a: a.rearrange("p (t g) -> p t g", t=T_)
            R = [b_rt]
            op("act", lambda e: e.activation(out=sc, in_=lg_all[:, :, :].rearrange("p t e -> p (t e)"), func=AF.Sigmoid), [b_lg], R)
            op("dve", lambda e: e.tensor_tensor(out=v3(sel), in0=v3(sc), in1=rbb[:, None, :].to_broadcast([128, T_, 16]), op=ALU.add), R + [b_rw], R)
            op("dve", lambda e: e.tensor_reduce(out=g3(m1), in_=v4(sel), axis=AX.X, op=ALU.max), R, R)
            op("dve", lambda e: e.tensor_tensor(out=v4(tmpa), in0=v4(sel), in1=g3(m1)[:, :, :, None].to_broadcast([128, T_, 4, 4]), op=ALU.is_equal), R, R)
            op("dve", lambda e: e.scalar_tensor_tensor(out=tmpb, in0=tmpa, scalar=-1.0e9, in1=sel, op0=ALU.mult, op1=ALU.add), R, R)
            op("dve", lambda e: e.tensor_reduce(out=g3(m2), in_=v4(tmpb), axis=AX.X, op=ALU.max), R, R)
            op("dve", lambda e: e.tensor_tensor(out=gs, in0=m1, in1=m2, op=ALU.add), R, R)
            op("dve", lambda e: e.tensor_reduce(out=gmax, in_=g3(gs), axis=AX.X, op=ALU.max), R, R)
            op("dve", lambda e: e.tensor_tensor(out=g3(ing), in0=g3(gs), in1=gmax[:, :, None].to_broadcast([128, T_, 4]), op=ALU.is_lt), R, R)
            op("dve", lambda e: e.tensor_scalar(out=ing, in0=ing, scalar1=-1.0e9, scalar2=None, op0=ALU.mult), R, R)
            op("dve", lambda e: e.tensor_tensor(out=v4(tmpa), in0=v4(sel), in1=g3(ing)[:, :, :, None].to_broadcast([128, T_, 4, 4]), op=ALU.add), R, R)
            op("dve", lambda e: e.tensor_reduce(out=a1, in_=v3(tmpa), axis=AX.X, op=ALU.max), R, R)
            op("dve", lambda e: e.tensor_tensor(out=v3(ch), in0=v3(tmpa), in1=a1[:, :, None].to_broadcast([128, T_, 16]), op=ALU.is_ge), R, R)
            op("dve", lambda e: e.scalar_tensor_tensor(out=tmpb, in0=ch, scalar=-1.0e9, in1=tmpa, op0=ALU.mult, op1=ALU.add), R, R)
            op("dve", lambda e: e.tensor_reduce(out=a2, in_=v3(tmpb), axis=AX.X, op=ALU.max), R, R)
            op("dve", lambda e: e.tensor_tensor(out=v3(tmpa), in0=v3(tmpb), in1=a2[:, :, None].to_broadcast([128, T_, 16]), op=ALU.is_ge), R, R)
            op("dve", lambda e: e.tensor_tensor(out=ch, in0=ch, in1=tmpa, op=ALU.add), R, R)
            op("dve", lambda e: e.tensor_tensor(out=tmpb, in0=ch, in1=sc, op=ALU.mult), R, R)
            op("dve", lambda e: e.tensor_reduce(out=ssum, in_=v3(tmpb), axis=AX.X, op=ALU.add), R, R)
            op("dve", lambda e: e.reciprocal(out=ssum, in_=ssum), R, R)
            op("dve", lambda e: e.tensor_tensor(out=comb[:, :, :], in0=v3(tmpb), in1=ssum[:, :, None].to_broadcast([128, T_, 16]), op=ALU.mult), R, [b_comb])
            cx.barrier()
            cx.cur, cx.cur2 = base_mark
            cx.cur2 = Hoff + 65536
            comb2 = alloc("comb2", [128, NT, 16], F32); b_comb2 = Buf("comb2")
            op("dve", lambda e: e.tensor_copy(out=comb2[:], in_=comb[:]), [b_comb], [b_comb2])
            cx.barrier()

            if stop == 'ln1':
                cx.barrier()
                return nc, cx
            m0 = cx.mark()
            Wg_ = [alloc(f"weg{i}", [128, 8, 512], BF16) for i in range(2)]; b_weg = [Buf("weg0"), Buf("weg1")]
            Wu_ = [alloc(f"weu{i}", [128, 8, 512], BF16) for i in range(2)]; b_weu = [Buf("weu0"), Buf("weu1")]
            Wd_ = [alloc(f"wed{i}", [128, 4, D], BF16) for i in range(2)]; b_wed = [Buf("wed0"), Buf("wed1")]
            actT = alloc("actT", [128, 4, L], BF16); b_act = [[Buf(f"act{f}_{tc}") for tc in range(4)] for f in range(4)]
            sgb = [(alloc(f"sgm{i}", [128, 512], F32), Buf(f"sgm{i}")) for i in range(3)]
            sgr = Rot(sgb)
            ps_gu = Rot(PS[0:4]); ps_d = Rot(PS[4:8])
            for ex in range(16):
                wg, wu, wd = Wg_[ex % 2], Wu_[ex % 2], Wd_[ex % 2]
                bg, bu, bd = b_weg[ex % 2], b_weu[ex % 2], b_wed[ex % 2]
                load_w(wg[:], w_eg[layer, ex], bg)
                load_w(wu[:], w_eu[layer, ex], bu)
                dma("pool", wd[:], w_ed[layer, ex].rearrange("(c p) f -> p c f", p=128), writes=[bd])
                for tc in range(4):
                    for f in range(4):
                        pg_, pgb = ps_gu.get(); pu_, pub = ps_gu.get()
                        for c in range(8):
                            op("pe", lambda e, c=c: e.matmul(pg_[:, 0:512], lhsT=wg[:, c, f * 128:(f + 1) * 128], rhs=HT[:, c, tc * 512:(tc + 1) * 512],
                                                             start=(c == 0), stop=(c == 7)), [bg] + HTb[tc * 4:tc * 4 + 4], [pgb])
                        for c in range(8):
                            op("pe", lambda e, c=c: e.matmul(pu_[:, 0:512], lhsT=wu[:, c, f * 128:(f + 1) * 128], rhs=HT[:, c, tc * 512:(tc + 1) * 512],
                                                             start=(c == 0), stop=(c == 7)), [bu] + HTb[tc * 4:tc * 4 + 4], [pub])
                        sgt, sgtb = sgr.get()
                        op("act", lambda e: e.activation(out=sgt[:], in_=pg_[:, 0:512], func=AF.Silu), [pgb], [sgtb])
                        op("dve", lambda e: e.tensor_tensor(out=actT[:, f, tc * 512:(tc + 1) * 512], in0=pu_[:, 0:512], in1=sgt[:], op=ALU.mult),
                           [pub, sgtb], [b_act[f][tc]])
                    for t4 in range(4):
                        t = tc * 4 + t4
                        for half in range(2):
                            pd_, pdb = ps_d.get()
                            for f in range(4):
                                op("pe", lambda e, f=f: e.matmul(pd_[:, 0:512], lhsT=actT[:, f, t * 128:(t + 1) * 128], rhs=wd[:, f, half * 512:(half + 1) * 512],
                                                                 start=(f == 0), stop=(f == 3)), [b_act[f][tc], bd], [pdb])
                            op("dve", lambda e: e.scalar_tensor_tensor(out=H[:, t, half * 512:(half + 1) * 512], in0=pd_[:, 0:512], scalar=comb2[:, t, ex:ex + 1],
                                                                       in1=H[:, t, half * 512:(half + 1) * 512], op0=ALU.mult, op1=ALU.add),
                               [pdb, b_comb2, Hb[t]], [Hb[t]])
            cx.release(m0)

            if stop == 'moe':
                cx.barrier()
                return nc, cx
            m0 = cx.mark()
            gbc = alloc("gbc", [128, D], F32); bbc = alloc("bbc", [128, D], F32); b_par = Buf("lnpar")
            stat = alloc("stat", [128, NT, 20], F32); b_stat = [Buf(f"stat{t}") for t in range(NT)]
            dma("sp", gbc[:], ln2g[layer, :].partition_broadcast(128), writes=[b_par])
            dma("sp", bbc[:], ln2b[layer, :].partition_broadcast(128), writes=[b_par], owner=Buf("lnb"))
            pool_tr = Rot(PS[0:4])
            last = (layer == layers[-1])
            ln_stats(0, stat, b_stat)
            for t in range(NT):
                ln_apply(t, gbc, bbc, b_par, stat, b_stat, between=((lambda t=t: ln_stats(t + 1, stat, b_stat)) if t + 1 < NT else None))
                if last:
                    dma("sp", out[seq, t * 128:(t + 1) * 128, :], H[:, t, :], reads=[Hb[t]], owner=Hb[t])
                else:
                    make_HT(t, pool_tr)
            cx.release(m0)
            cx.cur, cx.cur2 = base_mark

    cx.barrier()
    return nc, cx


_CACHE = {}


def _prep_inputs(inputs):
    cst, cst8 = _consts()
    w_in_x = np.ascontiguousarray(np.asarray(inputs["w_in"], np.float32)[:, :, COLMAP])
    shared = {"w_in_x": w_in_x, "cst": cst, "cst8": cst8}
    for k in ("gla_gate_up", "gla_gate_bias", "gla_norm_gain", "w_out", "ln_mix_g", "ln_mix_b", "router_w", "router_bias",
              "w_expert_gate", "w_expert_up", "w_expert_down", "ln_ffn_g", "ln_ffn_b"):
        shared[k] = np.ascontiguousarray(np.asarray(inputs[k], np.float32))
    return shared


def kernel(**inputs):
    x = np.ascontiguousarray(np.asarray(inputs["x"], np.float32))
    positions = np.ascontiguousarray(np.asarray(inputs["positions"], np.int32))
    shared = _prep_inputs(inputs)
    ncores, nseq = 8, 2
    nc = bass.Bass("TRN2", target_bir_lowering=False)
    build(nc, nseq, [0, 1])
    in_maps = []
    for c in range(ncores):
        m = dict(shared)
        m["x"] = x[c * nseq:(c + 1) * nseq]
        m["positions"] = positions[c * nseq:(c + 1) * nseq]
        in_maps.append(m)
    res = run_bass_kernel_spmd(nc, in_maps, core_ids=list(range(ncores)))
    return np.concatenate([r["out"] for r in res.results], axis=0)
```

```python
import numpy as np
import concourse.bass as bass
import concourse.mybir as mybir
from concourse.bass_utils import run_bass_kernel_spmd

F32 = mybir.dt.float32
BF16 = mybir.dt.bfloat16
I32 = mybir.dt.int32
ALU = mybir.AluOpType
AF = mybir.ActivationFunctionType
AX = mybir.AxisListType

L = 2048
D = 1024
NT = 16
NEG = -30000.0
BIG = 1.0e30
ALPHA = 4.0 ** 0.25
N_BISECT = 18
SBUF_BASE = 16512
SBUF_BYTES = 229000

IN_WIDTHS = (256, 256, 512, 16, 512, 256, 256, 256, 256, 256, 256, 512, 64, 8)
IN_OFF = np.concatenate([[0], np.cumsum(IN_WIDTHS)]).astype(int)
(O_GQ, O_GK, O_GV, O_GR, O_GG, O_MQ, O_MK, O_MV, O_SQ, O_SK, O_SV, O_IQ, O_IK, O_IW) = [int(v) for v in IN_OFF[:-1]]


def _rot_cols(base, nheads):
    cols = []
    for h in range(nheads):
        for d in range(64):
            if d < 8:
                pd = d + 8
            elif d < 16:
                pd = d - 8
            else:
                pd = d
            cols.append(base + h * 64 + pd)
    return cols


def _build_colmap():
    cols = []
    off = {}

    def add(name, c):
        off[name] = len(cols)
        cols.extend(c)

    for p in range(2):
        add(f"g{p}", list(range(O_GQ + p * 128, O_GQ + p * 128 + 128)) + list(range(O_GK + p * 128, O_GK + p * 128 + 128))
            + list(range(O_GV + p * 256, O_GV + p * 256 + 256)) + list(range(O_GG + p * 256, O_GG + p * 256 + 256))
            + list(range(O_GR, O_GR + 16)))
    add("mq", list(range(O_MQ, O_MQ + 256)))
    add("mqr", _rot_cols(O_MQ, 4))
    add("mk", list(range(O_MK, O_MK + 256)))
    add("mkr", _rot_cols(O_MK, 4))
    add("mv", list(range(O_MV, O_MV + 256)))
    add("sq", list(range(O_SQ, O_SQ + 256)))
    add("sqr", _rot_cols(O_SQ, 4))
    add("sk", list(range(O_SK, O_SK + 256)))
    add("skr", _rot_cols(O_SK, 4))
    add("sv", list(range(O_SV, O_SV + 256)))
    add("iq", list(range(O_IQ, O_IQ + 512)))
    add("iqr", _rot_cols(O_IQ, 8))
    add("ik", list(range(O_IK, O_IK + 64)) * 2)
    add("ikr", _rot_cols(O_IK, 1) * 2)
    add("iw", list(range(O_IW, O_IW + 8)))
    return np.array(cols, dtype=np.int64), off


COLMAP, COFF = _build_colmap()
WX = len(COLMAP)


def _consts():
    c = np.zeros((128, 1024), np.float32)
    c[:, 0:128] = np.eye(128, dtype=np.float32)
    j = np.arange(128)[:, None]
    i = np.arange(128)[None, :]
    c[:, 128:256] = (i >= j).astype(np.float32)
    c[:, 256:384] = np.where(i <= j, 0.0, NEG)
    c[:, 384:512] = np.where(i <= j, 0.0, -BIG)
    p = np.arange(128)
    d = p % 64
    invf = np.where(d < 16, 500000.0 ** (-(d % 8) / 8.0), 0.0)
    c[:, 512] = invf / (2 * np.pi)
    c[:, 513] = np.where(d < 8, -invf, invf) / (2 * np.pi)
    c8 = np.zeros((8, 2048 + 512 + 8), np.float32)
    for k in range(8):
        for n in range(8):
            c8[k, 2560 + n] = 0.0 if n < k else (BIG if n == k else -BIG)
    for n in range(8):
        c8[n, n * 256:(n + 1) * 256] = 1.0
    for k in range(8):
        pr, hh = k // 2, k % 2
        c8[k, 2048 + pr * 128 + hh * 64: 2048 + pr * 128 + hh * 64 + 64] = 1.0
    return c, c8


class Buf:
    __slots__ = ("name", "writer", "readers", "dsem", "dcnt")

    def __init__(self, name):
        self.name = name
        self.writer = None
        self.readers = {}
        self.dsem = None
        self.dcnt = 0


class Ctx:
    def __init__(self, nc):
        self.nc = nc
        self.E = {"pe": nc.tensor, "act": nc.scalar, "dve": nc.vector, "pool": nc.gpsimd, "sp": nc.sync}
        self.sem = {k: nc.alloc_semaphore("e_" + k) for k in self.E}
        self.cnt = {k: 0 for k in self.E}
        self.seen = {k: {} for k in self.E}
        self.dbufs = []
        self.slots = {}
        self.nalloc = 0
        self.cur = SBUF_BASE
        self.cur2 = 0
        self.lim2 = 0
        self.peak = 0

    def alloc(self, name, shape, dtype, reg=0):
        isz = 4 if dtype in (F32, I32) else 2
        n = isz
        for s in shape[1:]:
            n *= s
        n = (n + 63) // 64 * 64
        if reg == 0:
            off = self.cur
            self.cur += n
            self.peak = max(self.peak, self.cur)
            assert self.cur <= SBUF_BYTES, (name, self.cur)
        else:
            off = self.cur2
            self.cur2 += n
            assert self.cur2 <= self.lim2, (name, self.cur2, self.lim2)
        self.nalloc += 1
        return self.nc.alloc_sbuf_tensor_at(f"{name}_{self.nalloc}", list(shape), dtype, offset=off)

    def mark(self):
        return (self.cur, self.cur2)

    def release(self, m):
        self.barrier()
        self.cur, self.cur2 = m

    def _wait(self, eng, dep):
        kind, obj, val = dep
        key = obj if kind == "e" else id(obj)
        if self.seen[eng].get(key, 0) >= val:
            return
        sem = self.sem[obj] if kind == "e" else obj.dsem
        self.E[eng].wait_ge(sem, val)
        self.seen[eng][key] = val

    def _deps(self, eng, reads, writes):
        for b in reads:
            if b.writer is not None:
                self._wait(eng, b.writer)
        for b in writes:
            w = b.writer
            if w is not None and not (w[0] == "e" and w[1] == eng and eng == "pe"):
                self._wait(eng, w)
            for r in b.readers.values():
                if not (r[0] == "e" and r[1] == eng and eng == "pe"):
                    self._wait(eng, r)

    def op(self, eng, fn, reads=(), writes=()):
        self._deps(eng, reads, writes)
        inst = fn(self.E[eng])
        inst.then_inc(self.sem[eng], 1)
        self.cnt[eng] += 1
        tag = ("e", eng, self.cnt[eng])
        for b in reads:
            b.readers[eng] = tag
        for b in writes:
            b.writer = tag
            b.readers = {}

    def dma(self, q, out, in_, reads=(), writes=(), owner=None):
        self._deps(q, reads, writes)
        if owner is None:
            owner = writes[0] if writes else reads[0]
        slot = self.slots.get(owner.name)
        if slot is None:
            slot = Buf("slot_" + owner.name)
            slot.dsem = self.nc.alloc_semaphore("d_%d" % len(self.slots))
            self.slots[owner.name] = slot
            self.dbufs.append(slot)
        self.E[q].dma_start(out=out, in_=in_).then_inc(slot.dsem, 16)
        slot.dcnt += 16
        tag = ("d", slot, slot.dcnt)
        for b in reads:
            b.readers[("d", id(slot))] = tag
        for b in writes:
            b.writer = tag
            b.readers = {}

    def barrier(self):
        for e in self.E:
            for e2 in self.E:
                if e2 != e and self.cnt[e2] > 0:
                    self._wait(e, ("e", e2, self.cnt[e2]))
            for b in self.dbufs:
                if b.dcnt:
                    self._wait(e, ("d", b, b.dcnt))


class Rot:
    def __init__(self, items):
        self.items = items
        self.i = 0

    def get(self):
        it = self.items[self.i % len(self.items)]
        self.i += 1
        return it


class StopBuild(Exception):
    pass


def build(nc, nseq, layers, debug=False, stop=None):
    try:
        return _build(nc, nseq, layers, debug, stop)
    except StopBuild as ex:
        return nc, ex.args[0]


def _build(nc, nseq, layers, debug=False, stop=None):
    cx = Ctx(nc)
    op, dma, alloc = cx.op, cx.dma, cx.alloc

    def chk(name):
        if stop == name:
            cx.barrier()
            raise StopBuild(cx)

    x = nc.dram_tensor("x", [nseq, L, D], F32, kind="ExternalInput").ap()
    pos = nc.dram_tensor("positions", [nseq, L], I32, kind="ExternalInput").ap()
    w_in = nc.dram_tensor("w_in_x", [2, D, WX], F32, kind="ExternalInput").ap()
    g_up = nc.dram_tensor("gla_gate_up", [2, 16, 256], F32, kind="ExternalInput").ap()
    g_bias = nc.dram_tensor("gla_gate_bias", [2, 256], F32, kind="ExternalInput").ap()
    g_gain = nc.dram_tensor("gla_norm_gain", [2, 128], F32, kind="ExternalInput").ap()
    w_out = nc.dram_tensor("w_out", [2, D, D], F32, kind="ExternalInput").ap()
    ln1g = nc.dram_tensor("ln_mix_g", [2, D], F32, kind="ExternalInput").ap()
    ln1b = nc.dram_tensor("ln_mix_b", [2, D], F32, kind="ExternalInput").ap()
    r_w = nc.dram_tensor("router_w", [D, 16], F32, kind="ExternalInput").ap()
    r_b = nc.dram_tensor("router_bias", [16], F32, kind="ExternalInput").ap()
    w_eg = nc.dram_tensor("w_expert_gate", [2, 16, D, 512], F32, kind="ExternalInput").ap()
    w_eu = nc.dram_tensor("w_expert_up", [2, 16, D, 512], F32, kind="ExternalInput").ap()
    w_ed = nc.dram_tensor("w_expert_down", [2, 16, 512, D], F32, kind="ExternalInput").ap()
    ln2g = nc.dram_tensor("ln_ffn_g", [2, D], F32, kind="ExternalInput").ap()
    ln2b = nc.dram_tensor("ln_ffn_b", [2, D], F32, kind="ExternalInput").ap()
    cst_d = nc.dram_tensor("cst", [128, 1024], F32, kind="ExternalInput").ap()
    cst8_d = nc.dram_tensor("cst8", [8, 2568], F32, kind="ExternalInput").ap()
    out = nc.dram_tensor("out", [nseq, L, D], F32, kind="ExternalOutput").ap()
    hs = nc.dram_tensor("hspill", [L, D], F32, kind="Internal").ap()
    dbg = nc.dram_tensor("dbg", [128, 8, L], BF16, kind="ExternalOutput").ap() if debug else None

    cst = alloc("cst", [128, 1024], F32)
    c8b = alloc("c8b", [8, 2568], BF16)
    identB = alloc("identB", [128, 128], BF16)
    trinegB = alloc("trinegB", [128, 128], BF16)
    negidentB = alloc("negidentB", [128, 128], BF16)
    epsln = alloc("epsln", [128, 2], F32)
    zerosB = alloc("zerosB", [1, 512], BF16)
    HT = alloc("HT", [128, 8, L], BF16)
    Hoff = cx.cur
    H = alloc("H", [128, NT, D], F32)
    cosT = alloc("cosT", [128, L], F32)
    sinT = alloc("sinT", [128, L], F32)
    b_tab = Buf("tab")
    base_mark = cx.mark()

    b_cst = Buf("cst")
    Hb = [Buf(f"H{t}") for t in range(NT)]
    HTb = [Buf(f"HT{t}") for t in range(NT)]
    hsb = [Buf(f"hs{t}") for t in range(NT)]
    outb = Buf("out")

    identF = cst[:, 0:128]
    tri01 = cst[:, 128:256]
    trinegbig = cst[:, 384:512]
    invf = cst[:, 512:513]
    sinvf = cst[:, 513:514]
    onehotK = c8b[0:8, 0:2048]
    selw = c8b[0:8, 2048:2560]
    bpen = c8b[0:8, 2560:2568]

    PS = []
    for i in range(8):
        t = nc.alloc_psum_tensor(f"ps{i}", [128, 512], F32)
        PS.append((t, Buf(f"ps{i}")))

    dma("sp", cst[:], cst_d[:, :], writes=[b_cst])
    dma("pool", c8b[:], cst8_d[:, :], writes=[b_cst], owner=Buf("c8"))
    op("dve", lambda e: e.tensor_copy(out=identB[:], in_=cst[:, 0:128]), [b_cst], [b_cst])
    op("dve", lambda e: e.tensor_copy(out=trinegB[:], in_=cst[:, 256:384]), [b_cst], [b_cst])
    op("dve", lambda e: e.tensor_scalar(out=negidentB[:], in0=cst[:, 0:128], scalar1=-1.0, scalar2=None, op0=ALU.mult), [b_cst], [b_cst])
    op("dve", lambda e: e.memset(epsln[:, 0:1], 1e-5), [], [b_cst])
    op("dve", lambda e: e.memset(epsln[:, 1:2], 1e-6), [], [b_cst])
    op("dve", lambda e: e.memset(zerosB[:], 0.0), [], [b_cst])
    cx.barrier()

    def load_w(dst, src_rows_cols, buf, q="pool"):
        dma(q, dst, src_rows_cols.rearrange("(c p) f -> p c f", p=128), writes=[buf])

    def make_HT(t, pool):
        for half in range(2):
            pt, pb = pool.get()
            for c4 in range(4):
                c = half * 4 + c4
                op("pe", lambda e, c=c, c4=c4: e.transpose(out=pt[:, c4 * 128:(c4 + 1) * 128],
                                                           in_=H[:, t, c * 128:(c + 1) * 128], identity=identF),
                   [Hb[t], b_cst], [pb])
            op("act", lambda e: e.activation(out=HT[:, half * 4:(half + 1) * 4, t * 128:(t + 1) * 128],
                                             in_=pt[:, :].rearrange("p (a b) -> p a b", a=4), func=AF.Copy),
               [pb], [HTb[t]])

    def ln_stats(t, stat, b_stat):
        st = stat[:, t, :]
        bs_ = [b_stat[t]]
        op("dve", lambda e: e.bn_stats(out=st[:, 0:6], in_=H[:, t, 0:512]), [Hb[t]], bs_)
        op("dve", lambda e: e.bn_stats(out=st[:, 6:12], in_=H[:, t, 512:1024]), [Hb[t]], bs_)
        op("dve", lambda e: e.bn_aggr(out=st[:, 12:14], in_=st[:, 0:12]), bs_, bs_)
        op("act", lambda e: e.activation(out=st[:, 14:15], in_=st[:, 13:14], func=AF.Sqrt, bias=epsln[:, 0:1], scale=1.0),
           bs_ + [b_cst], bs_)
        op("dve", lambda e: e.reciprocal(out=st[:, 15:16], in_=st[:, 14:15]), bs_, bs_)
        op("dve", lambda e: e.scalar_tensor_tensor(out=st[:, 16:17], in0=st[:, 12:13], scalar=-1.0, in1=st[:, 15:16], op0=ALU.mult, op1=ALU.mult), bs_, bs_)

    def ln_apply(t, gbc, bbc, b_par, stat, b_stat, between=None):
        hv = H[:, t, :]
        st = stat[:, t, :]
        op("act", lambda e: e.activation(out=hv, in_=hv, func=AF.Identity, bias=st[:, 16:17], scale=st[:, 15:16]), [Hb[t], b_stat[t]], [Hb[t]])
        if between is not None:
            between()
        op("dve", lambda e: e.tensor_tensor(out=hv, in0=hv, in1=gbc[:], op=ALU.mult), [Hb[t], b_par], [Hb[t]])
        op("dve", lambda e: e.tensor_tensor(out=hv, in0=hv, in1=bbc[:], op=ALU.add), [Hb[t], b_par], [Hb[t]])

    def rope_tables(seq, cosT, sinT, b_tab):
        m = cx.mark()
        posf = alloc("posf", [128, L], F32)
        tmp = alloc("rtmp", [128, L], F32)
        tmi = alloc("rtmi", [128, L], I32)
        b_t = Buf("ropetmp")
        dma("pool", posf[:], pos[seq, :].partition_broadcast(128), writes=[b_t])
        for (tab, fcol, shift) in ((sinT, sinvf, 0.0), (cosT, invf, 0.25)):
            op("dve", lambda e, fcol=fcol, shift=shift: e.tensor_scalar(out=tmp[:], in0=posf[:], scalar1=fcol, scalar2=shift,
                                                                         op0=ALU.mult, op1=ALU.add), [b_t, b_cst], [b_t])
            op("dve", lambda e: e.tensor_copy(out=tmi[:], in_=tmp[:]), [b_t], [b_t])
            op("dve", lambda e: e.tensor_copy(out=tab[:], in_=tmi[:]), [b_t], [b_tab])
            op("dve", lambda e: e.tensor_tensor(out=tmp[:], in0=tmp[:], in1=tab[:], op=ALU.subtract), [b_t, b_tab], [b_t])
            op("dve", lambda e: e.tensor_scalar(out=tab[:], in0=tmp[:], scalar1=0.5, scalar2=None, op0=ALU.is_gt), [b_t], [b_tab])
            op("dve", lambda e: e.tensor_tensor(out=tmp[:], in0=tmp[:], in1=tab[:], op=ALU.subtract), [b_t, b_tab], [b_t])
            op("dve", lambda e: e.tensor_scalar(out=tab[:], in0=tmp[:], scalar1=-0.5, scalar2=None, op0=ALU.is_lt), [b_t], [b_tab])
            op("dve", lambda e: e.tensor_tensor(out=tmp[:], in0=tmp[:], in1=tab[:], op=ALU.add), [b_t, b_tab], [b_t])
            op("act", lambda e: e.activation(out=tab[:], in_=tmp[:], func=AF.Sin, scale=2 * np.pi), [b_t], [b_tab])
        cx.release(m)

    def proj_fm(wt, col0, M, tc, pt, pb, wbuf, pbase=0):
        for c in range(8):
            op("pe", lambda e, c=c: e.matmul(pt[pbase:pbase + M, 0:512], lhsT=wt[:, c, col0:col0 + M],
                                             rhs=HT[:, c, tc * 512:(tc + 1) * 512], start=(c == 0), stop=(c == 7)),
               [wbuf] + HTb[tc * 4:(tc + 1) * 4], [pb])

    def proj_tm(wt, col0, N, t, pt, pb, wbuf, ocol=0):
        for c in range(8):
            op("pe", lambda e, c=c: e.matmul(pt[:, ocol:ocol + N], lhsT=HT[:, c, t * 128:(t + 1) * 128],
                                             rhs=wt[:, c, col0:col0 + N], start=(c == 0), stop=(c == 7)),
               [wbuf, HTb[t]], [pb])

    def roped_proj(wt, wbuf, col, colr, dstT, b_dst, cosT, sinT, b_tab, tmps, pspool, ksum=None):
        ntile = dstT.shape[1]
        (t1, t2, b_tmp) = tmps
        for ti in range(ntile):
            for tc in range(4):
                py, pyb = pspool.get()
                pr, prb = pspool.get()
                proj_fm(wt, col + ti * 128, 128, tc, py, pyb, wbuf)
                proj_fm(wt, colr + ti * 128, 128, tc, pr, prb, wbuf)
                sl = slice(tc * 512, (tc + 1) * 512)
                op("dve", lambda e: e.tensor_tensor(out=t1[:], in0=pr[:, :], in1=sinT[:, sl], op=ALU.mult), [prb, b_tab], [b_tmp])
                op("dve", lambda e: e.tensor_tensor(out=t2[:], in0=py[:, :], in1=cosT[:, sl], op=ALU.mult), [pyb, b_tab], [b_tmp])
                if ksum is not None:
                    op("pool", lambda e: e.tensor_tensor(out=t2[:], in0=t1[:], in1=t2[:], op=ALU.add), [b_tmp], [b_tmp])
                    op("act", lambda e: e.activation(out=dstT[:, ti, sl], in_=t2[:], func=AF.Copy), [b_tmp], [b_dst])
                    (km, b_km) = ksum
                    op("dve", lambda e: e.tensor_reduce(out=km[:, ti, tc * 2:(tc + 1) * 2], in_=t2[:, :].rearrange("p (a b) -> p a b", a=2),
                                                        axis=AX.X, op=ALU.add), [b_tmp], [b_km])
                else:
                    op("pool", lambda e: e.tensor_tensor(out=dstT[:, ti, sl], in0=t1[:], in1=t2[:], op=ALU.add), [b_tmp], [b_dst])

    def v_aug_proj(wt, wbuf, col, vaug, b_v, pspool):
        op("dve", lambda e: e.memset(vaug[:, :, :, 64:65], 1.0), [], [b_v])
        for t in range(NT):
            pt, pb = pspool.get()
            proj_tm(wt, col, 256, t, pt, pb, wbuf)
            op("act", lambda e: e.activation(out=vaug[:, t, :, 0:64], in_=pt[:, 0:256].rearrange("p (a b) -> p a b", a=4), func=AF.Copy),
               [pb], [b_v])

    def attention(qT, kT, b_qk, vaug, b_v, maskfn, mix_chunk0, pools, otm, b_otm, pTs, rs, b_rs, mixT, mixb, qc_hook=None):
        (ps_s, ps_acc, ps_tr) = pools

        def att_gen(qc):
                for h in range(4):
                    ptile, bp = h // 2, (h % 2) * 64
                    acc, accb = ps_acc.get()
                    op("pe", lambda e: e.matmul(acc[:, 0:260], lhsT=zerosB[0:1, 0:128], rhs=zerosB[0:1, 0:260], start=True, stop=False,
                                                skip_group_check=True), [b_cst], [accb])
                    prev = None

                    def emit_pv(args):
                        (j_, t0_, pT_, pTb_) = args
                        for t in range(t0_, 4 * qc + 4):
                            i = t - 4 * qc
                            op("pe", lambda e, t=t, i=i: e.matmul(acc[:, i * 65:(i + 1) * 65], lhsT=pT_[:, (t - t0_) * 128:(t - t0_ + 1) * 128],
                                                                  rhs=vaug[:, j_, h, :], start=False, stop=(j_ == t), skip_group_check=True),
                               [pTb_, b_v], [accb])

                    for j in range(4 * qc + 4):
                        t0 = max(j, 4 * qc)
                        q0, q1 = t0 * 128, (4 * qc + 4) * 128
                        wq = q1 - q0
                        st, sb = ps_s.get()
                        op("pe", lambda e: e.matmul(st[:, 0:wq], lhsT=kT[bp:bp + 64, ptile, j * 128:(j + 1) * 128],
                                                    rhs=qT[bp:bp + 64, ptile, q0:q1], start=True, stop=False, skip_group_check=True),
                           [b_qk], [sb])
                        maskfn(h, qc, j, t0, st, sb, wq)
                        pT, pTb = pTs.get()
                        op("act", lambda e: e.activation(out=pT[:, 0:wq], in_=st[:, 0:wq], func=AF.Exp, scale=0.125), [sb], [pTb])
                        if prev is not None:
                            emit_pv(prev)
                        prev = (j, t0, pT, pTb)
                        yield
                    emit_pv(prev)
                    acc3 = acc[:, 0:260].rearrange("p (a b) -> p a b", a=4)
                    op("dve", lambda e: e.reciprocal(out=rs[:, :], in_=acc3[:, :, 64]), [accb], [b_rs])
                    for i in range(4):
                        op("dve", lambda e, i=i: e.tensor_scalar(out=otm[:, i, h * 64:(h + 1) * 64], in0=acc3[:, i, 0:64],
                                                                 scalar1=rs[:, i:i + 1], scalar2=None, op0=ALU.mult), [accb, b_rs], [b_otm])
                    yield
                for i in range(4):
                    t = 4 * qc + i
                    pt, pb = ps_tr.get()
                    for f in range(2):
                        op("pe", lambda e, f=f: e.transpose(out=pt[:, f * 128:(f + 1) * 128], in_=otm[:, i, f * 128:(f + 1) * 128], identity=identF),
                           [b_otm, b_cst], [pb])
                    op("act", lambda e: e.activation(out=mixT[:, mix_chunk0:mix_chunk0 + 2, t * 128:(t + 1) * 128],
                                                     in_=pt[:, 0:256].rearrange("p (a b) -> p a b", a=2), func=AF.Copy), [pb], [mixb[t]])
                    yield

        def drain(g):
            for _ in g:
                pass

        def merge(ga, na, gb, nb):
            ia = ib = 0
            da = db = False
            while not (da and db):
                if not da and (db or ia * nb <= ib * na):
                    try:
                        next(ga)
                    except StopIteration:
                        da = True
                    ia += 1
                else:
                    try:
                        next(gb)
                    except StopIteration:
                        db = True
                    ib += 1

        if qc_hook is not None:
            drain(qc_hook(0))
        for qc in range(4):
            ga = att_gen(qc)
            if qc_hook is not None and qc + 1 < 4:
                n_att = 4 * (4 * qc + 4) + 8
                n_hook = 4 + sum(((4 * (qc + 1) + i + 1) * 128 + 511) // 512 for i in range(4)) + N_BISECT + 2
                merge(ga, n_att, qc_hook(qc + 1), n_hook)
            else:
                drain(ga)

    for seq in range(nseq):
        pool_tr = Rot(PS[0:4])
        for t in range(NT):
            dma("sp", H[:, t, :], x[seq, t * 128:(t + 1) * 128, :], writes=[Hb[t]])
        for t in range(NT):
            make_HT(t, pool_tr)
        rope_tables(seq, cosT, sinT, b_tab)

        for layer in layers:
            for t in range(NT):
                dma("sp", hs[t * 128:(t + 1) * 128, :], H[:, t, :], reads=[Hb[t]], writes=[hsb[t]], owner=Hb[t])
            cx.barrier()
            cx.cur, cx.cur2 = base_mark
            cx.cur2, cx.lim2 = Hoff, Hoff + 65536
            mixT = alloc("mixT", [128, 8, L], BF16)
            mixb = [Buf(f"mix{t}") for t in range(NT)]

            if stop == 'ht':
                cx.barrier()
                return nc, cx
            mg = cx.mark()
            Wg2 = [alloc(f"Wg{p}", [128, 8, 784], BF16) for p in range(2)]; b_Wg2 = [Buf(f"Wg{p}") for p in range(2)]
            for p in range(2):
                load_w(Wg2[p][:], w_in[layer][:, COFF[f"g{p}"]:COFF[f"g{p}"] + 784], b_Wg2[p])
            for p in range(2):
                m0 = cx.mark()
                Wg = Wg2[p]; b_Wg = b_Wg2[p]
                gu = alloc("gu", [16, 128], BF16)
                nbias = alloc("nbias", [128, 1], F32)
                gain = alloc("gain", [128, 2, 128], F32)
                b_gp = Buf("gpar")
                rankT = alloc("rankT", [16, L], BF16); b_rank = Buf("rank")
                la = alloc("la", [128, L], F32, 1); b_la = Buf("la")
                bc = alloc("bc", [128, L], F32, 1); b_bc = Buf("bc")
                nbl = alloc("nbl", [128, 16], F32); dec = alloc("dec", [128, 16], F32); b_nbl = Buf("nbl")
                qdT = alloc("qdT", [128, L], BF16, 1); kiT = alloc("kiT", [128, L], BF16, 1); b_qk = Buf("gqk")
                kd = alloc("kd", [128, NT, 128], BF16, 1); b_kd = Buf("kd")
                vtm = alloc("vtm", [128, NT, 256], BF16, 1); b_v = Buf("gv")
                Sall = alloc("Sall", [128, NT, 128], BF16, 1); b_Sall = Buf("Sall")
                Sst = alloc("Sst", [128, 128], F32); b_S = Buf("S")
                e1 = alloc("e1", [128, 512], F32); e2 = alloc("e2", [128, 512], F32); e3 = alloc("e3", [128, 512], F32)
                b_e = [Buf("e1"), Buf("e2"), Buf("e3")]
                kdT = alloc("kdT", [128, 512], F32); b_kdT = Buf("kdT")
                AT = [alloc("AT0", [128, 128], BF16), alloc("AT1", [128, 128], BF16)]; b_AT = [Buf("AT0"), Buf("AT1")]
                sg = alloc("sg", [128, 256], F32); b_sg = Buf("sg")
                otm = alloc("otmg", [128, 256], F32); b_otm = Buf("otmg")
                sq = alloc("sq", [128, 256], F32); ssq = alloc("ssq", [128, 4], F32); b_ssq = Buf("ssq")

                dma("pool", gu[:], g_up[layer][:, p * 128:(p + 1) * 128], writes=[b_gp])
                dma("sp", nbias[:], g_bias[layer, p * 128:(p + 1) * 128].rearrange("(p o) -> p o", o=1), writes=[b_gp], owner=Buf("nb"))
                for hh in range(2):
                    dma("sp", gain[:, hh, :], g_gain[layer, :].partition_broadcast(128), writes=[b_gp], owner=Buf("gn"))
                op("dve", lambda e: e.tensor_scalar(out=nbias[:], in0=nbias[:], scalar1=-1.0, scalar2=None, op0=ALU.mult), [b_gp], [b_gp])
                chk('g1')
                pp = Rot(PS[0:4])
                for tc in range(4):
                    pt, pb = pp.get()
                    for c in range(8):
                        op("pe", lambda e, c=c: e.matmul(pt[0:16, 0:512], lhsT=Wg[:, c, 768:784], rhs=HT[:, c, tc * 512:(tc + 1) * 512],
                                                         start=(c == 0), stop=(c == 7)), [b_Wg] + HTb[tc * 4:tc * 4 + 4], [pb])
                    op("act", lambda e: e.activation(out=rankT[:, tc * 512:(tc + 1) * 512], in_=pt[0:16, 0:512], func=AF.Copy), [pb], [b_rank])
                for tc in range(4):
                    pt, pb = pp.get()
                    sl = slice(tc * 512, (tc + 1) * 512)
                    op("pe", lambda e: e.matmul(pt[:, 0:512], lhsT=gu[0:16, :], rhs=rankT[0:16, sl], start=True, stop=True), [b_gp, b_rank], [pb])
                    op("act", lambda e: e.activation(out=la[:, sl], in_=pt[:, 0:512], func=AF.Exp, bias=nbias[:, 0:1], scale=-1.0), [pb, b_gp], [b_la])
                op("act", lambda e: e.activation(out=la[:], in_=la[:], func=AF.Ln, bias=1.0, scale=1.0), [b_la], [b_la])
                chk('g2')
                for n in range(NT):
                    sl = slice(n * 128, (n + 1) * 128)
                    op("dve", lambda e: e.tensor_tensor_scan(out=bc[:, sl], data0=la[:, sl], data1=cst[:, 640:768], initial=0.0,
                                                             op0=ALU.add, op1=ALU.add), [b_la, b_cst], [b_bc])
                chk('g3')
                cx_chk = None
                op("dve", lambda e: e.tensor_scalar(out=nbl[:], in0=bc[:, :].rearrange("p (n c) -> p n c", c=128)[:, :, 127],
                                                    scalar1=-1.0 / 16.0, scalar2=None, op0=ALU.mult), [b_bc], [b_nbl])
                op("act", lambda e: e.activation(out=dec[:], in_=nbl[:], func=AF.Exp), [b_nbl], [b_nbl])
                chk('g3b')
                ptr = Rot(PS[4:6])
                for tc in range(4):
                    sl = slice(tc * 512, (tc + 1) * 512)
                    pq, pqb = pp.get()
                    pk, pkb = pp.get()
                    proj_fm(Wg, 0, 128, tc, pq, pqb, b_Wg)
                    proj_fm(Wg, 128, 128, tc, pk, pkb, b_Wg)
                    op("act", lambda e: e.activation(out=e1[:], in_=bc[:, sl], func=AF.Exp, scale=-1.0 / 16.0), [b_bc], [b_e[0]])
                    op("dve", lambda e: e.scalar_tensor_tensor(out=qdT[:, sl], in0=pq[:, :], scalar=0.125, in1=e1[:], op0=ALU.mult, op1=ALU.mult),
                       [pqb, b_e[0]], [b_qk])
                    op("act", lambda e: e.activation(out=e2[:], in_=bc[:, sl], func=AF.Exp, scale=1.0 / 16.0), [b_bc], [b_e[1]])
                    op("dve", lambda e: e.tensor_tensor(out=kiT[:, sl], in0=pk[:, :], in1=e2[:], op=ALU.mult), [pkb, b_e[1]], [b_qk])
                    for n4 in range(4):
                        n = tc * 4 + n4
                        op("act", lambda e, n=n, n4=n4: e.activation(out=e3[:, n4 * 128:(n4 + 1) * 128], in_=bc[:, n * 128:(n + 1) * 128], func=AF.Exp,
                                                                     bias=nbl[:, n:n + 1], scale=1.0 / 16.0), [b_bc, b_nbl], [b_e[2]])
                    op("dve", lambda e: e.tensor_tensor(out=kdT[:], in0=pk[:, :], in1=e3[:], op=ALU.mult), [pkb, b_e[2]], [b_kdT])
                    pt, pb = ptr.get()
                    for n4 in range(4):
                        op("pe", lambda e, n4=n4: e.transpose(out=pt[:, n4 * 128:(n4 + 1) * 128], in_=kdT[:, n4 * 128:(n4 + 1) * 128], identity=identF),
                           [b_kdT, b_cst], [pb])
                    op("act", lambda e: e.activation(out=kd[:, tc * 4:(tc + 1) * 4, :], in_=pt[:, :].rearrange("p (a b) -> p a b", a=4), func=AF.Copy),
                       [pb], [b_kd])
                chk('g4')
                for t in range(NT):
                    pt, pb = pp.get()
                    proj_tm(Wg, 256, 256, t, pt, pb, b_Wg)
                    op("act", lambda e: e.activation(out=vtm[:, t, :], in_=pt[:, 0:256], func=AF.Copy), [pb], [b_v])
                chk('g5')
                op("dve", lambda e: e.memset(Sst[:], 0.0), [], [b_S])
                op("dve", lambda e: e.memset(Sall[:, 0, :], 0.0), [], [b_Sall])
                for n in range(NT - 1):
                    pt, pb = pp.get()
                    for hh in range(2):
                        op("pe", lambda e, hh=hh: e.matmul(pt[hh * 64:(hh + 1) * 64, 0:128], lhsT=kd[:, n, hh * 64:(hh + 1) * 64],
                                                           rhs=vtm[:, n, hh * 128:(hh + 1) * 128], start=True, stop=True), [b_kd, b_v], [pb])
                    op("dve", lambda e: e.scalar_tensor_tensor(out=Sst[:], in0=Sst[:], scalar=dec[:, n:n + 1], in1=pt[:, 0:128],
                                                               op0=ALU.mult, op1=ALU.add), [b_S, b_nbl, pb], [b_S])
                    op("act", lambda e: e.activation(out=Sall[:, n + 1, :], in_=Sst[:], func=AF.Copy), [b_S], [b_Sall])
                chk('g6')
                ps_o = Rot(PS[4:6]); ps_g = Rot(PS[6:8])
                o_all = alloc("o_all", [128, NT, 256], F32, 1); b_oall = [Buf(f"oall{n}") for n in range(NT)]
                sg_all = alloc("sg_all", [128, NT, 256], BF16); b_sgall = [Buf(f"sgall{n}") for n in range(NT)]
                ssq_all = alloc("ssq_all", [128, NT, 2], F32); rstd_all = alloc("rstd_all", [128, NT, 2], F32); b_ssqa = Buf("ssqa")
                otms = [(alloc(f"otmg{i}", [128, 256], F32), Buf(f"otmg{i}")) for i in range(2)]
                for n in range(NT):
                    sl = slice(n * 128, (n + 1) * 128)
                    po, pob = ps_o.get()
                    for hh in range(2):
                        bp = hh * 64
                        pa, pab = pp.get()
                        op("pe", lambda e: e.matmul(pa[:, 0:128], lhsT=kiT[bp:bp + 64, sl], rhs=qdT[bp:bp + 64, sl], start=True, stop=True), [b_qk], [pab])
                        op("dve", lambda e: e.tensor_tensor(out=AT[hh][:], in0=pa[:, 0:128], in1=tri01, op=ALU.mult), [pab, b_cst], [b_AT[hh]])
                        op("pe", lambda e: e.matmul(po[:, hh * 128:(hh + 1) * 128], lhsT=AT[hh][:], rhs=vtm[:, n, hh * 128:(hh + 1) * 128],
                                                    start=True, stop=False, skip_group_check=True), [b_AT[hh], b_v], [pob])
                        op("pe", lambda e: e.matmul(po[:, hh * 128:(hh + 1) * 128], lhsT=qdT[bp:bp + 64, sl], rhs=Sall[bp:bp + 64, n, :],
                                                    start=False, stop=True, skip_group_check=True), [b_qk, b_Sall], [pob])
                    for hh in range(2):
                        op("act", lambda e, hh=hh: e.activation(out=sq[:, hh * 128:(hh + 1) * 128], in_=po[:, hh * 128:(hh + 1) * 128], func=AF.Square,
                                                                accum_out=ssq_all[:, n, hh:hh + 1]), [pob], [b_ssqa])
                    op("act", lambda e: e.activation(out=o_all[:, n, :], in_=po[:, 0:256], func=AF.Copy), [pob], [b_oall[n]])
                op("act", lambda e: e.activation(out=rstd_all[:, :, :], in_=ssq_all[:, :, :], func=AF.Sqrt, bias=epsln[:, 1:2], scale=1.0 / 128.0), [b_ssqa, b_cst], [b_ssqa])
                op("dve", lambda e: e.reciprocal(out=rstd_all[:, :, :], in_=rstd_all[:, :, :]), [b_ssqa], [b_ssqa])
                for n in range(NT):
                    pg, pgb = ps_g.get()
                    proj_tm(Wg, 512, 256, n, pg, pgb, b_Wg)
                    op("act", lambda e: e.activation(out=sg_all[:, n, :], in_=pg[:, 0:256], func=AF.Silu), [pgb], [b_sgall[n]])
                    op("pool", lambda e: e.tensor_tensor(out=sg_all[:, n, :], in0=sg_all[:, n, :], in1=gain[:, :, :].rearrange("p a b -> p (a b)"), op=ALU.mult),
                       [b_sgall[n], b_gp], [b_sgall[n]])
                for n in range(NT):
                    sl = slice(n * 128, (n + 1) * 128)
                    otm, b_otm = otms[n % 2]
                    for hh in range(2):
                        op("dve", lambda e, hh=hh: e.scalar_tensor_tensor(out=otm[:, hh * 128:(hh + 1) * 128], in0=o_all[:, n, hh * 128:(hh + 1) * 128],
                                                                          scalar=rstd_all[:, n, hh:hh + 1], in1=sg_all[:, n, hh * 128:(hh + 1) * 128],
                                                                          op0=ALU.mult, op1=ALU.mult), [b_oall[n], b_ssqa, b_sgall[n]], [b_otm])
                    pt, pb = ps_o.get()
                    for hh in range(2):
                        op("pe", lambda e, hh=hh: e.transpose(out=pt[:, hh * 128:(hh + 1) * 128], in_=otm[:, hh * 128:(hh + 1) * 128], identity=identF),
                           [b_otm, b_cst], [pb])
                    op("act", lambda e: e.activation(out=mixT[:, 2 * p:2 * p + 2, sl], in_=pt[:, 0:256].rearrange("p (a b) -> p a b", a=2), func=AF.Copy),
                       [pb], [mixb[n]])
                cx.release(m0)
                chk('p0')
            cx.release(mg)

            if stop == 'gla':
                cx.barrier()
                return nc, cx
            def attn_common():
                qT = alloc("qT", [128, 2, L], BF16, 1); kT = alloc("kT", [128, 2, L], BF16, 1)
                vaug = alloc("vaug", [128, NT, 4, 65], BF16, 1)
                return qT, kT, vaug, Buf("aqk"), Buf("av")

            m0 = cx.mark()
            qT, kT, vaug, b_qk, b_v = attn_common()
            kmT = alloc("kmT", [128, 2, 8], F32); kmB = alloc("kmB", [128, 2, 8], BF16); b_km = Buf("km")
            mbT = alloc("mbT", [8, 4, L], BF16); b_mb = Buf("mbT")
            m1 = cx.mark()
            chk('m1')
            Wa = alloc("Wa", [128, 8, 512], BF16); b_Wa = Buf("Wa")
            Wb = alloc("Wb", [128, 8, 512], BF16); b_Wb = Buf("Wb")
            t1 = alloc("t1", [128, 512], F32); t2 = alloc("t2", [128, 512], F32); b_tmp = Buf("rt")
            pp = Rot(PS[0:6])
            load_w(Wa[:], w_in[layer][:, COFF["mq"]:COFF["mq"] + 512], b_Wa)
            load_w(Wb[:], w_in[layer][:, COFF["mk"]:COFF["mk"] + 512], b_Wb)
            roped_proj(Wa, b_Wa, 0, 256, qT, b_qk, cosT, sinT, b_tab, (t1, t2, b_tmp), pp)
            roped_proj(Wb, b_Wb, 0, 256, kT, b_qk, cosT, sinT, b_tab, (t1, t2, b_tmp), pp, ksum=(kmT, b_km))
            chk('m2a')
            load_w(Wa[:, :, 0:256], w_in[layer][:, COFF["mv"]:COFF["mv"] + 256], b_Wa)
            v_aug_proj(Wa, b_Wa, 0, vaug, b_v, pp)
            op("dve", lambda e: e.tensor_copy(out=kmB[:], in_=kmT[:]), [b_km], [b_km])
            cx.release(m1)
            chk('m2')
            Z = alloc("Z", [128, 4, 8], F32); gm = alloc("gm", [128, 4, 8], F32); top8 = alloc("top8", [128, 4, 8], F32); b_Z = Buf("Z")
            otm = alloc("otm", [128, 4, 256], F32); b_otm = Buf("otm")
            pTl = [(alloc(f"pT{i}", [128, 512], BF16), Buf(f"pT{i}")) for i in range(3)]
            rs = alloc("rs", [128, 4], F32); b_rs = Buf("rs")
            op("pool", lambda e: e.memset(mbT[:], 0.0), [], [b_mb])
            pg = Rot(PS[0:2])
            for t in range(8, NT):
                pt, pb = pg.get()
                for h in range(4):
                    ptile, bp = h // 2, (h % 2) * 64
                    op("pe", lambda e, h=h: e.matmul(pt[:, h * 8:(h + 1) * 8], lhsT=qT[bp:bp + 64, ptile, t * 128:(t + 1) * 128],
                                                     rhs=kmB[bp:bp + 64, ptile, :], start=True, stop=False, skip_group_check=True), [b_qk, b_km], [pb])
                    op("pe", lambda e, h=h: e.matmul(pt[:, h * 8:(h + 1) * 8], lhsT=onehotK[0:8, t * 128:(t + 1) * 128],
                                                     rhs=bpen, start=False, stop=True, skip_group_check=True), [b_cst], [pb])
                op("dve", lambda e: e.tensor_copy(out=gm[:], in_=pt[:, 0:32].rearrange("p (a b) -> p a b", a=4)), [pb], [b_Z])
                for h in range(4):
                    op("dve", lambda e, h=h: e.max(out=top8[:, h, :], in_=gm[:, h, :]), [b_Z], [b_Z])
                for h in range(4):
                    op("dve", lambda e, h=h: e.tensor_scalar(out=Z[:, h, :], in0=gm[:, h, :], scalar1=top8[:, h, 3:4], scalar2=NEG,
                                                             op0=ALU.is_lt, op1=ALU.mult), [b_Z], [b_Z])
                pt2, pb2 = pg.get()
                for h in range(4):
                    op("pe", lambda e, h=h: e.transpose(out=pt2[0:8, h * 128:(h + 1) * 128], in_=Z[:, h, :], identity=identF), [b_Z, b_cst], [pb2])
                op("dve", lambda e: e.tensor_copy(out=mbT[0:8, :, t * 128:(t + 1) * 128], in_=pt2[0:8, :].rearrange("p (a b) -> p a b", a=4)),
                   [pb2], [b_mb])
            chk('m3')

            def moba_mask(h, qc, j, t0, st, sb, wq):
                q0 = t0 * 128
                diag = (j >= 4 * qc)
                op("pe", lambda e: e.matmul(st[:, 0:wq], lhsT=onehotK[0:8, j * 128:(j + 1) * 128], rhs=mbT[0:8, h, q0:q0 + wq],
                                            start=False, stop=(not diag), skip_group_check=True), [b_cst, b_mb], [sb])
                if diag:
                    op("pe", lambda e: e.matmul(st[:, 0:128], lhsT=trinegB[:], rhs=identB[:], start=False, stop=True, skip_group_check=True), [b_cst], [sb])

            attention(qT, kT, b_qk, vaug, b_v, moba_mask, 4, (Rot(PS[2:5]), Rot(PS[5:7]), Rot(PS[7:8])), otm, b_otm, Rot(pTl), rs, b_rs, mixT, mixb)
            cx.release(m0)

            if stop == 'moba':
                cx.barrier()
                return nc, cx
            m0 = cx.mark()
            qT, kT, vaug, b_qk, b_v = attn_common()
            kiT = alloc("kiT", [128, 1, L], BF16, 1); b_ki = Buf("ki")
            wT = alloc("wT", [8, L], BF16, 1); b_wT = Buf("wT")
            qiT = alloc("qiT", [128, 4, L], BF16, 1); b_qi = Buf("qi")
            m1 = cx.mark()
            Wa = alloc("Wa", [128, 8, 512], BF16); b_Wa = Buf("Wa")
            Wb = alloc("Wb", [128, 8, 512], BF16); b_Wb = Buf("Wb")
            t1 = alloc("t1", [128, 512], F32); t2 = alloc("t2", [128, 512], F32); b_tmp = Buf("rt")
            pp = Rot(PS[0:6])
            tm3 = (t1, t2, b_tmp)
            Wc = alloc("Wc", [128, 8, 512], BF16); b_Wc = Buf("Wc")
            Wd = alloc("Wd", [128, 8, 512], BF16); b_Wd = Buf("Wd")
            load_w(Wa[:], w_in[layer][:, COFF["sq"]:COFF["sq"] + 512], b_Wa)
            load_w(Wb[:], w_in[layer][:, COFF["sk"]:COFF["sk"] + 512], b_Wb)
            load_w(Wc[:], w_in[layer][:, COFF["iq"]:COFF["iq"] + 512], b_Wc)
            load_w(Wd[:], w_in[layer][:, COFF["iqr"]:COFF["iqr"] + 512], b_Wd)
            roped_proj(Wa, b_Wa, 0, 256, qT, b_qk, cosT, sinT, b_tab, tm3, pp)
            roped_proj(Wb, b_Wb, 0, 256, kT, b_qk, cosT, sinT, b_tab, tm3, pp)
            load_w(Wa[:, :, 0:264], w_in[layer][:, COFF["ik"]:COFF["ik"] + 264], b_Wa)
            load_w(Wb[:, :, 0:256], w_in[layer][:, COFF["sv"]:COFF["sv"] + 256], b_Wb)
            for ti in range(4):
                for tc in range(4):
                    py, pyb = pp.get(); pr, prb = pp.get()
                    proj_fm(Wc, ti * 128, 128, tc, py, pyb, b_Wc)
                    proj_fm(Wd, ti * 128, 128, tc, pr, prb, b_Wd)
                    sl = slice(tc * 512, (tc + 1) * 512)
                    op("dve", lambda e: e.tensor_tensor(out=t1[:], in0=pr[:, :], in1=sinT[:, sl], op=ALU.mult), [prb, b_tab], [b_tmp])
                    op("dve", lambda e: e.tensor_tensor(out=t2[:], in0=py[:, :], in1=cosT[:, sl], op=ALU.mult), [pyb, b_tab], [b_tmp])
                    op("pool", lambda e: e.tensor_tensor(out=qiT[:, ti, sl], in0=t1[:], in1=t2[:], op=ALU.add), [b_tmp], [b_qi])
            roped_proj(Wa, b_Wa, 0, 128, kiT, b_ki, cosT, sinT, b_tab, tm3, pp)
            for tc in range(4):
                pt, pb = pp.get()
                for c in range(8):
                    op("pe", lambda e, c=c: e.matmul(pt[0:8, 0:512], lhsT=Wa[:, c, 256:264], rhs=HT[:, c, tc * 512:(tc + 1) * 512],
                                                     start=(c == 0), stop=(c == 7)), [b_Wa] + HTb[tc * 4:tc * 4 + 4], [pb])
                op("act", lambda e: e.activation(out=wT[:, tc * 512:(tc + 1) * 512], in_=pt[0:8, 0:512], func=AF.Copy), [pb], [b_wT])
            v_aug_proj(Wb, b_Wb, 0, vaug, b_v, pp)
            cx.release(m1)
            mbs = [HT[:, 0:4, :], alloc("mbB", [128, 4, L], BF16)]
            b_mb2 = [[Buf(f"mb{a}_{i}") for i in range(4)] for a in range(2)]
            isc = [HT[:, 4:6, :].bitcast(F32).rearrange("p a b -> p (a b)"), HT[:, 6:8, :].bitcast(F32).rearrange("p a b -> p (a b)"),
                   alloc("isc2", [128, L], F32), alloc("isc3", [128, L], F32)]
            b_isc = [Buf(f"isc{i}") for i in range(4)]
            qs = alloc("qs", [128, 4, 2, 512], BF16, 1); b_qs = Buf("qs")
            otm = alloc("otm", [128, 4, 256], F32); b_otm = Buf("otm")
            pTl = [(alloc(f"pT{i}", [128, 512], BF16), Buf(f"pT{i}")) for i in range(3)]
            rs = alloc("rs", [128, 4], F32); b_rs = Buf("rs")
            bsA = alloc("bsA", [128, 32], F32); bsi = alloc("bsi", [128, 4], I32)
            lo = bsA[:, 0:4]; w0 = bsA[:, 4:8]; mid = bsA[:, 8:12]; nmid = bsA[:, 12:16]; cntv = bsA[:, 16:20]; thc = bsA[:, 20:24]; mx = bsA[:, 24:28]
            b_lo = Buf("lo"); b_mid = Buf("mid"); b_cnt = [Buf(f"cnt{i}") for i in range(4)]; b_msk = Buf("msk")
            junkD = alloc("junkD", [128, L], BF16); b_junkD = Buf("junkD")
            junkA = alloc("junkA", [128, L], BF16); b_junkA = Buf("junkA")
            ps_i = Rot(PS[0:3])
            rls = Rot([(alloc(f"rl{i}", [128, 512], BF16), Buf(f"rl{i}")) for i in range(4)])

            def dsa_qc(qc):
                mb = mbs[qc % 2]; b_mbq = b_mb2[qc % 2]
                qsl = slice(qc * 512, (qc + 1) * 512)
                for pr_ in range(4):
                    pt, pb = ps_i.get()
                    op("pe", lambda e: e.matmul(pt[:, 0:512], lhsT=selw[0:8, pr_ * 128:(pr_ + 1) * 128], rhs=wT[0:8, qsl], start=True, stop=True),
                       [b_cst, b_wT], [pb])
                    op("dve", lambda e: e.scalar_tensor_tensor(out=qs[:, pr_, 0, :], in0=pt[:, 0:512], scalar=0.0, in1=qiT[:, pr_, qsl],
                                                               op0=ALU.max, op1=ALU.mult), [pb, b_qi], [b_qs])
                    op("dve", lambda e: e.scalar_tensor_tensor(out=qs[:, pr_, 1, :], in0=pt[:, 0:512], scalar=0.0, in1=qiT[:, pr_, qsl],
                                                               op0=ALU.min, op1=ALU.mult), [pb, b_qi], [b_qs])
                yield
                act_tiles = []
                for i in range(4):
                    t = 4 * qc + i
                    S = (t + 1) * 128
                    if t < 2:
                        if t > 0:
                            op("pool", lambda e: e.memset(mb[:, i, 0:t * 128], 0.0), [], [b_mbq[i]])
                        op("pool", lambda e: e.tensor_copy(out=mb[:, i, t * 128:S], in_=trinegB[:]), [b_cst], [b_mbq[i]])
                        continue
                    act_tiles.append(i)
                    ic, icb = isc[i], b_isc[i]
                    for kc in range((S + 511) // 512):
                        w = min(512, S - kc * 512)
                        ksl = slice(kc * 512, kc * 512 + w)
                        first = True
                        nsum = 0
                        p2, p2b = PS[3]
                        pend = []

                        def flush_one():
                            nonlocal nsum
                            (rl_, rlb_, sg_) = pend.pop(0)
                            op("pe", lambda e: e.matmul(p2[:, 0:w], lhsT=(identB[:] if sg_ == 0 else negidentB[:]), rhs=rl_[:, 0:w],
                                                        start=(nsum == 0), stop=(nsum == 7)), [rlb_, b_cst], [p2b])
                            nsum += 1

                        for k in range(16):
                            hd, sgn = k // 2, k % 2
                            pr_, bp = hd // 2, (hd % 2) * 64
                            pt, pb = ps_i.get()
                            op("pe", lambda e: e.matmul(pt[:, 0:w], lhsT=qs[bp:bp + 64, pr_, sgn, i * 128:(i + 1) * 128], rhs=kiT[bp:bp + 64, 0, ksl],
                                                        start=True, stop=True), [b_qs, b_ki], [pb])
                            o = ALU.max if sgn == 0 else ALU.min
                            if hd % 2 == 1:
                                rl, rlb = rls.get()
                                op("act", lambda e: e.activation(out=rl[:, 0:w], in_=pt[:, 0:w], func=AF.Relu, scale=(1.0 if sgn == 0 else -1.0)), [pb], [rlb])
                                pend.append((rl, rlb, sgn))
                                if len(pend) > 2:
                                    flush_one()
                            elif first:
                                op("dve", lambda e: e.tensor_scalar(out=ic[:, ksl], in0=pt[:, 0:w], scalar1=0.0, scalar2=None, op0=o), [pb], [icb])
                                first = False
                            else:
                                op("dve", lambda e: e.scalar_tensor_tensor(out=ic[:, ksl], in0=pt[:, 0:w], scalar=0.0, in1=ic[:, ksl], op0=o, op1=ALU.add),
                                   [pb, icb], [icb])
                        while pend:
                            flush_one()
                        op("dve", lambda e: e.tensor_tensor(out=ic[:, ksl], in0=p2[:, 0:w], in1=ic[:, ksl], op=ALU.add), [p2b, icb], [icb])
                        yield
                    op("dve", lambda e: e.tensor_reduce(out=lo[:, i:i + 1], in_=ic[:, 0:S], axis=AX.X, op=ALU.min), [icb], [b_lo])
                    op("dve", lambda e: e.tensor_reduce(out=mx[:, i:i + 1], in_=ic[:, 0:S], axis=AX.X, op=ALU.max), [icb], [b_lo])
                    op("dve", lambda e: e.tensor_tensor(out=ic[:, t * 128:S], in0=ic[:, t * 128:S], in1=trinegbig, op=ALU.add), [icb, b_cst], [icb])
                    on_act = (i % 2 == 1)
                    op("dve", lambda e: e.memset(thc[:, i:i + 1], (S - 511.0) if on_act else (S - 255.5)), [], [b_lo])
                a0, a1 = act_tiles[0], act_tiles[-1] + 1
                asl = slice(a0, a1)
                op("dve", lambda e: e.scalar_tensor_tensor(out=w0[:, asl], in0=mx[:, asl], scalar=1.0, in1=lo[:, asl], op0=ALU.add, op1=ALU.subtract),
                   [b_lo], [b_lo])
                for it in range(N_BISECT):
                    ck = 0.5 ** (it + 1)
                    op("dve", lambda e, ck=ck: e.scalar_tensor_tensor(out=mid[:, asl], in0=w0[:, asl], scalar=ck, in1=lo[:, asl], op0=ALU.mult, op1=ALU.add),
                       [b_lo], [b_mid])
                    for i in act_tiles:
                        S = (4 * qc + i + 1) * 128
                        ic, icb = isc[i], b_isc[i]
                        if i % 2 == 1:
                            op("act", lambda e, i=i, S=S, ic=ic: e.activation(out=junkA[:, 0:S], in_=ic[:, 0:S], func=AF.Sign, bias=mid[:, i:i + 1], scale=-1.0,
                                                                              accum_out=cntv[:, i:i + 1]), [icb, b_mid], [b_cnt[i], b_junkA])
                        else:
                            op("dve", lambda e, i=i, S=S, ic=ic: e.tensor_scalar(out=junkD[:, 0:S], in0=ic[:, 0:S], scalar1=mid[:, i:i + 1], scalar2=None,
                                                                                 op0=ALU.is_lt, op1=ALU.add, accum_out=cntv[:, i:i + 1]), [icb, b_mid], [b_cnt[i], b_junkD])
                    op("dve", lambda e: e.tensor_tensor(out=bsi[:, asl], in0=cntv[:, asl], in1=thc[:, asl], op=ALU.is_le), [b_cnt[i] for i in act_tiles] + [b_lo], [b_msk])
                    op("dve", lambda e: e.copy_predicated(out=lo[:, asl], mask=bsi[:, asl], data=mid[:, asl]), [b_msk, b_mid], [b_lo])
                    yield
                for i in act_tiles:
                    S = (4 * qc + i + 1) * 128
                    ic, icb = isc[i], b_isc[i]
                    op("dve", lambda e, i=i, S=S, ic=ic: e.tensor_scalar(out=mb[:, i, 0:S], in0=ic[:, 0:S], scalar1=lo[:, i:i + 1], scalar2=NEG, op0=ALU.is_lt, op1=ALU.mult),
                       [icb, b_lo], [b_mbq[i]])

            def dsa_mask(h, qc, j, t0, st, sb, wq):
                last = 4 * qc + 3
                mb = mbs[qc % 2]; b_mbq = b_mb2[qc % 2]
                for t in range(t0, last + 1):
                    i = t - 4 * qc
                    op("pe", lambda e, t=t, i=i: e.matmul(st[:, (t - t0) * 128:(t - t0 + 1) * 128], lhsT=mb[:, i, j * 128:(j + 1) * 128], rhs=identB[:],
                                                          start=False, stop=(t == last), skip_group_check=True), [b_mbq[i], b_cst], [sb])

            attention(qT, kT, b_qk, vaug, b_v, dsa_mask, 6, (Rot(PS[4:6]), Rot(PS[6:8]), Rot(PS[3:4])), otm, b_otm, Rot(pTl), rs, b_rs, mixT, mixb,
                      qc_hook=dsa_qc)
            cx.release(m0)

            if stop == 'dsa':
                cx.barrier()
                return nc, cx
            if debug and seq == 0 and layer == layers[0]:
                dma("sp", dbg[:, :, :], mixT[:, :, :], reads=mixb, owner=Buf("dbg"))

            cx.cur2 = Hoff + 65536
            m0 = cx.mark()
            Wo = alloc("Wo", [128, 8, D], BF16); b_Wo = Buf("Wo")
            gbc = alloc("gbc", [128, D], F32); bbc = alloc("bbc", [128, D], F32); b_par = Buf("lnpar")
            hres = [alloc("hres0", [128, D], F32), alloc("hres1", [128, D], F32)]; b_hres = [Buf("hres0"), Buf("hres1")]
            stat = alloc("stat", [128, NT, 20], F32); b_stat = [Buf(f"stat{t}") for t in range(NT)]
            rw = alloc("rw", [128, 8, 16], F32); rwh = alloc("rwh", [128, 8, 16], BF16); rwl = alloc("rwl", [128, 8, 16], BF16); b_rw = Buf("rw")
            rbb = alloc("rbb", [128, 16], F32)
            HTlo = alloc("HTlo", [128, 8, 128], BF16); b_lo = Buf("HTlo")
            comb = alloc("comb", [128, NT, 16], F32); b_comb = Buf("comb")
            rt = alloc("rt", [128, 8, NT * 16], F32); b_rt = Buf("rt")
            lg_all = alloc("lg_all", [128, NT, 16], F32); b_lg = Buf("lg")
            load_w(Wo[:], w_out[layer][:, :], b_Wo)
            dma("sp", gbc[:], ln1g[layer, :].partition_broadcast(128), writes=[b_par])
            dma("sp", bbc[:], ln1b[layer, :].partition_broadcast(128), writes=[b_par], owner=Buf("lnb"))
            dma("sp", rw[:], r_w.rearrange("(c p) e -> p c e", p=128), writes=[b_rw])
            dma("sp", rbb[:], r_b.partition_broadcast(128), writes=[b_rw], owner=Buf("rbb"))
            op("dve", lambda e: e.tensor_copy(out=rwh[:], in_=rw[:]), [b_rw], [b_rw])
            op("dve", lambda e: e.tensor_tensor(out=rw[:], in0=rw[:], in1=rwh[:], op=ALU.subtract), [b_rw], [b_rw])
            op("dve", lambda e: e.tensor_copy(out=rwl[:], in_=rw[:]), [b_rw], [b_rw])
            ps_o = Rot(PS[0:4]); ps_t = Rot(PS[4:7]); ps_r = Rot(PS[7:8])
            def ln1_front(t):
                hr, hrb = hres[t % 2], b_hres[t % 2]
                dma("sp", hr[:], hs[t * 128:(t + 1) * 128, :], reads=[hsb[t]], writes=[hrb])
                for half in range(2):
                    po, pob = ps_o.get()
                    for c in range(8):
                        op("pe", lambda e, c=c: e.matmul(po[:, 0:512], lhsT=mixT[:, c, t * 128:(t + 1) * 128], rhs=Wo[:, c, half * 512:(half + 1) * 512],
                                                         start=(c == 0), stop=(c == 7)), [mixb[t], b_Wo], [pob])
                    op("dve", lambda e: e.scalar_tensor_tensor(out=H[:, t, half * 512:(half + 1) * 512], in0=hr[:, half * 512:(half + 1) * 512], scalar=ALPHA,
                                                               in1=po[:, 0:512], op0=ALU.mult, op1=ALU.add), [hrb, pob], [Hb[t]])
                ln_stats(t, stat, b_stat)

            def ln1_back(t):
                ln_apply(t, gbc, bbc, b_par, stat, b_stat)
                for half in range(2):
                    pt, pb = ps_t.get()
                    for c4 in range(4):
                        c = half * 4 + c4
                        op("pe", lambda e, c=c, c4=c4: e.transpose(out=pt[:, c4 * 128:(c4 + 1) * 128], in_=H[:, t, c * 128:(c + 1) * 128], identity=identF),
                           [Hb[t], b_cst], [pb])
                    pv = pt[:, :].rearrange("p (a b) -> p a b", a=4)
                    op("act", lambda e: e.activation(out=HT[:, half * 4:(half + 1) * 4, t * 128:(t + 1) * 128], in_=pv, func=AF.Copy), [pb], [HTb[t]])
                    op("dve", lambda e: e.tensor_tensor(out=HTlo[:, half * 4:(half + 1) * 4, :], in0=pv, in1=HT[:, half * 4:(half + 1) * 4, t * 128:(t + 1) * 128],
                                                        op=ALU.subtract), [pb, HTb[t]], [b_lo])
                pr_, prb = ps_r.get()
                k = 0
                for (A_, Ab, W_) in ((HT[:, :, t * 128:(t + 1) * 128], HTb[t], rwh), (HTlo[:, :, :], b_lo, rwh), (HT[:, :, t * 128:(t + 1) * 128], HTb[t], rwl)):
                    for c in range(8):
                        op("pe", lambda e, c=c, A_=A_, W_=W_, k=k: e.matmul(pr_[:, 0:16], lhsT=A_[:, c, :], rhs=W_[:, c, :], start=(k == 0), stop=(k == 23)),
                           [Ab, b_rw], [prb])
                        k += 1
                op("act", lambda e: e.activation(out=lg_all[:, t, :], in_=pr_[:, 0:16], func=AF.Copy), [prb], [b_lg])
                op("act", lambda e: e.activation(out=H[:, t, :], in_=H[:, t, :], func=AF.Copy, scale=ALPHA), [Hb[t]], [Hb[t]])

            ln1_front(0)
            for t in range(NT):
                if t + 1 < NT:
                    ln1_front(t + 1)
                ln1_back(t)
            T_ = NT
            sc = rt[:, 0, :]; sel = rt[:, 1, :]; tmpa = rt[:, 2, :]; tmpb = rt[:, 3, :]; ch = rt[:, 4, :]
            m1 = rt[:, 5, 0:T_ * 4]; m2 = rt[:, 5, T_ * 4:T_ * 8]; gs = rt[:, 5, T_ * 8:T_ * 12]; ing = rt[:, 5, T_ * 12:T_ * 16]
            gmax = rt[:, 6, 0:T_]; a1 = rt[:, 6, T_:2 * T_]; a2 = rt[:, 6, 2 * T_:3 * T_]; ssum = rt[:, 6, 3 * T_:4 * T_]
            v3 = lambda a: a.rearrange("p (t e) -> p t e", t=T_)
            v4 = lambda a: a.rearrange("p (t g e) -> p t g e", t=T_, g=4)
            g3 = lambda a: a.rearrange("p (t g) -> p t g", t=T_)
            R = [b_rt]
            op("act", lambda e: e.activation(out=sc, in_=lg_all[:, :, :].rearrange("p t e -> p (t e)"), func=AF.Sigmoid), [b_lg], R)
            op("dve", lambda e: e.tensor_tensor(out=v3(sel), in0=v3(sc), in1=rbb[:, None, :].to_broadcast([128, T_, 16]), op=ALU.add), R + [b_rw], R)
            op("dve", lambda e: e.tensor_reduce(out=g3(m1), in_=v4(sel), axis=AX.X, op=ALU.max), R, R)
            op("dve", lambda e: e.tensor_tensor(out=v4(tmpa), in0=v4(sel), in1=g3(m1)[:, :, :, None].to_broadcast([128, T_, 4, 4]), op=ALU.is_equal), R, R)
            op("dve", lambda e: e.scalar_tensor_tensor(out=tmpb, in0=tmpa, scalar=-1.0e9, in1=sel, op0=ALU.mult, op1=ALU.add), R, R)
            op("dve", lambda e: e.tensor_reduce(out=g3(m2), in_=v4(tmpb), axis=AX.X, op=ALU.max), R, R)
            op("dve", lambda e: e.tensor_tensor(out=gs, in0=m1, in1=m2, op=ALU.add), R, R)
            op("dve", lambda e: e.tensor_reduce(out=gmax, in_=g3(gs), axis=AX.X, op=ALU.max), R, R)
            op("dve", lambda e: e.tensor_tensor(out=g3(ing), in0=g3(gs), in1=gmax[:, :, None].to_broadcast([128, T_, 4]), op=ALU.is_lt), R, R)
            op("dve", lambda e: e.tensor_scalar(out=ing, in0=ing, scalar1=-1.0e9, scalar2=None, op0=ALU.mult), R, R)
            op("dve", lambda e: e.tensor_tensor(out=v4(tmpa), in0=v4(sel), in1=g3(ing)[:, :, :, None].to_broadcast([128, T_, 4, 4]), op=ALU.add), R, R)
            op("dve", lambda e: e.tensor_reduce(out=a1, in_=v3(tmpa), axis=AX.X, op=ALU.max), R, R)
            op("dve", lambda e: e.tensor_tensor(out=v3(ch), in0=v3(tmpa), in1=a1[:, :, None].to_broadcast([128, T_, 16]), op=ALU.is_ge), R, R)
            op("dve", lambda e: e.scalar_tensor_tensor(out=tmpb, in0=ch, scalar=-1.0e9, in1=tmpa, op0=ALU.mult, op1=ALU.add), R, R)
            op("dve", lambda e: e.tensor_reduce(out=a2, in_=v3(tmpb), axis=AX.X, op=ALU.max), R, R)
            op("dve", lambda e: e.tensor_tensor(out=v3(tmpa), in0=v3(tmpb), in1=a2[:, :, None].to_broadcast([128, T_, 16]), op=ALU.is_ge), R, R)
            op("dve", lambda e: e.tensor_tensor(out=ch, in0=ch, in1=tmpa, op=ALU.add), R, R)
            op("dve", lambda e: e.tensor_tensor(out=tmpb, in0=ch, in1=sc, op=ALU.mult), R, R)
            op("dve", lambda e: e.tensor_reduce(out=ssum, in_=v3(tmpb), axis=AX.X, op=ALU.add), R, R)
            op("dve", lambda e: e.reciprocal(out=ssum, in_=ssum), R, R)
            op("dve", lambda e: e.tensor_tensor(out=comb[:, :, :], in0=v3(tmpb), in1=ssum[:, :, None].to_broadcast([128, T_, 16]), op=ALU.mult), R, [b_comb])
            cx.barrier()
            cx.cur, cx.cur2 = base_mark
            cx.cur2 = Hoff + 65536
            comb2 = alloc("comb2", [128, NT, 16], F32); b_comb2 = Buf("comb2")
            op("dve", lambda e: e.tensor_copy(out=comb2[:], in_=comb[:]), [b_comb], [b_comb2])
            cx.barrier()

            if stop == 'ln1':
                cx.barrier()
                return nc, cx
            m0 = cx.mark()
            Wg_ = [alloc(f"weg{i}", [128, 8, 512], BF16) for i in range(2)]; b_weg = [Buf("weg0"), Buf("weg1")]
            Wu_ = [alloc(f"weu{i}", [128, 8, 512], BF16) for i in range(2)]; b_weu = [Buf("weu0"), Buf("weu1")]
            Wd_ = [alloc(f"wed{i}", [128, 4, D], BF16) for i in range(2)]; b_wed = [Buf("wed0"), Buf("wed1")]
            actT = alloc("actT", [128, 4, L], BF16); b_act = [[Buf(f"act{f}_{tc}") for tc in range(4)] for f in range(4)]
            sgb = [(alloc(f"sgm{i}", [128, 512], F32), Buf(f"sgm{i}")) for i in range(3)]
            sgr = Rot(sgb)
            ps_gu = Rot(PS[0:4]); ps_d = Rot(PS[4:8])
            for ex in range(16):
                wg, wu, wd = Wg_[ex % 2], Wu_[ex % 2], Wd_[ex % 2]
                bg, bu, bd = b_weg[ex % 2], b_weu[ex % 2], b_wed[ex % 2]
                load_w(wg[:], w_eg[layer, ex], bg)
                load_w(wu[:], w_eu[layer, ex], bu)
                dma("pool", wd[:], w_ed[layer, ex].rearrange("(c p) f -> p c f", p=128), writes=[bd])
                for tc in range(4):
                    for f in range(4):
                        pg_, pgb = ps_gu.get(); pu_, pub = ps_gu.get()
                        for c in range(8):
                            op("pe", lambda e, c=c: e.matmul(pg_[:, 0:512], lhsT=wg[:, c, f * 128:(f + 1) * 128], rhs=HT[:, c, tc * 512:(tc + 1) * 512],
                                                             start=(c == 0), stop=(c == 7)), [bg] + HTb[tc * 4:tc * 4 + 4], [pgb])
                        for c in range(8):
                            op("pe", lambda e, c=c: e.matmul(pu_[:, 0:512], lhsT=wu[:, c, f * 128:(f + 1) * 128], rhs=HT[:, c, tc * 512:(tc + 1) * 512],
                                                             start=(c == 0), stop=(c == 7)), [bu] + HTb[tc * 4:tc * 4 + 4], [pub])
                        sgt, sgtb = sgr.get()
                        op("act", lambda e: e.activation(out=sgt[:], in_=pg_[:, 0:512], func=AF.Silu), [pgb], [sgtb])
                        op("dve", lambda e: e.tensor_tensor(out=actT[:, f, tc * 512:(tc + 1) * 512], in0=pu_[:, 0:512], in1=sgt[:], op=ALU.mult),
                           [pub, sgtb], [b_act[f][tc]])
                    for t4 in range(4):
                        t = tc * 4 + t4
                        for half in range(2):
                            pd_, pdb = ps_d.get()
                            for f in range(4):
                                op("pe", lambda e, f=f: e.matmul(pd_[:, 0:512], lhsT=actT[:, f, t * 128:(t + 1) * 128], rhs=wd[:, f, half * 512:(half + 1) * 512],
                                                                 start=(f == 0), stop=(f == 3)), [b_act[f][tc], bd], [pdb])
                            op("dve", lambda e: e.scalar_tensor_tensor(out=H[:, t, half * 512:(half + 1) * 512], in0=pd_[:, 0:512], scalar=comb2[:, t, ex:ex + 1],
                                                                       in1=H[:, t, half * 512:(half + 1) * 512], op0=ALU.mult, op1=ALU.add),
                               [pdb, b_comb2, Hb[t]], [Hb[t]])
            cx.release(m0)

            if stop == 'moe':
                cx.barrier()
                return nc, cx
            m0 = cx.mark()
            gbc = alloc("gbc", [128, D], F32); bbc = alloc("bbc", [128, D], F32); b_par = Buf("lnpar")
            stat = alloc("stat", [128, NT, 20], F32); b_stat = [Buf(f"stat{t}") for t in range(NT)]
            dma("sp", gbc[:], ln2g[layer, :].partition_broadcast(128), writes=[b_par])
            dma("sp", bbc[:], ln2b[layer, :].partition_broadcast(128), writes=[b_par], owner=Buf("lnb"))
            pool_tr = Rot(PS[0:4])
            last = (layer == layers[-1])
            ln_stats(0, stat, b_stat)
            for t in range(NT):
                ln_apply(t, gbc, bbc, b_par, stat, b_stat, between=((lambda t=t: ln_stats(t + 1, stat, b_stat)) if t + 1 < NT else None))
                if last:
                    dma("sp", out[seq, t * 128:(t + 1) * 128, :], H[:, t, :], reads=[Hb[t]], owner=Hb[t])
                else:
                    make_HT(t, pool_tr)
            cx.release(m0)
            cx.cur, cx.cur2 = base_mark

    cx.barrier()
    return nc, cx


_CACHE = {}


def _prep_inputs(inputs):
    cst, cst8 = _consts()
    w_in_x = np.ascontiguousarray(np.asarray(inputs["w_in"], np.float32)[:, :, COLMAP])
    shared = {"w_in_x": w_in_x, "cst": cst, "cst8": cst8}
    for k in ("gla_gate_up", "gla_gate_bias", "gla_norm_gain", "w_out", "ln_mix_g", "ln_mix_b", "router_w", "router_bias",
              "w_expert_gate", "w_expert_up", "w_expert_down", "ln_ffn_g", "ln_ffn_b"):
        shared[k] = np.ascontiguousarray(np.asarray(inputs[k], np.float32))
    return shared


def kernel(**inputs):
    x = np.ascontiguousarray(np.asarray(inputs["x"], np.float32))
    positions = np.ascontiguousarray(np.asarray(inputs["positions"], np.int32))
    shared = _prep_inputs(inputs)
    ncores, nseq = 8, 2
    nc = bass.Bass("TRN2", target_bir_lowering=False)
    build(nc, nseq, [0, 1])
    in_maps = []
    for c in range(ncores):
        m = dict(shared)
        m["x"] = x[c * nseq:(c + 1) * nseq]
        m["positions"] = positions[c * nseq:(c + 1) * nseq]
        in_maps.append(m)
    res = run_bass_kernel_spmd(nc, in_maps, core_ids=list(range(ncores)))
    return np.concatenate([r["out"] for r in res.results], axis=0)
```

```python
import numpy as np
import concourse.bass as bass
import concourse.mybir as mybir
from concourse.bass_utils import run_bass_kernel_spmd

F32 = mybir.dt.float32
BF16 = mybir.dt.bfloat16
I32 = mybir.dt.int32
ALU = mybir.AluOpType
AF = mybir.ActivationFunctionType
AX = mybir.AxisListType

L = 2048
D = 1024
NT = 16
NEG = -30000.0
BIG = 1.0e30
ALPHA = 4.0 ** 0.25
N_BISECT = 18
SBUF_BASE = 16512
SBUF_BYTES = 229000

IN_WIDTHS = (256, 256, 512, 16, 512, 256, 256, 256, 256, 256, 256, 512, 64, 8)
IN_OFF = np.concatenate([[0], np.cumsum(IN_WIDTHS)]).astype(int)
(O_GQ, O_GK, O_GV, O_GR, O_GG, O_MQ, O_MK, O_MV, O_SQ, O_SK, O_SV, O_IQ, O_IK, O_IW) = [int(v) for v in IN_OFF[:-1]]


def _rot_cols(base, nheads):
    cols = []
    for h in range(nheads):
        for d in range(64):
            if d < 8:
                pd = d + 8
            elif d < 16:
                pd = d - 8
            else:
                pd = d
            cols.append(base + h * 64 + pd)
    return cols


def _build_colmap():
    cols = []
    off = {}

    def add(name, c):
        off[name] = len(cols)
        cols.extend(c)

    for p in range(2):
        add(f"g{p}", list(range(O_GQ + p * 128, O_GQ + p * 128 + 128)) + list(range(O_GK + p * 128, O_GK + p * 128 + 128))
            + list(range(O_GV + p * 256, O_GV + p * 256 + 256)) + list(range(O_GG + p * 256, O_GG + p * 256 + 256))
            + list(range(O_GR, O_GR + 16)))
    add("mq", list(range(O_MQ, O_MQ + 256)))
    add("mqr", _rot_cols(O_MQ, 4))
    add("mk", list(range(O_MK, O_MK + 256)))
    add("mkr", _rot_cols(O_MK, 4))
    add("mv", list(range(O_MV, O_MV + 256)))
    add("sq", list(range(O_SQ, O_SQ + 256)))
    add("sqr", _rot_cols(O_SQ, 4))
    add("sk", list(range(O_SK, O_SK + 256)))
    add("skr", _rot_cols(O_SK, 4))
    add("sv", list(range(O_SV, O_SV + 256)))
    add("iq", list(range(O_IQ, O_IQ + 512)))
    add("iqr", _rot_cols(O_IQ, 8))
    add("ik", list(range(O_IK, O_IK + 64)) * 2)
    add("ikr", _rot_cols(O_IK, 1) * 2)
    add("iw", list(range(O_IW, O_IW + 8)))
    return np.array(cols, dtype=np.int64), off


COLMAP, COFF = _build_colmap()
WX = len(COLMAP)


def _consts():
    c = np.zeros((128, 1024), np.float32)
    c[:, 0:128] = np.eye(128, dtype=np.float32)
    j = np.arange(128)[:, None]
    i = np.arange(128)[None, :]
    c[:, 128:256] = (i >= j).astype(np.float32)
    c[:, 256:384] = np.where(i <= j, 0.0, NEG)
    c[:, 384:512] = np.where(i <= j, 0.0, -BIG)
    p = np.arange(128)
    d = p % 64
    invf = np.where(d < 16, 500000.0 ** (-(d % 8) / 8.0), 0.0)
    c[:, 512] = invf / (2 * np.pi)
    c[:, 513] = np.where(d < 8, -invf, invf) / (2 * np.pi)
    c8 = np.zeros((8, 2048 + 512 + 8), np.float32)
    for k in range(8):
        for n in range(8):
            c8[k, 2560 + n] = 0.0 if n < k else (BIG if n == k else -BIG)
    for n in range(8):
        c8[n, n * 256:(n + 1) * 256] = 1.0
    for k in range(8):
        pr, hh = k // 2, k % 2
        c8[k, 2048 + pr * 128 + hh * 64: 2048 + pr * 128 + hh * 64 + 64] = 1.0
    return c, c8


class Buf:
    __slots__ = ("name", "writer", "readers", "dsem", "dcnt")

    def __init__(self, name):
        self.name = name
        self.writer = None
        self.readers = {}
        self.dsem = None
        self.dcnt = 0


class Ctx:
    def __init__(self, nc):
        self.nc = nc
        self.E = {"pe": nc.tensor, "act": nc.scalar, "dve": nc.vector, "pool": nc.gpsimd, "sp": nc.sync}
        self.sem = {k: nc.alloc_semaphore("e_" + k) for k in self.E}
        self.cnt = {k: 0 for k in self.E}
        self.seen = {k: {} for k in self.E}
        self.dbufs = []
        self.slots = {}
        self.nalloc = 0
        self.cur = SBUF_BASE
        self.cur2 = 0
        self.lim2 = 0
        self.peak = 0

    def alloc(self, name, shape, dtype, reg=0):
        isz = 4 if dtype in (F32, I32) else 2
        n = isz
        for s in shape[1:]:
            n *= s
        n = (n + 63) // 64 * 64
        if reg == 0:
            off = self.cur
            self.cur += n
            self.peak = max(self.peak, self.cur)
            assert self.cur <= SBUF_BYTES, (name, self.cur)
        else:
            off = self.cur2
            self.cur2 += n
            assert self.cur2 <= self.lim2, (name, self.cur2, self.lim2)
        self.nalloc += 1
        return self.nc.alloc_sbuf_tensor_at(f"{name}_{self.nalloc}", list(shape), dtype, offset=off)

    def mark(self):
        return (self.cur, self.cur2)

    def release(self, m):
        self.barrier()
        self.cur, self.cur2 = m

    def _wait(self, eng, dep):
        kind, obj, val = dep
        key = obj if kind == "e" else id(obj)
        if self.seen[eng].get(key, 0) >= val:
            return
        sem = self.sem[obj] if kind == "e" else obj.dsem
        self.E[eng].wait_ge(sem, val)
        self.seen[eng][key] = val

    def _deps(self, eng, reads, writes):
        for b in reads:
            if b.writer is not None:
                self._wait(eng, b.writer)
        for b in writes:
            w = b.writer
            if w is not None and not (w[0] == "e" and w[1] == eng and eng == "pe"):
                self._wait(eng, w)
            for r in b.readers.values():
                if not (r[0] == "e" and r[1] == eng and eng == "pe"):
                    self._wait(eng, r)

    def op(self, eng, fn, reads=(), writes=()):
        self._deps(eng, reads, writes)
        inst = fn(self.E[eng])
        inst.then_inc(self.sem[eng], 1)
        self.cnt[eng] += 1
        tag = ("e", eng, self.cnt[eng])
        for b in reads:
            b.readers[eng] = tag
        for b in writes:
            b.writer = tag
            b.readers = {}

    def dma(self, q, out, in_, reads=(), writes=(), owner=None):
        self._deps(q, reads, writes)
        if owner is None:
            owner = writes[0] if writes else reads[0]
        slot = self.slots.get(owner.name)
        if slot is None:
            slot = Buf("slot_" + owner.name)
            slot.dsem = self.nc.alloc_semaphore("d_%d" % len(self.slots))
            self.slots[owner.name] = slot
            self.dbufs.append(slot)
        self.E[q].dma_start(out=out, in_=in_).then_inc(slot.dsem, 16)
        slot.dcnt += 16
        tag = ("d", slot, slot.dcnt)
        for b in reads:
            b.readers[("d", id(slot))] = tag
        for b in writes:
            b.writer = tag
            b.readers = {}

    def barrier(self):
        for e in self.E:
            for e2 in self.E:
                if e2 != e and self.cnt[e2] > 0:
                    self._wait(e, ("e", e2, self.cnt[e2]))
            for b in self.dbufs:
                if b.dcnt:
                    self._wait(e, ("d", b, b.dcnt))


class Rot:
    def __init__(self, items):
        self.items = items
        self.i = 0

    def get(self):
        it = self.items[self.i % len(self.items)]
        self.i += 1
        return it


class StopBuild(Exception):
    pass


def build(nc, nseq, layers, debug=False, stop=None):
    try:
        return _build(nc, nseq, layers, debug, stop)
    except StopBuild as ex:
        return nc, ex.args[0]


def _build(nc, nseq, layers, debug=False, stop=None):
    cx = Ctx(nc)
    op, dma, alloc = cx.op, cx.dma, cx.alloc

    def chk(name):
        if stop == name:
            cx.barrier()
            raise StopBuild(cx)

    x = nc.dram_tensor("x", [nseq, L, D], F32, kind="ExternalInput").ap()
    pos = nc.dram_tensor("positions", [nseq, L], I32, kind="ExternalInput").ap()
    w_in = nc.dram_tensor("w_in_x", [2, D, WX], F32, kind="ExternalInput").ap()
    g_up = nc.dram_tensor("gla_gate_up", [2, 16, 256], F32, kind="ExternalInput").ap()
    g_bias = nc.dram_tensor("gla_gate_bias", [2, 256], F32, kind="ExternalInput").ap()
    g_gain = nc.dram_tensor("gla_norm_gain", [2, 128], F32, kind="ExternalInput").ap()
    w_out = nc.dram_tensor("w_out", [2, D, D], F32, kind="ExternalInput").ap()
    ln1g = nc.dram_tensor("ln_mix_g", [2, D], F32, kind="ExternalInput").ap()
    ln1b = nc.dram_tensor("ln_mix_b", [2, D], F32, kind="ExternalInput").ap()
    r_w = nc.dram_tensor("router_w", [D, 16], F32, kind="ExternalInput").ap()
    r_b = nc.dram_tensor("router_bias", [16], F32, kind="ExternalInput").ap()
    w_eg = nc.dram_tensor("w_expert_gate", [2, 16, D, 512], F32, kind="ExternalInput").ap()
    w_eu = nc.dram_tensor("w_expert_up", [2, 16, D, 512], F32, kind="ExternalInput").ap()
    w_ed = nc.dram_tensor("w_expert_down", [2, 16, 512, D], F32, kind="ExternalInput").ap()
    ln2g = nc.dram_tensor("ln_ffn_g", [2, D], F32, kind="ExternalInput").ap()
    ln2b = nc.dram_tensor("ln_ffn_b", [2, D], F32, kind="ExternalInput").ap()
    cst_d = nc.dram_tensor("cst", [128, 1024], F32, kind="ExternalInput").ap()
    cst8_d = nc.dram_tensor("cst8", [8, 2568], F32, kind="ExternalInput").ap()
    out = nc.dram_tensor("out", [nseq, L, D], F32, kind="ExternalOutput").ap()
    hs = nc.dram_tensor("hspill", [L, D], F32, kind="Internal").ap()
    dbg = nc.dram_tensor("dbg", [128, 8, L], BF16, kind="ExternalOutput").ap() if debug else None

    cst = alloc("cst", [128, 1024], F32)
    c8b = alloc("c8b", [8, 2568], BF16)
    identB = alloc("identB", [128, 128], BF16)
    trinegB = alloc("trinegB", [128, 128], BF16)
    negidentB = alloc("negidentB", [128, 128], BF16)
    epsln = alloc("epsln", [128, 2], F32)
    zerosB = alloc("zerosB", [1, 512], BF16)
    HT = alloc("HT", [128, 8, L], BF16)
    Hoff = cx.cur
    H = alloc("H", [128, NT, D], F32)
    cosT = alloc("cosT", [128, L], F32)
    sinT = alloc("sinT", [128, L], F32)
    b_tab = Buf("tab")
    base_mark = cx.mark()

    b_cst = Buf("cst")
    Hb = [Buf(f"H{t}") for t in range(NT)]
    HTb = [Buf(f"HT{t}") for t in range(NT)]
    hsb = [Buf(f"hs{t}") for t in range(NT)]
    outb = Buf("out")

    identF = cst[:, 0:128]
    tri01 = cst[:, 128:256]
    trinegbig = cst[:, 384:512]
    invf = cst[:, 512:513]
    sinvf = cst[:, 513:514]
    onehotK = c8b[0:8, 0:2048]
    selw = c8b[0:8, 2048:2560]
    bpen = c8b[0:8, 2560:2568]

    PS = []
    for i in range(8):
        t = nc.alloc_psum_tensor(f"ps{i}", [128, 512], F32)
        PS.append((t, Buf(f"ps{i}")))

    dma("sp", cst[:], cst_d[:, :], writes=[b_cst])
    dma("pool", c8b[:], cst8_d[:, :], writes=[b_cst], owner=Buf("c8"))
    op("dve", lambda e: e.tensor_copy(out=identB[:], in_=cst[:, 0:128]), [b_cst], [b_cst])
    op("dve", lambda e: e.tensor_copy(out=trinegB[:], in_=cst[:, 256:384]), [b_cst], [b_cst])
    op("dve", lambda e: e.tensor_scalar(out=negidentB[:], in0=cst[:, 0:128], scalar1=-1.0, scalar2=None, op0=ALU.mult), [b_cst], [b_cst])
    op("dve", lambda e: e.memset(epsln[:, 0:1], 1e-5), [], [b_cst])
    op("dve", lambda e: e.memset(epsln[:, 1:2], 1e-6), [], [b_cst])
    op("dve", lambda e: e.memset(zerosB[:], 0.0), [], [b_cst])
    cx.barrier()

    def load_w(dst, src_rows_cols, buf, q="pool"):
        dma(q, dst, src_rows_cols.rearrange("(c p) f -> p c f", p=128), writes=[buf])

    def make_HT(t, pool):
        for half in range(2):
            pt, pb = pool.get()
            for c4 in range(4):
                c = half * 4 + c4
                op("pe", lambda e, c=c, c4=c4: e.transpose(out=pt[:, c4 * 128:(c4 + 1) * 128],
                                                           in_=H[:, t, c * 128:(c + 1) * 128], identity=identF),
                   [Hb[t], b_cst], [pb])
            op("act", lambda e: e.activation(out=HT[:, half * 4:(half + 1) * 4, t * 128:(t + 1) * 128],
                                             in_=pt[:, :].rearrange("p (a b) -> p a b", a=4), func=AF.Copy),
               [pb], [HTb[t]])

    def ln_stats(t, stat, b_stat):
        st = stat[:, t, :]
        bs_ = [b_stat[t]]
        op("dve", lambda e: e.bn_stats(out=st[:, 0:6], in_=H[:, t, 0:512]), [Hb[t]], bs_)
        op("dve", lambda e: e.bn_stats(out=st[:, 6:12], in_=H[:, t, 512:1024]), [Hb[t]], bs_)
        op("dve", lambda e: e.bn_aggr(out=st[:, 12:14], in_=st[:, 0:12]), bs_, bs_)
        op("act", lambda e: e.activation(out=st[:, 14:15], in_=st[:, 13:14], func=AF.Sqrt, bias=epsln[:, 0:1], scale=1.0),
           bs_ + [b_cst], bs_)
        op("dve", lambda e: e.reciprocal(out=st[:, 15:16], in_=st[:, 14:15]), bs_, bs_)
        op("dve", lambda e: e.scalar_tensor_tensor(out=st[:, 16:17], in0=st[:, 12:13], scalar=-1.0, in1=st[:, 15:16], op0=ALU.mult, op1=ALU.mult), bs_, bs_)

    def ln_apply(t, gbc, bbc, b_par, stat, b_stat, between=None):
        hv = H[:, t, :]
        st = stat[:, t, :]
        op("act", lambda e: e.activation(out=hv, in_=hv, func=AF.Identity, bias=st[:, 16:17], scale=st[:, 15:16]), [Hb[t], b_stat[t]], [Hb[t]])
        if between is not None:
            between()
        op("dve", lambda e: e.tensor_tensor(out=hv, in0=hv, in1=gbc[:], op=ALU.mult), [Hb[t], b_par], [Hb[t]])
        op("dve", lambda e: e.tensor_tensor(out=hv, in0=hv, in1=bbc[:], op=ALU.add), [Hb[t], b_par], [Hb[t]])

    def rope_tables(seq, cosT, sinT, b_tab):
        m = cx.mark()
        posf = alloc("posf", [128, L], F32)
        tmp = alloc("rtmp", [128, L], F32)
        tmi = alloc("rtmi", [128, L], I32)
        b_t = Buf("ropetmp")
        dma("pool", posf[:], pos[seq, :].partition_broadcast(128), writes=[b_t])
        for (tab, fcol, shift) in ((sinT, sinvf, 0.0), (cosT, invf, 0.25)):
            op("dve", lambda e, fcol=fcol, shift=shift: e.tensor_scalar(out=tmp[:], in0=posf[:], scalar1=fcol, scalar2=shift,
                                                                         op0=ALU.mult, op1=ALU.add), [b_t, b_cst], [b_t])
            op("dve", lambda e: e.tensor_copy(out=tmi[:], in_=tmp[:]), [b_t], [b_t])
            op("dve", lambda e: e.tensor_copy(out=tab[:], in_=tmi[:]), [b_t], [b_tab])
            op("dve", lambda e: e.tensor_tensor(out=tmp[:], in0=tmp[:], in1=tab[:], op=ALU.subtract), [b_t, b_tab], [b_t])
            op("dve", lambda e: e.tensor_scalar(out=tab[:], in0=tmp[:], scalar1=0.5, scalar2=None, op0=ALU.is_gt), [b_t], [b_tab])
            op("dve", lambda e: e.tensor_tensor(out=tmp[:], in0=tmp[:], in1=tab[:], op=ALU.subtract), [b_t, b_tab], [b_t])
            op("dve", lambda e: e.tensor_scalar(out=tab[:], in0=tmp[:], scalar1=-0.5, scalar2=None, op0=ALU.is_lt), [b_t], [b_tab])
            op("dve", lambda e: e.tensor_tensor(out=tmp[:], in0=tmp[:], in1=tab[:], op=ALU.add), [b_t, b_tab], [b_t])
            op("act", lambda e: e.activation(out=tab[:], in_=tmp[:], func=AF.Sin, scale=2 * np.pi), [b_t], [b_tab])
        cx.release(m)

    def proj_fm(wt, col0, M, tc, pt, pb, wbuf, pbase=0):
        for c in range(8):
            op("pe", lambda e, c=c: e.matmul(pt[pbase:pbase + M, 0:512], lhsT=wt[:, c, col0:col0 + M],
                                             rhs=HT[:, c, tc * 512:(tc + 1) * 512], start=(c == 0), stop=(c == 7)),
               [wbuf] + HTb[tc * 4:(tc + 1) * 4], [pb])

    def proj_tm(wt, col0, N, t, pt, pb, wbuf, ocol=0):
        for c in range(8):
            op("pe", lambda e, c=c: e.matmul(pt[:, ocol:ocol + N], lhsT=HT[:, c, t * 128:(t + 1) * 128],
                                             rhs=wt[:, c, col0:col0 + N], start=(c == 0), stop=(c == 7)),
               [wbuf, HTb[t]], [pb])

    def roped_proj(wt, wbuf, col, colr, dstT, b_dst, cosT, sinT, b_tab, tmps, pspool, ksum=None):
        ntile = dstT.shape[1]
        (t1, t2, b_tmp) = tmps
        for ti in range(ntile):
            for tc in range(4):
                py, pyb = pspool.get()
                pr, prb = pspool.get()
                proj_fm(wt, col + ti * 128, 128, tc, py, pyb, wbuf)
                proj_fm(wt, colr + ti * 128, 128, tc, pr, prb, wbuf)
                sl = slice(tc * 512, (tc + 1) * 512)
                op("dve", lambda e: e.tensor_tensor(out=t1[:], in0=pr[:, :], in1=sinT[:, sl], op=ALU.mult), [prb, b_tab], [b_tmp])
                op("dve", lambda e: e.tensor_tensor(out=t2[:], in0=py[:, :], in1=cosT[:, sl], op=ALU.mult), [pyb, b_tab], [b_tmp])
                if ksum is not None:
                    op("pool", lambda e: e.tensor_tensor(out=t2[:], in0=t1[:], in1=t2[:], op=ALU.add), [b_tmp], [b_tmp])
                    op("act", lambda e: e.activation(out=dstT[:, ti, sl], in_=t2[:], func=AF.Copy), [b_tmp], [b_dst])
                    (km, b_km) = ksum
                    op("dve", lambda e: e.tensor_reduce(out=km[:, ti, tc * 2:(tc + 1) * 2], in_=t2[:, :].rearrange("p (a b) -> p a b", a=2),
                                                        axis=AX.X, op=ALU.add), [b_tmp], [b_km])
                else:
                    op("pool", lambda e: e.tensor_tensor(out=dstT[:, ti, sl], in0=t1[:], in1=t2[:], op=ALU.add), [b_tmp], [b_dst])

    def v_aug_proj(wt, wbuf, col, vaug, b_v, pspool):
        op("dve", lambda e: e.memset(vaug[:, :, :, 64:65], 1.0), [], [b_v])
        for t in range(NT):
            pt, pb = pspool.get()
            proj_tm(wt, col, 256, t, pt, pb, wbuf)
            op("act", lambda e: e.activation(out=vaug[:, t, :, 0:64], in_=pt[:, 0:256].rearrange("p (a b) -> p a b", a=4), func=AF.Copy),
               [pb], [b_v])

    def attention(qT, kT, b_qk, vaug, b_v, maskfn, mix_chunk0, pools, otm, b_otm, pTs, rs, b_rs, mixT, mixb, qc_hook=None):
        (ps_s, ps_acc, ps_tr) = pools

        def att_gen(qc):
                for h in range(4):
                    ptile, bp = h // 2, (h % 2) * 64
                    acc, accb = ps_acc.get()
                    op("pe", lambda e: e.matmul(acc[:, 0:260], lhsT=zerosB[0:1, 0:128], rhs=zerosB[0:1, 0:260], start=True, stop=False,
                                                skip_group_check=True), [b_cst], [accb])
                    prev = None

                    def emit_pv(args):
                        (j_, t0_, pT_, pTb_) = args
                        for t in range(t0_, 4 * qc + 4):
                            i = t - 4 * qc
                            op("pe", lambda e, t=t, i=i: e.matmul(acc[:, i * 65:(i + 1) * 65], lhsT=pT_[:, (t - t0_) * 128:(t - t0_ + 1) * 128],
                                                                  rhs=vaug[:, j_, h, :], start=False, stop=(j_ == t), skip_group_check=True),
                               [pTb_, b_v], [accb])

                    for j in range(4 * qc + 4):
                        t0 = max(j, 4 * qc)
                        q0, q1 = t0 * 128, (4 * qc + 4) * 128
                        wq = q1 - q0
                        st, sb = ps_s.get()
                        op("pe", lambda e: e.matmul(st[:, 0:wq], lhsT=kT[bp:bp + 64, ptile, j * 128:(j + 1) * 128],
                                                    rhs=qT[bp:bp + 64, ptile, q0:q1], start=True, stop=False, skip_group_check=True),
                           [b_qk], [sb])
                        maskfn(h, qc, j, t0, st, sb, wq)
                        pT, pTb = pTs.get()
                        op("act", lambda e: e.activation(out=pT[:, 0:wq], in_=st[:, 0:wq], func=AF.Exp, scale=0.125), [sb], [pTb])
                        if prev is not None:
                            emit_pv(prev)
                        prev = (j, t0, pT, pTb)
                        yield
                    emit_pv(prev)
                    acc3 = acc[:, 0:260].rearrange("p (a b) -> p a b", a=4)
                    op("dve", lambda e: e.reciprocal(out=rs[:, :], in_=acc3[:, :, 64]), [accb], [b_rs])
                    for i in range(4):
                        op("dve", lambda e, i=i: e.tensor_scalar(out=otm[:, i, h * 64:(h + 1) * 64], in0=acc3[:, i, 0:64],
                                                                 scalar1=rs[:, i:i + 1], scalar2=None, op0=ALU.mult), [accb, b_rs], [b_otm])
                    yield
                for i in range(4):
                    t = 4 * qc + i
                    pt, pb = ps_tr.get()
                    for f in range(2):
                        op("pe", lambda e, f=f: e.transpose(out=pt[:, f * 128:(f + 1) * 128], in_=otm[:, i, f * 128:(f + 1) * 128], identity=identF),
                           [b_otm, b_cst], [pb])
                    op("act", lambda e: e.activation(out=mixT[:, mix_chunk0:mix_chunk0 + 2, t * 128:(t + 1) * 128],
                                                     in_=pt[:, 0:256].rearrange("p (a b) -> p a b", a=2), func=AF.Copy), [pb], [mixb[t]])
                    yield

        def drain(g):
            for _ in g:
                pass

        def merge(ga, na, gb, nb):
            ia = ib = 0
            da = db = False
            while not (da and db):
                if not da and (db or ia * nb <= ib * na):
                    try:
                        next(ga)
                    except StopIteration:
                        da = True
                    ia += 1
                else:
                    try:
                        next(gb)
                    except StopIteration:
                        db = True
                    ib += 1

        if qc_hook is not None:
            drain(qc_hook(0))
        for qc in range(4):
            ga = att_gen(qc)
            if qc_hook is not None and qc + 1 < 4:
                n_att = 4 * (4 * qc + 4) + 8
                n_hook = 4 + sum(((4 * (qc + 1) + i + 1) * 128 + 511) // 512 for i in range(4)) + N_BISECT + 2
                merge(ga, n_att, qc_hook(qc + 1), n_hook)
            else:
                drain(ga)

    for seq in range(nseq):
        pool_tr = Rot(PS[0:4])
        for t in range(NT):
            dma("sp", H[:, t, :], x[seq, t * 128:(t + 1) * 128, :], writes=[Hb[t]])
        for t in range(NT):
            make_HT(t, pool_tr)
        rope_tables(seq, cosT, sinT, b_tab)

        for layer in layers:
            for t in range(NT):
                dma("sp", hs[t * 128:(t + 1) * 128, :], H[:, t, :], reads=[Hb[t]], writes=[hsb[t]], owner=Hb[t])
            cx.barrier()
            cx.cur, cx.cur2 = base_mark
            cx.cur2, cx.lim2 = Hoff, Hoff + 65536
            mixT = alloc("mixT", [128, 8, L], BF16)
            mixb = [Buf(f"mix{t}") for t in range(NT)]

            if stop == 'ht':
                cx.barrier()
                return nc, cx
            mg = cx.mark()
            Wg2 = [alloc(f"Wg{p}", [128, 8, 784], BF16) for p in range(2)]; b_Wg2 = [Buf(f"Wg{p}") for p in range(2)]
            for p in range(2):
                load_w(Wg2[p][:], w_in[layer][:, COFF[f"g{p}"]:COFF[f"g{p}"] + 784], b_Wg2[p])
            for p in range(2):
                m0 = cx.mark()
                Wg = Wg2[p]; b_Wg = b_Wg2[p]
                gu = alloc("gu", [16, 128], BF16)
                nbias = alloc("nbias", [128, 1], F32)
                gain = alloc("gain", [128, 2, 128], F32)
                b_gp = Buf("gpar")
                rankT = alloc("rankT", [16, L], BF16); b_rank = Buf("rank")
                la = alloc("la", [128, L], F32, 1); b_la = Buf("la")
                bc = alloc("bc", [128, L], F32, 1); b_bc = Buf("bc")
                nbl = alloc("nbl", [128, 16], F32); dec = alloc("dec", [128, 16], F32); b_nbl = Buf("nbl")
                qdT = alloc("qdT", [128, L], BF16, 1); kiT = alloc("kiT", [128, L], BF16, 1); b_qk = Buf("gqk")
                kd = alloc("kd", [128, NT, 128], BF16, 1); b_kd = Buf("kd")
                vtm = alloc("vtm", [128, NT, 256], BF16, 1); b_v = Buf("gv")
                Sall = alloc("Sall", [128, NT, 128], BF16, 1); b_Sall = Buf("Sall")
                Sst = alloc("Sst", [128, 128], F32); b_S = Buf("S")
                e1 = alloc("e1", [128, 512], F32); e2 = alloc("e2", [128, 512], F32); e3 = alloc("e3", [128, 512], F32)
                b_e = [Buf("e1"), Buf("e2"), Buf("e3")]
                kdT = alloc("kdT", [128, 512], F32); b_kdT = Buf("kdT")
                AT = [alloc("AT0", [128, 128], BF16), alloc("AT1", [128, 128], BF16)]; b_AT = [Buf("AT0"), Buf("AT1")]
                sg = alloc("sg", [128, 256], F32); b_sg = Buf("sg")
                otm = alloc("otmg", [128, 256], F32); b_otm = Buf("otmg")
                sq = alloc("sq", [128, 256], F32); ssq = alloc("ssq", [128, 4], F32); b_ssq = Buf("ssq")

                dma("pool", gu[:], g_up[layer][:, p * 128:(p + 1) * 128], writes=[b_gp])
                dma("sp", nbias[:], g_bias[layer, p * 128:(p + 1) * 128].rearrange("(p o) -> p o", o=1), writes=[b_gp], owner=Buf("nb"))
                for hh in range(2):
                    dma("sp", gain[:, hh, :], g_gain[layer, :].partition_broadcast(128), writes=[b_gp], owner=Buf("gn"))
                op("dve", lambda e: e.tensor_scalar(out=nbias[:], in0=nbias[:], scalar1=-1.0, scalar2=None, op0=ALU.mult), [b_gp], [b_gp])
                chk('g1')
                pp = Rot(PS[0:4])
                for tc in range(4):
                    pt, pb = pp.get()
                    for c in range(8):
                        op("pe", lambda e, c=c: e.matmul(pt[0:16, 0:512], lhsT=Wg[:, c, 768:784], rhs=HT[:, c, tc * 512:(tc + 1) * 512],
                                                         start=(c == 0), stop=(c == 7)), [b_Wg] + HTb[tc * 4:tc * 4 + 4], [pb])
                    op("act", lambda e: e.activation(out=rankT[:, tc * 512:(tc + 1) * 512], in_=pt[0:16, 0:512], func=AF.Copy), [pb], [b_rank])
                for tc in range(4):
                    pt, pb = pp.get()
                    sl = slice(tc * 512, (tc + 1) * 512)
                    op("pe", lambda e: e.matmul(pt[:, 0:512], lhsT=gu[0:16, :], rhs=rankT[0:16, sl], start=True, stop=True), [b_gp, b_rank], [pb])
                    op("act", lambda e: e.activation(out=la[:, sl], in_=pt[:, 0:512], func=AF.Exp, bias=nbias[:, 0:1], scale=-1.0), [pb, b_gp], [b_la])
                op("act", lambda e: e.activation(out=la[:], in_=la[:], func=AF.Ln, bias=1.0, scale=1.0), [b_la], [b_la])
                chk('g2')
                for n in range(NT):
                    sl = slice(n * 128, (n + 1) * 128)
                    op("dve", lambda e: e.tensor_tensor_scan(out=bc[:, sl], data0=la[:, sl], data1=cst[:, 640:768], initial=0.0,
                                                             op0=ALU.add, op1=ALU.add), [b_la, b_cst], [b_bc])
                chk('g3')
                cx_chk = None
                op("dve", lambda e: e.tensor_scalar(out=nbl[:], in0=bc[:, :].rearrange("p (n c) -> p n c", c=128)[:, :, 127],
                                                    scalar1=-1.0 / 16.0, scalar2=None, op0=ALU.mult), [b_bc], [b_nbl])
                op("act", lambda e: e.activation(out=dec[:], in_=nbl[:], func=AF.Exp), [b_nbl], [b_nbl])
                chk('g3b')
                ptr = Rot(PS[4:6])
                for tc in range(4):
                    sl = slice(tc * 512, (tc + 1) * 512)
                    pq, pqb = pp.get()
                    pk, pkb = pp.get()
                    proj_fm(Wg, 0, 128, tc, pq, pqb, b_Wg)
                    proj_fm(Wg, 128, 128, tc, pk, pkb, b_Wg)
                    op("act", lambda e: e.activation(out=e1[:], in_=bc[:, sl], func=AF.Exp, scale=-1.0 / 16.0), [b_bc], [b_e[0]])
                    op("dve", lambda e: e.scalar_tensor_tensor(out=qdT[:, sl], in0=pq[:, :], scalar=0.125, in1=e1[:], op0=ALU.mult, op1=ALU.mult),
                       [pqb, b_e[0]], [b_qk])
                    op("act", lambda e: e.activation(out=e2[:], in_=bc[:, sl], func=AF.Exp, scale=1.0 / 16.0), [b_bc], [b_e[1]])
                    op("dve", lambda e: e.tensor_tensor(out=kiT[:, sl], in0=pk[:, :], in1=e2[:], op=ALU.mult), [pkb, b_e[1]], [b_qk])
                    for n4 in range(4):
                        n = tc * 4 + n4
                        op("act", lambda e, n=n, n4=n4: e.activation(out=e3[:, n4 * 128:(n4 + 1) * 128], in_=bc[:, n * 128:(n + 1) * 128], func=AF.Exp,
                                                                     bias=nbl[:, n:n + 1], scale=1.0 / 16.0), [b_bc, b_nbl], [b_e[2]])
                    op("dve", lambda e: e.tensor_tensor(out=kdT[:], in0=pk[:, :], in1=e3[:], op=ALU.mult), [pkb, b_e[2]], [b_kdT])
                    pt, pb = ptr.get()
                    for n4 in range(4):
                        op("pe", lambda e, n4=n4: e.transpose(out=pt[:, n4 * 128:(n4 + 1) * 128], in_=kdT[:, n4 * 128:(n4 + 1) * 128], identity=identF),
                           [b_kdT, b_cst], [pb])
                    op("act", lambda e: e.activation(out=kd[:, tc * 4:(tc + 1) * 4, :], in_=pt[:, :].rearrange("p (a b) -> p a b", a=4), func=AF.Copy),
                       [pb], [b_kd])
                chk('g4')
                for t in range(NT):
                    pt, pb = pp.get()
                    proj_tm(Wg, 256, 256, t, pt, pb, b_Wg)
                    op("act", lambda e: e.activation(out=vtm[:, t, :], in_=pt[:, 0:256], func=AF.Copy), [pb], [b_v])
                chk('g5')
                op("dve", lambda e: e.memset(Sst[:], 0.0), [], [b_S])
                op("dve", lambda e: e.memset(Sall[:, 0, :], 0.0), [], [b_Sall])
                for n in range(NT - 1):
                    pt, pb = pp.get()
                    for hh in range(2):
                        op("pe", lambda e, hh=hh: e.matmul(pt[hh * 64:(hh + 1) * 64, 0:128], lhsT=kd[:, n, hh * 64:(hh + 1) * 64],
                                                           rhs=vtm[:, n, hh * 128:(hh + 1) * 128], start=True, stop=True), [b_kd, b_v], [pb])
                    op("dve", lambda e: e.scalar_tensor_tensor(out=Sst[:], in0=Sst[:], scalar=dec[:, n:n + 1], in1=pt[:, 0:128],
                                                               op0=ALU.mult, op1=ALU.add), [b_S, b_nbl, pb], [b_S])
                    op("act", lambda e: e.activation(out=Sall[:, n + 1, :], in_=Sst[:], func=AF.Copy), [b_S], [b_Sall])
                chk('g6')
                ps_o = Rot(PS[4:6]); ps_g = Rot(PS[6:8])
                o_all = alloc("o_all", [128, NT, 256], F32, 1); b_oall = [Buf(f"oall{n}") for n in range(NT)]
                sg_all = alloc("sg_all", [128, NT, 256], BF16); b_sgall = [Buf(f"sgall{n}") for n in range(NT)]
                ssq_all = alloc("ssq_all", [128, NT, 2], F32); rstd_all = alloc("rstd_all", [128, NT, 2], F32); b_ssqa = Buf("ssqa")
                otms = [(alloc(f"otmg{i}", [128, 256], F32), Buf(f"otmg{i}")) for i in range(2)]
                for n in range(NT):
                    sl = slice(n * 128, (n + 1) * 128)
                    po, pob = ps_o.get()
                    for hh in range(2):
                        bp = hh * 64
                        pa, pab = pp.get()
                        op("pe", lambda e: e.matmul(pa[:, 0:128], lhsT=kiT[bp:bp + 64, sl], rhs=qdT[bp:bp + 64, sl], start=True, stop=True), [b_qk], [pab])
                        op("dve", lambda e: e.tensor_tensor(out=AT[hh][:], in0=pa[:, 0:128], in1=tri01, op=ALU.mult), [pab, b_cst], [b_AT[hh]])
                        op("pe", lambda e: e.matmul(po[:, hh * 128:(hh + 1) * 128], lhsT=AT[hh][:], rhs=vtm[:, n, hh * 128:(hh + 1) * 128],
                                                    start=True, stop=False, skip_group_check=True), [b_AT[hh], b_v], [pob])
                        op("pe", lambda e: e.matmul(po[:, hh * 128:(hh + 1) * 128], lhsT=qdT[bp:bp + 64, sl], rhs=Sall[bp:bp + 64, n, :],
                                                    start=False, stop=True, skip_group_check=True), [b_qk, b_Sall], [pob])
                    for hh in range(2):
                        op("act", lambda e, hh=hh: e.activation(out=sq[:, hh * 128:(hh + 1) * 128], in_=po[:, hh * 128:(hh + 1) * 128], func=AF.Square,
                                                                accum_out=ssq_all[:, n, hh:hh + 1]), [pob], [b_ssqa])
                    op("act", lambda e: e.activation(out=o_all[:, n, :], in_=po[:, 0:256], func=AF.Copy), [pob], [b_oall[n]])
                op("act", lambda e: e.activation(out=rstd_all[:, :, :], in_=ssq_all[:, :, :], func=AF.Sqrt, bias=epsln[:, 1:2], scale=1.0 / 128.0), [b_ssqa, b_cst], [b_ssqa])
                op("dve", lambda e: e.reciprocal(out=rstd_all[:, :, :], in_=rstd_all[:, :, :]), [b_ssqa], [b_ssqa])
                for n in range(NT):
                    pg, pgb = ps_g.get()
                    proj_tm(Wg, 512, 256, n, pg, pgb, b_Wg)
                    op("act", lambda e: e.activation(out=sg_all[:, n, :], in_=pg[:, 0:256], func=AF.Silu), [pgb], [b_sgall[n]])
                    op("pool", lambda e: e.tensor_tensor(out=sg_all[:, n, :], in0=sg_all[:, n, :], in1=gain[:, :, :].rearrange("p a b -> p (a b)"), op=ALU.mult),
                       [b_sgall[n], b_gp], [b_sgall[n]])
                for n in range(NT):
                    sl = slice(n * 128, (n + 1) * 128)
                    otm, b_otm = otms[n % 2]
                    for hh in range(2):
                        op("dve", lambda e, hh=hh: e.scalar_tensor_tensor(out=otm[:, hh * 128:(hh + 1) * 128], in0=o_all[:, n, hh * 128:(hh + 1) * 128],
                                                                          scalar=rstd_all[:, n, hh:hh + 1], in1=sg_all[:, n, hh * 128:(hh + 1) * 128],
                                                                          op0=ALU.mult, op1=ALU.mult), [b_oall[n], b_ssqa, b_sgall[n]], [b_otm])
                    pt, pb = ps_o.get()
                    for hh in range(2):
                        op("pe", lambda e, hh=hh: e.transpose(out=pt[:, hh * 128:(hh + 1) * 128], in_=otm[:, hh * 128:(hh + 1) * 128], identity=identF),
                           [b_otm, b_cst], [pb])
                    op("act", lambda e: e.activation(out=mixT[:, 2 * p:2 * p + 2, sl], in_=pt[:, 0:256].rearrange("p (a b) -> p a b", a=2), func=AF.Copy),
                       [pb], [mixb[n]])
                cx.release(m0)
                chk('p0')
            cx.release(mg)

            if stop == 'gla':
                cx.barrier()
                return nc, cx
            def attn_common():
                qT = alloc("qT", [128, 2, L], BF16, 1); kT = alloc("kT", [128, 2, L], BF16, 1)
                vaug = alloc("vaug", [128, NT, 4, 65], BF16, 1)
                return qT, kT, vaug, Buf("aqk"), Buf("av")

            m0 = cx.mark()
            qT, kT, vaug, b_qk, b_v = attn_common()
            kmT = alloc("kmT", [128, 2, 8], F32); kmB = alloc("kmB", [128, 2, 8], BF16); b_km = Buf("km")
            mbT = alloc("mbT", [8, 4, L], BF16); b_mb = Buf("mbT")
            m1 = cx.mark()
            chk('m1')
            Wa = alloc("Wa", [128, 8, 512], BF16); b_Wa = Buf("Wa")
            Wb = alloc("Wb", [128, 8, 512], BF16); b_Wb = Buf("Wb")
            t1 = alloc("t1", [128, 512], F32); t2 = alloc("t2", [128, 512], F32); b_tmp = Buf("rt")
            pp = Rot(PS[0:6])
            load_w(Wa[:], w_in[layer][:, COFF["mq"]:COFF["mq"] + 512], b_Wa)
            load_w(Wb[:], w_in[layer][:, COFF["mk"]:COFF["mk"] + 512], b_Wb)
            roped_proj(Wa, b_Wa, 0, 256, qT, b_qk, cosT, sinT, b_tab, (t1, t2, b_tmp), pp)
            roped_proj(Wb, b_Wb, 0, 256, kT, b_qk, cosT, sinT, b_tab, (t1, t2, b_tmp), pp, ksum=(kmT, b_km))
            chk('m2a')
            load_w(Wa[:, :, 0:256], w_in[layer][:, COFF["mv"]:COFF["mv"] + 256], b_Wa)
            v_aug_proj(Wa, b_Wa, 0, vaug, b_v, pp)
            op("dve", lambda e: e.tensor_copy(out=kmB[:], in_=kmT[:]), [b_km], [b_km])
            cx.release(m1)
            chk('m2')
            Z = alloc("Z", [128, 4, 8], F32); gm = alloc("gm", [128, 4, 8], F32); top8 = alloc("top8", [128, 4, 8], F32); b_Z = Buf("Z")
            otm = alloc("otm", [128, 4, 256], F32); b_otm = Buf("otm")
            pTl = [(alloc(f"pT{i}", [128, 512], BF16), Buf(f"pT{i}")) for i in range(3)]
            rs = alloc("rs", [128, 4], F32); b_rs = Buf("rs")
            op("pool", lambda e: e.memset(mbT[:], 0.0), [], [b_mb])
            pg = Rot(PS[0:2])
            for t in range(8, NT):
                pt, pb = pg.get()
                for h in range(4):
                    ptile, bp = h // 2, (h % 2) * 64
                    op("pe", lambda e, h=h: e.matmul(pt[:, h * 8:(h + 1) * 8], lhsT=qT[bp:bp + 64, ptile, t * 128:(t + 1) * 128],
                                                     rhs=kmB[bp:bp + 64, ptile, :], start=True, stop=False, skip_group_check=True), [b_qk, b_km], [pb])
                    op("pe", lambda e, h=h: e.matmul(pt[:, h * 8:(h + 1) * 8], lhsT=onehotK[0:8, t * 128:(t + 1) * 128],
                                                     rhs=bpen, start=False, stop=True, skip_group_check=True), [b_cst], [pb])
                op("dve", lambda e: e.tensor_copy(out=gm[:], in_=pt[:, 0:32].rearrange("p (a b) -> p a b", a=4)), [pb], [b_Z])
                for h in range(4):
                    op("dve", lambda e, h=h: e.max(out=top8[:, h, :], in_=gm[:, h, :]), [b_Z], [b_Z])
                for h in range(4):
                    op("dve", lambda e, h=h: e.tensor_scalar(out=Z[:, h, :], in0=gm[:, h, :], scalar1=top8[:, h, 3:4], scalar2=NEG,
                                                             op0=ALU.is_lt, op1=ALU.mult), [b_Z], [b_Z])
                pt2, pb2 = pg.get()
                for h in range(4):
                    op("pe", lambda e, h=h: e.transpose(out=pt2[0:8, h * 128:(h + 1) * 128], in_=Z[:, h, :], identity=identF), [b_Z, b_cst], [pb2])
                op("dve", lambda e: e.tensor_copy(out=mbT[0:8, :, t * 128:(t + 1) * 128], in_=pt2[0:8, :].rearrange("p (a b) -> p a b", a=4)),
                   [pb2], [b_mb])
            chk('m3')

            def moba_mask(h, qc, j, t0, st, sb, wq):
                q0 = t0 * 128
                diag = (j >= 4 * qc)
                op("pe", lambda e: e.matmul(st[:, 0:wq], lhsT=onehotK[0:8, j * 128:(j + 1) * 128], rhs=mbT[0:8, h, q0:q0 + wq],
                                            start=False, stop=(not diag), skip_group_check=True), [b_cst, b_mb], [sb])
                if diag:
                    op("pe", lambda e: e.matmul(st[:, 0:128], lhsT=trinegB[:], rhs=identB[:], start=False, stop=True, skip_group_check=True), [b_cst], [sb])

            attention(qT, kT, b_qk, vaug, b_v, moba_mask, 4, (Rot(PS[2:5]), Rot(PS[5:7]), Rot(PS[7:8])), otm, b_otm, Rot(pTl), rs, b_rs, mixT, mixb)
            cx.release(m0)

            if stop == 'moba':
                cx.barrier()
                return nc, cx
            m0 = cx.mark()
            qT, kT, vaug, b_qk, b_v = attn_common()
            kiT = alloc("kiT", [128, 1, L], BF16, 1); b_ki = Buf("ki")
            wT = alloc("wT", [8, L], BF16, 1); b_wT = Buf("wT")
            qiT = alloc("qiT", [128, 4, L], BF16, 1); b_qi = Buf("qi")
            m1 = cx.mark()
            Wa = alloc("Wa", [128, 8, 512], BF16); b_Wa = Buf("Wa")
            Wb = alloc("Wb", [128, 8, 512], BF16); b_Wb = Buf("Wb")
            t1 = alloc("t1", [128, 512], F32); t2 = alloc("t2", [128, 512], F32); b_tmp = Buf("rt")
            pp = Rot(PS[0:6])
            tm3 = (t1, t2, b_tmp)
            Wc = alloc("Wc", [128, 8, 512], BF16); b_Wc = Buf("Wc")
            Wd = alloc("Wd", [128, 8, 512], BF16); b_Wd = Buf("Wd")
            load_w(Wa[:], w_in[layer][:, COFF["sq"]:COFF["sq"] + 512], b_Wa)
            load_w(Wb[:], w_in[layer][:, COFF["sk"]:COFF["sk"] + 512], b_Wb)
            load_w(Wc[:], w_in[layer][:, COFF["iq"]:COFF["iq"] + 512], b_Wc)
            load_w(Wd[:], w_in[layer][:, COFF["iqr"]:COFF["iqr"] + 512], b_Wd)
            roped_proj(Wa, b_Wa, 0, 256, qT, b_qk, cosT, sinT, b_tab, tm3, pp)
            roped_proj(Wb, b_Wb, 0, 256, kT, b_qk, cosT, sinT, b_tab, tm3, pp)
            load_w(Wa[:, :, 0:264], w_in[layer][:, COFF["ik"]:COFF["ik"] + 264], b_Wa)
            load_w(Wb[:, :, 0:256], w_in[layer][:, COFF["sv"]:COFF["sv"] + 256], b_Wb)
            for ti in range(4):
                for tc in range(4):
                    py, pyb = pp.get(); pr, prb = pp.get()
                    proj_fm(Wc, ti * 128, 128, tc, py, pyb, b_Wc)
                    proj_fm(Wd, ti * 128, 128, tc, pr, prb, b_Wd)
                    sl = slice(tc * 512, (tc + 1) * 512)
                    op("dve", lambda e: e.tensor_tensor(out=t1[:], in0=pr[:, :], in1=sinT[:, sl], op=ALU.mult), [prb, b_tab], [b_tmp])
                    op("dve", lambda e: e.tensor_tensor(out=t2[:], in0=py[:, :], in1=cosT[:, sl], op=ALU.mult), [pyb, b_tab], [b_tmp])
                    op("pool", lambda e: e.tensor_tensor(out=qiT[:, ti, sl], in0=t1[:], in1=t2[:], op=ALU.add), [b_tmp], [b_qi])
            roped_proj(Wa, b_Wa, 0, 128, kiT, b_ki, cosT, sinT, b_tab, tm3, pp)
            for tc in range(4):
                pt, pb = pp.get()
                for c in range(8):
                    op("pe", lambda e, c=c: e.matmul(pt[0:8, 0:512], lhsT=Wa[:, c, 256:264], rhs=HT[:, c, tc * 512:(tc + 1) * 512],
                                                     start=(c == 0), stop=(c == 7)), [b_Wa] + HTb[tc * 4:tc * 4 + 4], [pb])
                op("act", lambda e: e.activation(out=wT[:, tc * 512:(tc + 1) * 512], in_=pt[0:8, 0:512], func=AF.Copy), [pb], [b_wT])
            v_aug_proj(Wb, b_Wb, 0, vaug, b_v, pp)
            cx.release(m1)
            mbs = [HT[:, 0:4, :], alloc("mbB", [128, 4, L], BF16)]
            b_mb2 = [[Buf(f"mb{a}_{i}") for i in range(4)] for a in range(2)]
            isc = [HT[:, 4:6, :].bitcast(F32).rearrange("p a b -> p (a b)"), HT[:, 6:8, :].bitcast(F32).rearrange("p a b -> p (a b)"),
                   alloc("isc2", [128, L], F32), alloc("isc3", [128, L], F32)]
            b_isc = [Buf(f"isc{i}") for i in range(4)]
            qs = alloc("qs", [128, 4, 2, 512], BF16, 1); b_qs = Buf("qs")
            otm = alloc("otm", [128, 4, 256], F32); b_otm = Buf("otm")
            pTl = [(alloc(f"pT{i}", [128, 512], BF16), Buf(f"pT{i}")) for i in range(3)]
            rs = alloc("rs", [128, 4], F32); b_rs = Buf("rs")
            bsA = alloc("bsA", [128, 32], F32); bsi = alloc("bsi", [128, 4], I32)
            lo = bsA[:, 0:4]; w0 = bsA[:, 4:8]; mid = bsA[:, 8:12]; nmid = bsA[:, 12:16]; cntv = bsA[:, 16:20]; thc = bsA[:, 20:24]; mx = bsA[:, 24:28]
            b_lo = Buf("lo"); b_mid = Buf("mid"); b_cnt = [Buf(f"cnt{i}") for i in range(4)]; b_msk = Buf("msk")
            junkD = alloc("junkD", [128, L], BF16); b_junkD = Buf("junkD")
            junkA = alloc("junkA", [128, L], BF16); b_junkA = Buf("junkA")
            ps_i = Rot(PS[0:3])
            rls = Rot([(alloc(f"rl{i}", [128, 512], BF16), Buf(f"rl{i}")) for i in range(4)])

            def dsa_qc(qc):
                mb = mbs[qc % 2]; b_mbq = b_mb2[qc % 2]
                qsl = slice(qc * 512, (qc + 1) * 512)
                for pr_ in range(4):
                    pt, pb = ps_i.get()
                    op("pe", lambda e: e.matmul(pt[:, 0:512], lhsT=selw[0:8, pr_ * 128:(pr_ + 1) * 128], rhs=wT[0:8, qsl], start=True, stop=True),
                       [b_cst, b_wT], [pb])
                    op("dve", lambda e: e.scalar_tensor_tensor(out=qs[:, pr_, 0, :], in0=pt[:, 0:512], scalar=0.0, in1=qiT[:, pr_, qsl],
                                                               op0=ALU.max, op1=ALU.mult), [pb, b_qi], [b_qs])
                    op("dve", lambda e: e.scalar_tensor_tensor(out=qs[:, pr_, 1, :], in0=pt[:, 0:512], scalar=0.0, in1=qiT[:, pr_, qsl],
                                                               op0=ALU.min, op1=ALU.mult), [pb, b_qi], [b_qs])
                yield
                act_tiles = []
                for i in range(4):
                    t = 4 * qc + i
                    S = (t + 1) * 128
                    if t < 2:
                        if t > 0:
                            op("pool", lambda e: e.memset(mb[:, i, 0:t * 128], 0.0), [], [b_mbq[i]])
                        op("pool", lambda e: e.tensor_copy(out=mb[:, i, t * 128:S], in_=trinegB[:]), [b_cst], [b_mbq[i]])
                        continue
                    act_tiles.append(i)
                    ic, icb = isc[i], b_isc[i]
                    for kc in range((S + 511) // 512):
                        w = min(512, S - kc * 512)
                        ksl = slice(kc * 512, kc * 512 + w)
                        first = True
                        nsum = 0
                        p2, p2b = PS[3]
                        pend = []

                        def flush_one():
                            nonlocal nsum
                            (rl_, rlb_, sg_) = pend.pop(0)
                            op("pe", lambda e: e.matmul(p2[:, 0:w], lhsT=(identB[:] if sg_ == 0 else negidentB[:]), rhs=rl_[:, 0:w],
                                                        start=(nsum == 0), stop=(nsum == 7)), [rlb_, b_cst], [p2b])
                            nsum += 1

                        for k in range(16):
                            hd, sgn = k // 2, k % 2
                            pr_, bp = hd // 2, (hd % 2) * 64
                            pt, pb = ps_i.get()
                            op("pe", lambda e: e.matmul(pt[:, 0:w], lhsT=qs[bp:bp + 64, pr_, sgn, i * 128:(i + 1) * 128], rhs=kiT[bp:bp + 64, 0, ksl],
                                                        start=True, stop=True), [b_qs, b_ki], [pb])
                            o = ALU.max if sgn == 0 else ALU.min
                            if hd % 2 == 1:
                                rl, rlb = rls.get()
                                op("act", lambda e: e.activation(out=rl[:, 0:w], in_=pt[:, 0:w], func=AF.Relu, scale=(1.0 if sgn == 0 else -1.0)), [pb], [rlb])
                                pend.append((rl, rlb, sgn))
                                if len(pend) > 2:
                                    flush_one()
                            elif first:
                                op("dve", lambda e: e.tensor_scalar(out=ic[:, ksl], in0=pt[:, 0:w], scalar1=0.0, scalar2=None, op0=o), [pb], [icb])
                                first = False
                            else:
                                op("dve", lambda e: e.scalar_tensor_tensor(out=ic[:, ksl], in0=pt[:, 0:w], scalar=0.0, in1=ic[:, ksl], op0=o, op1=ALU.add),
                                   [pb, icb], [icb])
                        while pend:
                            flush_one()
                        op("dve", lambda e: e.tensor_tensor(out=ic[:, ksl], in0=p2[:, 0:w], in1=ic[:, ksl], op=ALU.add), [p2b, icb], [icb])
                        yield
                    op("dve", lambda e: e.tensor_reduce(out=lo[:, i:i + 1], in_=ic[:, 0:S], axis=AX.X, op=ALU.min), [icb], [b_lo])
                    op("dve", lambda e: e.tensor_reduce(out=mx[:, i:i + 1], in_=ic[:, 0:S], axis=AX.X, op=ALU.max), [icb], [b_lo])
                    op("dve", lambda e: e.tensor_tensor(out=ic[:, t * 128:S], in0=ic[:, t * 128:S], in1=trinegbig, op=ALU.add), [icb, b_cst], [icb])
                    on_act = (i % 2 == 1)
                    op("dve", lambda e: e.memset(thc[:, i:i + 1], (S - 511.0) if on_act else (S - 255.5)), [], [b_lo])
                a0, a1 = act_tiles[0], act_tiles[-1] + 1
                asl = slice(a0, a1)
                op("dve", lambda e: e.scalar_tensor_tensor(out=w0[:, asl], in0=mx[:, asl], scalar=1.0, in1=lo[:, asl], op0=ALU.add, op1=ALU.subtract),
                   [b_lo], [b_lo])
                for it in range(N_BISECT):
                    ck = 0.5 ** (it + 1)
                    op("dve", lambda e, ck=ck: e.scalar_tensor_tensor(out=mid[:, asl], in0=w0[:, asl], scalar=ck, in1=lo[:, asl], op0=ALU.mult, op1=ALU.add),
                       [b_lo], [b_mid])
                    for i in act_tiles:
                        S = (4 * qc + i + 1) * 128
                        ic, icb = isc[i], b_isc[i]
                        if i % 2 == 1:
                            op("act", lambda e, i=i, S=S, ic=ic: e.activation(out=junkA[:, 0:S], in_=ic[:, 0:S], func=AF.Sign, bias=mid[:, i:i + 1], scale=-1.0,
                                                                              accum_out=cntv[:, i:i + 1]), [icb, b_mid], [b_cnt[i], b_junkA])
                        else:
                            op("dve", lambda e, i=i, S=S, ic=ic: e.tensor_scalar(out=junkD[:, 0:S], in0=ic[:, 0:S], scalar1=mid[:, i:i + 1], scalar2=None,
                                                                                 op0=ALU.is_lt, op1=ALU.add, accum_out=cntv[:, i:i + 1]), [icb, b_mid], [b_cnt[i], b_junkD])
                    op("dve", lambda e: e.tensor_tensor(out=bsi[:, asl], in0=cntv[:, asl], in1=thc[:, asl], op=ALU.is_le), [b_cnt[i] for i in act_tiles] + [b_lo], [b_msk])
                    op("dve", lambda e: e.copy_predicated(out=lo[:, asl], mask=bsi[:, asl], data=mid[:, asl]), [b_msk, b_mid], [b_lo])
                    yield
                for i in act_tiles:
                    S = (4 * qc + i + 1) * 128
                    ic, icb = isc[i], b_isc[i]
                    op("dve", lambda e, i=i, S=S, ic=ic: e.tensor_scalar(out=mb[:, i, 0:S], in0=ic[:, 0:S], scalar1=lo[:, i:i + 1], scalar2=NEG, op0=ALU.is_lt, op1=ALU.mult),
                       [icb, b_lo], [b_mbq[i]])

            def dsa_mask(h, qc, j, t0, st, sb, wq):
                last = 4 * qc + 3
                mb = mbs[qc % 2]; b_mbq = b_mb2[qc % 2]
                for t in range(t0, last + 1):
                    i = t - 4 * qc
                    op("pe", lambda e, t=t, i=i: e.matmul(st[:, (t - t0) * 128:(t - t0 + 1) * 128], lhsT=mb[:, i, j * 128:(j + 1) * 128], rhs=identB[:],
                                                          start=False, stop=(t == last), skip_group_check=True), [b_mbq[i], b_cst], [sb])

            attention(qT, kT, b_qk, vaug, b_v, dsa_mask, 6, (Rot(PS[4:6]), Rot(PS[6:8]), Rot(PS[3:4])), otm, b_otm, Rot(pTl), rs, b_rs, mixT, mixb,
                      qc_hook=dsa_qc)
            cx.release(m0)

            if stop == 'dsa':
                cx.barrier()
                return nc, cx
            if debug and seq == 0 and layer == layers[0]:
                dma("sp", dbg[:, :, :], mixT[:, :, :], reads=mixb, owner=Buf("dbg"))

            cx.cur2 = Hoff + 65536
            m0 = cx.mark()
            Wo = alloc("Wo", [128, 8, D], BF16); b_Wo = Buf("Wo")
            gbc = alloc("gbc", [128, D], F32); bbc = alloc("bbc", [128, D], F32); b_par = Buf("lnpar")
            hres = [alloc("hres0", [128, D], F32), alloc("hres1", [128, D], F32)]; b_hres = [Buf("hres0"), Buf("hres1")]
            stat = alloc("stat", [128, NT, 20], F32); b_stat = [Buf(f"stat{t}") for t in range(NT)]
            rw = alloc("rw", [128, 8, 16], F32); rwh = alloc("rwh", [128, 8, 16], BF16); rwl = alloc("rwl", [128, 8, 16], BF16); b_rw = Buf("rw")
            rbb = alloc("rbb", [128, 16], F32)
            HTlo = alloc("HTlo", [128, 8, 128], BF16); b_lo = Buf("HTlo")
            comb = alloc("comb", [128, NT, 16], F32); b_comb = Buf("comb")
            rt = alloc("rt", [128, 8, NT * 16], F32); b_rt = Buf("rt")
            lg_all = alloc("lg_all", [128, NT, 16], F32); b_lg = Buf("lg")
            load_w(Wo[:], w_out[layer][:, :], b_Wo)
            dma("sp", gbc[:], ln1g[layer, :].partition_broadcast(128), writes=[b_par])
            dma("sp", bbc[:], ln1b[layer, :].partition_broadcast(128), writes=[b_par], owner=Buf("lnb"))
            dma("sp", rw[:], r_w.rearrange("(c p) e -> p c e", p=128), writes=[b_rw])
            dma("sp", rbb[:], r_b.partition_broadcast(128), writes=[b_rw], owner=Buf("rbb"))
            op("dve", lambda e: e.tensor_copy(out=rwh[:], in_=rw[:]), [b_rw], [b_rw])
            op("dve", lambda e: e.tensor_tensor(out=rw[:], in0=rw[:], in1=rwh[:], op=ALU.subtract), [b_rw], [b_rw])
            op("dve", lambda e: e.tensor_copy(out=rwl[:], in_=rw[:]), [b_rw], [b_rw])
            ps_o = Rot(PS[0:4]); ps_t = Rot(PS[4:7]); ps_r = Rot(PS[7:8])
            def ln1_front(t):
                hr, hrb = hres[t % 2], b_hres[t % 2]
                dma("sp", hr[:], hs[t * 128:(t + 1) * 128, :], reads=[hsb[t]], writes=[hrb])
                for half in range(2):
                    po, pob = ps_o.get()
                    for c in range(8):
                        op("pe", lambda e, c=c: e.matmul(po[:, 0:512], lhsT=mixT[:, c, t * 128:(t + 1) * 128], rhs=Wo[:, c, half * 512:(half + 1) * 512],
                                                         start=(c == 0), stop=(c == 7)), [mixb[t], b_Wo], [pob])
                    op("dve", lambda e: e.scalar_tensor_tensor(out=H[:, t, half * 512:(half + 1) * 512], in0=hr[:, half * 512:(half + 1) * 512], scalar=ALPHA,
                                                               in1=po[:, 0:512], op0=ALU.mult, op1=ALU.add), [hrb, pob], [Hb[t]])
                ln_stats(t, stat, b_stat)

            def ln1_back(t):
                ln_apply(t, gbc, bbc, b_par, stat, b_stat)
                for half in range(2):
                    pt, pb = ps_t.get()
                    for c4 in range(4):
                        c = half * 4 + c4
                        op("pe", lambda e, c=c, c4=c4: e.transpose(out=pt[:, c4 * 128:(c4 + 1) * 128], in_=H[:, t, c * 128:(c + 1) * 128], identity=identF),
                           [Hb[t], b_cst], [pb])
                    pv = pt[:, :].rearrange("p (a b) -> p a b", a=4)
                    op("act", lambda e: e.activation(out=HT[:, half * 4:(half + 1) * 4, t * 128:(t + 1) * 128], in_=pv, func=AF.Copy), [pb], [HTb[t]])
                    op("dve", lambda e: e.tensor_tensor(out=HTlo[:, half * 4:(half + 1) * 4, :], in0=pv, in1=HT[:, half * 4:(half + 1) * 4, t * 128:(t + 1) * 128],
                                                        op=ALU.subtract), [pb, HTb[t]], [b_lo])
                pr_, prb = ps_r.get()
                k = 0
                for (A_, Ab, W_) in ((HT[:, :, t * 128:(t + 1) * 128], HTb[t], rwh), (HTlo[:, :, :], b_lo, rwh), (HT[:, :, t * 128:(t + 1) * 128], HTb[t], rwl)):
                    for c in range(8):
                        op("pe", lambda e, c=c, A_=A_, W_=W_, k=k: e.matmul(pr_[:, 0:16], lhsT=A_[:, c, :], rhs=W_[:, c, :], start=(k == 0), stop=(k == 23)),
                           [Ab, b_rw], [prb])
                        k += 1
                op("act", lambda e: e.activation(out=lg_all[:, t, :], in_=pr_[:, 0:16], func=AF.Copy), [prb], [b_lg])
                op("act", lambda e: e.activation(out=H[:, t, :], in_=H[:, t, :], func=AF.Copy, scale=ALPHA), [Hb[t]], [Hb[t]])

            ln1_front(0)
            for t in range(NT):
                if t + 1 < NT:
                    ln1_front(t + 1)
                ln1_back(t)
            T_ = NT
            sc = rt[:, 0, :]; sel = rt[:, 1, :]; tmpa = rt[:, 2, :]; tmpb = rt[:, 3, :]; ch = rt[:, 4, :]
            m1 = rt[:, 5, 0:T_ * 4]; m2 = rt[:, 5, T_ * 4:T_ * 8]; gs = rt[:, 5, T_ * 8:T_ * 12]; ing = rt[:, 5, T_ * 12:T_ * 16]
            gmax = rt[:, 6, 0:T_]; a1 = rt[:, 6, T_:2 * T_]; a2 = rt[:, 6, 2 * T_:3 * T_]; ssum = rt[:, 6, 3 * T_:4 * T_]
            v3 = lambda a: a.rearrange("p (t e) -> p t e", t=T_)
            v4 = lambda a: a.rearrange("p (t g e) -> p t g e", t=T_, g=4)
            g3 = lambda a: a.rearrange("p (t g) -> p t g", t=T_)
            R = [b_rt]
            op("act", lambda e: e.activation(out=sc, in_=lg_all[:, :, :].rearrange("p t e -> p (t e)"), func=AF.Sigmoid), [b_lg], R)
            op("dve", lambda e: e.tensor_tensor(out=v3(sel), in0=v3(sc), in1=rbb[:, None, :].to_broadcast([128, T_, 16]), op=ALU.add), R + [b_rw], R)
            op("dve", lambda e: e.tensor_reduce(out=g3(m1), in_=v4(sel), axis=AX.X, op=ALU.max), R, R)
            op("dve", lambda e: e.tensor_tensor(out=v4(tmpa), in0=v4(sel), in1=g3(m1)[:, :, :, None].to_broadcast([128, T_, 4, 4]), op=ALU.is_equal), R, R)
            op("dve", lambda e: e.scalar_tensor_tensor(out=tmpb, in0=tmpa, scalar=-1.0e9, in1=sel, op0=ALU.mult, op1=ALU.add), R, R)
            op("dve", lambda e: e.tensor_reduce(out=g3(m2), in_=v4(tmpb), axis=AX.X, op=ALU.max), R, R)
            op("dve", lambda e: e.tensor_tensor(out=gs, in0=m1, in1=m2, op=ALU.add), R, R)
            op("dve", lambda e: e.tensor_reduce(out=gmax, in_=g3(gs), axis=AX.X, op=ALU.max), R, R)
            op("dve", lambda e: e.tensor_tensor(out=g3(ing), in0=g3(gs), in1=gmax[:, :, None].to_broadcast([128, T_, 4]), op=ALU.is_lt), R, R)
            op("dve", lambda e: e.tensor_scalar(out=ing, in0=ing, scalar1=-1.0e9, scalar2=None, op0=ALU.mult), R, R)
            op("dve", lambda e: e.tensor_tensor(out=v4(tmpa), in0=v4(sel), in1=g3(ing)[:, :, :, None].to_broadcast([128, T_, 4, 4]), op=ALU.add), R, R)
            op("dve", lambda e: e.tensor_reduce(out=a1, in_=v3(tmpa), axis=AX.X, op=ALU.max), R, R)
            op("dve", lambda e: e.tensor_tensor(out=v3(ch), in0=v3(tmpa), in1=a1[:, :, None].to_broadcast([128, T_, 16]), op=ALU.is_ge), R, R)
            op("dve", lambda e: e.scalar_tensor_tensor(out=tmpb, in0=ch, scalar=-1.0e9, in1=tmpa, op0=ALU.mult, op1=ALU.add), R, R)
            op("dve", lambda e: e.tensor_reduce(out=a2, in_=v3(tmpb), axis=AX.X, op=ALU.max), R, R)
            op("dve", lambda e: e.tensor_tensor(out=v3(tmpa), in0=v3(tmpb), in1=a2[:, :, None].to_broadcast([128, T_, 16]), op=ALU.is_ge), R, R)
            op("dve", lambda e: e.tensor_tensor(out=ch, in0=ch, in1=tmpa, op=ALU.add), R, R)
            op("dve", lambda e: e.tensor_tensor(out=tmpb, in0=ch, in1=sc, op=ALU.mult), R, R)
            op("dve", lambda e: e.tensor_reduce(out=ssum, in_=v3(tmpb), axis=AX.X, op=ALU.add), R, R)
            op("dve", lambda e: e.reciprocal(out=ssum, in_=ssum), R, R)
            op("dve", lambda e: e.tensor_tensor(out=comb[:, :, :], in0=v3(tmpb), in1=ssum[:, :, None].to_broadcast([128, T_, 16]), op=ALU.mult), R, [b_comb])
            cx.barrier()
            cx.cur, cx.cur2 = base_mark
            cx.cur2 = Hoff + 65536
            comb2 = alloc("comb2", [128, NT, 16], F32); b_comb2 = Buf("comb2")
            op("dve", lambda e: e.tensor_copy(out=comb2[:], in_=comb[:]), [b_comb], [b_comb2])
            cx.barrier()

            if stop == 'ln1':
                cx.barrier()
                return nc, cx
            m0 = cx.mark()
            Wg_ = [alloc(f"weg{i}", [128, 8, 512], BF16) for i in range(2)]; b_weg = [Buf("weg0"), Buf("weg1")]
            Wu_ = [alloc(f"weu{i}", [128, 8, 512], BF16) for i in range(2)]; b_weu = [Buf("weu0"), Buf("weu1")]
            Wd_ = [alloc(f"wed{i}", [128, 4, D], BF16) for i in range(2)]; b_wed = [Buf("wed0"), Buf("wed1")]
            actT = alloc("actT", [128, 4, L], BF16); b_act = [[Buf(f"act{f}_{tc}") for tc in range(4)] for f in range(4)]
            sgb = [(alloc(f"sgm{i}", [128, 512], F32), Buf(f"sgm{i}")) for i in range(3)]
            sgr = Rot(sgb)
            ps_gu = Rot(PS[0:4]); ps_d = Rot(PS[4:8])
            gbc = alloc("gbc", [128, D], F32); bbc = alloc("bbc", [128, D], F32); b_par = Buf("lnpar")
            stat = alloc("stat", [128, NT, 20], F32); b_stat = [Buf(f"stat{t}") for t in range(NT)]
            dma("sp", gbc[:], ln2g[layer, :].partition_broadcast(128), writes=[b_par])
            dma("sp", bbc[:], ln2b[layer, :].partition_broadcast(128), writes=[b_par], owner=Buf("lnb"))
            pool_tr = Rot(PS[0:4])
            last = (layer == layers[-1])

            def ln2_group(ts):
                for t in ts:
                    st = stat[:, t, :]
                    op("dve", lambda e, t=t, st=st: e.bn_stats(out=st[:, 0:6], in_=H[:, t, 0:512]), [Hb[t]], [b_stat[t]])
                    op("dve", lambda e, t=t, st=st: e.bn_stats(out=st[:, 6:12], in_=H[:, t, 512:1024]), [Hb[t]], [b_stat[t]])
                    op("dve", lambda e, st=st: e.bn_aggr(out=st[:, 12:14], in_=st[:, 0:12]), [b_stat[t]], [b_stat[t]])
                for t in ts:
                    st = stat[:, t, :]
                    op("act", lambda e, st=st: e.activation(out=st[:, 14:15], in_=st[:, 13:14], func=AF.Sqrt, bias=epsln[:, 0:1], scale=1.0),
                       [b_stat[t], b_cst], [b_stat[t]])
                for t in ts:
                    st = stat[:, t, :]
                    op("dve", lambda e, st=st: e.reciprocal(out=st[:, 15:16], in_=st[:, 14:15]), [b_stat[t]], [b_stat[t]])
                    op("dve", lambda e, st=st: e.scalar_tensor_tensor(out=st[:, 16:17], in0=st[:, 12:13], scalar=-1.0, in1=st[:, 15:16],
                                                                      op0=ALU.mult, op1=ALU.mult), [b_stat[t]], [b_stat[t]])
                for t in ts:
                    st = stat[:, t, :]
                    op("act", lambda e, t=t, st=st: e.activation(out=H[:, t, :], in_=H[:, t, :], func=AF.Identity, bias=st[:, 16:17], scale=st[:, 15:16]),
                       [Hb[t], b_stat[t]], [Hb[t]])
                for t in ts:
                    op("dve", lambda e, t=t: e.tensor_tensor(out=H[:, t, :], in0=H[:, t, :], in1=gbc[:], op=ALU.mult), [Hb[t], b_par], [Hb[t]])
                    op("dve", lambda e, t=t: e.tensor_tensor(out=H[:, t, :], in0=H[:, t, :], in1=bbc[:], op=ALU.add), [Hb[t], b_par], [Hb[t]])
                    if last:
                        dma("sp", out[seq, t * 128:(t + 1) * 128, :], H[:, t, :], reads=[Hb[t]], owner=Hb[t])
                    else:
                        make_HT(t, pool_tr)

            for ex in range(16):
                wg, wu, wd = Wg_[ex % 2], Wu_[ex % 2], Wd_[ex % 2]
                bg, bu, bd = b_weg[ex % 2], b_weu[ex % 2], b_wed[ex % 2]
                load_w(wg[:], w_eg[layer, ex], bg)
                load_w(wu[:], w_eu[layer, ex], bu)
                dma("pool", wd[:], w_ed[layer, ex].rearrange("(c p) f -> p c f", p=128), writes=[bd])
                for tc in range(4):
                    for f in range(4):
                        pg_, pgb = ps_gu.get(); pu_, pub = ps_gu.get()
                        for c in range(8):
                            op("pe", lambda e, c=c: e.matmul(pg_[:, 0:512], lhsT=wg[:, c, f * 128:(f + 1) * 128], rhs=HT[:, c, tc * 512:(tc + 1) * 512],
                                                             start=(c == 0), stop=(c == 7)), [bg] + HTb[tc * 4:tc * 4 + 4], [pgb])
                        for c in range(8):
                            op("pe", lambda e, c=c: e.matmul(pu_[:, 0:512], lhsT=wu[:, c, f * 128:(f + 1) * 128], rhs=HT[:, c, tc * 512:(tc + 1) * 512],
                                                             start=(c == 0), stop=(c == 7)), [bu] + HTb[tc * 4:tc * 4 + 4], [pub])
                        sgt, sgtb = sgr.get()
                        op("act", lambda e: e.activation(out=sgt[:], in_=pg_[:, 0:512], func=AF.Silu), [pgb], [sgtb])
                        op("dve", lambda e: e.tensor_tensor(out=actT[:, f, tc * 512:(tc + 1) * 512], in0=pu_[:, 0:512], in1=sgt[:], op=ALU.mult),
                           [pub, sgtb], [b_act[f][tc]])
                    for t4 in range(4):
                        t = tc * 4 + t4
                        for half in range(2):
                            pd_, pdb = ps_d.get()
                            for f in range(4):
                                op("pe", lambda e, f=f: e.matmul(pd_[:, 0:512], lhsT=actT[:, f, t * 128:(t + 1) * 128], rhs=wd[:, f, half * 512:(half + 1) * 512],
                                                                 start=(f == 0), stop=(f == 3)), [b_act[f][tc], bd], [pdb])
                            op("dve", lambda e: e.scalar_tensor_tensor(out=H[:, t, half * 512:(half + 1) * 512], in0=pd_[:, 0:512], scalar=comb2[:, t, ex:ex + 1],
                                                                       in1=H[:, t, half * 512:(half + 1) * 512], op0=ALU.mult, op1=ALU.add),
                               [pdb, b_comb2, Hb[t]], [Hb[t]])
                    if ex == 15:
                        ln2_group(list(range(tc * 4, tc * 4 + 4)))
            cx.release(m0)

            if stop == 'moe':
                cx.barrier()
                return nc, cx
            cx.cur, cx.cur2 = base_mark

    cx.barrier()
    return nc, cx


_CACHE = {}


def _prep_inputs(inputs):
    cst, cst8 = _consts()
    w_in_x = np.ascontiguousarray(np.asarray(inputs["w_in"], np.float32)[:, :, COLMAP])
    shared = {"w_in_x": w_in_x, "cst": cst, "cst8": cst8}
    for k in ("gla_gate_up", "gla_gate_bias", "gla_norm_gain", "w_out", "ln_mix_g", "ln_mix_b", "router_w", "router_bias",
              "w_expert_gate", "w_expert_up", "w_expert_down", "ln_ffn_g", "ln_ffn_b"):
        shared[k] = np.ascontiguousarray(np.asarray(inputs[k], np.float32))
    return shared


def kernel(**inputs):
    x = np.ascontiguousarray(np.asarray(inputs["x"], np.float32))
    positions = np.ascontiguousarray(np.asarray(inputs["positions"], np.int32))
    shared = _prep_inputs(inputs)
    ncores, nseq = 8, 2
    nc = bass.Bass("TRN2", target_bir_lowering=False)
    build(nc, nseq, [0, 1])
    in_maps = []
    for c in range(ncores):
        m = dict(shared)
        m["x"] = x[c * nseq:(c + 1) * nseq]
        m["positions"] = positions[c * nseq:(c + 1) * nseq]
        in_maps.append(m)
    res = run_bass_kernel_spmd(nc, in_maps, core_ids=list(range(ncores)))
    return np.concatenate([r["out"] for r in res.results], axis=0)
```
